# Optimizing a Trainium2 kernel written in Bass

```python
import math
import jax, jax.numpy as jnp
from jax import lax
import numpy as np

D_MODEL = 1024
BATCH = 8
SEQ = 2048
DEPTH = 1
DEC_BATCH = 128
DEC_SEQ = 1
PAST_LEN = 2048
PAGE_SIZE = 128

MIX_WIDTH = D_MODEL
SSM_WIDTH = MIX_WIDTH // 2
ATT_WIDTH = MIX_WIDTH - SSM_WIDTH
SSM_GROUP = 16
SSM_GROUPS = SSM_WIDTH // SSM_GROUP
SSM_STATE = 64
DT_MIN = 0.001
DT_MAX = 0.1
DIFF_HEADS = 4
V_DIM = ATT_WIDTH // DIFF_HEADS
QK_DIM = V_DIM // 2
ROPE_THETA = 10000.0
Q_BLOCK = 128
N_EXPERTS = 32
TOP_K = 4
D_EXPERT = D_MODEL
SWIGLU_ALPHA = 1.702
SWIGLU_LIMIT = 7.0
MOE_BLOCK = 128
LN_EPS = 1e-5
RMS_EPS = 1e-5
IN_COLS = SSM_WIDTH + 3 * ATT_WIDTH
DEEPNORM_ALPHA = (2 * DEPTH) ** 0.25
DEEPNORM_BETA = (8 * DEPTH) ** -0.25

kernel_name = "hymba_s5_diffattn_moe_decode_step"

F32 = jnp.float32


def lambda_init_fn(layer_idx):
    return 0.8 - 0.6 * math.exp(-0.3 * layer_idx)


def layer_norm(x, g, b):
    xf = x.astype(F32)
    mu = jnp.mean(xf, axis=-1, keepdims=True)
    var = jnp.mean(jnp.square(xf - mu), axis=-1, keepdims=True)
    return ((xf - mu) * lax.rsqrt(var + LN_EPS) * g.astype(F32) + b.astype(F32)).astype(x.dtype)


def rope(x, pos):
    half = QK_DIM // 2
    inv = ROPE_THETA ** (-jnp.arange(half, dtype=F32) / half)
    ang = pos.astype(F32)[:, None] * inv[None, :]
    cos = jnp.cos(ang)[None, :, None, None, :]
    sin = jnp.sin(ang)[None, :, None, None, :]
    xf = x.astype(F32)
    x1, x2 = xf[..., :half], xf[..., half:]
    return jnp.concatenate([x1 * cos - x2 * sin, x2 * cos + x1 * sin], axis=-1).astype(x.dtype)


def mixer_inputs(x, pos, w_in):
    b_, l_ = x.shape[:2]
    proj = x @ w_in
    u = proj[..., :SSM_WIDTH]
    q = proj[..., SSM_WIDTH:SSM_WIDTH + ATT_WIDTH].reshape(b_, l_, DIFF_HEADS, 2, QK_DIM)
    k = proj[..., SSM_WIDTH + ATT_WIDTH:SSM_WIDTH + 2 * ATT_WIDTH].reshape(b_, l_, DIFF_HEADS, 2, QK_DIM)
    v = proj[..., SSM_WIDTH + 2 * ATT_WIDTH:].reshape(b_, l_, DIFF_HEADS, V_DIM)
    return u, rope(q, pos), rope(k, pos), v


def ssm_discretize(a_re, a_im, log_dt, b_re, b_im):
    a_re = jnp.minimum(a_re.astype(F32), -1e-4)
    a_im = a_im.astype(F32)
    dt = jnp.exp(log_dt.astype(F32))[:, None]
    mag = jnp.exp(a_re * dt)
    lb_re = mag * jnp.cos(a_im * dt)
    lb_im = mag * jnp.sin(a_im * dt)
    den = jnp.square(a_re) + jnp.square(a_im)
    nr, ni = lb_re - 1.0, lb_im
    f_re = (nr * a_re + ni * a_im) / den
    f_im = (ni * a_re - nr * a_im) / den
    b_re = b_re.astype(F32)
    b_im = b_im.astype(F32)
    bb_re = f_re[..., None] * b_re - f_im[..., None] * b_im
    bb_im = f_re[..., None] * b_im + f_im[..., None] * b_re
    return lb_re, lb_im, bb_re, bb_im


def complex_affine_combine(e1, e2):
    a1r, a1i, b1r, b1i = e1
    a2r, a2i, b2r, b2i = e2
    return (a2r * a1r - a2i * a1i,
            a2r * a1i + a2i * a1r,
            a2r * b1r - a2i * b1i + b2r,
            a2r * b1i + a2i * b1r + b2i)


def ssm_mixer(u, s0_re, s0_im, p):
    b_, l_ = u.shape[:2]
    ug = u.reshape(b_, l_, SSM_GROUPS, SSM_GROUP).astype(F32)
    lb_re, lb_im, bb_re, bb_im = ssm_discretize(p['ssm_a_re'], p['ssm_a_im'], p['ssm_log_dt'], p['ssm_b_re'], p['ssm_b_im'])
    b_re = jnp.einsum('blgh,gph->blgp', ug, bb_re)
    b_im = jnp.einsum('blgh,gph->blgp', ug, bb_im)
    s0r = s0_re.astype(F32)
    s0i = s0_im.astype(F32)
    b_re = b_re.at[:, 0].add(lb_re * s0r - lb_im * s0i)
    b_im = b_im.at[:, 0].add(lb_re * s0i + lb_im * s0r)
    a_re = jnp.broadcast_to(lb_re, b_re.shape)
    a_im = jnp.broadcast_to(lb_im, b_re.shape)
    _, _, s_re, s_im = lax.associative_scan(complex_affine_combine, (a_re, a_im, b_re, b_im), axis=1)
    y = (jnp.einsum('blgp,ghp->blgh', s_re, p['ssm_c_re'].astype(F32))
         - jnp.einsum('blgp,ghp->blgh', s_im, p['ssm_c_im'].astype(F32))
         + p['ssm_d'].astype(F32)[None, None] * ug)
    y = jax.nn.gelu(y.reshape(b_, l_, SSM_WIDTH)).astype(u.dtype)
    z = y @ p['w_glu'] + p['b_glu']
    out = z[..., :SSM_WIDTH] * jax.nn.sigmoid(z[..., SSM_WIDTH:])
    return out, s_re[:, -1], s_im[:, -1]


def diff_lambda(p, lam_init):
    return (jnp.exp(jnp.sum(p['lambda_q1'].astype(F32) * p['lambda_k1'].astype(F32)))
            - jnp.exp(jnp.sum(p['lambda_q2'].astype(F32) * p['lambda_k2'].astype(F32)))
            + lam_init)


def diff_attend(q, k, v, q_pos, k_pos, lam):
    s = jnp.einsum('bqhmd,bkhmd->bhmqk', q, k, preferred_element_type=F32) * (QK_DIM ** -0.5)
    mask = k_pos[None, :] <= q_pos[:, None]
    s = jnp.where(mask[None, None, None], s, -jnp.inf)
    pr = jax.nn.softmax(s, axis=-1)
    a = pr[:, :, 0] - lam * pr[:, :, 1]
    return jnp.einsum('bhqk,bkhd->bqhd', a.astype(v.dtype), v)


def diff_attend_prompt(q, k, v, lam):
    b_, l_ = q.shape[:2]
    nb = l_ // Q_BLOCK
    qb = jnp.moveaxis(q.reshape(b_, nb, Q_BLOCK, DIFF_HEADS, 2, QK_DIM), 1, 0)
    pos = jnp.arange(l_, dtype=jnp.int32)
    qpos = pos.reshape(nb, Q_BLOCK)
    out = lax.map(lambda a: diff_attend(a[0], k, v, a[1], pos, lam), (qb, qpos))
    return jnp.moveaxis(out, 0, 1).reshape(b_, l_, DIFF_HEADS, V_DIM)


def diff_head_norm(o, subln_g, lam_init):
    b_, l_ = o.shape[:2]
    of = o.astype(F32)
    of = of * lax.rsqrt(jnp.mean(jnp.square(of), axis=-1, keepdims=True) + RMS_EPS) * subln_g.astype(F32)
    return (of * (1.0 - lam_init)).reshape(b_, l_, ATT_WIDTH).astype(o.dtype)


def moe_ffn(x2d, p):
    t_ = x2d.shape[0]
    logits = (x2d @ p['w_router'] + p['b_router']).astype(F32)
    top_val, top_idx = lax.top_k(logits, TOP_K)
    gates = jax.nn.softmax(top_val, axis=-1)
    n_assign = t_ * TOP_K
    e_flat = top_idx.reshape(n_assign).astype(jnp.int32)
    tok_flat = jnp.arange(n_assign, dtype=jnp.int32) // TOP_K
    g_flat = gates.reshape(n_assign)
    order = jnp.argsort(e_flat, stable=True)
    e_sorted = e_flat[order]
    counts = jnp.bincount(e_flat, length=N_EXPERTS).astype(jnp.int32)
    start = jnp.cumsum(counts) - counts
    padded = ((counts + MOE_BLOCK - 1) // MOE_BLOCK) * MOE_BLOCK
    pend = jnp.cumsum(padded)
    pstart = pend - padded
    dest = pstart[e_sorted] + (jnp.arange(n_assign, dtype=jnp.int32) - start[e_sorted])
    n_blocks = -(-(n_assign + N_EXPERTS * (MOE_BLOCK - 1)) // MOE_BLOCK)
    n_rows = n_blocks * MOE_BLOCK
    tok_buf = jnp.zeros((n_rows,), jnp.int32).at[dest].set(tok_flat[order])
    gate_buf = jnp.zeros((n_rows,), F32).at[dest].set(g_flat[order])
    block_start = jnp.arange(n_blocks, dtype=jnp.int32) * MOE_BLOCK
    block_expert = jnp.minimum(jnp.searchsorted(pend, block_start, side='right'), N_EXPERTS - 1).astype(jnp.int32)
    x_buf = x2d[tok_buf].reshape(n_blocks, MOE_BLOCK, x2d.shape[1])
    w_gu, b_gu, w_dn, b_dn = p['w_gate_up'], p['b_gate_up'], p['w_down'], p['b_down']

    def expert_block(args):
        xb, e = args
        h = (xb @ w_gu[e] + b_gu[e]).astype(F32)
        glu = jnp.minimum(h[:, :D_EXPERT], SWIGLU_LIMIT)
        lin = jnp.clip(h[:, D_EXPERT:], -SWIGLU_LIMIT, SWIGLU_LIMIT)
        act = (glu * jax.nn.sigmoid(SWIGLU_ALPHA * glu) * (lin + 1.0)).astype(xb.dtype)
        return act @ w_dn[e] + b_dn[e]

    y_buf = lax.map(expert_block, (x_buf, block_expert)).reshape(n_rows, x2d.shape[1])
    return jax.ops.segment_sum(y_buf * gate_buf.astype(y_buf.dtype)[:, None], tok_buf, num_segments=t_)


def layer_output(x, ssm_out, att_o, lam_init, p):
    att_out = diff_head_norm(att_o, p['subln_g'], lam_init)
    mix = jnp.concatenate([ssm_out, att_out], axis=-1) @ p['w_out']
    h = layer_norm(DEEPNORM_ALPHA * x + mix, p['ln1_g'], p['ln1_b'])
    b_, l_, d_ = h.shape
    f = moe_ffn(h.reshape(b_ * l_, d_), p).reshape(b_, l_, d_)
    return layer_norm(DEEPNORM_ALPHA * h + f, p['ln2_g'], p['ln2_b'])


def setup_inputs(seed: int = 0) -> dict:
    key = jax.random.key(seed)
    ks = jax.random.split(key, 40)
    n_pages = PAST_LEN // PAGE_SIZE
    n_phys = (DEC_BATCH * n_pages * 5) // 4

    def nrm(k, shape, scale):
        return jax.random.normal(k, shape, F32) * scale

    x_prompt = nrm(ks[0], (BATCH, SEQ, D_MODEL), 1.0)
    x_sample = nrm(ks[1], (DEC_BATCH, DEC_SEQ, D_MODEL), 1.0)
    cache_k = nrm(ks[2], (DEPTH, n_phys, PAGE_SIZE, DIFF_HEADS, 2 * QK_DIM), 1.0)
    cache_v = nrm(ks[3], (DEPTH, n_phys, PAGE_SIZE, DIFF_HEADS, V_DIM), 1.0)
    state_ssm_re = nrm(ks[4], (DEPTH, DEC_BATCH, SSM_GROUPS, SSM_STATE), 0.5)
    state_ssm_im = nrm(ks[5], (DEPTH, DEC_BATCH, SSM_GROUPS, SSM_STATE), 0.5)
    page_table = jax.random.permutation(ks[6], n_phys)[:DEC_BATCH * n_pages].reshape(DEC_BATCH, n_pages).astype(jnp.int32)

    w_in = jnp.concatenate([
        nrm(ks[7], (DEPTH, D_MODEL, SSM_WIDTH + 2 * ATT_WIDTH), D_MODEL ** -0.5),
        nrm(ks[8], (DEPTH, D_MODEL, ATT_WIDTH), D_MODEL ** -0.5 * DEEPNORM_BETA)], axis=-1)
    w_out = nrm(ks[9], (DEPTH, MIX_WIDTH, D_MODEL), MIX_WIDTH ** -0.5 * DEEPNORM_BETA)
    ssm_a_re = -0.5 + nrm(ks[10], (DEPTH, SSM_GROUPS, SSM_STATE), 0.01)
    ssm_a_im = math.pi * jnp.arange(SSM_STATE, dtype=F32)[None, None, :] + nrm(ks[11], (DEPTH, SSM_GROUPS, SSM_STATE), 0.01)
    ssm_log_dt = jax.random.uniform(ks[12], (DEPTH, SSM_GROUPS), F32, math.log(DT_MIN), math.log(DT_MAX))
    ssm_b_re = nrm(ks[13], (DEPTH, SSM_GROUPS, SSM_STATE, SSM_GROUP), SSM_GROUP ** -0.5)
    ssm_b_im = nrm(ks[14], (DEPTH, SSM_GROUPS, SSM_STATE, SSM_GROUP), SSM_GROUP ** -0.5)
    ssm_c_re = nrm(ks[15], (DEPTH, SSM_GROUPS, SSM_GROUP, SSM_STATE), SSM_STATE ** -0.5)
    ssm_c_im = nrm(ks[16], (DEPTH, SSM_GROUPS, SSM_GROUP, SSM_STATE), SSM_STATE ** -0.5)
    ssm_d = nrm(ks[17], (DEPTH, SSM_GROUPS, SSM_GROUP), 1.0)
    w_glu = nrm(ks[18], (DEPTH, SSM_WIDTH, 2 * SSM_WIDTH), SSM_WIDTH ** -0.5)
    b_glu = nrm(ks[19], (DEPTH, 2 * SSM_WIDTH), 0.01)
    lambda_q1 = nrm(ks[20], (DEPTH, QK_DIM), 0.1)
    lambda_k1 = nrm(ks[21], (DEPTH, QK_DIM), 0.1)
    lambda_q2 = nrm(ks[22], (DEPTH, QK_DIM), 0.1)
    lambda_k2 = nrm(ks[23], (DEPTH, QK_DIM), 0.1)
    subln_g = 1.0 + nrm(ks[24], (DEPTH, V_DIM), 0.01)
    ln1_g = 1.0 + nrm(ks[25], (DEPTH, D_MODEL), 0.01)
    ln1_b = nrm(ks[26], (DEPTH, D_MODEL), 0.01)
    w_router = nrm(ks[27], (DEPTH, D_MODEL, N_EXPERTS), D_MODEL ** -0.5)
    b_router = nrm(ks[28], (DEPTH, N_EXPERTS), 0.01)
    w_gate_up = nrm(ks[29], (DEPTH, N_EXPERTS, D_MODEL, 2 * D_EXPERT), D_MODEL ** -0.5 * DEEPNORM_BETA)
    b_gate_up = nrm(ks[30], (DEPTH, N_EXPERTS, 2 * D_EXPERT), 0.01)
    w_down = nrm(ks[31], (DEPTH, N_EXPERTS, D_EXPERT, D_MODEL), D_EXPERT ** -0.5 * DEEPNORM_BETA)
    b_down = nrm(ks[32], (DEPTH, N_EXPERTS, D_MODEL), 0.01)
    ln2_g = 1.0 + nrm(ks[33], (DEPTH, D_MODEL), 0.01)
    ln2_b = nrm(ks[34], (DEPTH, D_MODEL), 0.01)
    return {
        'x_prompt': x_prompt, 'x_sample': x_sample,
        'cache_k': cache_k, 'cache_v': cache_v,
        'state_ssm_re': state_ssm_re, 'state_ssm_im': state_ssm_im,
        'page_table': page_table,
        'w_in': w_in, 'w_out': w_out,
        'ssm_a_re': ssm_a_re, 'ssm_a_im': ssm_a_im, 'ssm_log_dt': ssm_log_dt,
        'ssm_b_re': ssm_b_re, 'ssm_b_im': ssm_b_im, 'ssm_c_re': ssm_c_re, 'ssm_c_im': ssm_c_im,
        'ssm_d': ssm_d, 'w_glu': w_glu, 'b_glu': b_glu,
        'lambda_q1': lambda_q1, 'lambda_k1': lambda_k1, 'lambda_q2': lambda_q2, 'lambda_k2': lambda_k2,
        'subln_g': subln_g, 'ln1_g': ln1_g, 'ln1_b': ln1_b,
        'w_router': w_router, 'b_router': b_router,
        'w_gate_up': w_gate_up, 'b_gate_up': b_gate_up, 'w_down': w_down, 'b_down': b_down,
        'ln2_g': ln2_g, 'ln2_b': ln2_b,
    }


def reference(x_prompt, x_sample, cache_k, cache_v, state_ssm_re, state_ssm_im, page_table,
              w_in, w_out, ssm_a_re, ssm_a_im, ssm_log_dt, ssm_b_re, ssm_b_im, ssm_c_re, ssm_c_im,
              ssm_d, w_glu, b_glu, lambda_q1, lambda_k1, lambda_q2, lambda_k2, subln_g, ln1_g, ln1_b,
              w_router, b_router, w_gate_up, b_gate_up, w_down, b_down, ln2_g, ln2_b):
    b_p, l_p = x_prompt.shape[:2]
    b_s, l_s = x_sample.shape[:2]
    past_len = page_table.shape[1] * PAGE_SIZE
    pos_p = jnp.arange(l_p, dtype=jnp.int32)
    pos_s = past_len + jnp.arange(l_s, dtype=jnp.int32)
    k_pos_s = jnp.arange(past_len + l_s, dtype=jnp.int32)
    hp, hs = x_prompt, x_sample
    kp_l, vp_l, srp_l, sip_l = [], [], [], []
    ks_l, vs_l, srs_l, sis_l = [], [], [], []
    for l in range(DEPTH):
        p = dict(w_in=w_in[l], w_out=w_out[l], ssm_a_re=ssm_a_re[l], ssm_a_im=ssm_a_im[l],
                 ssm_log_dt=ssm_log_dt[l], ssm_b_re=ssm_b_re[l], ssm_b_im=ssm_b_im[l],
                 ssm_c_re=ssm_c_re[l], ssm_c_im=ssm_c_im[l], ssm_d=ssm_d[l], w_glu=w_glu[l], b_glu=b_glu[l],
                 lambda_q1=lambda_q1[l], lambda_k1=lambda_k1[l], lambda_q2=lambda_q2[l], lambda_k2=lambda_k2[l],
                 subln_g=subln_g[l], ln1_g=ln1_g[l], ln1_b=ln1_b[l], w_router=w_router[l], b_router=b_router[l],
                 w_gate_up=w_gate_up[l], b_gate_up=b_gate_up[l], w_down=w_down[l], b_down=b_down[l],
                 ln2_g=ln2_g[l], ln2_b=ln2_b[l])
        lam_init = lambda_init_fn(l)
        lam = diff_lambda(p, lam_init)

        u, q, k, v = mixer_inputs(hp, pos_p, p['w_in'])
        z0 = jnp.zeros((b_p, SSM_GROUPS, SSM_STATE), F32)
        ssm_o, s_re, s_im = ssm_mixer(u, z0, z0, p)
        att_o = diff_attend_prompt(q, k, v, lam)
        hp = layer_output(hp, ssm_o, att_o, lam_init, p)
        kp_l.append(k.reshape(b_p, l_p, DIFF_HEADS, 2 * QK_DIM))
        vp_l.append(v)
        srp_l.append(s_re)
        sip_l.append(s_im)

        u, q, k, v = mixer_inputs(hs, pos_s, p['w_in'])
        ssm_o, s_re, s_im = ssm_mixer(u, state_ssm_re[l], state_ssm_im[l], p)
        k_past = cache_k[l][page_table].reshape(b_s, past_len, DIFF_HEADS, 2, QK_DIM).astype(k.dtype)
        v_past = cache_v[l][page_table].reshape(b_s, past_len, DIFF_HEADS, V_DIM).astype(v.dtype)
        k_all = jnp.concatenate([k_past, k], axis=1)
        v_all = jnp.concatenate([v_past, v], axis=1)
        att_o = diff_attend(q, k_all, v_all, pos_s, k_pos_s, lam)
        hs = layer_output(hs, ssm_o, att_o, lam_init, p)
        ks_l.append(k.reshape(b_s, l_s, DIFF_HEADS, 2 * QK_DIM))
        vs_l.append(v)
        srs_l.append(s_re)
        sis_l.append(s_im)

    return (hp, hs,
            jnp.stack(kp_l), jnp.stack(vp_l), jnp.stack(srp_l), jnp.stack(sip_l),
            jnp.stack(ks_l), jnp.stack(vs_l), jnp.stack(srs_l), jnp.stack(sis_l))
```

```python
import math
from contextlib import ExitStack

import numpy as np
import concourse.bass as bass
import concourse.mybir as mybir
from concourse.bass_utils import run_bass_kernel_spmd

F32 = mybir.dt.float32
BF16 = mybir.dt.bfloat16
I32 = mybir.dt.int32
AF = mybir.ActivationFunctionType
ALU = mybir.AluOpType
AX = mybir.AxisListType

D = 1024
NCORES = 8
LN_EPS = 1e-5
RMS_EPS = 1e-5
LAM_INIT = 0.8 - 0.6 * math.exp(-0.3 * 0)
ALPHA = (2 * 1) ** 0.25
SW_ALPHA = 1.702
SW_LIM = 7.0
ARENA_W = 52992


class Cfg:
    def __init__(self, T=2048, NS=16, NPG=16, NPHYS=2560, NE=32, TOPK=4):
        self.T, self.NS, self.NPG, self.NPHYS, self.NE, self.TOPK = T, NS, NPG, NPHYS, NE, TOPK
        self.NT = T + NS
        self.NTP = T // 128
        self.TB = [(i * 512, min(512, T - i * 512)) for i in range((T + 511) // 512)] + [(T, NS)]
        self.TL = [(i * 128, 128) for i in range(self.NTP)] + [(T, NS)]


FULL = Cfg()

ENGS = ("pe", "act", "dve", "pool", "sp")


class Ctx:
    def __init__(self, nc, stack):
        self.nc, self.stack = nc, stack
        self.esem = {e: stack.enter_context(nc.semaphore("es_" + e)) for e in ("pe", "act", "dve", "pool")}
        self.ecnt = {e: 0 for e in self.esem}
        self.dsem, self.dcnt = {}, {}
        self.known = {e: {} for e in ENGS}
        self.phase_no = 0
        self.stop_after = None
        self.max_ops = None

    def dma_slot(self, slot):
        if slot not in self.dsem:
            self.dsem[slot] = self.stack.enter_context(self.nc.semaphore("ds%d" % len(self.dsem)))
            self.dcnt[slot] = 0
            assert len(self.dsem) < 150, "too many dma semaphores"
        return self.dsem[slot]


class Phase:
    def __init__(self, ctx):
        self.ctx, self.ops = ctx, []

    def add(self, eng, fn, r=(), w=(), dma=None):
        self.ops.append(dict(eng=eng, fn=fn, r=tuple(r), w=tuple(w), dma=dma, dep=False))

    def mm(self, items, r, w):
        def fn(e, items=items):
            return [e.matmul(o, l, rh, start=s, stop=t) for (o, l, rh, s, t) in items]
        self.add("pe", fn, r, w)

    def tr(self, items, r, w):
        def fn(e, items=items):
            return [e.transpose(o, i, idn) for (o, i, idn) in items]
        self.add("pe", fn, r, w)

    def act(self, out, in_, func, r, w, bias=None, scale=None, accum=None):
        def fn(e):
            kw = {}
            if bias is not None:
                kw["bias"] = bias
            if scale is not None:
                kw["scale"] = scale
            if accum is not None:
                kw["accum_out"] = accum
            return e.activation(out, in_, func, **kw)
        self.add("act", fn, r, w)

    def ts(self, out, in0, s1, op0, r, w, s2=None, op1=None, eng="dve"):
        def fn(e):
            if op1 is None:
                return e.tensor_scalar(out, in0, s1, None, op0)
            return e.tensor_scalar(out, in0, s1, s2, op0, op1)
        self.add(eng, fn, r, w)

    def stt(self, out, in0, scalar, in1, op0, op1, r, w):
        self.add("dve", lambda e: e.scalar_tensor_tensor(out, in0, scalar, in1, op0, op1), r, w)

    def tt(self, out, in0, in1, op, r, w, eng="dve"):
        self.add(eng, lambda e: e.tensor_tensor(out, in0, in1, op), r, w)

    def cp(self, out, in_, r, w, eng="dve"):
        if eng == "act":
            self.add("act", lambda e: e.copy(out, in_), r, w)
        else:
            self.add(eng, lambda e: e.tensor_copy(out, in_), r, w)

    def red(self, out, in_, op, r, w, axis=None):
        ax = AX.X if axis is None else axis
        self.add("dve", lambda e: e.tensor_reduce(out, in_, ax, op), r, w)

    def memset(self, out, val, w, eng="dve"):
        self.add(eng, lambda e: e.memset(out, val), (), w)

    def recip(self, out, in_, r, w):
        self.add("dve", lambda e: e.reciprocal(out, in_), r, w)

    def dma(self, q, out, in_, r, w, slot):
        self.add(q, lambda e: e.dma_start(out=out, in_=in_), r, w, dma=slot)

    def run(self):
        ops, ctx, nc = self.ops, self.ctx, self.ctx.nc
        ctx.phase_no += 1
        if ctx.stop_after is not None and ctx.phase_no > ctx.stop_after:
            return
        if ctx.stop_after is not None and ctx.phase_no == ctx.stop_after and ctx.max_ops is not None:
            print("[bisect] phase %d has %d ops, keeping %d; last kept: %s" % (
                ctx.phase_no, len(ops), ctx.max_ops, [(o["eng"], o["r"], o["w"], o["dma"]) for o in ops[max(0, ctx.max_ops - 2):ctx.max_ops]]))
            del ops[ctx.max_ops:]
        lastw, readers = {}, {}
        def _excl(b):
            return len(b) == 3 and b[:2] == "ps" and b[2].isdigit()
        for o in ops:
            xr = tuple(b for b in o["r"] if _excl(b))
            if xr:
                o["w"] = tuple(o["w"]) + xr
                o["r"] = tuple(b for b in o["r"] if not _excl(b))
        for i, o in enumerate(ops):
            deps = set()
            for b in o["r"]:
                if b in lastw:
                    deps.add(lastw[b])
            for b in o["w"]:
                if b in lastw:
                    deps.add(lastw[b])
                deps |= readers.get(b, set())
            deps.discard(i)
            o["deps"] = deps
            for d in deps:
                ops[d]["dep"] = True
            for b in o["r"]:
                readers.setdefault(b, set()).add(i)
            for b in o["w"]:
                lastw[b] = i
                readers[b] = set()
        touched = []
        for o in ops:
            if o["dma"] is not None:
                sem = ctx.dma_slot(o["dma"])
                ctx.dcnt[o["dma"]] += 16
                o["tok"] = ("d:" + o["dma"], sem, ctx.dcnt[o["dma"]])
                if o["dma"] not in touched:
                    touched.append(o["dma"])
            elif o["dep"]:
                e = o["eng"]
                ctx.ecnt[e] += 1
                o["tok"] = ("e:" + e, ctx.esem[e], ctx.ecnt[e])
            else:
                o["tok"] = None
        per = {e: [o for o in ops if o["eng"] == e] for e in ENGS}

        def mk(ename):
            def body(eng):
                kn = ctx.known[ename]
                for o in per[ename]:
                    for d in sorted(o["deps"]):
                        key, sem, val = ops[d]["tok"]
                        if kn.get(key, 0) < val:
                            eng.wait_ge(sem, val)
                            kn[key] = val
                    res = o["fn"](eng)
                    last = res[-1] if isinstance(res, (list, tuple)) else res
                    if o["tok"] is not None:
                        last.then_inc(o["tok"][1], 16 if o["dma"] is not None else 1)
                if ename == "sp":
                    for slot in touched:
                        key, val = "d:" + slot, ctx.dcnt[slot]
                        if kn.get(key, 0) < val:
                            eng.wait_ge(ctx.dsem[slot], val)
                            kn[key] = val
            return body

        with nc.Block() as blk:
            blk.tensor(mk("pe"))
            blk.scalar(mk("act"))
            blk.vector(mk("dve"))
            blk.gpsimd(mk("pool"))
            blk.sync(mk("sp"))


class Arena:
    def __init__(self, ap_all):
        self.a = ap_all

    def at(self, off_w, shape, dt=F32, parts=128):
        n = int(np.prod(shape[1:]))
        words = n if dt in (F32, I32) else (n + 1) // 2
        assert off_w + words <= ARENA_W, ("arena overflow", off_w, words)
        v = self.a[0:parts, off_w:off_w + words]
        if dt not in (F32,):
            v = v.bitcast(dt)
            if dt == BF16 and n % 2:
                v = v[:, 0:n]
        if len(shape) == 3:
            v = v.rearrange("p (a b) -> p a b", a=shape[1])
        elif len(shape) == 4:
            v = v.rearrange("p (a b c) -> p a b c", a=shape[1], b=shape[2])
        return v


class Bump:
    def __init__(self, arena, lo_kb, hi_kb):
        self.ar, self.p, self.hi = arena, int(lo_kb * 256), int(hi_kb * 256)

    def __call__(self, shape, dt=F32, parts=128):
        n = int(np.prod(shape[1:]))
        words = n if dt in (F32, I32) else (n + 1) // 2
        words = (words + 7) // 8 * 8
        v = self.ar.at(self.p, shape, dt, parts)
        self.p += words
        assert self.p <= self.hi, ("bump overflow", self.p, self.hi)
        return v


def build(cfg, stop_after=None, max_ops=None):
    T, NS, NT, NTP, NPG, NE = cfg.T, cfg.NS, cfg.NT, cfg.NTP, cfg.NPG, cfg.NE
    TB, TL = cfg.TB, cfg.TL
    TT = len(TL)
    NSL = NPG + 1
    nc = bass.Bass("TRN2", target_bir_lowering=False)

    def din(name, shape, dt=F32):
        return nc.dram_tensor(name, list(shape), dt, kind="ExternalInput").ap()

    def dout(name, shape, dt=F32):
        return nc.dram_tensor(name, list(shape), dt, kind="ExternalOutput").ap()

    xT_d = din("xT", [D, NT]); x_d = din("x", [NT, D])
    ck_d = din("cache_k", [cfg.NPHYS * 128, 512]); cv_d = din("cache_v", [cfg.NPHYS * 128, 512])
    pt_d = din("pt", [1, NS * NPG], I32)
    st_re_d = din("st_re", [128, 16 * NS]); st_im_d = din("st_im", [128, 16 * NS])
    w_in_d = din("w_in", [D, 2048]); w_out_d = din("w_out", [D, D])
    aP_d = din("aP", [128, 48])
    bP_d = din("bP", [128, 2 * 2048])
    cT_d = din("cT", [128, 2 * 2048])
    dP_d = din("dP", [128, 4])
    w_glu_d = din("w_glu", [512, 1024]); bglu_d = din("bglu", [128, 8])
    lam4_d = din("lam4", [1, 256])
    gsub_d = din("gsub", [1, 128]); gcol_d = din("gcol", [128, 1])
    ln_d = din("ln", [1, 4 * D])
    wr_d = din("wr", [D, 32 if NE <= 32 else NE]); br_d = din("br", [1, NE])
    wgu_d = din("wgu", [NE * 8 * 128, 2048]); bgu_d = din("bgu", [128, NE * 16])
    wdn_d = din("wdn", [NE * 8 * 128, 1024]); bdn_d = din("bdn", [NE, D])
    ropeC_d = din("ropeC", [NT, 32]); ropeS_d = din("ropeS", [NT, 32])
    ident_d = din("ident", [128, 128]); tri_d = din("tri", [128, 128])
    selB_d = din("selB", [NS, NS * 128])
    pidx_d = din("pidx", [128, 1]); negm_d = din("negm", [128, 1])

    y_d = dout("y", [NT, D]); ko_d = dout("ko", [NT, 512]); vo_d = dout("vo", [NT, 512])
    sre_d = dout("sre", [128, 16 * (1 + NS)]); sim_d = dout("sim", [128, 16 * (1 + NS)])

    stack = ExitStack()
    with stack:
        arena_t = stack.enter_context(nc.sbuf_tensor("arena", [128, ARENA_W], F32))
        ps_t = stack.enter_context(nc.psum_tensor("ps", [128, 4096], F32))
        ar = Arena(arena_t)
        ctx = Ctx(nc, stack)
        ctx.stop_after = stop_after
        ctx.max_ops = max_ops

        def bank(b, n=512):
            return ps_t[:, b * 512:b * 512 + n]

        def bank_bf(b):
            return ps_t[:, b * 512:(b + 1) * 512].bitcast(BF16)

        KB = 256
        cb = Bump(ar, 0, 10)
        ident = cb([128, 128]); identb = cb([128, 128], BF16); tri = cb([128, 128]); onesf = cb([128, 128])
        cosT = cb([128, TT, 32]); sinT = cb([128, TT, 32])
        gsub = cb([128, 128]); gcol = cb([128, 1]); lam_t = cb([128, 1]); nlam_t = cb([128, 1])
        pidx = cb([128, 1]); negm = cb([128, 1]); dP = cb([128, 4]); bglu = cb([128, 8])
        aoT = ar.at(10 * KB, [128, 4, NT], BF16)
        soT = ar.at(int(26.5 * KB), [128, 4, NT], BF16)
        actT = ar.at(10 * KB, [128, 8, NT], BF16)
        uT = ar.at(43 * KB, [128, 4, NT])
        gss = ar.at(76 * KB, [128, 4, NT], BF16)
        qs = ar.at(76 * KB, [128, 512])
        qT = ar.at(int(94.5 * KB), [128, 4, NT], BF16)
        kT = ar.at(int(94.5 * KB) + 2 * NT, [128, 4, NT], BF16)
        vbf = ar.at(int(127.5 * KB), [128, max(NTP, 1), 512], BF16)
        fT = ar.at(43 * KB, [128, 8, NT])
        hTb = ar.at(109 * KB, [128, 8, NT], BF16)
        gatesT = ar.at(142 * KB, [128, NT])

        ph = Phase(ctx)
        tb_ = Bump(ar, 150, 207)
        l4 = tb_([128, 256]); pr = tb_([128, 128]); sm = tb_([128, 2]); ee = tb_([128, 2])
        ph.dma("sp", ident, ident_d, (), ("ident",), "c0")
        ph.dma("sp", tri, tri_d, (), ("tri",), "c1")
        ph.dma("sp", gsub, gsub_d.broadcast_to([128, 128]), (), ("gsub",), "c2")
        ph.dma("sp", gcol, gcol_d, (), ("gcol",), "c3")
        ph.dma("sp", pidx, pidx_d, (), ("pidx",), "c4")
        ph.dma("sp", negm, negm_d, (), ("negm",), "c5")
        ph.dma("sp", dP, dP_d, (), ("dP",), "c6")
        ph.dma("sp", bglu, bglu_d, (), ("bglu",), "c7")
        ph.dma("sp", l4, lam4_d.broadcast_to([128, 256]), (), ("l4",), "c8")
        if NTP:
            ph.dma("sp", cosT[:, 0:NTP, :], ropeC_d[0:T, :].rearrange("(i p) f -> p i f", p=128), (), ("cosT",), "c9")
            ph.dma("sp", sinT[:, 0:NTP, :], ropeS_d[0:T, :].rearrange("(i p) f -> p i f", p=128), (), ("sinT",), "c10")
        ph.dma("sp", cosT[0:NS, NTP, :], ropeC_d[T:NT, :], (), ("cosTs",), "c11")
        ph.dma("sp", sinT[0:NS, NTP, :], ropeS_d[T:NT, :], (), ("sinTs",), "c12")
        ph.cp(identb, ident, ("ident",), ("identb",))
        ph.memset(onesf, 1.0, ("onesf",))
        ph.tt(pr.rearrange("p (a b) -> p a b", a=2), l4.rearrange("p (a two b) -> p a two b", a=2, two=2)[:, :, 0, :],
              l4.rearrange("p (a two b) -> p a two b", a=2, two=2)[:, :, 1, :], ALU.mult, ("l4",), ("pr",))
        ph.red(sm, pr.rearrange("p (a b) -> p a b", a=2), ALU.add, ("pr",), ("sm",))
        ph.act(ee, sm, AF.Exp, ("sm",), ("ee",))
        ph.tt(lam_t, ee[:, 0:1], ee[:, 1:2], ALU.subtract, ("ee",), ("lam0",))
        ph.ts(lam_t, lam_t, LAM_INIT, ALU.add, ("lam0",), ("lam",))
        ph.ts(nlam_t, lam_t, -1.0, ALU.mult, ("lam",), ("nlam",))
        ph.run()

        ph = Phase(ctx)
        xTb = ar.at(int(143.5 * KB), [128, 8, NT], BF16)
        sb = Bump(ar, 78, 94.5)
        xst = [sb([128, NT]), sb([128, NT])]
        wb_ = Bump(ar, 26.5, 43)
        wpc = [wb_([128, 8, 512], BF16), wb_([128, 8, 512], BF16)]
        tb_ = Bump(ar, 176.5, 207)
        wst = [tb_([128, 512]), tb_([128, 512])]
        ev = [tb_([128, 512]), tb_([128, 512])]
        ta = tb_([128, 256]); tbb = tb_([128, 256])
        for kc in range(8):
            s = kc % 2
            ph.dma("sp", xst[s], xT_d[kc * 128:(kc + 1) * 128, :], (), ("xst%d" % s,), "xst%d" % s)
            ph.cp(xTb[:, kc, :], xst[s], ("xst%d" % s,), ("xTb%d" % kc,), eng="act" if kc % 2 else "dve")
        xall = tuple("xTb%d" % k for k in range(8))
        nev = 0
        for grp in range(4):
            pw = wpc[grp % 2]
            pwn = "wpc%d" % (grp % 2)
            for kc in range(8):
                s = kc % 2
                ph.dma("act" if kc % 2 else "sp", wst[s], w_in_d[kc * 128:(kc + 1) * 128, grp * 512:(grp + 1) * 512],
                       (), ("wst%d" % s,), "wst%d" % s)
                ph.cp(pw[:, kc, :], wst[s], ("wst%d" % s,), (pwn,), eng="pool")
            if grp == 0:
                for c in range(4):
                    for bi, (t0, n) in enumerate(TB):
                        b = (c * len(TB) + bi) % 2
                        ph.mm([(bank(b, n), pw[:, kc, c * 128:(c + 1) * 128], xTb[:, kc, t0:t0 + n], kc == 0, kc == 7)
                               for kc in range(8)], xall + (pwn,), ("ps%d" % b,))
                        ph.cp(uT[:, c, t0:t0 + n], bank(b, n), ("ps%d" % b,), ("uT%d_%d" % (c, bi),), eng="act")
                continue
            for i, (t0, n) in enumerate(TL):
                b = 2 + (i % 2)
                pb = ps_t[0:n, b * 512:(b + 1) * 512]
                ph.mm([(pb, xTb[:, kc, t0:t0 + n], pw[:, kc, :], kc == 0, kc == 7) for kc in range(8)],
                      xall + (pwn,), ("ps%d" % b,))
                e_ = ev[nev % 2]; en = "ev%d" % (nev % 2); nev += 1
                eo = e_[0:n, :]
                if grp == 3:
                    ph.cp(eo, pb, ("ps%d" % b,), (en,), eng="act")
                    ph.dma("pool", vo_d[t0:t0 + n, :], eo, (en,), ("vo%d" % i,), "st_" + en)
                    if i < NTP:
                        ph.cp(vbf[:, i, :], pb, ("ps%d" % b,), ("vbf%d" % i,))
                    continue
                pv = pb.rearrange("p (g two f) -> p g two f", g=8, two=2)
                ov = eo.rearrange("p (g two f) -> p g two f", g=8, two=2)
                cs = cosT[0:n, i:i + 1, :].broadcast_to([n, 8, 32]); sn = sinT[0:n, i:i + 1, :].broadcast_to([n, 8, 32])
                t1 = ta[0:n, :].rearrange("p (g f) -> p g f", g=8); t2 = tbb[0:n, :].rearrange("p (g f) -> p g f", g=8)
                rp = ("ps%d" % b, "cosT", "sinT", "cosTs", "sinTs")
                ph.tt(t1, pv[:, :, 0, :], cs, ALU.mult, rp, ("ta",))
                ph.tt(t2, pv[:, :, 1, :], sn, ALU.mult, rp, ("tb",))
                ph.tt(ov[:, :, 0, :], t1, t2, ALU.subtract, ("ta", "tb"), (en,))
                ph.tt(t1, pv[:, :, 1, :], cs, ALU.mult, rp, ("ta",))
                ph.tt(t2, pv[:, :, 0, :], sn, ALU.mult, rp, ("tb",))
                ph.tt(ov[:, :, 1, :], t1, t2, ALU.add, ("ta", "tb"), (en + "b",))
                if grp == 2:
                    ph.dma("pool", ko_d[t0:t0 + n, :], eo, (en, en + "b"), ("ko%d" % i,), "st_" + en)
                if i >= NTP:
                    if grp == 1:
                        ph.cp(qs[0:n, :], eo, (en, en + "b"), ("qs",), eng="act")
                    continue
                tb2 = i % 2
                ph.tr([(bank(tb2)[:, h * 128:(h + 1) * 128], eo[:, h * 128:(h + 1) * 128], ident) for h in range(4)],
                      (en, en + "b", "ident"), ("ps%d" % tb2,))
                dst = qT if grp == 1 else kT
                ph.cp(dst[:, :, t0:t0 + 128], bank(tb2).rearrange("p (h t) -> p h t", h=4), ("ps%d" % tb2,),
                      ("%sT%d" % ("q" if grp == 1 else "k", i),), eng="act" if i % 2 else "dve")
        ph.run()

        if NTP:
            ph = Phase(ctx)
            tb_ = Bump(ar, 143.5, 207)
            Psb = [tb_([128, T], BF16), tb_([128, T], BF16)]
            PTs = [tb_([128, T], BF16), tb_([128, T], BF16)]
            mx = tb_([128, 2]); nb = tb_([128, 2]); lsum = tb_([128, 2]); rl = tb_([128, 2])
            tS = tb_([128, 128]); att = tb_([128, 128]); junk = tb_([128, 128]); ss = tb_([128, 1]); rs = tb_([128, 1])
            for h in range(4):
                for qt in range(NTP):
                    nk = qt + 1
                    small = nk <= 8
                    for m in range(2):
                        pn, tn = "P%d" % m, "PT%d" % m
                        sb0 = 2 * m if small else 0
                        SBK = tuple("ps%d" % (sb0 + k) for k in range(2 if small else 4))
                        Sv = ps_t[:, sb0 * 512:sb0 * 512 + (1024 if small else 2048)]
                        if small:
                            PTv = ps_t[:, (4 + m) * 512:(5 + m) * 512].bitcast(BF16); PTK = ("ps%d" % (4 + m),)
                        else:
                            PTv = ps_t[:, 2048:3072].bitcast(BF16); PTK = ("ps4", "ps5")
                        Om = bank(6 + m)[:, 0:128]; OK_ = "ps%d" % (6 + m)
                        items = []
                        for j in range(0, nk, 4):
                            cols = min(4, nk - j) * 128
                            items.append((Sv[:, j * 128:j * 128 + cols], qT[m * 64:(m + 1) * 64, h, qt * 128:(qt + 1) * 128],
                                          kT[m * 64:(m + 1) * 64, h, j * 128:j * 128 + cols], True, True))
                        ph.mm(items, ("qT%d" % qt,) + tuple("kT%d" % k for k in range(nk)), SBK)
                        ph.tt(Sv[:, qt * 128:(qt + 1) * 128], Sv[:, qt * 128:(qt + 1) * 128], tri, ALU.add, SBK + ("tri",), SBK)
                        ph.red(mx[:, m:m + 1], Sv[:, 0:nk * 128], ALU.max, SBK, ("mx%d" % m,))
                        ph.ts(nb[:, m:m + 1], mx[:, m:m + 1], -0.125, ALU.mult, ("mx%d" % m,), ("nb%d" % m,))
                        ph.act(Psb[m][:, 0:nk * 128], Sv[:, 0:nk * 128], AF.Exp, SBK + ("nb%d" % m,), (pn, "l%d" % m),
                               bias=nb[:, m:m + 1], scale=0.125, accum=lsum[:, m:m + 1])
                        ph.tr([(PTv[:, k * 128:(k + 1) * 128], Psb[m][:, k * 128:(k + 1) * 128], identb) for k in range(nk)],
                              (pn, "identb"), PTK)
                        ph.cp(PTs[m][:, 0:nk * 128], PTv[:, 0:nk * 128], PTK, (tn,), eng="act" if m else "dve")
                        ph.mm([(Om, PTs[m][:, k * 128:(k + 1) * 128], vbf[:, k, h * 128:(h + 1) * 128],
                                k == 0, k == nk - 1) for k in range(nk)], (tn,) + tuple("vbf%d" % k for k in range(nk)), (OK_,))
                    ph.recip(rl, lsum, ("l0", "l1"), ("rl",))
                    ph.tt(rl[:, 1:2], rl[:, 1:2], lam_t, ALU.mult, ("rl", "lam"), ("rl2",))
                    ph.ts(tS, bank(7)[:, 0:128], rl[:, 1:2], ALU.mult, ("ps7", "rl2"), ("tS",))
                    ph.stt(att, bank(6)[:, 0:128], rl[:, 0:1], tS, ALU.mult, ALU.subtract, ("ps6", "rl", "tS"), ("att",))
                    ph.act(junk, att, AF.Square, ("att",), ("junk", "ss"), accum=ss)
                    ph.ts(ss, ss, 1.0 / 128, ALU.mult, ("ss",), ("ss1",), s2=RMS_EPS, op1=ALU.add)
                    ph.act(ss, ss, AF.Sqrt, ("ss1",), ("ss2",))
                    ph.recip(rs, ss, ("ss2",), ("rs",))
                    ph.ts(att, att, rs, ALU.mult, ("att", "rs"), ("att1",), s2=1.0 - LAM_INIT, op1=ALU.mult)
                    ph.tt(att, att, gsub, ALU.mult, ("att1", "gsub"), ("att2",))
                    ph.tr([(bank(6)[:, 256:384], att, ident)], ("att2", "ident"), ("ps6",))
                    ph.cp(aoT[:, h, qt * 128:(qt + 1) * 128], bank(6)[:, 256:384], ("ps6",), ("aoT%d_%d" % (h, qt),), eng="act")
            ph.run()

        ph = Phase(ctx)
        tb_ = Bump(ar, 94.5, 207)
        selB = tb_([128, NS * 128])
        pti = tb_([128, NS * NPG], I32); ptf = tb_([128, NS * NPG]); idx = tb_([128, NS * NPG], I32)
        NR = 3
        Kp = [tb_([128, 4, 512]) for _ in range(NR)]
        Vp = [tb_([128, 4, 512]) for _ in range(NR)]
        Ksf = [tb_([128, 512]), tb_([128, 512])]; Vsf = [tb_([128, 512]), tb_([128, 512])]
        prod = [tb_([128, 512]), tb_([128, 512])]
        Ss = tb_([128, NS, NSL, 8])
        mp = tb_([128, NS * 8]); gmx = tb_([128, 1]); dg = tb_([128, NS * 8]); rls = tb_([128, NS * 8])
        t1s = tb_([128, NS, 2]); atts = tb_([128, 4, NS]); sqs = tb_([128, 4 * NS]); rss = tb_([128, 4 * NS])
        H8 = NS * 8
        OTB = ("ps3", "ps4", "ps5", "ps6", "ps7")
        ph.dma("sp", selB[0:NS, :], selB_d, (), ("selB",), "d0")
        ph.dma("sp", pti, pt_d.broadcast_to([128, NS * NPG]), (), ("pti",), "d1")
        ph.cp(ptf, pti, ("pti",), ("ptf",))
        ph.ts(ptf, ptf, 128.0, ALU.mult, ("ptf", "pidx"), ("ptf2",), s2=pidx, op1=ALU.add)
        ph.cp(idx, ptf, ("ptf2",), ("idx",))
        for s in range(2):
            ph.memset(Ksf[s], 0.0, ("Ksf%d" % s,)); ph.memset(Vsf[s], 0.0, ("Vsf%d" % s,))
        ngr = (NPG + 3) // 4
        gi = 0
        for b in range(NS):
            qb = b % 2
            ph.mm([(bank(qb), selB[0:NS, b * 128:(b + 1) * 128], qs[0:NS, :], True, True)], ("selB", "qs"), ("ps%d" % qb,))
            for g in range(ngr):
                r = gi % NR; gi += 1
                for jj in range(min(4, NPG - g * 4)):
                    j = g * 4 + jj
                    col = b * NPG + j
                    bn = "Kp%d_%d" % (r, jj)
                    ph.add("pool", (lambda e, o=Kp[r][:, jj, :], ia=idx[:, col:col + 1]: e.indirect_dma_start(
                        out=o, out_offset=None, in_=ck_d, in_offset=bass.IndirectOffsetOnAxis(ap=ia, axis=0))),
                        ("idx",), (bn,), dma=bn)
                    p_ = prod[j % 2]; pn = "prod%d" % (j % 2)
                    ph.tt(p_, Kp[r][:, jj, :], bank(qb), ALU.mult, (bn, "ps%d" % qb), (pn,))
                    ph.red(Ss[:, b, j, :], p_.rearrange("p (g f) -> p g f", g=8), ALU.add, (pn,), ("Ss%d" % b,))
            s = b % 2
            ph.dma("sp", Ksf[s][0:1, :], ko_d[T + b:T + b + 1, :], ("ko%d" % NTP,), ("Ksf%d" % s,), "ksf%d" % s)
            ph.tt(prod[0], Ksf[s], bank(qb), ALU.mult, ("Ksf%d" % s, "ps%d" % qb), ("prod0",))
            ph.red(Ss[:, b, NPG, :], prod[0].rearrange("p (g f) -> p g f", g=8), ALU.add, ("prod0",), ("Ss%d" % b,))
        allS = tuple("Ss%d" % b for b in range(NS))
        ph.ts(Ss[:, :, NPG, :], Ss[:, :, NPG, :], negm, ALU.add, allS + ("negm",), ("SsA",))
        ph.red(mp.rearrange("p (b h) -> p b h", b=NS), Ss.rearrange("p b s h -> p b h s"), ALU.max, ("SsA",), ("mp",))
        ph.tr([(bank(2)[0:H8, 0:128], mp, ident)], ("mp", "ident"), ("ps2",))
        ph.red(gmx[0:H8, :], bank(2)[0:H8, 0:128], ALU.max, ("ps2",), ("gmx",))
        ph.ts(dg[0:H8, :], ident[0:H8, 0:H8], gmx[0:H8, :], ALU.mult, ("gmx", "ident"), ("dg",))
        ph.mm([(bank(2)[:, 0:H8], onesf[0:H8, :], dg[0:H8, :], True, True)], ("dg", "onesf"), ("ps2",))
        ph.tt(Ss, Ss, bank(2)[:, 0:H8].rearrange("p (b o h) -> p b o h", b=NS, o=1).broadcast_to([128, NS, NSL, 8]),
              ALU.subtract, ("SsA", "ps2"), ("SsB",))
        ph.act(Ss, Ss, AF.Exp, ("SsB",), ("P",), scale=0.125)
        gi = 0
        for b in range(NS):
            for g in range(ngr):
                r = gi % NR; gi += 1
                for jj in range(min(4, NPG - g * 4)):
                    j = g * 4 + jj
                    col = b * NPG + j
                    bn = "Vp%d_%d" % (r, jj)
                    ph.add("pool", (lambda e, o=Vp[r][:, jj, :], ia=idx[:, col:col + 1]: e.indirect_dma_start(
                        out=o, out_offset=None, in_=cv_d, in_offset=bass.IndirectOffsetOnAxis(ap=ia, axis=0))),
                        ("idx",), (bn,), dma=bn)
                    items = [(bank(3 + h)[:, b * 2:b * 2 + 2], Vp[r][:, jj, h * 128:(h + 1) * 128], Ss[:, b, j, 2 * h:2 * h + 2],
                              j == 0, False) for h in range(4)]
                    items.append((bank(7)[:, b * 8:b * 8 + 8], onesf, Ss[:, b, j, :], j == 0, False))
                    ph.mm(items, (bn, "P", "onesf"), OTB)
            s = b % 2
            ph.dma("sp", Vsf[s][0:1, :], vo_d[T + b:T + b + 1, :], ("vo%d" % NTP,), ("Vsf%d" % s,), "vsf%d" % s)
            items = [(bank(3 + h)[:, b * 2:b * 2 + 2], Vsf[s][:, h * 128:(h + 1) * 128], Ss[:, b, NPG, 2 * h:2 * h + 2],
                      False, True) for h in range(4)]
            items.append((bank(7)[:, b * 8:b * 8 + 8], onesf, Ss[:, b, NPG, :], False, True))
            ph.mm(items, ("Vsf%d" % s, "P", "onesf"), OTB)
        ph.recip(rls, bank(7)[:, 0:H8], ("ps7",), ("rls",))
        rl4 = rls.rearrange("p (b h m) -> p b h m", b=NS, h=4)
        for h in range(4):
            ph.tt(t1s, bank(3 + h)[:, 0:2 * NS].rearrange("p (b m) -> p b m", b=NS), rl4[:, :, h, :], ALU.mult,
                  ("ps%d" % (3 + h), "rls"), ("t1s",))
            ph.stt(atts[:, h, :], t1s[:, :, 1], nlam_t, t1s[:, :, 0], ALU.mult, ALU.add, ("t1s", "nlam"), ("atts%d" % h,))
        alla = tuple("atts%d" % h for h in range(4))
        af = atts.rearrange("p h b -> p (h b)")
        ph.tt(sqs, af, af, ALU.mult, alla, ("sqs",))
        ph.mm([(bank(2)[:, 0:4 * NS], onesf, sqs, True, True)], ("sqs", "onesf"), ("ps2",))
        ph.ts(rss, bank(2)[:, 0:4 * NS], 1.0 / 128, ALU.mult, ("ps2",), ("rss0",), s2=RMS_EPS, op1=ALU.add)
        ph.act(rss, rss, AF.Sqrt, ("rss0",), ("rss1",))
        ph.recip(rss, rss, ("rss1",), ("rss2",))
        ph.tt(af, af, rss, ALU.mult, alla + ("rss2",), ("attn",))
        ph.ts(aoT[:, :, T:NT], atts, gcol, ALU.mult, ("attn", "gcol"), ("aoTs",), s2=1.0 - LAM_INIT, op1=ALU.mult)
        ph.run()

        E_LO = 92.5
        pb_ = Bump(ar, E_LO, 207)
        WB = pb_([128, 2, 16, 128])
        CTr = pb_([128, 16, 128], BF16); CTi = pb_([128, 16, 128], BF16)
        magP = pb_([128, 16]); ec1 = pb_([128, 16]); es1 = pb_([128, 16]); lbrP = pb_([128, 16, 1]); lbiP = pb_([128, 16, 1])
        sout = [pb_([128, 16, 1 + NS]), pb_([128, 16, 1 + NS])]
        e_mark = pb_.p
        ph = Phase(ctx)
        tb_ = Bump(ar, e_mark / 256.0, 207)
        aPt = tb_([128, 48]); bPt = tb_([128, 2, 16, 128]); cst = tb_([128, 2048])
        BB = tb_([128, 2, 16, 128]); w2 = tb_([128, 16, 128]); w3 = tb_([128, 16, 128])
        ph.dma("sp", aPt, aP_d, (), ("Pin",), "e0")
        ph.dma("sp", bPt[:, 0], bP_d[:, 0:2048].rearrange("p (a b) -> p a b", a=16), (), ("bPt0",), "e1")
        ph.dma("act", bPt[:, 1], bP_d[:, 2048:4096].rearrange("p (a b) -> p a b", a=16), (), ("bPt1",), "e2")
        ph.dma("sp", cst, cT_d[:, 0:2048], (), ("cst",), "e3")
        ph.cp(CTr.rearrange("p a b -> p (a b)"), cst, ("cst",), ("CTr",))
        ph.dma("sp", cst, cT_d[:, 2048:4096], ("CTr",), ("cst2",), "e3")
        ph.act(CTi.rearrange("p a b -> p (a b)"), cst, AF.Copy, ("cst2",), ("CTi",), scale=-1.0)

        def disc(tag, a_re, a_im, ldt, W, want_f):
            keys = ("ar", "dt", "xr", "th", "acc", "tmp", "s", "a", "x2", "u", "s2", "a2")
            t = {k: tb_([128, W]) for k in keys}
            N = lambda k: tag + k
            IN = tag + "in"

            def TS(o, i, s1, op0, s2=None, op1=None, extra=()):
                ph.ts(t[o], t[i], s1, op0, (N(i),) + extra, (N(o),), s2=s2, op1=op1)

            def TT(o, i0, i1, op):
                ph.tt(t[o], t[i0], t[i1], op, (N(i0), N(i1)), (N(o),))

            ph.ts(t["ar"], a_re, -1e-4, ALU.min, (IN,), (N("ar"),))
            ph.act(t["dt"], ldt, AF.Exp, (IN,), (N("dt"),))
            TT("xr", "ar", "dt", ALU.mult)
            ph.tt(t["th"], a_im, t["dt"], ALU.mult, (IN, N("dt")), (N("th"),))
            TS("acc", "xr", 0.1, ALU.mult, s2=1.0, op1=ALU.add)
            for k in range(9, 0, -1):
                TT("tmp", "xr", "acc", ALU.mult)
                if k > 1:
                    TS("acc", "tmp", 1.0 / k, ALU.mult, s2=1.0, op1=ALU.add)
            TS("acc", "tmp", 1.0, ALU.add)
            TS("u", "th", 1.0 / 1024, ALU.mult)
            TT("x2", "u", "u", ALU.mult)
            TS("s", "x2", -1.0 / 20, ALU.mult, s2=1.0, op1=ALU.add)
            TT("s", "s", "x2", ALU.mult)
            TS("s", "s", -1.0 / 6, ALU.mult, s2=1.0, op1=ALU.add)
            TT("s", "s", "u", ALU.mult)
            TS("a", "x2", -1.0 / 30, ALU.mult, s2=1.0, op1=ALU.add)
            TT("a", "a", "x2", ALU.mult)
            TS("a", "a", -1.0 / 12, ALU.mult, s2=1.0, op1=ALU.add)
            TT("a", "a", "x2", ALU.mult)
            TS("a", "a", 0.5, ALU.mult)
            s_, a_, s2_, a2_ = "s", "a", "s2", "a2"
            for _ in range(10):
                TS("x2", a_, -1.0, ALU.mult, s2=1.0, op1=ALU.add)
                ph.stt(t[a2_], t[s_], 2.0, t[s_], ALU.mult, ALU.mult, (N(s_),), (N(a2_),))
                ph.stt(t[s2_], t[s_], 2.0, t["x2"], ALU.mult, ALU.mult, (N(s_), N("x2")), (N(s2_),))
                s_, s2_, a_, a2_ = s2_, s_, a2_, a_
            res = dict(mag=t["acc"], magn=N("acc"), s=t[s_], sn=N(s_), a=t[a_], an=N(a_))
            if want_f:
                TT("x2", "acc", a_, ALU.mult)
                TT("x2", "tmp", "x2", ALU.subtract)
                TT("th", "acc", s_, ALU.mult)
                TT("dt", "ar", "ar", ALU.mult)
                ph.tt(t["xr"], a_im, a_im, ALU.mult, (IN,), (N("xr"),))
                TT("dt", "dt", "xr", ALU.add)
                ph.recip(t["dt"], t["dt"], (N("dt"),), (N("dt"),))
                TT("xr", "x2", "ar", ALU.mult)
                ph.tt(t["u"], t["th"], a_im, ALU.mult, (N("th"), IN), (N("u"),))
                TT("xr", "xr", "u", ALU.add)
                TT("xr", "xr", "dt", ALU.mult)
                TT(s2_, "th", "ar", ALU.mult)
                ph.tt(t["u"], t["x2"], a_im, ALU.mult, (N("x2"), IN), (N("u"),))
                TT(s2_, s2_, "u", ALU.subtract)
                TT(s2_, s2_, "dt", ALU.mult)
                res.update(fre=t["xr"], fren=N("xr"), fim=t[s2_], fimn=N(s2_))
            return res

        rP = disc("P", aPt[:, 0:16], aPt[:, 16:32], aPt[:, 32:48], 16, True)
        fre = rP["fre"].unsqueeze(2).broadcast_to([128, 16, 128]); fim = rP["fim"].unsqueeze(2).broadcast_to([128, 16, 128])
        ph.tt(w2, bPt[:, 0], fre, ALU.mult, (rP["fren"], "bPt0"), ("w2",))
        ph.tt(w3, bPt[:, 1], fim, ALU.mult, (rP["fimn"], "bPt1"), ("w3",))
        ph.tt(BB[:, 0], w2, w3, ALU.subtract, ("w2", "w3"), ("BB0",))
        ph.tt(w2, bPt[:, 1], fre, ALU.mult, (rP["fren"], "bPt1"), ("w2",))
        ph.tt(w3, bPt[:, 0], fim, ALU.mult, (rP["fimn"], "bPt0"), ("w3",))
        ph.tt(BB[:, 1], w2, w3, ALU.add, ("w2", "w3"), ("BB1",))
        for ri_ in range(2):
            for g4 in range(4):
                bk = (ri_ * 4 + g4) % 4
                ph.tr([(bank(bk)[:, q * 128:(q + 1) * 128], BB[:, ri_, g4 * 4 + q, :], ident) for q in range(4)],
                      ("BB%d" % ri_, "ident"), ("ps%d" % bk,))
                ph.cp(WB[:, ri_, g4 * 4:g4 * 4 + 4, :], bank(bk).rearrange("p (q f) -> p q f", q=4), ("ps%d" % bk,), ("WB",),
                      eng="act" if g4 % 2 else "dve")
        cP = tb_([128, 16]); nP = tb_([128, 16]); n2 = tb_([128, 16])
        ph.ts(cP, rP["a"], -1.0, ALU.mult, (rP["an"],), ("cP",), s2=1.0, op1=ALU.add)
        ph.tt(nP, cP, cP, ALU.mult, ("cP",), ("nP",))
        ph.tt(n2, rP["s"], rP["s"], ALU.mult, (rP["sn"],), ("n2",))
        ph.tt(nP, nP, n2, ALU.add, ("nP", "n2"), ("nP",))
        ph.ts(nP, nP, -0.5, ALU.mult, ("nP",), ("nP",), s2=1.5, op1=ALU.add)
        ph.tt(ec1, cP, nP, ALU.mult, ("cP", "nP"), ("ec1",))
        ph.tt(es1, rP["s"], nP, ALU.mult, (rP["sn"], "nP"), ("es1",))
        ph.cp(magP, rP["mag"], (rP["magn"],), ("magP",))
        ph.tt(lbrP.rearrange("p a b -> p (a b)"), rP["mag"], ec1, ALU.mult, (rP["magn"], "ec1"), ("lbrP",))
        ph.tt(lbiP.rearrange("p a b -> p (a b)"), rP["mag"], es1, ALU.mult, (rP["magn"], "es1"), ("lbiP",))
        ph.run()

        if NTP:
            ph = Phase(ctx)
            tb_ = Bump(ar, e_mark / 256.0, 207)
            EcB = [tb_([128, T]), tb_([128, T])]; EsB = [tb_([128, T]), tb_([128, T])]
            zr = tb_([128, T]); zi = tb_([128, T]); rr = tb_([128, T]); ri = tb_([128, T])
            TW = max(512, T // 2)
            tA = tb_([128, TW]); tBt = tb_([128, TW]); pA = tb_([128, 512]); pB = tb_([128, 512])
            Sr = tb_([128, T], BF16); Si = tb_([128, T], BF16)
            en_ = [tb_([128, 2]), tb_([128, 2])]; e2_ = tb_([128, 2])
            PB = [(t0, n) for (t0, n) in TB if t0 < T]
            LOGT = int(math.log2(T))
            assert 1 << LOGT == T
            for gp in range(16):
                c = gp // 4
                Ec, Es = EcB[gp % 2], EsB[gp % 2]
                ecn, esn = "Ec%d" % (gp % 2), "Es%d" % (gp % 2)
                ph.memset(Ec[:, 0:1], 1.0, (ecn,)); ph.memset(Es[:, 0:1], 0.0, (esn,))
                ph.cp(en_[0][:, 0:1], ec1[:, gp:gp + 1], (), ("en0",)); ph.cp(en_[0][:, 1:2], es1[:, gp:gp + 1], (), ("en0",))
                for k in range(LOGT):
                    n = 1 << k
                    e0, e1_ = en_[k % 2], en_[(k + 1) % 2]
                    n0, n1 = "en%d" % (k % 2), "en%d" % ((k + 1) % 2)
                    ph.ts(tA[:, 0:n], Es[:, 0:n], e0[:, 1:2], ALU.mult, (esn, n0), ("tA",))
                    ph.stt(Ec[:, n:2 * n], Ec[:, 0:n], e0[:, 0:1], tA[:, 0:n], ALU.mult, ALU.subtract, (ecn, n0, "tA"), (ecn,))
                    ph.ts(tBt[:, 0:n], Es[:, 0:n], e0[:, 0:1], ALU.mult, (esn, n0), ("tB",))
                    ph.stt(Es[:, n:2 * n], Ec[:, 0:n], e0[:, 1:2], tBt[:, 0:n], ALU.mult, ALU.add, (ecn, n0, "tB"), (esn,))
                    if k < LOGT - 1:
                        ph.tt(e2_, e0, e0, ALU.mult, (n0,), ("e2",))
                        ph.tt(e1_[:, 0:1], e2_[:, 0:1], e2_[:, 1:2], ALU.subtract, ("e2",), (n1,))
                        ph.stt(e1_[:, 1:2], e0[:, 0:1], 2.0, e0[:, 1:2], ALU.mult, ALU.mult, (n0,), (n1,))
                for bi, (t0, n) in enumerate(PB):
                    xb = 2 * (bi % 2)
                    ph.mm([(bank(xb, n), WB[:, 0, gp, :], uT[:, c, t0:t0 + n], True, True),
                           (bank(xb + 1, n), WB[:, 1, gp, :], uT[:, c, t0:t0 + n], True, True)],
                          (), ("ps%d" % xb, "ps%d" % (xb + 1)))
                    xn0, xn1 = "ps%d" % xb, "ps%d" % (xb + 1)
                    sl = slice(t0, t0 + n)
                    a_, b_ = tA[:, 0:n], tBt[:, 0:n]
                    ph.tt(a_, bank(xb, n), Ec[:, sl], ALU.mult, (xn0, ecn), ("tA",))
                    ph.tt(b_, bank(xb + 1, n), Es[:, sl], ALU.mult, (xn1, esn), ("tB",))
                    ph.tt(zr[:, sl], a_, b_, ALU.add, ("tA", "tB"), ("zr",))
                    ph.tt(a_, bank(xb + 1, n), Ec[:, sl], ALU.mult, (xn1, ecn), ("tA",))
                    ph.tt(b_, bank(xb, n), Es[:, sl], ALU.mult, (xn0, esn), ("tB",))
                    ph.tt(zi[:, sl], a_, b_, ALU.subtract, ("tA", "tB"), ("zi",))
                dec = magP[:, gp:gp + 1].broadcast_to([128, T])
                ph.add("dve", (lambda e, o=rr, d0=dec, d1=zr: e.tensor_tensor_scan(o, d0, d1, 0.0, ALU.mult, ALU.add)), ("zr",), ("rr",))
                ph.add("dve", (lambda e, o=ri, d0=dec, d1=zi: e.tensor_tensor_scan(o, d0, d1, 0.0, ALU.mult, ALU.add)), ("zi",), ("ri",))
                for bi, (t0, n) in enumerate(PB):
                    sl = slice(t0, t0 + n)
                    a_, b_ = pA[:, 0:n], pB[:, 0:n]
                    last = (t0 + n == T)
                    ph.tt(a_, rr[:, sl], Ec[:, sl], ALU.mult, ("rr", ecn), ("pA",), eng="pool")
                    ph.tt(b_, ri[:, sl], Es[:, sl], ALU.mult, ("ri", esn), ("pB",), eng="pool")
                    ph.tt(Sr[:, sl], a_, b_, ALU.subtract, ("pA", "pB"), ("Sr%d" % bi,), eng="pool")
                    if last:
                        ph.tt(sout[0][:, gp, 0:1], pA[:, n - 1:n], pB[:, n - 1:n], ALU.subtract, ("pA", "pB"), ("so_re",), eng="pool")
                    ph.tt(a_, ri[:, sl], Ec[:, sl], ALU.mult, ("ri", ecn), ("pA",), eng="pool")
                    ph.tt(b_, rr[:, sl], Es[:, sl], ALU.mult, ("rr", esn), ("pB",), eng="pool")
                    ph.tt(Si[:, sl], a_, b_, ALU.add, ("pA", "pB"), ("Si%d" % bi,), eng="pool")
                    if last:
                        ph.tt(sout[1][:, gp, 0:1], pA[:, n - 1:n], pB[:, n - 1:n], ALU.add, ("pA", "pB"), ("so_im",), eng="pool")
                    ph.mm([(bank(4 + bi, n), CTr[:, gp, :], Sr[:, sl], gp % 4 == 0, False),
                           (bank(4 + bi, n), CTi[:, gp, :], Si[:, sl], False, gp % 4 == 3)], ("Sr%d" % bi, "Si%d" % bi), ("ps%d" % (4 + bi),))
                if gp % 4 == 3:
                    for bi, (t0, n) in enumerate(PB):
                        y_ = tA[:, 0:n]; a_ = tA[:, 512:512 + n] if TW >= 1024 else pA[:, 0:n]; b_ = tBt[:, 0:n]
                        an = "tA" if TW >= 1024 else "pA"
                        ph.stt(y_, uT[:, c, t0:t0 + n], dP[:, c:c + 1], bank(4 + bi, n), ALU.mult, ALU.add, ("ps%d" % (4 + bi),), ("tA",))
                        ph.tt(a_, y_, y_, ALU.mult, ("tA",), (an,))
                        ph.ts(a_, a_, 0.044715, ALU.mult, (an,), (an,), s2=1.0, op1=ALU.add)
                        ph.tt(a_, a_, y_, ALU.mult, (an, "tA"), (an,))
                        ph.act(b_, a_, AF.Sigmoid, (an,), ("tB",), scale=1.5957691216057308)
                        ph.tt(gss[:, c, t0:t0 + n], y_, b_, ALU.mult, ("tA", "tB"), ("gss%d_%d" % (c, bi),))
            ph.run()

        ph = Phase(ctx)
        tb_ = Bump(ar, e_mark / 256.0, 207)
        s0r = tb_([128, 16, NS]); s0i = tb_([128, 16, NS]); q1 = tb_([128, 16, NS]); q2 = tb_([128, 16, NS])
        Ssr = tb_([128, 16, NS], BF16); Ssi = tb_([128, 16, NS], BF16)
        yv = tb_([128, 4, NS]); g1 = tb_([128, 4, NS]); g2 = tb_([128, 4, NS])
        ph.dma("sp", s0r, st_re_d.rearrange("p (a b) -> p a b", a=16), (), ("s0r",), "f0")
        ph.dma("sp", s0i, st_im_d.rearrange("p (a b) -> p a b", a=16), (), ("s0i",), "f1")
        items = []
        for gp in range(16):
            c, rows = gp // 4, (gp % 4) * 32
            for ri_ in range(2):
                items.append((bank(0)[:, (gp * 2 + ri_) * NS:(gp * 2 + ri_ + 1) * NS], WB[:, ri_, gp, :], uT[:, c, T:NT], True, True))
        ph.mm(items, (), ("ps0",))
        Xv = bank(0)[:, 0:32 * NS].rearrange("p (g r b) -> p g r b", g=16, r=2)
        lbr = lbrP.broadcast_to([128, 16, NS]); lbi = lbiP.broadcast_to([128, 16, NS])
        ph.tt(q1, s0i, lbi, ALU.mult, ("s0i",), ("q1",))
        ph.tt(q2, s0r, lbr, ALU.mult, ("s0r",), ("q2",))
        ph.tt(q2, q2, q1, ALU.subtract, ("q1", "q2"), ("q2",))
        ph.tt(sout[0][:, :, 1:1 + NS], q2, Xv[:, :, 0, :], ALU.add, ("q2", "ps0"), ("so_re",))
        ph.cp(Ssr, sout[0][:, :, 1:1 + NS], ("so_re",), ("Ssr",))
        ph.tt(q1, s0r, lbi, ALU.mult, ("s0r", "q2"), ("q1",))
        ph.tt(q2, s0i, lbr, ALU.mult, ("s0i", "so_re"), ("q2",))
        ph.tt(q2, q2, q1, ALU.add, ("q1", "q2"), ("q2",))
        ph.tt(sout[1][:, :, 1:1 + NS], q2, Xv[:, :, 1, :], ALU.add, ("q2", "ps0"), ("so_im",))
        ph.cp(Ssi, sout[1][:, :, 1:1 + NS], ("so_im",), ("Ssi",))
        ph.dma("sp", sre_d.rearrange("p (a b) -> p a b", a=16), sout[0], ("so_re",), ("sre_d",), "f2")
        ph.dma("sp", sim_d.rearrange("p (a b) -> p a b", a=16), sout[1], ("so_im",), ("sim_d",), "f3")
        for c in range(4):
            items = []
            for gl in range(4):
                gp = 4 * c + gl
                items.append((bank(1)[:, c * NS:(c + 1) * NS], CTr[:, gp, :], Ssr[:, gp, :], gl == 0, False))
                items.append((bank(1)[:, c * NS:(c + 1) * NS], CTi[:, gp, :], Ssi[:, gp, :], False, gl == 3))
            ph.mm(items, ("Ssr", "Ssi"), ("ps1",))
        for c in range(4):
            ph.stt(yv[:, c, :], uT[:, c, T:NT], dP[:, c:c + 1], bank(1)[:, c * NS:(c + 1) * NS], ALU.mult, ALU.add, ("ps1",), ("yv%d" % c,))
        ally = tuple("yv%d" % c for c in range(4))
        ph.tt(g1, yv, yv, ALU.mult, ally, ("g1",))
        ph.ts(g1, g1, 0.044715, ALU.mult, ("g1",), ("g1",), s2=1.0, op1=ALU.add)
        ph.tt(g1, g1, yv, ALU.mult, ("g1",) + ally, ("g1",))
        ph.act(g2, g1, AF.Sigmoid, ("g1",), ("g2",), scale=1.5957691216057308)
        ph.tt(gss[:, :, T:NT], yv, g2, ALU.mult, ally + ("g2",), ("gss_s",))
        ph.run()

        ph = Phase(ctx)
        tb_ = Bump(ar, 92.5, 207)
        wgl = tb_([128, 4, 1024], BF16)
        wst = [tb_([128, 1024]), tb_([128, 1024])]
        sg = [tb_([128, 512]), tb_([128, 512])]
        for kc in range(4):
            s = kc % 2
            ph.dma("sp", wst[s], w_glu_d[kc * 128:(kc + 1) * 128, :], (), ("wst%d" % s,), "g%d" % s)
            ph.cp(wgl[:, kc, :], wst[s], ("wst%d" % s,), ("wgl",), eng="pool")
        it = 0
        for oc in range(4):
            for bi, (t0, n) in enumerate(TB):
                b = 2 * (it % 2); it += 1
                ph.mm([(bank(b, n), wgl[:, kc, oc * 128:(oc + 1) * 128], gss[:, kc, t0:t0 + n], kc == 0, kc == 3) for kc in range(4)]
                      + [(bank(b + 1, n), wgl[:, kc, 512 + oc * 128:512 + (oc + 1) * 128], gss[:, kc, t0:t0 + n], kc == 0, kc == 3) for kc in range(4)],
                      ("wgl",), ("ps%d" % b, "ps%d" % (b + 1)))
                s_ = sg[it % 2][:, 0:n]
                ph.act(s_, bank(b + 1, n), AF.Sigmoid, ("ps%d" % (b + 1),), ("sg%d" % (it % 2),), bias=bglu[:, 4 + oc:5 + oc])
                ph.stt(soT[:, oc, t0:t0 + n], bank(b, n), bglu[:, oc:oc + 1], s_, ALU.add, ALU.mult, ("ps%d" % b, "sg%d" % (it % 2)), ("soT",))
        ph.run()

        def layer_norm(ph, src, srcn, n, gam, bet, out_tile, outn, tmp, tmpn, stat):
            st6, mv, rs_, nmr = stat
            for j in range(2):
                ph.add("dve", (lambda e, o=st6[0:n, j, :], i=src[j]: e.bn_stats(o, i)), (srcn[j],), ("st6",))
            ph.add("dve", (lambda e, o=mv[0:n, :], i=st6[0:n, :, :].rearrange("p a b -> p (a b)"): e.bn_aggr(o, i)), ("st6",), ("mv",))
            ph.ts(rs_[0:n, :], mv[0:n, 1:2], LN_EPS, ALU.add, ("mv",), ("rs",))
            ph.act(rs_[0:n, :], rs_[0:n, :], AF.Sqrt, ("rs",), ("rs",))
            ph.recip(rs_[0:n, :], rs_[0:n, :], ("rs",), ("rs",))
            ph.stt(nmr[0:n, :], mv[0:n, 0:1], -1.0, rs_[0:n, :], ALU.mult, ALU.mult, ("mv", "rs"), ("nmr",))
            for j in range(2):
                ph.act(tmp[0:n, j * 512:(j + 1) * 512], src[j], AF.Identity, (srcn[j], "rs", "nmr"), (tmpn[j],),
                       bias=nmr[0:n, :], scale=rs_[0:n, :])
            ph.tt(tmp[0:n, :], tmp[0:n, :], gam[0:n, :], ALU.mult, tuple(tmpn) + ("lng",), tuple(tmpn))
            ph.tt(out_tile[0:n, :], tmp[0:n, :], bet[0:n, :], ALU.add, tuple(tmpn) + ("lng",), (outn,))

        ph = Phase(ctx)
        tb_ = Bump(ar, 150.5, 207)
        wob = tb_([128, 8, 1024], BF16)
        wst = [tb_([128, 1024]), tb_([128, 1024])]
        lng = tb_([128, 1024]); lnb = tb_([128, 1024])
        xt = [tb_([128, 1024]), tb_([128, 1024])]
        tl = tb_([128, 1024]); ht = tb_([128, 1024])
        wrt = tb_([128, 8, 32]); brt = tb_([128, 32])
        st6 = tb_([128, 2, 6]); mv = tb_([128, 2]); rs_ = tb_([128, 1]); nmr = tb_([128, 1])
        lg = tb_([128, 32]); m8 = tb_([128, 8]); sel = tb_([128, 32]); nm_ = tb_([128, 1]); ex = tb_([128, 32])
        den = tb_([128, 1]); gt = tb_([128, 32])
        for kc in range(8):
            s = kc % 2
            ph.dma("sp", wst[s], w_out_d[kc * 128:(kc + 1) * 128, :], (), ("wst%d" % s,), "h%d" % s)
            ph.cp(wob[:, kc, :], wst[s], ("wst%d" % s,), ("wob",), eng="pool")
        ph.dma("act", lng, ln_d[:, 0:D].broadcast_to([128, D]), (), ("lng",), "h2")
        ph.dma("act", lnb, ln_d[:, D:2 * D].broadcast_to([128, D]), (), ("lng",), "h3")
        ph.dma("act", wrt[:, :, 0:NE], wr_d[:, 0:NE].rearrange("(c p) e -> p c e", p=128), (), ("wrt",), "h4")
        ph.dma("act", brt[:, 0:NE], br_d.broadcast_to([128, NE]), (), ("brt",), "h5")
        for i, (t0, n) in enumerate(TL):
            s = i % 2
            ph.dma("sp", xt[s][0:n, :], x_d[t0:t0 + n, :], (), ("xt%d" % s,), "x%d" % s)
            for j in range(2):
                ph.mm([(ps_t[0:n, j * 512:(j + 1) * 512], (soT[:, kc, t0:t0 + n] if kc < 4 else aoT[:, kc - 4, t0:t0 + n]),
                        wob[:, kc, j * 512:(j + 1) * 512], kc == 0, kc == 7) for kc in range(8)], ("wob",), ("ps%d" % j,))
                ph.stt(tl[0:n, j * 512:(j + 1) * 512], xt[s][0:n, j * 512:(j + 1) * 512], ALPHA, ps_t[0:n, j * 512:(j + 1) * 512],
                       ALU.mult, ALU.add, ("xt%d" % s, "ps%d" % j), ("tl%d" % j,))
            layer_norm(ph, [tl[0:n, 0:512], tl[0:n, 512:1024]], ("tl0", "tl1"), n, lng, lnb, ht, "ht", tl, ("tl0", "tl1"),
                       (st6, mv, rs_, nmr))
            for j in range(2):
                ph.tr([(ps_t[:, (2 + j) * 512 + cc * 128:(2 + j) * 512 + cc * 128 + n], ht[0:n, (4 * j + cc) * 128:(4 * j + cc + 1) * 128],
                        ident[0:n, 0:n]) for cc in range(4)], ("ht", "ident"), ("ps%d" % (2 + j),))
                src = bank(2 + j).rearrange("p (c t) -> p c t", c=4)[:, :, 0:n]
                ph.act(fT[:, 4 * j:4 * j + 4, t0:t0 + n], src, AF.Copy, ("ps%d" % (2 + j),), ("fT%d" % i,), scale=ALPHA)
                ph.cp(hTb[:, 4 * j:4 * j + 4, t0:t0 + n], src, ("ps%d" % (2 + j),), ("hTb%d" % i,))
            ph.mm([(ps_t[0:n, 4 * 512:4 * 512 + NE], fT[:, c, t0:t0 + n], wrt[:, c, 0:NE], c == 0, c == 7) for c in range(8)],
                  ("fT%d" % i, "wrt"), ("ps4",))
            ph.stt(lg[0:n, 0:NE], ps_t[0:n, 4 * 512:4 * 512 + NE], 1.0 / ALPHA, brt[0:n, 0:NE], ALU.mult, ALU.add, ("ps4", "brt"), ("lg",))
            ph.add("dve", (lambda e, o=m8[0:n, :], i_=lg[0:n, 0:NE]: e.max(o, i_)), ("lg",), ("m8",))
            ph.ts(sel[0:n, 0:NE], lg[0:n, 0:NE], m8[0:n, cfg.TOPK - 1:cfg.TOPK], ALU.is_ge, ("lg", "m8"), ("sel",))
            ph.ts(nm_[0:n, :], m8[0:n, 0:1], -1.0, ALU.mult, ("m8",), ("nm",))
            ph.act(ex[0:n, 0:NE], lg[0:n, 0:NE], AF.Exp, ("lg", "nm"), ("ex",), bias=nm_[0:n, :])
            ph.tt(ex[0:n, 0:NE], ex[0:n, 0:NE], sel[0:n, 0:NE], ALU.mult, ("ex", "sel"), ("ex2",))
            ph.red(den[0:n, :], ex[0:n, 0:NE], ALU.add, ("ex2",), ("den",))
            ph.recip(den[0:n, :], den[0:n, :], ("den",), ("den2",))
            ph.ts(gt[0:n, 0:NE], ex[0:n, 0:NE], den[0:n, :], ALU.mult, ("ex2", "den2"), ("gt",))
            ph.tr([(ps_t[0:NE, 5 * 512:5 * 512 + n], gt[0:n, 0:NE], ident[0:n, 0:n])], ("gt", "ident"), ("ps5",))
            ph.cp(gatesT[0:NE, t0:t0 + n], ps_t[0:NE, 5 * 512:5 * 512 + n], ("ps5",), ("gatesT",), eng="act")
        ph.run()

        ph = Phase(ctx)
        tb_ = Bump(ar, 159, 207)
        bdn = tb_([128, D])
        ph.dma("act", bdn[0:NE, :], bdn_d, (), ("bdn",), "m1")
        for dc in range(8):
            for bi, (t0, n) in enumerate(TB):
                b = (dc * len(TB) + bi) % 4
                ph.mm([(bank(b, n), bdn[0:NE, dc * 128:(dc + 1) * 128], gatesT[0:NE, t0:t0 + n], True, True)], ("bdn",), ("ps%d" % b,))
                ph.tt(fT[:, dc, t0:t0 + n], fT[:, dc, t0:t0 + n], bank(b, n), ALU.add, ("ps%d" % b,), ("fT%d_%d" % (dc, bi),))
        ph.run()

        ph = Phase(ctx)
        GeS = ar.at(int(150.5 * KB), [128, NT])
        tb_ = Bump(ar, 159, 207)
        stg = [tb_([128, 2048]), tb_([128, 2048])]
        wpb = [tb_([128, 8, 256], BF16) for _ in range(2)]
        Gt = [tb_([128, 512]) for _ in range(3)]; Sg = [tb_([128, 512]) for _ in range(3)]; Lt = [tb_([128, 512]) for _ in range(3)]
        bguT = tb_([128, NE, 16]); bl1 = tb_([128, NE, 8]); selt = [tb_([128, 128]), tb_([128, 128])]
        ph.dma("act", bguT, bgu_d.rearrange("p (e c) -> p e c", e=NE), (), ("bguT",), "m0")
        ph.ts(bl1, bguT[:, :, 8:16], 1.0, ALU.add, ("bguT",), ("bl1",))
        pieces = [(e_, kind, j) for e_ in range(NE) for kind in ("gu", "dn") for j in range(8)]
        cnt = dict(it=0, un=0)

        def emit_load(i):
            e_, kind, j = pieces[i]
            s2_ = i % 2
            row0 = (e_ * 8 + j) * 128
            if kind == "gu":
                ph.dma("sp", stg[s2_], wgu_d[row0:row0 + 128, :], (), ("stg%d" % s2_,), "stg%d" % s2_)
                ph.cp(wpb[s2_].rearrange("p a b -> p (a b)"), stg[s2_], ("stg%d" % s2_,), ("wpb%d" % s2_,), eng="act")
            else:
                ph.dma("sp", stg[s2_][:, 0:1024], wdn_d[row0:row0 + 128, :], (), ("stg%d" % s2_,), "stg%d" % s2_)
                ph.cp(wpb[s2_].rearrange("p a b -> p (a b)")[:, 0:1024], stg[s2_][:, 0:1024], ("stg%d" % s2_,), ("wpb%d" % s2_,), eng="act")

        def emit_compute(i):
            e_, kind, j = pieces[i]
            s2_ = i % 2
            if kind == "gu" and j == 0:
                st_ = selt[e_ % 2]; sn_ = "selt%d" % (e_ % 2)
                ph.cp(st_[0:NE, :], ident[0:NE, e_:e_ + 1].broadcast_to([NE, 128]), (), (sn_,), eng="pool")
                for bi, (t0, n) in enumerate(TB):
                    b = 6 + bi % 2
                    ph.mm([(bank(b, n), st_[0:NE, :], gatesT[0:NE, t0:t0 + n], True, True)], (sn_,), ("ps%d" % b,))
                    ph.cp(GeS[:, t0:t0 + n], bank(b, n), ("ps%d" % b,), ("GeS%d" % bi,), eng="act")
            if kind == "gu":
                fc = j
                for bi, (t0, n) in enumerate(TB):
                    b = 2 * (cnt["it"] % 3); cnt["it"] += 1
                    k3 = cnt["un"] % 3; cnt["un"] += 1
                    ph.mm([(bank(b, n), wpb[s2_][:, kc, 0:128], hTb[:, kc, t0:t0 + n], kc == 0, kc == 7) for kc in range(8)]
                          + [(bank(b + 1, n), wpb[s2_][:, kc, 128:256], hTb[:, kc, t0:t0 + n], kc == 0, kc == 7) for kc in range(8)],
                          ("wpb%d" % s2_,), ("ps%d" % b, "ps%d" % (b + 1)))
                    G_ = Gt[k3][:, 0:n]; S_ = Sg[k3][:, 0:n]; L_ = Lt[k3][:, 0:n]
                    gn, sn, ln_ = "G%d" % k3, "S%d" % k3, "L%d" % k3
                    ph.ts(G_, bank(b, n), bguT[:, e_, fc:fc + 1], ALU.add, ("ps%d" % b, "bguT"), (gn,), s2=SW_LIM, op1=ALU.min)
                    ph.act(L_, bank(b + 1, n), AF.Identity, ("ps%d" % (b + 1), "bl1"), (ln_,), bias=bl1[:, e_, fc:fc + 1])
                    ph.act(S_, G_, AF.Sigmoid, (gn,), (sn,), scale=SW_ALPHA)
                    ph.ts(L_, L_, 1.0 - SW_LIM, ALU.max, (ln_,), (ln_,), s2=SW_LIM + 1.0, op1=ALU.min)
                    ph.tt(L_, L_, G_, ALU.mult, (ln_, gn), (ln_,))
                    ph.tt(S_, S_, L_, ALU.mult, (sn, ln_), (sn,), eng="pool")
                    ph.tt(actT[:, fc, t0:t0 + n], S_, GeS[:, t0:t0 + n], ALU.mult, (sn, "GeS%d" % bi), ("actT%d_%d" % (fc, bi),), eng="pool")
            else:
                dc = j
                wd3 = wpb[s2_].rearrange("p a b -> p (a b)")[:, 0:1024].rearrange("p (a b) -> p a b", a=8)
                for bi, (t0, n) in enumerate(TB):
                    b = 6 + (cnt["it"] % 2); cnt["it"] += 1
                    ph.mm([(bank(b, n), wd3[:, f, :], actT[:, f, t0:t0 + n], f == 0, f == 7) for f in range(8)],
                          ("wpb%d" % s2_,) + tuple("actT%d_%d" % (f, bi) for f in range(8)), ("ps%d" % b,))
                    ph.tt(fT[:, dc, t0:t0 + n], fT[:, dc, t0:t0 + n], bank(b, n), ALU.add, ("ps%d" % b,), ("fT%d_%d" % (dc, bi),))

        emit_load(0)
        for i in range(len(pieces)):
            if i + 1 < len(pieces):
                emit_load(i + 1)
            emit_compute(i)
        ph.run()

        ph = Phase(ctx)
        tb_ = Bump(ar, 150.5, 207)
        lng = tb_([128, 1024]); lnb = tb_([128, 1024])
        yt = [tb_([128, 1024]), tb_([128, 1024])]; tmp = tb_([128, 1024])
        st6 = tb_([128, 2, 6]); mv = tb_([128, 2]); rs_ = tb_([128, 1]); nmr = tb_([128, 1])
        ph.dma("sp", lng, ln_d[:, 2 * D:3 * D].broadcast_to([128, D]), (), ("lng",), "n0")
        ph.dma("sp", lnb, ln_d[:, 3 * D:4 * D].broadcast_to([128, D]), (), ("lng",), "n1")
        for i, (t0, n) in enumerate(TL):
            s = i % 2
            bb = 2 * (i % 2)
            for j in range(2):
                ph.tr([(ps_t[0:n, (bb + j) * 512 + cc * 128:(bb + j) * 512 + (cc + 1) * 128], fT[:, 4 * j + cc, t0:t0 + n], ident)
                       for cc in range(4)], ("ident",), ("ps%d" % (bb + j),))
            layer_norm(ph, [ps_t[0:n, (bb + j) * 512:(bb + j + 1) * 512] for j in range(2)], ("ps%d" % bb, "ps%d" % (bb + 1)), n, lng, lnb,
                       yt[s], "yt%d" % s, tmp, ("tmp0", "tmp1"), (st6, mv, rs_, nmr))
            ph.add("pool", (lambda e, o=y_d[t0:t0 + n, :], i_=yt[s][0:n, :]: e.dma_start(out=o, in_=i_)), ("yt%d" % s,), ("yd%d" % i,), dma="y%d" % s)
        ph.run()
    return nc


def _consts(cfg, past_len):
    T, NS, NT = cfg.T, cfg.NS, cfg.NT
    half = 32
    inv = (np.float32(10000.0) ** (-np.arange(half, dtype=np.float32) / np.float32(half))).astype(np.float32)
    pos = np.concatenate([np.arange(T, dtype=np.float32), np.full((NS,), past_len, np.float32)])
    ang = (pos[:, None] * inv[None, :]).astype(np.float32)
    ropeC = np.cos(ang.astype(np.float64)).astype(np.float32)
    ropeS = np.sin(ang.astype(np.float64)).astype(np.float32)
    ident = np.eye(128, dtype=np.float32)
    tri = np.where(np.arange(128)[None, :] <= np.arange(128)[:, None], 0.0, -30000.0).astype(np.float32)
    selB = np.zeros((NS, NS, 128), np.float32)
    for b in range(NS):
        selB[b, b, :] = 1.0
    pidx = np.arange(128, dtype=np.float32)[:, None].copy()
    negm = np.full((128, 1), -30000.0, np.float32); negm[0, 0] = 0.0
    return dict(ropeC=ropeC, ropeS=ropeS, ident=ident, tri=tri, selB=selB.reshape(NS, NS * 128), pidx=pidx, negm=negm)


def _shared(cfg, I):
    NE = cfg.NE
    f = lambda a: np.ascontiguousarray(a, dtype=np.float32)
    a_re, a_im, ldt = I["ssm_a_re"][0], I["ssm_a_im"][0], I["ssm_log_dt"][0]
    toP = lambda a: a.reshape(16, 2, 64).transpose(1, 2, 0).reshape(128, 16)
    aP = np.concatenate([toP(a_re), toP(a_im), toP(np.repeat(ldt[:, None], 64, 1))], axis=1)

    def bP(b):
        out = np.zeros((2, 64, 16, 4, 2, 16), np.float32)
        v = b.reshape(16, 2, 64, 16)
        for gp in range(16):
            for g2 in range(2):
                out[g2, :, gp, gp % 4, g2, :] = v[gp, g2]
        return out.reshape(128, 2048)
    bPc = np.concatenate([bP(I["ssm_b_re"][0]), bP(I["ssm_b_im"][0])], axis=1)

    def cT(cm):
        out = np.zeros((2, 64, 16, 4, 2, 16), np.float32)
        v = cm.reshape(16, 2, 16, 64)
        for gp in range(16):
            for g2 in range(2):
                out[g2, :, gp, gp % 4, g2, :] = v[gp, g2].T
        return out.reshape(128, 2048)
    cTc = np.concatenate([cT(I["ssm_c_re"][0]), cT(I["ssm_c_im"][0])], axis=1)
    dP = I["ssm_d"][0].reshape(4, 128).T
    bglu = I["b_glu"][0].reshape(8, 128).T
    lam4 = np.concatenate([I["lambda_q1"][0], I["lambda_k1"][0], I["lambda_q2"][0], I["lambda_k2"][0]])[None, :]
    ln = np.concatenate([I["ln1_g"][0], I["ln1_b"][0], I["ln2_g"][0], I["ln2_b"][0]])[None, :]
    wgu = I["w_gate_up"][0]
    wg = wgu[:, :, :1024].reshape(NE, 8, 128, 8, 128)
    wl = wgu[:, :, 1024:].reshape(NE, 8, 128, 8, 128)
    wgu_t = np.empty((NE, 8, 128, 8, 256), np.float32)
    wgu_t[..., :128] = wg.transpose(0, 3, 2, 1, 4)
    wgu_t[..., 128:] = wl.transpose(0, 3, 2, 1, 4)
    wdn = I["w_down"][0].reshape(NE, 8, 128, 8, 128)
    wdn_t = np.ascontiguousarray(wdn.transpose(0, 3, 2, 1, 4))
    bgu = I["b_gate_up"][0].reshape(NE, 16, 128).transpose(2, 0, 1).reshape(128, NE * 16)
    wr = I["w_router"][0]
    if wr.shape[1] < 32:
        wr = np.concatenate([wr, np.zeros((D, 32 - wr.shape[1]), np.float32)], axis=1)
    return dict(
        cache_k=f(I["cache_k"][0].reshape(-1, 512)), cache_v=f(I["cache_v"][0].reshape(-1, 512)),
        w_in=f(I["w_in"][0]), w_out=f(I["w_out"][0]), aP=f(aP), bP=f(bPc), cT=f(cTc), dP=f(dP),
        w_glu=f(I["w_glu"][0]), bglu=f(bglu), lam4=f(lam4), gsub=f(I["subln_g"][0][None, :]), gcol=f(I["subln_g"][0][:, None]),
        ln=f(ln), wr=f(wr), br=f(I["b_router"][0][None, :]), wgu=f(wgu_t.reshape(NE * 8 * 128, 2048)), bgu=f(bgu),
        wdn=f(wdn_t.reshape(NE * 8 * 128, 1024)), bdn=f(I["b_down"][0]))


def run(cfg, I, trace=False, stop_after=None, max_ops=None):
    T, NS, NPG = cfg.T, cfg.NS, cfg.NPG
    nc = build(cfg, stop_after, max_ops)
    shared = _shared(cfg, I)
    shared.update(_consts(cfg, NPG * 128))
    in_maps = []
    for c in range(NCORES):
        x = np.concatenate([I["x_prompt"][c], I["x_sample"][c * NS:(c + 1) * NS, 0]], axis=0).astype(np.float32)
        m = dict(shared)
        m["x"] = np.ascontiguousarray(x)
        m["xT"] = np.ascontiguousarray(x.T)
        m["pt"] = np.ascontiguousarray(I["page_table"][c * NS:(c + 1) * NS].reshape(1, NS * NPG).astype(np.int32))
        for k_, nm in (("state_ssm_re", "st_re"), ("state_ssm_im", "st_im")):
            s = I[k_][0, c * NS:(c + 1) * NS].reshape(NS, 16, 2, 64)
            m[nm] = np.ascontiguousarray(s.transpose(2, 3, 1, 0).reshape(128, 16 * NS).astype(np.float32))
        in_maps.append(m)
    res = run_bass_kernel_spmd(nc, in_maps, core_ids=list(range(NCORES)), trace=trace) if trace else \
        run_bass_kernel_spmd(nc, in_maps, core_ids=list(range(NCORES)))
    R = res.results
    B = NCORES
    y = np.stack([r["y"] for r in R])
    ko = np.stack([r["ko"] for r in R]); vo = np.stack([r["vo"] for r in R])
    sre = np.stack([r["sre"].reshape(2, 64, 16, 1 + NS) for r in R])
    sim = np.stack([r["sim"].reshape(2, 64, 16, 1 + NS) for r in R])

    def st_p(s):
        return np.ascontiguousarray(s[..., 0].transpose(0, 3, 1, 2).reshape(B, 32, 64))[None]

    def st_s(s):
        v = s[..., 1:].transpose(0, 4, 3, 1, 2)
        return np.ascontiguousarray(v.reshape(B * NS, 32, 64))[None]
    outs = (
        np.ascontiguousarray(y[:, :T]), np.ascontiguousarray(y[:, T:].reshape(B * NS, 1, D)),
        np.ascontiguousarray(ko[:, :T].reshape(B, T, 4, 128))[None], np.ascontiguousarray(vo[:, :T].reshape(B, T, 4, 128))[None],
        st_p(sre), st_p(sim),
        np.ascontiguousarray(ko[:, T:].reshape(B * NS, 1, 4, 128))[None], np.ascontiguousarray(vo[:, T:].reshape(B * NS, 1, 4, 128))[None],
        st_s(sre), st_s(sim))
    return tuple(o.astype(np.float32) for o in outs), res


def kernel(**inputs):
    I = {k: np.asarray(v) for k, v in inputs.items()}
    outs, _ = run(FULL, I)
    return outs
```

```python
import math
from contextlib import ExitStack

import numpy as np
import concourse.bass as bass
import concourse.mybir as mybir
from concourse.bass_utils import run_bass_kernel_spmd

F32 = mybir.dt.float32
BF16 = mybir.dt.bfloat16
I32 = mybir.dt.int32
AF = mybir.ActivationFunctionType
ALU = mybir.AluOpType
AX = mybir.AxisListType

D = 1024
NCORES = 8
LN_EPS = 1e-5
RMS_EPS = 1e-5
LAM_INIT = 0.8 - 0.6 * math.exp(-0.3 * 0)
ALPHA = (2 * 1) ** 0.25
SW_ALPHA = 1.702
SW_LIM = 7.0
ARENA_W = 52992


class Cfg:
    def __init__(self, T=2048, NS=16, NPG=16, NPHYS=2560, NE=32, TOPK=4):
        self.T, self.NS, self.NPG, self.NPHYS, self.NE, self.TOPK = T, NS, NPG, NPHYS, NE, TOPK
        self.NT = T + NS
        self.NTP = T // 128
        self.TB = [(i * 512, min(512, T - i * 512)) for i in range((T + 511) // 512)] + [(T, NS)]
        self.TL = [(i * 128, 128) for i in range(self.NTP)] + [(T, NS)]


FULL = Cfg()

ENGS = ("pe", "act", "dve", "pool", "sp")


class Ctx:
    def __init__(self, nc, stack):
        self.nc, self.stack = nc, stack
        self.esem = {e: stack.enter_context(nc.semaphore("es_" + e)) for e in ("pe", "act", "dve", "pool")}
        self.ecnt = {e: 0 for e in self.esem}
        self.dsem, self.dcnt = {}, {}
        self.known = {e: {} for e in ENGS}
        self.phase_no = 0
        self.stop_after = None
        self.max_ops = None

    def dma_slot(self, slot):
        if slot not in self.dsem:
            self.dsem[slot] = self.stack.enter_context(self.nc.semaphore("ds%d" % len(self.dsem)))
            self.dcnt[slot] = 0
            assert len(self.dsem) < 150, "too many dma semaphores"
        return self.dsem[slot]


class Phase:
    def __init__(self, ctx):
        self.ctx, self.ops = ctx, []

    def add(self, eng, fn, r=(), w=(), dma=None):
        self.ops.append(dict(eng=eng, fn=fn, r=tuple(r), w=tuple(w), dma=dma, dep=False))

    def mm(self, items, r, w):
        def fn(e, items=items):
            return [e.matmul(o, l, rh, start=s, stop=t) for (o, l, rh, s, t) in items]
        self.add("pe", fn, r, w)

    def tr(self, items, r, w):
        def fn(e, items=items):
            return [e.transpose(o, i, idn) for (o, i, idn) in items]
        self.add("pe", fn, r, w)

    def act(self, out, in_, func, r, w, bias=None, scale=None, accum=None):
        def fn(e):
            kw = {}
            if bias is not None:
                kw["bias"] = bias
            if scale is not None:
                kw["scale"] = scale
            if accum is not None:
                kw["accum_out"] = accum
            return e.activation(out, in_, func, **kw)
        self.add("act", fn, r, w)

    def ts(self, out, in0, s1, op0, r, w, s2=None, op1=None, eng="dve"):
        def fn(e):
            if op1 is None:
                return e.tensor_scalar(out, in0, s1, None, op0)
            return e.tensor_scalar(out, in0, s1, s2, op0, op1)
        self.add(eng, fn, r, w)

    def stt(self, out, in0, scalar, in1, op0, op1, r, w, accum=None):
        if accum is None:
            self.add("dve", lambda e: e.scalar_tensor_tensor(out, in0, scalar, in1, op0, op1), r, w)
        else:
            self.add("dve", lambda e: e.scalar_tensor_tensor(out, in0, scalar, in1, op0, op1, accum_out=accum), r, w)

    def tt(self, out, in0, in1, op, r, w, eng="dve"):
        self.add(eng, lambda e: e.tensor_tensor(out, in0, in1, op), r, w)

    def cp(self, out, in_, r, w, eng="dve"):
        if eng == "act":
            self.add("act", lambda e: e.copy(out, in_), r, w)
        else:
            self.add(eng, lambda e: e.tensor_copy(out, in_), r, w)

    def red(self, out, in_, op, r, w, axis=None):
        ax = AX.X if axis is None else axis
        self.add("dve", lambda e: e.tensor_reduce(out, in_, ax, op), r, w)

    def memset(self, out, val, w, eng="dve"):
        self.add(eng, lambda e: e.memset(out, val), (), w)

    def recip(self, out, in_, r, w):
        self.add("dve", lambda e: e.reciprocal(out, in_), r, w)

    def dma(self, q, out, in_, r, w, slot):
        self.add(q, lambda e: e.dma_start(out=out, in_=in_), r, w, dma=slot)

    def run(self):
        ops, ctx, nc = self.ops, self.ctx, self.ctx.nc
        ctx.phase_no += 1
        if ctx.stop_after is not None and ctx.phase_no > ctx.stop_after:
            return
        if ctx.stop_after is not None and ctx.phase_no == ctx.stop_after and ctx.max_ops is not None:
            print("[bisect] phase %d has %d ops, keeping %d; last kept: %s" % (
                ctx.phase_no, len(ops), ctx.max_ops, [(o["eng"], o["r"], o["w"], o["dma"]) for o in ops[max(0, ctx.max_ops - 2):ctx.max_ops]]))
            del ops[ctx.max_ops:]
        lastw, readers = {}, {}
        def _excl(b):
            return len(b) == 3 and b[:2] == "ps" and b[2].isdigit()
        for o in ops:
            xr = tuple(b for b in o["r"] if _excl(b))
            if xr:
                o["w"] = tuple(o["w"]) + xr
                o["r"] = tuple(b for b in o["r"] if not _excl(b))
        for i, o in enumerate(ops):
            deps = set()
            for b in o["r"]:
                if b in lastw:
                    deps.add(lastw[b])
            for b in o["w"]:
                if b in lastw:
                    deps.add(lastw[b])
                deps |= readers.get(b, set())
            deps.discard(i)
            o["deps"] = deps
            for d in deps:
                ops[d]["dep"] = True
            for b in o["r"]:
                readers.setdefault(b, set()).add(i)
            for b in o["w"]:
                lastw[b] = i
                readers[b] = set()
        touched = []
        for o in ops:
            if o["dma"] is not None:
                sem = ctx.dma_slot(o["dma"])
                ctx.dcnt[o["dma"]] += 16
                o["tok"] = ("d:" + o["dma"], sem, ctx.dcnt[o["dma"]])
                if o["dma"] not in touched:
                    touched.append(o["dma"])
            elif o["dep"]:
                e = o["eng"]
                ctx.ecnt[e] += 1
                o["tok"] = ("e:" + e, ctx.esem[e], ctx.ecnt[e])
            else:
                o["tok"] = None
        per = {e: [o for o in ops if o["eng"] == e] for e in ENGS}

        def mk(ename):
            def body(eng):
                kn = ctx.known[ename]
                for o in per[ename]:
                    for d in sorted(o["deps"]):
                        key, sem, val = ops[d]["tok"]
                        if kn.get(key, 0) < val:
                            eng.wait_ge(sem, val)
                            kn[key] = val
                    res = o["fn"](eng)
                    last = res[-1] if isinstance(res, (list, tuple)) else res
                    if o["tok"] is not None:
                        last.then_inc(o["tok"][1], 16 if o["dma"] is not None else 1)
                if ename == "sp":
                    for slot in touched:
                        key, val = "d:" + slot, ctx.dcnt[slot]
                        if kn.get(key, 0) < val:
                            eng.wait_ge(ctx.dsem[slot], val)
                            kn[key] = val
            return body

        with nc.Block() as blk:
            blk.tensor(mk("pe"))
            blk.scalar(mk("act"))
            blk.vector(mk("dve"))
            blk.gpsimd(mk("pool"))
            blk.sync(mk("sp"))


class Arena:
    def __init__(self, ap_all):
        self.a = ap_all

    def at(self, off_w, shape, dt=F32, parts=128):
        n = int(np.prod(shape[1:]))
        words = n if dt in (F32, I32) else (n + 1) // 2
        assert off_w + words <= ARENA_W, ("arena overflow", off_w, words)
        v = self.a[0:parts, off_w:off_w + words]
        if dt not in (F32,):
            v = v.bitcast(dt)
            if dt == BF16 and n % 2:
                v = v[:, 0:n]
        if len(shape) == 3:
            v = v.rearrange("p (a b) -> p a b", a=shape[1])
        elif len(shape) == 4:
            v = v.rearrange("p (a b c) -> p a b c", a=shape[1], b=shape[2])
        return v


class Bump:
    def __init__(self, arena, lo_kb, hi_kb):
        self.ar, self.p, self.hi = arena, int(lo_kb * 256), int(hi_kb * 256)

    def __call__(self, shape, dt=F32, parts=128):
        n = int(np.prod(shape[1:]))
        words = n if dt in (F32, I32) else (n + 1) // 2
        words = (words + 7) // 8 * 8
        v = self.ar.at(self.p, shape, dt, parts)
        self.p += words
        assert self.p <= self.hi, ("bump overflow", self.p, self.hi)
        return v


def build(cfg, stop_after=None, max_ops=None):
    T, NS, NT, NTP, NPG, NE = cfg.T, cfg.NS, cfg.NT, cfg.NTP, cfg.NPG, cfg.NE
    TB, TL = cfg.TB, cfg.TL
    TT = len(TL)
    NSL = NPG + 1
    nc = bass.Bass("TRN2", target_bir_lowering=False)

    def din(name, shape, dt=F32):
        return nc.dram_tensor(name, list(shape), dt, kind="ExternalInput").ap()

    def dout(name, shape, dt=F32):
        return nc.dram_tensor(name, list(shape), dt, kind="ExternalOutput").ap()

    xT_d = din("xT", [D, NT]); x_d = din("x", [NT, D])
    ck_d = din("cache_k", [cfg.NPHYS * 128, 512]); cv_d = din("cache_v", [cfg.NPHYS * 128, 512])
    pt_d = din("pt", [1, NS * NPG], I32)
    st_re_d = din("st_re", [128, 16 * NS]); st_im_d = din("st_im", [128, 16 * NS])
    w_in_d = din("w_in", [D, 2048]); w_out_d = din("w_out", [D, D])
    aP_d = din("aP", [128, 48])
    bP_d = din("bP", [128, 2 * 2048])
    cT_d = din("cT", [128, 2 * 2048])
    dP_d = din("dP", [128, 4])
    w_glu_d = din("w_glu", [512, 1024]); bglu_d = din("bglu", [128, 8])
    lam4_d = din("lam4", [1, 256])
    gsub_d = din("gsub", [1, 128]); gcol_d = din("gcol", [128, 1])
    ln_d = din("ln", [1, 4 * D])
    wr_d = din("wr", [D, 32 if NE <= 32 else NE]); br_d = din("br", [1, NE])
    wgu_d = din("wgu", [NE * 8 * 128, 2048]); bgu_d = din("bgu", [128, NE * 16])
    wdn_d = din("wdn", [NE * 8 * 128, 1024]); bdn_d = din("bdn", [NE, D])
    ropeC_d = din("ropeC", [NT, 32]); ropeS_d = din("ropeS", [NT, 32])
    ident_d = din("ident", [128, 128]); tri_d = din("tri", [128, 128])
    selB_d = din("selB", [NS, NS * 128])
    pidx_d = din("pidx", [128, 1]); negm_d = din("negm", [128, 1])

    y_d = dout("y", [NT, D]); ko_d = dout("ko", [NT, 512]); vo_d = dout("vo", [NT, 512])
    sre_d = dout("sre", [128, 16 * (1 + NS)]); sim_d = dout("sim", [128, 16 * (1 + NS)])

    stack = ExitStack()
    with stack:
        arena_t = stack.enter_context(nc.sbuf_tensor("arena", [128, ARENA_W], F32))
        ps_t = stack.enter_context(nc.psum_tensor("ps", [128, 4096], F32))
        ar = Arena(arena_t)
        ctx = Ctx(nc, stack)
        ctx.stop_after = stop_after
        ctx.max_ops = max_ops

        def bank(b, n=512):
            return ps_t[:, b * 512:b * 512 + n]

        def bank_bf(b):
            return ps_t[:, b * 512:(b + 1) * 512].bitcast(BF16)

        KB = 256
        cb = Bump(ar, 0, 10)
        ident = cb([128, 128]); identb = cb([128, 128], BF16); tri = cb([128, 128]); onesf = cb([128, 128])
        cosT = cb([128, TT, 32]); sinT = cb([128, TT, 32])
        gsub = cb([128, 128]); gcol = cb([128, 1]); lam_t = cb([128, 1]); nlam_t = cb([128, 1])
        pidx = cb([128, 1]); negm = cb([128, 1]); dP = cb([128, 4]); bglu = cb([128, 8])
        aoT = ar.at(10 * KB, [128, 4, NT], BF16)
        soT = ar.at(int(26.5 * KB), [128, 4, NT], BF16)
        actT = ar.at(10 * KB, [128, 8, NT], BF16)
        uT = ar.at(43 * KB, [128, 4, NT])
        gss = ar.at(76 * KB, [128, 4, NT], BF16)
        qs = ar.at(76 * KB, [128, 512])
        qT = ar.at(int(94.5 * KB), [128, 4, NT], BF16)
        kT = ar.at(int(94.5 * KB) + 2 * NT, [128, 4, NT], BF16)
        vbf = ar.at(int(127.5 * KB), [128, max(NTP, 1), 512], BF16)
        fT = ar.at(43 * KB, [128, 8, NT])
        hTb = ar.at(109 * KB, [128, 8, NT], BF16)
        gatesT = ar.at(142 * KB, [128, NT])

        ph = Phase(ctx)
        tb_ = Bump(ar, 150, 207)
        l4 = tb_([128, 256]); pr = tb_([128, 128]); sm = tb_([128, 2]); ee = tb_([128, 2])
        ph.dma("sp", ident, ident_d, (), ("ident",), "c0")
        ph.dma("sp", tri, tri_d, (), ("tri",), "c1")
        ph.dma("sp", gsub, gsub_d.broadcast_to([128, 128]), (), ("gsub",), "c2")
        ph.dma("sp", gcol, gcol_d, (), ("gcol",), "c3")
        ph.dma("sp", pidx, pidx_d, (), ("pidx",), "c4")
        ph.dma("sp", negm, negm_d, (), ("negm",), "c5")
        ph.dma("sp", dP, dP_d, (), ("dP",), "c6")
        ph.dma("sp", bglu, bglu_d, (), ("bglu",), "c7")
        ph.dma("sp", l4, lam4_d.broadcast_to([128, 256]), (), ("l4",), "c8")
        if NTP:
            ph.dma("sp", cosT[:, 0:NTP, :], ropeC_d[0:T, :].rearrange("(i p) f -> p i f", p=128), (), ("cosT",), "c9")
            ph.dma("sp", sinT[:, 0:NTP, :], ropeS_d[0:T, :].rearrange("(i p) f -> p i f", p=128), (), ("sinT",), "c10")
        ph.dma("sp", cosT[0:NS, NTP, :], ropeC_d[T:NT, :], (), ("cosTs",), "c11")
        ph.dma("sp", sinT[0:NS, NTP, :], ropeS_d[T:NT, :], (), ("sinTs",), "c12")
        ph.cp(identb, ident, ("ident",), ("identb",))
        ph.memset(onesf, 1.0, ("onesf",))
        ph.tt(pr.rearrange("p (a b) -> p a b", a=2), l4.rearrange("p (a two b) -> p a two b", a=2, two=2)[:, :, 0, :],
              l4.rearrange("p (a two b) -> p a two b", a=2, two=2)[:, :, 1, :], ALU.mult, ("l4",), ("pr",))
        ph.red(sm, pr.rearrange("p (a b) -> p a b", a=2), ALU.add, ("pr",), ("sm",))
        ph.act(ee, sm, AF.Exp, ("sm",), ("ee",))
        ph.tt(lam_t, ee[:, 0:1], ee[:, 1:2], ALU.subtract, ("ee",), ("lam0",))
        ph.ts(lam_t, lam_t, LAM_INIT, ALU.add, ("lam0",), ("lam",))
        ph.ts(nlam_t, lam_t, -1.0, ALU.mult, ("lam",), ("nlam",))
        ph.run()

        ph = Phase(ctx)
        xTb = ar.at(int(143.5 * KB), [128, 8, NT], BF16)
        sb = Bump(ar, 78, 94.5)
        xst = [sb([128, NT]), sb([128, NT])]
        wb_ = Bump(ar, 26.5, 43)
        wpc = [wb_([128, 8, 512], BF16), wb_([128, 8, 512], BF16)]
        tb_ = Bump(ar, 176.5, 207)
        wst = [tb_([128, 512]), tb_([128, 512])]
        ev = [tb_([128, 512]), tb_([128, 512])]
        ta = tb_([128, 256]); tbb = tb_([128, 256])
        for kc in range(8):
            s = kc % 2
            ph.dma("sp", xst[s], xT_d[kc * 128:(kc + 1) * 128, :], (), ("xst%d" % s,), "xst%d" % s)
            ph.cp(xTb[:, kc, :], xst[s], ("xst%d" % s,), ("xTb%d" % kc,), eng="act" if kc % 2 else "dve")
        xall = tuple("xTb%d" % k for k in range(8))
        nev = 0
        for grp in range(4):
            pw = wpc[grp % 2]
            pwn = "wpc%d" % (grp % 2)
            for kc in range(8):
                s = kc % 2
                ph.dma("act" if kc % 2 else "sp", wst[s], w_in_d[kc * 128:(kc + 1) * 128, grp * 512:(grp + 1) * 512],
                       (), ("wst%d" % s,), "wst%d" % s)
                ph.cp(pw[:, kc, :], wst[s], ("wst%d" % s,), (pwn,), eng="pool")
            if grp == 0:
                for c in range(4):
                    for bi, (t0, n) in enumerate(TB):
                        b = (c * len(TB) + bi) % 2
                        ph.mm([(bank(b, n), pw[:, kc, c * 128:(c + 1) * 128], xTb[:, kc, t0:t0 + n], kc == 0, kc == 7)
                               for kc in range(8)], xall + (pwn,), ("ps%d" % b,))
                        ph.cp(uT[:, c, t0:t0 + n], bank(b, n), ("ps%d" % b,), ("uT%d_%d" % (c, bi),), eng="act")
                continue
            for i, (t0, n) in enumerate(TL):
                b = 2 + (i % 2)
                pb = ps_t[0:n, b * 512:(b + 1) * 512]
                ph.mm([(pb, xTb[:, kc, t0:t0 + n], pw[:, kc, :], kc == 0, kc == 7) for kc in range(8)],
                      xall + (pwn,), ("ps%d" % b,))
                e_ = ev[nev % 2]; en = "ev%d" % (nev % 2); nev += 1
                eo = e_[0:n, :]
                if grp == 3:
                    ph.cp(eo, pb, ("ps%d" % b,), (en,), eng="act")
                    ph.dma("pool", vo_d[t0:t0 + n, :], eo, (en,), ("vo%d" % i,), "st_" + en)
                    if i < NTP:
                        ph.cp(vbf[:, i, :], pb, ("ps%d" % b,), ("vbf%d" % i,))
                    continue
                pv = pb.rearrange("p (g two f) -> p g two f", g=8, two=2)
                ov = eo.rearrange("p (g two f) -> p g two f", g=8, two=2)
                cs = cosT[0:n, i:i + 1, :].broadcast_to([n, 8, 32]); sn = sinT[0:n, i:i + 1, :].broadcast_to([n, 8, 32])
                t1 = ta[0:n, :].rearrange("p (g f) -> p g f", g=8); t2 = tbb[0:n, :].rearrange("p (g f) -> p g f", g=8)
                rp = ("ps%d" % b, "cosT", "sinT", "cosTs", "sinTs")
                ph.tt(t1, pv[:, :, 0, :], cs, ALU.mult, rp, ("ta",))
                ph.tt(t2, pv[:, :, 1, :], sn, ALU.mult, rp, ("tb",))
                ph.tt(ov[:, :, 0, :], t1, t2, ALU.subtract, ("ta", "tb"), (en,))
                ph.tt(t1, pv[:, :, 1, :], cs, ALU.mult, rp, ("ta",))
                ph.tt(t2, pv[:, :, 0, :], sn, ALU.mult, rp, ("tb",))
                ph.tt(ov[:, :, 1, :], t1, t2, ALU.add, ("ta", "tb"), (en + "b",))
                if grp == 2:
                    ph.dma("pool", ko_d[t0:t0 + n, :], eo, (en, en + "b"), ("ko%d" % i,), "st_" + en)
                if i >= NTP:
                    if grp == 1:
                        ph.cp(qs[0:n, :], eo, (en, en + "b"), ("qs",), eng="act")
                    continue
                tb2 = i % 2
                ph.tr([(bank(tb2)[:, h * 128:(h + 1) * 128], eo[:, h * 128:(h + 1) * 128], ident) for h in range(4)],
                      (en, en + "b", "ident"), ("ps%d" % tb2,))
                dst = qT if grp == 1 else kT
                ph.cp(dst[:, :, t0:t0 + 128], bank(tb2).rearrange("p (h t) -> p h t", h=4), ("ps%d" % tb2,),
                      ("%sT%d" % ("q" if grp == 1 else "k", i),), eng="act" if i % 2 else "dve")
        ph.run()

        if NTP:
            ph = Phase(ctx)
            tb_ = Bump(ar, 143.5, 207)
            Psb = [tb_([128, T], BF16), tb_([128, T], BF16)]
            PTs = [tb_([128, T], BF16), tb_([128, T], BF16)]
            mx = tb_([128, 2]); nb = tb_([128, 2]); lsum = tb_([128, 2]); rl = tb_([128, 2])
            tS = tb_([128, 128]); att = tb_([128, 128]); junk = tb_([128, 128]); ss = tb_([128, 1]); rs = tb_([128, 1])
            lsum2 = [lsum, tb_([128, 2])]

            def geom(qt, m):
                nk = qt + 1
                small = nk <= 8
                sb0 = 2 * m if small else 0
                SBK = tuple("ps%d" % (sb0 + k) for k in range(2 if small else 4))
                Sv = ps_t[:, sb0 * 512:sb0 * 512 + (1024 if small else 2048)]
                if small:
                    PTv = ps_t[:, (4 + m) * 512:(5 + m) * 512].bitcast(BF16); PTK = ("ps%d" % (4 + m),)
                else:
                    PTv = ps_t[:, 2048:3072].bitcast(BF16); PTK = ("ps4", "ps5")
                return nk, SBK, Sv, PTv, PTK

            def stageA(idx, h, qt, m):
                nk, SBK, Sv, PTv, PTK = geom(qt, m)
                ls = lsum2[idx % 2]; lname = "l%d_%d" % (idx % 2, m)
                items = []
                for j in range(0, nk, 4):
                    cols = min(4, nk - j) * 128
                    items.append((Sv[:, j * 128:j * 128 + cols], qT[m * 64:(m + 1) * 64, h, qt * 128:(qt + 1) * 128],
                                  kT[m * 64:(m + 1) * 64, h, j * 128:j * 128 + cols], True, True))
                ph.mm(items, (), SBK)
                ph.tt(Sv[:, qt * 128:(qt + 1) * 128], Sv[:, qt * 128:(qt + 1) * 128], tri, ALU.add, SBK + ("tri",), SBK)
                ph.red(mx[:, m:m + 1], Sv[:, 0:nk * 128], ALU.max, SBK, ("mx%d" % m,))
                ph.ts(nb[:, m:m + 1], mx[:, m:m + 1], -0.125, ALU.mult, ("mx%d" % m,), ("nb%d" % m,))
                ph.act(Psb[m][:, 0:nk * 128], Sv[:, 0:nk * 128], AF.Exp, SBK + ("nb%d" % m,), ("P%d" % m, lname),
                       bias=nb[:, m:m + 1], scale=0.125, accum=ls[:, m:m + 1])

            def stageB(idx, h, qt, m):
                nk, SBK, Sv, PTv, PTK = geom(qt, m)
                pn, tn = "P%d" % m, "PT%d" % m
                ph.tr([(PTv[:, k * 128:(k + 1) * 128], Psb[m][:, k * 128:(k + 1) * 128], identb) for k in range(nk)], (pn, "identb"), PTK)
                ph.cp(PTs[m][:, 0:nk * 128], PTv[:, 0:nk * 128], PTK, (tn,), eng="act" if m else "dve")
                ph.mm([(bank(6 + m)[:, 0:128], PTs[m][:, k * 128:(k + 1) * 128], vbf[:, k, h * 128:(h + 1) * 128],
                        k == 0, k == nk - 1) for k in range(nk)], (tn,), ("ps%d" % (6 + m),))

            def stageC(idx, h, qt):
                ls = lsum2[idx % 2]
                ph.recip(rl, ls, ("l%d_0" % (idx % 2), "l%d_1" % (idx % 2)), ("rl",))
                ph.tt(rl[:, 1:2], rl[:, 1:2], lam_t, ALU.mult, ("rl", "lam"), ("rl",))
                ph.ts(tS, bank(7)[:, 0:128], rl[:, 1:2], ALU.mult, ("ps7", "rl"), ("tS",))
                ph.stt(att, bank(6)[:, 0:128], rl[:, 0:1], tS, ALU.mult, ALU.subtract, ("ps6", "rl", "tS"), ("att",))
                ph.stt(junk, att, 1.0, att, ALU.mult, ALU.mult, ("att",), ("junk", "ss"), accum=ss)
                ph.ts(ss, ss, 1.0 / 128, ALU.mult, ("ss",), ("ss",), s2=RMS_EPS, op1=ALU.add)
                ph.act(ss, ss, AF.Ln, ("ss",), ("ss",))
                ph.act(rs, ss, AF.Exp, ("ss",), ("rs",), scale=-0.5)
                ph.ts(att, att, rs, ALU.mult, ("att", "rs"), ("att",), s2=1.0 - LAM_INIT, op1=ALU.mult)
                ph.tt(att, att, gsub, ALU.mult, ("att", "gsub"), ("att",))
                ph.tr([(bank(6)[:, 256:384], att, ident)], ("att", "ident"), ("ps6",))
                ph.cp(aoT[:, h, qt * 128:(qt + 1) * 128], bank(6)[:, 256:384], ("ps6",), ("aoT%d_%d" % (h, qt),), eng="act")

            iters = [(h, qt) for h in range(4) for qt in range(NTP)]
            stageA(0, iters[0][0], iters[0][1], 0); stageA(0, iters[0][0], iters[0][1], 1)
            for idx, (h, qt) in enumerate(iters):
                stageB(idx, h, qt, 0); stageB(idx, h, qt, 1)
                if idx + 1 < len(iters):
                    stageA(idx + 1, iters[idx + 1][0], iters[idx + 1][1], 0)
                    stageA(idx + 1, iters[idx + 1][0], iters[idx + 1][1], 1)
                stageC(idx, h, qt)
            ph.run()

        ph = Phase(ctx)
        tb_ = Bump(ar, 94.5, 207)
        selB = tb_([128, NS * 128])
        pti = tb_([128, NS * NPG], I32); ptf = tb_([128, NS * NPG]); idx = tb_([128, NS * NPG], I32)
        NR = 3
        Kp = [tb_([128, 4, 512]) for _ in range(NR)]
        Vp = [tb_([128, 4, 512]) for _ in range(NR)]
        Ksf = [tb_([128, 512]), tb_([128, 512])]; Vsf = [tb_([128, 512]), tb_([128, 512])]
        prod = [tb_([128, 512]), tb_([128, 512])]
        Ss = tb_([128, NS, NSL, 8])
        mp = tb_([128, NS * 8]); gmx = tb_([128, 1]); dg = tb_([128, NS * 8]); rls = tb_([128, NS * 8])
        t1s = tb_([128, NS, 2]); atts = tb_([128, 4, NS]); sqs = tb_([128, 4 * NS]); rss = tb_([128, 4 * NS])
        H8 = NS * 8
        OTB = ("ps3", "ps4", "ps5", "ps6", "ps7")
        ph.dma("sp", selB[0:NS, :], selB_d, (), ("selB",), "d0")
        ph.dma("sp", pti, pt_d.broadcast_to([128, NS * NPG]), (), ("pti",), "d1")
        ph.cp(ptf, pti, ("pti",), ("ptf",))
        ph.ts(ptf, ptf, 128.0, ALU.mult, ("ptf", "pidx"), ("ptf2",), s2=pidx, op1=ALU.add)
        ph.cp(idx, ptf, ("ptf2",), ("idx",))
        for s in range(2):
            ph.memset(Ksf[s], 0.0, ("Ksf%d" % s,)); ph.memset(Vsf[s], 0.0, ("Vsf%d" % s,))
        ngr = (NPG + 3) // 4
        gi = 0
        for b in range(NS):
            qb = b % 2
            ph.mm([(bank(qb), selB[0:NS, b * 128:(b + 1) * 128], qs[0:NS, :], True, True)], ("selB", "qs"), ("ps%d" % qb,))
            for g in range(ngr):
                r = gi % NR; gi += 1
                for jj in range(min(4, NPG - g * 4)):
                    j = g * 4 + jj
                    col = b * NPG + j
                    bn = "Kp%d_%d" % (r, jj)
                    ph.add("pool", (lambda e, o=Kp[r][:, jj, :], ia=idx[:, col:col + 1]: e.indirect_dma_start(
                        out=o, out_offset=None, in_=ck_d, in_offset=bass.IndirectOffsetOnAxis(ap=ia, axis=0))),
                        ("idx",), (bn,), dma=bn)
                    p_ = prod[j % 2]; pn = "prod%d" % (j % 2)
                    ph.tt(p_, Kp[r][:, jj, :], bank(qb), ALU.mult, (bn, "ps%d" % qb), (pn,))
                    ph.red(Ss[:, b, j, :], p_.rearrange("p (g f) -> p g f", g=8), ALU.add, (pn,), ("Ss%d" % b,))
            s = b % 2
            ph.dma("sp", Ksf[s][0:1, :], ko_d[T + b:T + b + 1, :], ("ko%d" % NTP,), ("Ksf%d" % s,), "ksf%d" % s)
            ph.tt(prod[0], Ksf[s], bank(qb), ALU.mult, ("Ksf%d" % s, "ps%d" % qb), ("prod0",))
            ph.red(Ss[:, b, NPG, :], prod[0].rearrange("p (g f) -> p g f", g=8), ALU.add, ("prod0",), ("Ss%d" % b,))
        allS = tuple("Ss%d" % b for b in range(NS))
        ph.ts(Ss[:, :, NPG, :], Ss[:, :, NPG, :], negm, ALU.add, allS + ("negm",), ("SsA",))
        ph.red(mp.rearrange("p (b h) -> p b h", b=NS), Ss.rearrange("p b s h -> p b h s"), ALU.max, ("SsA",), ("mp",))
        ph.tr([(bank(2)[0:H8, 0:128], mp, ident)], ("mp", "ident"), ("ps2",))
        ph.red(gmx[0:H8, :], bank(2)[0:H8, 0:128], ALU.max, ("ps2",), ("gmx",))
        ph.ts(dg[0:H8, :], ident[0:H8, 0:H8], gmx[0:H8, :], ALU.mult, ("gmx", "ident"), ("dg",))
        ph.mm([(bank(2)[:, 0:H8], onesf[0:H8, :], dg[0:H8, :], True, True)], ("dg", "onesf"), ("ps2",))
        ph.tt(Ss, Ss, bank(2)[:, 0:H8].rearrange("p (b o h) -> p b o h", b=NS, o=1).broadcast_to([128, NS, NSL, 8]),
              ALU.subtract, ("SsA", "ps2"), ("SsB",))
        ph.act(Ss, Ss, AF.Exp, ("SsB",), ("P",), scale=0.125)
        gi = 0
        for b in range(NS):
            for g in range(ngr):
                r = gi % NR; gi += 1
                for jj in range(min(4, NPG - g * 4)):
                    j = g * 4 + jj
                    col = b * NPG + j
                    bn = "Vp%d_%d" % (r, jj)
                    ph.add("pool", (lambda e, o=Vp[r][:, jj, :], ia=idx[:, col:col + 1]: e.indirect_dma_start(
                        out=o, out_offset=None, in_=cv_d, in_offset=bass.IndirectOffsetOnAxis(ap=ia, axis=0))),
                        ("idx",), (bn,), dma=bn)
                    items = [(bank(3 + h)[:, b * 2:b * 2 + 2], Vp[r][:, jj, h * 128:(h + 1) * 128], Ss[:, b, j, 2 * h:2 * h + 2],
                              j == 0, False) for h in range(4)]
                    items.append((bank(7)[:, b * 8:b * 8 + 8], onesf, Ss[:, b, j, :], j == 0, False))
                    ph.mm(items, (bn, "P", "onesf"), OTB)
            s = b % 2
            ph.dma("sp", Vsf[s][0:1, :], vo_d[T + b:T + b + 1, :], ("vo%d" % NTP,), ("Vsf%d" % s,), "vsf%d" % s)
            items = [(bank(3 + h)[:, b * 2:b * 2 + 2], Vsf[s][:, h * 128:(h + 1) * 128], Ss[:, b, NPG, 2 * h:2 * h + 2],
                      False, True) for h in range(4)]
            items.append((bank(7)[:, b * 8:b * 8 + 8], onesf, Ss[:, b, NPG, :], False, True))
            ph.mm(items, ("Vsf%d" % s, "P", "onesf"), OTB)
        ph.recip(rls, bank(7)[:, 0:H8], ("ps7",), ("rls",))
        rl4 = rls.rearrange("p (b h m) -> p b h m", b=NS, h=4)
        for h in range(4):
            ph.tt(t1s, bank(3 + h)[:, 0:2 * NS].rearrange("p (b m) -> p b m", b=NS), rl4[:, :, h, :], ALU.mult,
                  ("ps%d" % (3 + h), "rls"), ("t1s",))
            ph.stt(atts[:, h, :], t1s[:, :, 1], nlam_t, t1s[:, :, 0], ALU.mult, ALU.add, ("t1s", "nlam"), ("atts%d" % h,))
        alla = tuple("atts%d" % h for h in range(4))
        af = atts.rearrange("p h b -> p (h b)")
        ph.tt(sqs, af, af, ALU.mult, alla, ("sqs",))
        ph.mm([(bank(2)[:, 0:4 * NS], onesf, sqs, True, True)], ("sqs", "onesf"), ("ps2",))
        ph.ts(rss, bank(2)[:, 0:4 * NS], 1.0 / 128, ALU.mult, ("ps2",), ("rss0",), s2=RMS_EPS, op1=ALU.add)
        ph.act(rss, rss, AF.Sqrt, ("rss0",), ("rss1",))
        ph.recip(rss, rss, ("rss1",), ("rss2",))
        ph.tt(af, af, rss, ALU.mult, alla + ("rss2",), ("attn",))
        ph.ts(aoT[:, :, T:NT], atts, gcol, ALU.mult, ("attn", "gcol"), ("aoTs",), s2=1.0 - LAM_INIT, op1=ALU.mult)
        ph.run()

        E_LO = 92.5
        pb_ = Bump(ar, E_LO, 207)
        WB = pb_([128, 2, 16, 128])
        CTr = pb_([128, 16, 128], BF16); CTi = pb_([128, 16, 128], BF16)
        magP = pb_([128, 16]); ec1 = pb_([128, 16]); es1 = pb_([128, 16]); lbrP = pb_([128, 16, 1]); lbiP = pb_([128, 16, 1])
        sout = [pb_([128, 16, 1 + NS]), pb_([128, 16, 1 + NS])]
        e_mark = pb_.p
        ph = Phase(ctx)
        tb_ = Bump(ar, e_mark / 256.0, 207)
        aPt = tb_([128, 48]); bPt = tb_([128, 2, 16, 128]); cst = tb_([128, 2048])
        BB = tb_([128, 2, 16, 128]); w2 = tb_([128, 16, 128]); w3 = tb_([128, 16, 128])
        ph.dma("sp", aPt, aP_d, (), ("Pin",), "e0")
        ph.dma("sp", bPt[:, 0], bP_d[:, 0:2048].rearrange("p (a b) -> p a b", a=16), (), ("bPt0",), "e1")
        ph.dma("act", bPt[:, 1], bP_d[:, 2048:4096].rearrange("p (a b) -> p a b", a=16), (), ("bPt1",), "e2")
        ph.dma("sp", cst, cT_d[:, 0:2048], (), ("cst",), "e3")
        ph.cp(CTr.rearrange("p a b -> p (a b)"), cst, ("cst",), ("CTr",))
        ph.dma("sp", cst, cT_d[:, 2048:4096], ("CTr",), ("cst2",), "e3")
        ph.act(CTi.rearrange("p a b -> p (a b)"), cst, AF.Copy, ("cst2",), ("CTi",), scale=-1.0)

        def disc(tag, a_re, a_im, ldt, W, want_f):
            keys = ("ar", "dt", "xr", "th", "acc", "tmp", "s", "a", "x2", "u", "s2", "a2")
            t = {k: tb_([128, W]) for k in keys}
            N = lambda k: tag + k
            IN = tag + "in"

            def TS(o, i, s1, op0, s2=None, op1=None, extra=()):
                ph.ts(t[o], t[i], s1, op0, (N(i),) + extra, (N(o),), s2=s2, op1=op1)

            def TT(o, i0, i1, op):
                ph.tt(t[o], t[i0], t[i1], op, (N(i0), N(i1)), (N(o),))

            ph.ts(t["ar"], a_re, -1e-4, ALU.min, (IN,), (N("ar"),))
            ph.act(t["dt"], ldt, AF.Exp, (IN,), (N("dt"),))
            TT("xr", "ar", "dt", ALU.mult)
            ph.tt(t["th"], a_im, t["dt"], ALU.mult, (IN, N("dt")), (N("th"),))
            TS("acc", "xr", 0.1, ALU.mult, s2=1.0, op1=ALU.add)
            for k in range(9, 0, -1):
                TT("tmp", "xr", "acc", ALU.mult)
                if k > 1:
                    TS("acc", "tmp", 1.0 / k, ALU.mult, s2=1.0, op1=ALU.add)
            TS("acc", "tmp", 1.0, ALU.add)
            TS("u", "th", 1.0 / 1024, ALU.mult)
            TT("x2", "u", "u", ALU.mult)
            TS("s", "x2", -1.0 / 20, ALU.mult, s2=1.0, op1=ALU.add)
            TT("s", "s", "x2", ALU.mult)
            TS("s", "s", -1.0 / 6, ALU.mult, s2=1.0, op1=ALU.add)
            TT("s", "s", "u", ALU.mult)
            TS("a", "x2", -1.0 / 30, ALU.mult, s2=1.0, op1=ALU.add)
            TT("a", "a", "x2", ALU.mult)
            TS("a", "a", -1.0 / 12, ALU.mult, s2=1.0, op1=ALU.add)
            TT("a", "a", "x2", ALU.mult)
            TS("a", "a", 0.5, ALU.mult)
            s_, a_, s2_, a2_ = "s", "a", "s2", "a2"
            for _ in range(10):
                TS("x2", a_, -1.0, ALU.mult, s2=1.0, op1=ALU.add)
                ph.stt(t[a2_], t[s_], 2.0, t[s_], ALU.mult, ALU.mult, (N(s_),), (N(a2_),))
                ph.stt(t[s2_], t[s_], 2.0, t["x2"], ALU.mult, ALU.mult, (N(s_), N("x2")), (N(s2_),))
                s_, s2_, a_, a2_ = s2_, s_, a2_, a_
            res = dict(mag=t["acc"], magn=N("acc"), s=t[s_], sn=N(s_), a=t[a_], an=N(a_))
            if want_f:
                TT("x2", "acc", a_, ALU.mult)
                TT("x2", "tmp", "x2", ALU.subtract)
                TT("th", "acc", s_, ALU.mult)
                TT("dt", "ar", "ar", ALU.mult)
                ph.tt(t["xr"], a_im, a_im, ALU.mult, (IN,), (N("xr"),))
                TT("dt", "dt", "xr", ALU.add)
                ph.recip(t["dt"], t["dt"], (N("dt"),), (N("dt"),))
                TT("xr", "x2", "ar", ALU.mult)
                ph.tt(t["u"], t["th"], a_im, ALU.mult, (N("th"), IN), (N("u"),))
                TT("xr", "xr", "u", ALU.add)
                TT("xr", "xr", "dt", ALU.mult)
                TT(s2_, "th", "ar", ALU.mult)
                ph.tt(t["u"], t["x2"], a_im, ALU.mult, (N("x2"), IN), (N("u"),))
                TT(s2_, s2_, "u", ALU.subtract)
                TT(s2_, s2_, "dt", ALU.mult)
                res.update(fre=t["xr"], fren=N("xr"), fim=t[s2_], fimn=N(s2_))
            return res

        rP = disc("P", aPt[:, 0:16], aPt[:, 16:32], aPt[:, 32:48], 16, True)
        fre = rP["fre"].unsqueeze(2).broadcast_to([128, 16, 128]); fim = rP["fim"].unsqueeze(2).broadcast_to([128, 16, 128])
        ph.tt(w2, bPt[:, 0], fre, ALU.mult, (rP["fren"], "bPt0"), ("w2",))
        ph.tt(w3, bPt[:, 1], fim, ALU.mult, (rP["fimn"], "bPt1"), ("w3",))
        ph.tt(BB[:, 0], w2, w3, ALU.subtract, ("w2", "w3"), ("BB0",))
        ph.tt(w2, bPt[:, 1], fre, ALU.mult, (rP["fren"], "bPt1"), ("w2",))
        ph.tt(w3, bPt[:, 0], fim, ALU.mult, (rP["fimn"], "bPt0"), ("w3",))
        ph.tt(BB[:, 1], w2, w3, ALU.add, ("w2", "w3"), ("BB1",))
        for ri_ in range(2):
            for g4 in range(4):
                bk = (ri_ * 4 + g4) % 4
                ph.tr([(bank(bk)[:, q * 128:(q + 1) * 128], BB[:, ri_, g4 * 4 + q, :], ident) for q in range(4)],
                      ("BB%d" % ri_, "ident"), ("ps%d" % bk,))
                ph.cp(WB[:, ri_, g4 * 4:g4 * 4 + 4, :], bank(bk).rearrange("p (q f) -> p q f", q=4), ("ps%d" % bk,), ("WB",),
                      eng="act" if g4 % 2 else "dve")
        cP = tb_([128, 16]); nP = tb_([128, 16]); n2 = tb_([128, 16])
        ph.ts(cP, rP["a"], -1.0, ALU.mult, (rP["an"],), ("cP",), s2=1.0, op1=ALU.add)
        ph.tt(nP, cP, cP, ALU.mult, ("cP",), ("nP",))
        ph.tt(n2, rP["s"], rP["s"], ALU.mult, (rP["sn"],), ("n2",))
        ph.tt(nP, nP, n2, ALU.add, ("nP", "n2"), ("nP",))
        ph.ts(nP, nP, -0.5, ALU.mult, ("nP",), ("nP",), s2=1.5, op1=ALU.add)
        ph.tt(ec1, cP, nP, ALU.mult, ("cP", "nP"), ("ec1",))
        ph.tt(es1, rP["s"], nP, ALU.mult, (rP["sn"], "nP"), ("es1",))
        ph.cp(magP, rP["mag"], (rP["magn"],), ("magP",))
        ph.tt(lbrP.rearrange("p a b -> p (a b)"), rP["mag"], ec1, ALU.mult, (rP["magn"], "ec1"), ("lbrP",))
        ph.tt(lbiP.rearrange("p a b -> p (a b)"), rP["mag"], es1, ALU.mult, (rP["magn"], "es1"), ("lbiP",))
        ph.run()

        if NTP:
            ph = Phase(ctx)
            tb_ = Bump(ar, e_mark / 256.0, 207)
            Ec = tb_([128, T]); Es = tb_([128, T]); zr = tb_([128, T]); zi = tb_([128, T])
            rr = tb_([128, T]); ri = tb_([128, T]); tA = tb_([128, T]); tBt = tb_([128, T])
            Sr = tb_([128, T], BF16); Si = tb_([128, T], BF16)
            en_ = [tb_([128, 2]), tb_([128, 2])]; e2_ = tb_([128, 2]); u1 = tb_([128, 2])
            yv = tb_([128, 512]); g1 = tb_([128, 512]); g2 = tb_([128, 512])
            PB = [(t0, n) for (t0, n) in TB if t0 < T]
            LOGT = int(math.log2(T))
            assert 1 << LOGT == T
            for gp in range(16):
                c, rows = gp // 4, (gp % 4) * 32
                ph.memset(Ec[:, 0:1], 1.0, ("Ec",)); ph.memset(Es[:, 0:1], 0.0, ("Es",))
                ph.cp(en_[0][:, 0:1], ec1[:, gp:gp + 1], (), ("en0",)); ph.cp(en_[0][:, 1:2], es1[:, gp:gp + 1], (), ("en0",))
                for k in range(LOGT):
                    n = 1 << k
                    e0, e1_ = en_[k % 2], en_[(k + 1) % 2]
                    n0, n1 = "en%d" % (k % 2), "en%d" % ((k + 1) % 2)
                    ph.ts(tA[:, 0:n], Es[:, 0:n], e0[:, 1:2], ALU.mult, ("Es", n0), ("tA",))
                    ph.stt(Ec[:, n:2 * n], Ec[:, 0:n], e0[:, 0:1], tA[:, 0:n], ALU.mult, ALU.subtract, ("Ec", n0, "tA"), ("Ec",))
                    ph.ts(tBt[:, 0:n], Es[:, 0:n], e0[:, 0:1], ALU.mult, ("Es", n0), ("tB",))
                    ph.stt(Es[:, n:2 * n], Ec[:, 0:n], e0[:, 1:2], tBt[:, 0:n], ALU.mult, ALU.add, ("Ec", n0, "tB"), ("Es",))
                    if k < LOGT - 1:
                        ph.tt(e2_, e0, e0, ALU.mult, (n0,), ("e2",))
                        ph.tt(e1_[:, 0:1], e2_[:, 0:1], e2_[:, 1:2], ALU.subtract, ("e2",), (n1,))
                        ph.stt(e1_[:, 1:2], e0[:, 0:1], 2.0, e0[:, 1:2], ALU.mult, ALU.mult, (n0,), (n1,))
                for bi, (t0, n) in enumerate(PB):
                    xb = 2 * (bi % 2)
                    un = "uT%d_%d" % (c, bi)
                    ph.mm([(bank(xb, n), WB[:, 0, gp, :], uT[:, c, t0:t0 + n], True, True),
                           (bank(xb + 1, n), WB[:, 1, gp, :], uT[:, c, t0:t0 + n], True, True)],
                          (un,), ("ps%d" % xb, "ps%d" % (xb + 1)))
                    xn0, xn1 = "ps%d" % xb, "ps%d" % (xb + 1)
                    sl = slice(t0, t0 + n)
                    ph.tt(tA[:, sl], bank(xb, n), Ec[:, sl], ALU.mult, (xn0, "Ec"), ("tA",))
                    ph.tt(tBt[:, sl], bank(xb + 1, n), Es[:, sl], ALU.mult, (xn1, "Es"), ("tB",))
                    ph.tt(zr[:, sl], tA[:, sl], tBt[:, sl], ALU.add, ("tA", "tB"), ("zr",))
                    ph.tt(tA[:, sl], bank(xb + 1, n), Ec[:, sl], ALU.mult, (xn1, "Ec"), ("tA",))
                    ph.tt(tBt[:, sl], bank(xb, n), Es[:, sl], ALU.mult, (xn0, "Es"), ("tB",))
                    ph.tt(zi[:, sl], tA[:, sl], tBt[:, sl], ALU.subtract, ("tA", "tB"), ("zi",))
                dec = magP[:, gp:gp + 1].broadcast_to([128, T])
                ph.add("dve", (lambda e, o=rr, d0=dec, d1=zr: e.tensor_tensor_scan(o, d0, d1, 0.0, ALU.mult, ALU.add)), ("zr",), ("rr",))
                ph.add("dve", (lambda e, o=ri, d0=dec, d1=zi: e.tensor_tensor_scan(o, d0, d1, 0.0, ALU.mult, ALU.add)), ("zi",), ("ri",))
                ph.tt(tA, rr, Ec, ALU.mult, ("rr", "Ec"), ("tA",))
                ph.tt(tBt, ri, Es, ALU.mult, ("ri", "Es"), ("tB",))
                ph.tt(Sr, tA, tBt, ALU.subtract, ("tA", "tB"), ("Sr",))
                ph.tt(sout[0][:, gp, 0:1], tA[:, T - 1:T], tBt[:, T - 1:T], ALU.subtract, ("tA", "tB"), ("so_re",))
                ph.tt(tA, ri, Ec, ALU.mult, ("ri", "Ec"), ("tA",))
                ph.tt(tBt, rr, Es, ALU.mult, ("rr", "Es"), ("tB",))
                ph.tt(Si, tA, tBt, ALU.add, ("tA", "tB"), ("Si",))
                ph.tt(sout[1][:, gp, 0:1], tA[:, T - 1:T], tBt[:, T - 1:T], ALU.add, ("tA", "tB"), ("so_im",))
                for bi, (t0, n) in enumerate(PB):
                    ph.mm([(bank(4 + bi, n), CTr[:, gp, :], Sr[:, t0:t0 + n], gp % 4 == 0, False),
                           (bank(4 + bi, n), CTi[:, gp, :], Si[:, t0:t0 + n], False, gp % 4 == 3)], ("Sr", "Si"), ("ps%d" % (4 + bi),))
                if gp % 4 == 3:
                    for bi, (t0, n) in enumerate(PB):
                        y_ = yv[:, 0:n]; a_ = g1[:, 0:n]; b_ = g2[:, 0:n]
                        ph.stt(y_, uT[:, c, t0:t0 + n], dP[:, c:c + 1], bank(4 + bi, n), ALU.mult, ALU.add, ("ps%d" % (4 + bi),), ("yv",))
                        ph.tt(a_, y_, y_, ALU.mult, ("yv",), ("g1",))
                        ph.ts(a_, a_, 0.044715, ALU.mult, ("g1",), ("g1",), s2=1.0, op1=ALU.add)
                        ph.tt(a_, a_, y_, ALU.mult, ("g1", "yv"), ("g1",))
                        ph.act(b_, a_, AF.Sigmoid, ("g1",), ("g2",), scale=1.5957691216057308)
                        ph.tt(gss[:, c, t0:t0 + n], y_, b_, ALU.mult, ("yv", "g2"), ("gss%d_%d" % (c, bi),))
            ph.run()

        ph = Phase(ctx)
        tb_ = Bump(ar, e_mark / 256.0, 207)
        s0r = tb_([128, 16, NS]); s0i = tb_([128, 16, NS]); q1 = tb_([128, 16, NS]); q2 = tb_([128, 16, NS])
        Ssr = tb_([128, 16, NS], BF16); Ssi = tb_([128, 16, NS], BF16)
        yv = tb_([128, 4, NS]); g1 = tb_([128, 4, NS]); g2 = tb_([128, 4, NS])
        ph.dma("sp", s0r, st_re_d.rearrange("p (a b) -> p a b", a=16), (), ("s0r",), "f0")
        ph.dma("sp", s0i, st_im_d.rearrange("p (a b) -> p a b", a=16), (), ("s0i",), "f1")
        items = []
        for gp in range(16):
            c, rows = gp // 4, (gp % 4) * 32
            for ri_ in range(2):
                items.append((bank(0)[:, (gp * 2 + ri_) * NS:(gp * 2 + ri_ + 1) * NS], WB[:, ri_, gp, :], uT[:, c, T:NT], True, True))
        ph.mm(items, (), ("ps0",))
        Xv = bank(0)[:, 0:32 * NS].rearrange("p (g r b) -> p g r b", g=16, r=2)
        lbr = lbrP.broadcast_to([128, 16, NS]); lbi = lbiP.broadcast_to([128, 16, NS])
        ph.tt(q1, s0i, lbi, ALU.mult, ("s0i",), ("q1",))
        ph.tt(q2, s0r, lbr, ALU.mult, ("s0r",), ("q2",))
        ph.tt(q2, q2, q1, ALU.subtract, ("q1", "q2"), ("q2",))
        ph.tt(sout[0][:, :, 1:1 + NS], q2, Xv[:, :, 0, :], ALU.add, ("q2", "ps0"), ("so_re",))
        ph.cp(Ssr, sout[0][:, :, 1:1 + NS], ("so_re",), ("Ssr",))
        ph.tt(q1, s0r, lbi, ALU.mult, ("s0r", "q2"), ("q1",))
        ph.tt(q2, s0i, lbr, ALU.mult, ("s0i", "so_re"), ("q2",))
        ph.tt(q2, q2, q1, ALU.add, ("q1", "q2"), ("q2",))
        ph.tt(sout[1][:, :, 1:1 + NS], q2, Xv[:, :, 1, :], ALU.add, ("q2", "ps0"), ("so_im",))
        ph.cp(Ssi, sout[1][:, :, 1:1 + NS], ("so_im",), ("Ssi",))
        ph.dma("sp", sre_d.rearrange("p (a b) -> p a b", a=16), sout[0], ("so_re",), ("sre_d",), "f2")
        ph.dma("sp", sim_d.rearrange("p (a b) -> p a b", a=16), sout[1], ("so_im",), ("sim_d",), "f3")
        for c in range(4):
            items = []
            for gl in range(4):
                gp = 4 * c + gl
                items.append((bank(1)[:, c * NS:(c + 1) * NS], CTr[:, gp, :], Ssr[:, gp, :], gl == 0, False))
                items.append((bank(1)[:, c * NS:(c + 1) * NS], CTi[:, gp, :], Ssi[:, gp, :], False, gl == 3))
            ph.mm(items, ("Ssr", "Ssi"), ("ps1",))
        for c in range(4):
            ph.stt(yv[:, c, :], uT[:, c, T:NT], dP[:, c:c + 1], bank(1)[:, c * NS:(c + 1) * NS], ALU.mult, ALU.add, ("ps1",), ("yv%d" % c,))
        ally = tuple("yv%d" % c for c in range(4))
        ph.tt(g1, yv, yv, ALU.mult, ally, ("g1",))
        ph.ts(g1, g1, 0.044715, ALU.mult, ("g1",), ("g1",), s2=1.0, op1=ALU.add)
        ph.tt(g1, g1, yv, ALU.mult, ("g1",) + ally, ("g1",))
        ph.act(g2, g1, AF.Sigmoid, ("g1",), ("g2",), scale=1.5957691216057308)
        ph.tt(gss[:, :, T:NT], yv, g2, ALU.mult, ally + ("g2",), ("gss_s",))
        ph.run()

        ph = Phase(ctx)
        tb_ = Bump(ar, 92.5, 207)
        wgl = tb_([128, 4, 1024], BF16)
        wst = [tb_([128, 1024]), tb_([128, 1024])]
        sg = [tb_([128, 512]), tb_([128, 512])]
        for kc in range(4):
            s = kc % 2
            ph.dma("sp", wst[s], w_glu_d[kc * 128:(kc + 1) * 128, :], (), ("wst%d" % s,), "g%d" % s)
            ph.cp(wgl[:, kc, :], wst[s], ("wst%d" % s,), ("wgl",), eng="pool")
        it = 0
        for oc in range(4):
            for bi, (t0, n) in enumerate(TB):
                b = 2 * (it % 2); it += 1
                ph.mm([(bank(b, n), wgl[:, kc, oc * 128:(oc + 1) * 128], gss[:, kc, t0:t0 + n], kc == 0, kc == 3) for kc in range(4)]
                      + [(bank(b + 1, n), wgl[:, kc, 512 + oc * 128:512 + (oc + 1) * 128], gss[:, kc, t0:t0 + n], kc == 0, kc == 3) for kc in range(4)],
                      ("wgl",), ("ps%d" % b, "ps%d" % (b + 1)))
                s_ = sg[it % 2][:, 0:n]
                ph.act(s_, bank(b + 1, n), AF.Sigmoid, ("ps%d" % (b + 1),), ("sg%d" % (it % 2),), bias=bglu[:, 4 + oc:5 + oc])
                ph.stt(soT[:, oc, t0:t0 + n], bank(b, n), bglu[:, oc:oc + 1], s_, ALU.add, ALU.mult, ("ps%d" % b, "sg%d" % (it % 2)), ("soT",))
        ph.run()

        def layer_norm(ph, src, srcn, n, gam, bet, out_tile, outn, tmp, tmpn, stat):
            st6, mv, rs_, nmr = stat
            for j in range(2):
                ph.add("dve", (lambda e, o=st6[0:n, j, :], i=src[j]: e.bn_stats(o, i)), (srcn[j],), ("st6",))
            ph.add("dve", (lambda e, o=mv[0:n, :], i=st6[0:n, :, :].rearrange("p a b -> p (a b)"): e.bn_aggr(o, i)), ("st6",), ("mv",))
            ph.ts(rs_[0:n, :], mv[0:n, 1:2], LN_EPS, ALU.add, ("mv",), ("rs",))
            ph.act(rs_[0:n, :], rs_[0:n, :], AF.Ln, ("rs",), ("rs",))
            ph.act(rs_[0:n, :], rs_[0:n, :], AF.Exp, ("rs",), ("rs",), scale=-0.5)
            ph.stt(nmr[0:n, :], mv[0:n, 0:1], -1.0, rs_[0:n, :], ALU.mult, ALU.mult, ("mv", "rs"), ("nmr",))
            for j in range(2):
                ph.act(tmp[0:n, j * 512:(j + 1) * 512], src[j], AF.Identity, (srcn[j], "rs", "nmr"), (tmpn[j],),
                       bias=nmr[0:n, :], scale=rs_[0:n, :])
            ph.tt(tmp[0:n, :], tmp[0:n, :], gam[0:n, :], ALU.mult, tuple(tmpn) + ("lng",), tuple(tmpn))
            ph.tt(out_tile[0:n, :], tmp[0:n, :], bet[0:n, :], ALU.add, tuple(tmpn) + ("lng",), (outn,))

        ph = Phase(ctx)
        tb_ = Bump(ar, 150.5, 207)
        wob = tb_([128, 8, 1024], BF16)
        wst = [tb_([128, 1024]), tb_([128, 1024])]
        lng = tb_([128, 1024]); lnb = tb_([128, 1024])
        xt = [tb_([128, 1024]), tb_([128, 1024])]
        tl = tb_([128, 1024]); ht = tb_([128, 1024])
        wrt = tb_([128, 8, 32]); brt = tb_([128, 32])
        st6 = tb_([128, 2, 6]); mv = tb_([128, 2]); rs_ = tb_([128, 1]); nmr = tb_([128, 1])
        lg = tb_([128, 32]); m8 = tb_([128, 8]); sel = tb_([128, 32]); nm_ = tb_([128, 1]); ex = tb_([128, 32])
        den = tb_([128, 1]); gt = tb_([128, 32])
        for kc in range(8):
            s = kc % 2
            ph.dma("sp", wst[s], w_out_d[kc * 128:(kc + 1) * 128, :], (), ("wst%d" % s,), "h%d" % s)
            ph.cp(wob[:, kc, :], wst[s], ("wst%d" % s,), ("wob",), eng="pool")
        ph.dma("act", lng, ln_d[:, 0:D].broadcast_to([128, D]), (), ("lng",), "h2")
        ph.dma("act", lnb, ln_d[:, D:2 * D].broadcast_to([128, D]), (), ("lng",), "h3")
        ph.dma("act", wrt[:, :, 0:NE], wr_d[:, 0:NE].rearrange("(c p) e -> p c e", p=128), (), ("wrt",), "h4")
        ph.dma("act", brt[:, 0:NE], br_d.broadcast_to([128, NE]), (), ("brt",), "h5")
        for i, (t0, n) in enumerate(TL):
            s = i % 2
            ph.dma("sp", xt[s][0:n, :], x_d[t0:t0 + n, :], (), ("xt%d" % s,), "x%d" % s)
            for j in range(2):
                ph.mm([(ps_t[0:n, j * 512:(j + 1) * 512], (soT[:, kc, t0:t0 + n] if kc < 4 else aoT[:, kc - 4, t0:t0 + n]),
                        wob[:, kc, j * 512:(j + 1) * 512], kc == 0, kc == 7) for kc in range(8)], ("wob",), ("ps%d" % j,))
                ph.stt(tl[0:n, j * 512:(j + 1) * 512], xt[s][0:n, j * 512:(j + 1) * 512], ALPHA, ps_t[0:n, j * 512:(j + 1) * 512],
                       ALU.mult, ALU.add, ("xt%d" % s, "ps%d" % j), ("tl%d" % j,))
            layer_norm(ph, [tl[0:n, 0:512], tl[0:n, 512:1024]], ("tl0", "tl1"), n, lng, lnb, ht, "ht", tl, ("tl0", "tl1"),
                       (st6, mv, rs_, nmr))
            for j in range(2):
                ph.tr([(ps_t[:, (2 + j) * 512 + cc * 128:(2 + j) * 512 + cc * 128 + n], ht[0:n, (4 * j + cc) * 128:(4 * j + cc + 1) * 128],
                        ident[0:n, 0:n]) for cc in range(4)], ("ht", "ident"), ("ps%d" % (2 + j),))
                src = bank(2 + j).rearrange("p (c t) -> p c t", c=4)[:, :, 0:n]
                ph.act(fT[:, 4 * j:4 * j + 4, t0:t0 + n], src, AF.Copy, ("ps%d" % (2 + j),), ("fT%d" % i,), scale=ALPHA)
                ph.cp(hTb[:, 4 * j:4 * j + 4, t0:t0 + n], src, ("ps%d" % (2 + j),), ("hTb%d" % i,))
            ph.mm([(ps_t[0:n, 4 * 512:4 * 512 + NE], fT[:, c, t0:t0 + n], wrt[:, c, 0:NE], c == 0, c == 7) for c in range(8)],
                  ("fT%d" % i, "wrt"), ("ps4",))
            ph.stt(lg[0:n, 0:NE], ps_t[0:n, 4 * 512:4 * 512 + NE], 1.0 / ALPHA, brt[0:n, 0:NE], ALU.mult, ALU.add, ("ps4", "brt"), ("lg",))
            ph.add("dve", (lambda e, o=m8[0:n, :], i_=lg[0:n, 0:NE]: e.max(o, i_)), ("lg",), ("m8",))
            ph.ts(sel[0:n, 0:NE], lg[0:n, 0:NE], m8[0:n, cfg.TOPK - 1:cfg.TOPK], ALU.is_ge, ("lg", "m8"), ("sel",))
            ph.ts(nm_[0:n, :], m8[0:n, 0:1], -1.0, ALU.mult, ("m8",), ("nm",))
            ph.act(ex[0:n, 0:NE], lg[0:n, 0:NE], AF.Exp, ("lg", "nm"), ("ex",), bias=nm_[0:n, :])
            ph.tt(ex[0:n, 0:NE], ex[0:n, 0:NE], sel[0:n, 0:NE], ALU.mult, ("ex", "sel"), ("ex2",))
            ph.red(den[0:n, :], ex[0:n, 0:NE], ALU.add, ("ex2",), ("den",))
            ph.recip(den[0:n, :], den[0:n, :], ("den",), ("den2",))
            ph.ts(gt[0:n, 0:NE], ex[0:n, 0:NE], den[0:n, :], ALU.mult, ("ex2", "den2"), ("gt",))
            ph.tr([(ps_t[0:NE, 5 * 512:5 * 512 + n], gt[0:n, 0:NE], ident[0:n, 0:n])], ("gt", "ident"), ("ps5",))
            ph.cp(gatesT[0:NE, t0:t0 + n], ps_t[0:NE, 5 * 512:5 * 512 + n], ("ps5",), ("gatesT",), eng="act")
        ph.run()

        ph = Phase(ctx)
        tb_ = Bump(ar, 159, 207)
        bdn = tb_([128, D])
        ph.dma("act", bdn[0:NE, :], bdn_d, (), ("bdn",), "m1")
        for dc in range(8):
            for bi, (t0, n) in enumerate(TB):
                b = (dc * len(TB) + bi) % 4
                ph.mm([(bank(b, n), bdn[0:NE, dc * 128:(dc + 1) * 128], gatesT[0:NE, t0:t0 + n], True, True)], ("bdn",), ("ps%d" % b,))
                ph.tt(fT[:, dc, t0:t0 + n], fT[:, dc, t0:t0 + n], bank(b, n), ALU.add, ("ps%d" % b,), ("fT%d_%d" % (dc, bi),))
        ph.run()

        ph = Phase(ctx)
        GeS = ar.at(int(150.5 * KB), [128, NT])
        tb_ = Bump(ar, 159, 207)
        stg = [tb_([128, 2048]), tb_([128, 2048])]
        wpb = [tb_([128, 8, 256], BF16) for _ in range(2)]
        Gt = [tb_([128, 512]) for _ in range(3)]; Sg = [tb_([128, 512]) for _ in range(3)]; Lt = [tb_([128, 512]) for _ in range(3)]
        bguT = tb_([128, NE, 16]); bl1 = tb_([128, NE, 8]); selt = [tb_([128, 128]), tb_([128, 128])]
        ph.dma("act", bguT, bgu_d.rearrange("p (e c) -> p e c", e=NE), (), ("bguT",), "m0")
        ph.ts(bl1, bguT[:, :, 8:16], 1.0, ALU.add, ("bguT",), ("bl1",))
        pieces = [(e_, kind, j) for e_ in range(NE) for kind in ("gu", "dn") for j in range(8)]
        cnt = dict(it=0, un=0)

        def emit_load(i):
            e_, kind, j = pieces[i]
            s2_ = i % 2
            row0 = (e_ * 8 + j) * 128
            if kind == "gu":
                ph.dma("sp", stg[s2_], wgu_d[row0:row0 + 128, :], (), ("stg%d" % s2_,), "stg%d" % s2_)
                ph.cp(wpb[s2_].rearrange("p a b -> p (a b)"), stg[s2_], ("stg%d" % s2_,), ("wpb%d" % s2_,), eng="act")
            else:
                ph.dma("sp", stg[s2_][:, 0:1024], wdn_d[row0:row0 + 128, :], (), ("stg%d" % s2_,), "stg%d" % s2_)
                ph.cp(wpb[s2_].rearrange("p a b -> p (a b)")[:, 0:1024], stg[s2_][:, 0:1024], ("stg%d" % s2_,), ("wpb%d" % s2_,), eng="act")

        def emit_compute(i):
            e_, kind, j = pieces[i]
            s2_ = i % 2
            if kind == "gu" and j == 0:
                st_ = selt[e_ % 2]; sn_ = "selt%d" % (e_ % 2)
                ph.cp(st_[0:NE, :], ident[0:NE, e_:e_ + 1].broadcast_to([NE, 128]), (), (sn_,), eng="pool")
                for bi, (t0, n) in enumerate(TB):
                    b = 6 + bi % 2
                    ph.mm([(bank(b, n), st_[0:NE, :], gatesT[0:NE, t0:t0 + n], True, True)], (sn_,), ("ps%d" % b,))
                    ph.cp(GeS[:, t0:t0 + n], bank(b, n), ("ps%d" % b,), ("GeS%d" % bi,), eng="act")
            if kind == "gu":
                fc = j
                for bi, (t0, n) in enumerate(TB):
                    b = 2 * (cnt["it"] % 3); cnt["it"] += 1
                    k3 = cnt["un"] % 3; cnt["un"] += 1
                    ph.mm([(bank(b, n), wpb[s2_][:, kc, 0:128], hTb[:, kc, t0:t0 + n], kc == 0, kc == 7) for kc in range(8)]
                          + [(bank(b + 1, n), wpb[s2_][:, kc, 128:256], hTb[:, kc, t0:t0 + n], kc == 0, kc == 7) for kc in range(8)],
                          ("wpb%d" % s2_,), ("ps%d" % b, "ps%d" % (b + 1)))
                    G_ = Gt[k3][:, 0:n]; S_ = Sg[k3][:, 0:n]; L_ = Lt[k3][:, 0:n]
                    gn, sn, ln_ = "G%d" % k3, "S%d" % k3, "L%d" % k3
                    ph.ts(G_, bank(b, n), bguT[:, e_, fc:fc + 1], ALU.add, ("ps%d" % b, "bguT"), (gn,), s2=SW_LIM, op1=ALU.min)
                    ph.act(L_, bank(b + 1, n), AF.Identity, ("ps%d" % (b + 1), "bl1"), (ln_,), bias=bl1[:, e_, fc:fc + 1])
                    ph.act(S_, G_, AF.Sigmoid, (gn,), (sn,), scale=SW_ALPHA)
                    ph.ts(L_, L_, 1.0 - SW_LIM, ALU.max, (ln_,), (ln_,), s2=SW_LIM + 1.0, op1=ALU.min)
                    ph.tt(L_, L_, G_, ALU.mult, (ln_, gn), (ln_,))
                    ph.tt(S_, S_, L_, ALU.mult, (sn, ln_), (sn,), eng="pool")
                    ph.tt(actT[:, fc, t0:t0 + n], S_, GeS[:, t0:t0 + n], ALU.mult, (sn, "GeS%d" % bi), ("actT%d_%d" % (fc, bi),), eng="pool")
            else:
                dc = j
                wd3 = wpb[s2_].rearrange("p a b -> p (a b)")[:, 0:1024].rearrange("p (a b) -> p a b", a=8)
                for bi, (t0, n) in enumerate(TB):
                    b = 6 + (cnt["it"] % 2); cnt["it"] += 1
                    ph.mm([(bank(b, n), wd3[:, f, :], actT[:, f, t0:t0 + n], f == 0, f == 7) for f in range(8)],
                          ("wpb%d" % s2_,) + tuple("actT%d_%d" % (f, bi) for f in range(8)), ("ps%d" % b,))
                    ph.tt(fT[:, dc, t0:t0 + n], fT[:, dc, t0:t0 + n], bank(b, n), ALU.add, ("ps%d" % b,), ("fT%d_%d" % (dc, bi),))

        emit_load(0)
        for i in range(len(pieces)):
            if i + 1 < len(pieces):
                emit_load(i + 1)
            emit_compute(i)
        ph.run()

        ph = Phase(ctx)
        tb_ = Bump(ar, 150.5, 207)
        lng = tb_([128, 1024]); lnb = tb_([128, 1024])
        yt = [tb_([128, 1024]), tb_([128, 1024])]; tmp = tb_([128, 1024])
        st6 = tb_([128, 2, 6]); mv = tb_([128, 2]); rs_ = tb_([128, 1]); nmr = tb_([128, 1])
        ph.dma("sp", lng, ln_d[:, 2 * D:3 * D].broadcast_to([128, D]), (), ("lng",), "n0")
        ph.dma("sp", lnb, ln_d[:, 3 * D:4 * D].broadcast_to([128, D]), (), ("lng",), "n1")
        for i, (t0, n) in enumerate(TL):
            s = i % 2
            bb = 2 * (i % 2)
            for j in range(2):
                ph.tr([(ps_t[0:n, (bb + j) * 512 + cc * 128:(bb + j) * 512 + (cc + 1) * 128], fT[:, 4 * j + cc, t0:t0 + n], ident)
                       for cc in range(4)], ("ident",), ("ps%d" % (bb + j),))
            layer_norm(ph, [ps_t[0:n, (bb + j) * 512:(bb + j + 1) * 512] for j in range(2)], ("ps%d" % bb, "ps%d" % (bb + 1)), n, lng, lnb,
                       yt[s], "yt%d" % s, tmp, ("tmp0", "tmp1"), (st6, mv, rs_, nmr))
            ph.add("pool", (lambda e, o=y_d[t0:t0 + n, :], i_=yt[s][0:n, :]: e.dma_start(out=o, in_=i_)), ("yt%d" % s,), ("yd%d" % i,), dma="y%d" % s)
        ph.run()
    return nc


def _consts(cfg, past_len):
    T, NS, NT = cfg.T, cfg.NS, cfg.NT
    half = 32
    inv = (np.float32(10000.0) ** (-np.arange(half, dtype=np.float32) / np.float32(half))).astype(np.float32)
    pos = np.concatenate([np.arange(T, dtype=np.float32), np.full((NS,), past_len, np.float32)])
    ang = (pos[:, None] * inv[None, :]).astype(np.float32)
    ropeC = np.cos(ang.astype(np.float64)).astype(np.float32)
    ropeS = np.sin(ang.astype(np.float64)).astype(np.float32)
    ident = np.eye(128, dtype=np.float32)
    tri = np.where(np.arange(128)[None, :] <= np.arange(128)[:, None], 0.0, -30000.0).astype(np.float32)
    selB = np.zeros((NS, NS, 128), np.float32)
    for b in range(NS):
        selB[b, b, :] = 1.0
    pidx = np.arange(128, dtype=np.float32)[:, None].copy()
    negm = np.full((128, 1), -30000.0, np.float32); negm[0, 0] = 0.0
    return dict(ropeC=ropeC, ropeS=ropeS, ident=ident, tri=tri, selB=selB.reshape(NS, NS * 128), pidx=pidx, negm=negm)


def _shared(cfg, I):
    NE = cfg.NE
    f = lambda a: np.ascontiguousarray(a, dtype=np.float32)
    a_re, a_im, ldt = I["ssm_a_re"][0], I["ssm_a_im"][0], I["ssm_log_dt"][0]
    toP = lambda a: a.reshape(16, 2, 64).transpose(1, 2, 0).reshape(128, 16)
    aP = np.concatenate([toP(a_re), toP(a_im), toP(np.repeat(ldt[:, None], 64, 1))], axis=1)

    def bP(b):
        out = np.zeros((2, 64, 16, 4, 2, 16), np.float32)
        v = b.reshape(16, 2, 64, 16)
        for gp in range(16):
            for g2 in range(2):
                out[g2, :, gp, gp % 4, g2, :] = v[gp, g2]
        return out.reshape(128, 2048)
    bPc = np.concatenate([bP(I["ssm_b_re"][0]), bP(I["ssm_b_im"][0])], axis=1)

    def cT(cm):
        out = np.zeros((2, 64, 16, 4, 2, 16), np.float32)
        v = cm.reshape(16, 2, 16, 64)
        for gp in range(16):
            for g2 in range(2):
                out[g2, :, gp, gp % 4, g2, :] = v[gp, g2].T
        return out.reshape(128, 2048)
    cTc = np.concatenate([cT(I["ssm_c_re"][0]), cT(I["ssm_c_im"][0])], axis=1)
    dP = I["ssm_d"][0].reshape(4, 128).T
    bglu = I["b_glu"][0].reshape(8, 128).T
    lam4 = np.concatenate([I["lambda_q1"][0], I["lambda_k1"][0], I["lambda_q2"][0], I["lambda_k2"][0]])[None, :]
    ln = np.concatenate([I["ln1_g"][0], I["ln1_b"][0], I["ln2_g"][0], I["ln2_b"][0]])[None, :]
    wgu = I["w_gate_up"][0]
    wg = wgu[:, :, :1024].reshape(NE, 8, 128, 8, 128)
    wl = wgu[:, :, 1024:].reshape(NE, 8, 128, 8, 128)
    wgu_t = np.empty((NE, 8, 128, 8, 256), np.float32)
    wgu_t[..., :128] = wg.transpose(0, 3, 2, 1, 4)
    wgu_t[..., 128:] = wl.transpose(0, 3, 2, 1, 4)
    wdn = I["w_down"][0].reshape(NE, 8, 128, 8, 128)
    wdn_t = np.ascontiguousarray(wdn.transpose(0, 3, 2, 1, 4))
    bgu = I["b_gate_up"][0].reshape(NE, 16, 128).transpose(2, 0, 1).reshape(128, NE * 16)
    wr = I["w_router"][0]
    if wr.shape[1] < 32:
        wr = np.concatenate([wr, np.zeros((D, 32 - wr.shape[1]), np.float32)], axis=1)
    return dict(
        cache_k=f(I["cache_k"][0].reshape(-1, 512)), cache_v=f(I["cache_v"][0].reshape(-1, 512)),
        w_in=f(I["w_in"][0]), w_out=f(I["w_out"][0]), aP=f(aP), bP=f(bPc), cT=f(cTc), dP=f(dP),
        w_glu=f(I["w_glu"][0]), bglu=f(bglu), lam4=f(lam4), gsub=f(I["subln_g"][0][None, :]), gcol=f(I["subln_g"][0][:, None]),
        ln=f(ln), wr=f(wr), br=f(I["b_router"][0][None, :]), wgu=f(wgu_t.reshape(NE * 8 * 128, 2048)), bgu=f(bgu),
        wdn=f(wdn_t.reshape(NE * 8 * 128, 1024)), bdn=f(I["b_down"][0]))


def run(cfg, I, trace=False, stop_after=None, max_ops=None):
    T, NS, NPG = cfg.T, cfg.NS, cfg.NPG
    nc = build(cfg, stop_after, max_ops)
    shared = _shared(cfg, I)
    shared.update(_consts(cfg, NPG * 128))
    in_maps = []
    for c in range(NCORES):
        x = np.concatenate([I["x_prompt"][c], I["x_sample"][c * NS:(c + 1) * NS, 0]], axis=0).astype(np.float32)
        m = dict(shared)
        m["x"] = np.ascontiguousarray(x)
        m["xT"] = np.ascontiguousarray(x.T)
        m["pt"] = np.ascontiguousarray(I["page_table"][c * NS:(c + 1) * NS].reshape(1, NS * NPG).astype(np.int32))
        for k_, nm in (("state_ssm_re", "st_re"), ("state_ssm_im", "st_im")):
            s = I[k_][0, c * NS:(c + 1) * NS].reshape(NS, 16, 2, 64)
            m[nm] = np.ascontiguousarray(s.transpose(2, 3, 1, 0).reshape(128, 16 * NS).astype(np.float32))
        in_maps.append(m)
    res = run_bass_kernel_spmd(nc, in_maps, core_ids=list(range(NCORES)), trace=trace) if trace else \
        run_bass_kernel_spmd(nc, in_maps, core_ids=list(range(NCORES)))
    R = res.results
    B = NCORES
    y = np.stack([r["y"] for r in R])
    ko = np.stack([r["ko"] for r in R]); vo = np.stack([r["vo"] for r in R])
    sre = np.stack([r["sre"].reshape(2, 64, 16, 1 + NS) for r in R])
    sim = np.stack([r["sim"].reshape(2, 64, 16, 1 + NS) for r in R])

    def st_p(s):
        return np.ascontiguousarray(s[..., 0].transpose(0, 3, 1, 2).reshape(B, 32, 64))[None]

    def st_s(s):
        v = s[..., 1:].transpose(0, 4, 3, 1, 2)
        return np.ascontiguousarray(v.reshape(B * NS, 32, 64))[None]
    outs = (
        np.ascontiguousarray(y[:, :T]), np.ascontiguousarray(y[:, T:].reshape(B * NS, 1, D)),
        np.ascontiguousarray(ko[:, :T].reshape(B, T, 4, 128))[None], np.ascontiguousarray(vo[:, :T].reshape(B, T, 4, 128))[None],
        st_p(sre), st_p(sim),
        np.ascontiguousarray(ko[:, T:].reshape(B * NS, 1, 4, 128))[None], np.ascontiguousarray(vo[:, T:].reshape(B * NS, 1, 4, 128))[None],
        st_s(sre), st_s(sim))
    return tuple(o.astype(np.float32) for o in outs), res


def kernel(**inputs):
    I = {k: np.asarray(v) for k, v in inputs.items()}
    outs, _ = run(FULL, I)
    return outs
```

```python
import math
from contextlib import ExitStack

import numpy as np
import concourse.bass as bass
import concourse.mybir as mybir
from concourse.bass_utils import run_bass_kernel_spmd

F32 = mybir.dt.float32
BF16 = mybir.dt.bfloat16
I32 = mybir.dt.int32
AF = mybir.ActivationFunctionType
ALU = mybir.AluOpType
AX = mybir.AxisListType

D = 1024
NCORES = 8
LN_EPS = 1e-5
RMS_EPS = 1e-5
LAM_INIT = 0.8 - 0.6 * math.exp(-0.3 * 0)
ALPHA = (2 * 1) ** 0.25
SW_ALPHA = 1.702
SW_LIM = 7.0
ARENA_W = 52992


class Cfg:
    def __init__(self, T=2048, NS=16, NPG=16, NPHYS=2560, NE=32, TOPK=4):
        self.T, self.NS, self.NPG, self.NPHYS, self.NE, self.TOPK = T, NS, NPG, NPHYS, NE, TOPK
        self.NT = T + NS
        self.NTP = T // 128
        self.TB = [(i * 512, min(512, T - i * 512)) for i in range((T + 511) // 512)] + [(T, NS)]
        self.TL = [(i * 128, 128) for i in range(self.NTP)] + [(T, NS)]


FULL = Cfg()

ENGS = ("pe", "act", "dve", "pool", "sp")


class Ctx:
    def __init__(self, nc, stack):
        self.nc, self.stack = nc, stack
        self.esem = {e: stack.enter_context(nc.semaphore("es_" + e)) for e in ("pe", "act", "dve", "pool")}
        self.ecnt = {e: 0 for e in self.esem}
        self.dsem, self.dcnt = {}, {}
        self.known = {e: {} for e in ENGS}
        self.phase_no = 0
        self.stop_after = None
        self.max_ops = None

    def dma_slot(self, slot):
        if slot not in self.dsem:
            self.dsem[slot] = self.stack.enter_context(self.nc.semaphore("ds%d" % len(self.dsem)))
            self.dcnt[slot] = 0
            assert len(self.dsem) < 150, "too many dma semaphores"
        return self.dsem[slot]


class Phase:
    def __init__(self, ctx):
        self.ctx, self.ops = ctx, []

    def add(self, eng, fn, r=(), w=(), dma=None):
        self.ops.append(dict(eng=eng, fn=fn, r=tuple(r), w=tuple(w), dma=dma, dep=False))

    def mm(self, items, r, w):
        def fn(e, items=items):
            return [e.matmul(o, l, rh, start=s, stop=t) for (o, l, rh, s, t) in items]
        self.add("pe", fn, r, w)

    def tr(self, items, r, w):
        def fn(e, items=items):
            return [e.transpose(o, i, idn) for (o, i, idn) in items]
        self.add("pe", fn, r, w)

    def act(self, out, in_, func, r, w, bias=None, scale=None, accum=None):
        def fn(e):
            kw = {}
            if bias is not None:
                kw["bias"] = bias
            if scale is not None:
                kw["scale"] = scale
            if accum is not None:
                kw["accum_out"] = accum
            return e.activation(out, in_, func, **kw)
        self.add("act", fn, r, w)

    def ts(self, out, in0, s1, op0, r, w, s2=None, op1=None, eng="dve"):
        def fn(e):
            if op1 is None:
                return e.tensor_scalar(out, in0, s1, None, op0)
            return e.tensor_scalar(out, in0, s1, s2, op0, op1)
        self.add(eng, fn, r, w)

    def stt(self, out, in0, scalar, in1, op0, op1, r, w, accum=None):
        if accum is None:
            self.add("dve", lambda e: e.scalar_tensor_tensor(out, in0, scalar, in1, op0, op1), r, w)
        else:
            self.add("dve", lambda e: e.scalar_tensor_tensor(out, in0, scalar, in1, op0, op1, accum_out=accum), r, w)

    def tt(self, out, in0, in1, op, r, w, eng="dve"):
        self.add(eng, lambda e: e.tensor_tensor(out, in0, in1, op), r, w)

    def cp(self, out, in_, r, w, eng="dve"):
        if eng == "act":
            self.add("act", lambda e: e.copy(out, in_), r, w)
        else:
            self.add(eng, lambda e: e.tensor_copy(out, in_), r, w)

    def red(self, out, in_, op, r, w, axis=None):
        ax = AX.X if axis is None else axis
        self.add("dve", lambda e: e.tensor_reduce(out, in_, ax, op), r, w)

    def memset(self, out, val, w, eng="dve"):
        self.add(eng, lambda e: e.memset(out, val), (), w)

    def recip(self, out, in_, r, w):
        self.add("dve", lambda e: e.reciprocal(out, in_), r, w)

    def dma(self, q, out, in_, r, w, slot):
        self.add(q, lambda e: e.dma_start(out=out, in_=in_), r, w, dma=slot)

    def run(self):
        ops, ctx, nc = self.ops, self.ctx, self.ctx.nc
        ctx.phase_no += 1
        if ctx.stop_after is not None and ctx.phase_no > ctx.stop_after:
            return
        if ctx.stop_after is not None and ctx.phase_no == ctx.stop_after and ctx.max_ops is not None:
            print("[bisect] phase %d has %d ops, keeping %d; last kept: %s" % (
                ctx.phase_no, len(ops), ctx.max_ops, [(o["eng"], o["r"], o["w"], o["dma"]) for o in ops[max(0, ctx.max_ops - 2):ctx.max_ops]]))
            del ops[ctx.max_ops:]
        lastw, readers = {}, {}
        def _excl(b):
            return len(b) == 3 and b[:2] == "ps" and b[2].isdigit()
        for o in ops:
            xr = tuple(b for b in o["r"] if _excl(b))
            if xr:
                o["w"] = tuple(o["w"]) + xr
                o["r"] = tuple(b for b in o["r"] if not _excl(b))
        for i, o in enumerate(ops):
            deps = set()
            for b in o["r"]:
                if b in lastw:
                    deps.add(lastw[b])
            for b in o["w"]:
                if b in lastw:
                    deps.add(lastw[b])
                deps |= readers.get(b, set())
            deps.discard(i)
            o["deps"] = deps
            for d in deps:
                ops[d]["dep"] = True
            for b in o["r"]:
                readers.setdefault(b, set()).add(i)
            for b in o["w"]:
                lastw[b] = i
                readers[b] = set()
        touched = []
        for o in ops:
            if o["dma"] is not None:
                sem = ctx.dma_slot(o["dma"])
                ctx.dcnt[o["dma"]] += 16
                o["tok"] = ("d:" + o["dma"], sem, ctx.dcnt[o["dma"]])
                if o["dma"] not in touched:
                    touched.append(o["dma"])
            elif o["dep"]:
                e = o["eng"]
                ctx.ecnt[e] += 1
                o["tok"] = ("e:" + e, ctx.esem[e], ctx.ecnt[e])
            else:
                o["tok"] = None
        per = {e: [o for o in ops if o["eng"] == e] for e in ENGS}

        def mk(ename):
            def body(eng):
                kn = ctx.known[ename]
                for o in per[ename]:
                    for d in sorted(o["deps"]):
                        key, sem, val = ops[d]["tok"]
                        if kn.get(key, 0) < val:
                            eng.wait_ge(sem, val)
                            kn[key] = val
                    res = o["fn"](eng)
                    last = res[-1] if isinstance(res, (list, tuple)) else res
                    if o["tok"] is not None:
                        last.then_inc(o["tok"][1], 16 if o["dma"] is not None else 1)
                if ename == "sp":
                    for slot in touched:
                        key, val = "d:" + slot, ctx.dcnt[slot]
                        if kn.get(key, 0) < val:
                            eng.wait_ge(ctx.dsem[slot], val)
                            kn[key] = val
            return body

        with nc.Block() as blk:
            blk.tensor(mk("pe"))
            blk.scalar(mk("act"))
            blk.vector(mk("dve"))
            blk.gpsimd(mk("pool"))
            blk.sync(mk("sp"))


class Arena:
    def __init__(self, ap_all):
        self.a = ap_all

    def at(self, off_w, shape, dt=F32, parts=128):
        n = int(np.prod(shape[1:]))
        words = n if dt in (F32, I32) else (n + 1) // 2
        assert off_w + words <= ARENA_W, ("arena overflow", off_w, words)
        v = self.a[0:parts, off_w:off_w + words]
        if dt not in (F32,):
            v = v.bitcast(dt)
            if dt == BF16 and n % 2:
                v = v[:, 0:n]
        if len(shape) == 3:
            v = v.rearrange("p (a b) -> p a b", a=shape[1])
        elif len(shape) == 4:
            v = v.rearrange("p (a b c) -> p a b c", a=shape[1], b=shape[2])
        return v


class Bump:
    def __init__(self, arena, lo_kb, hi_kb):
        self.ar, self.p, self.hi = arena, int(lo_kb * 256), int(hi_kb * 256)

    def __call__(self, shape, dt=F32, parts=128):
        n = int(np.prod(shape[1:]))
        words = n if dt in (F32, I32) else (n + 1) // 2
        words = (words + 7) // 8 * 8
        v = self.ar.at(self.p, shape, dt, parts)
        self.p += words
        assert self.p <= self.hi, ("bump overflow", self.p, self.hi)
        return v


def build(cfg, stop_after=None, max_ops=None):
    T, NS, NT, NTP, NPG, NE = cfg.T, cfg.NS, cfg.NT, cfg.NTP, cfg.NPG, cfg.NE
    TB, TL = cfg.TB, cfg.TL
    TT = len(TL)
    NSL = NPG + 1
    nc = bass.Bass("TRN2", target_bir_lowering=False)

    def din(name, shape, dt=F32):
        return nc.dram_tensor(name, list(shape), dt, kind="ExternalInput").ap()

    def dout(name, shape, dt=F32):
        return nc.dram_tensor(name, list(shape), dt, kind="ExternalOutput").ap()

    xT_d = din("xT", [D, NT]); x_d = din("x", [NT, D])
    ck_d = din("cache_k", [cfg.NPHYS * 128, 512]); cv_d = din("cache_v", [cfg.NPHYS * 128, 512])
    pt_d = din("pt", [1, NS * NPG], I32)
    st_re_d = din("st_re", [128, 16 * NS]); st_im_d = din("st_im", [128, 16 * NS])
    w_in_d = din("w_in", [D, 2048]); w_out_d = din("w_out", [D, D])
    aP_d = din("aP", [128, 48])
    bP_d = din("bP", [128, 2 * 2048])
    cT_d = din("cT", [128, 2 * 2048])
    dP_d = din("dP", [128, 4])
    w_glu_d = din("w_glu", [512, 1024]); bglu_d = din("bglu", [128, 8])
    lam4_d = din("lam4", [1, 256])
    gsub_d = din("gsub", [1, 128]); gcol_d = din("gcol", [128, 1])
    ln_d = din("ln", [1, 4 * D])
    wr_d = din("wr", [D, 32 if NE <= 32 else NE]); br_d = din("br", [1, NE])
    wgu_d = din("wgu", [NE * 8 * 128, 2048]); bgu_d = din("bgu", [128, NE * 16])
    wdn_d = din("wdn", [NE * 8 * 128, 1024]); bdn_d = din("bdn", [NE, D])
    ropeC_d = din("ropeC", [NT, 32]); ropeS_d = din("ropeS", [NT, 32])
    ident_d = din("ident", [128, 128]); tri_d = din("tri", [128, 128])
    selB_d = din("selB", [NS, NS * 128])
    pidx_d = din("pidx", [128, 1]); negm_d = din("negm", [128, 1])

    y_d = dout("y", [NT, D]); ko_d = dout("ko", [NT, 512]); vo_d = dout("vo", [NT, 512])
    sre_d = dout("sre", [128, 16 * (1 + NS)]); sim_d = dout("sim", [128, 16 * (1 + NS)])

    stack = ExitStack()
    with stack:
        arena_t = stack.enter_context(nc.sbuf_tensor("arena", [128, ARENA_W], F32))
        ps_t = stack.enter_context(nc.psum_tensor("ps", [128, 4096], F32))
        ar = Arena(arena_t)
        ctx = Ctx(nc, stack)
        ctx.stop_after = stop_after
        ctx.max_ops = max_ops

        def bank(b, n=512):
            return ps_t[:, b * 512:b * 512 + n]

        def bank_bf(b):
            return ps_t[:, b * 512:(b + 1) * 512].bitcast(BF16)

        KB = 256
        cb = Bump(ar, 0, 10)
        ident = cb([128, 128]); identb = cb([128, 128], BF16); tri = cb([128, 128]); onesf = cb([128, 128])
        cosT = cb([128, TT, 32]); sinT = cb([128, TT, 32])
        gsub = cb([128, 128]); gcol = cb([128, 1]); lam_t = cb([128, 1]); nlam_t = cb([128, 1])
        pidx = cb([128, 1]); negm = cb([128, 1]); dP = cb([128, 4]); bglu = cb([128, 8])
        aoT = ar.at(10 * KB, [128, 4, NT], BF16)
        soT = ar.at(int(26.5 * KB), [128, 4, NT], BF16)
        actT = ar.at(10 * KB, [128, 8, NT], BF16)
        uT = ar.at(43 * KB, [128, 4, NT])
        gss = ar.at(76 * KB, [128, 4, NT], BF16)
        qs = ar.at(76 * KB, [128, 512])
        qT = ar.at(int(94.5 * KB), [128, 4, NT], BF16)
        kT = ar.at(int(94.5 * KB) + 2 * NT, [128, 4, NT], BF16)
        vbf = ar.at(int(127.5 * KB), [128, max(NTP, 1), 512], BF16)
        fT = ar.at(43 * KB, [128, 8, NT])
        hTb = ar.at(109 * KB, [128, 8, NT], BF16)
        gatesT = ar.at(142 * KB, [128, NT])

        ph = Phase(ctx)
        tb_ = Bump(ar, 150, 207)
        l4 = tb_([128, 256]); pr = tb_([128, 128]); sm = tb_([128, 2]); ee = tb_([128, 2])
        ph.dma("sp", ident, ident_d, (), ("ident",), "c0")
        ph.dma("sp", tri, tri_d, (), ("tri",), "c1")
        ph.dma("sp", gsub, gsub_d.broadcast_to([128, 128]), (), ("gsub",), "c2")
        ph.dma("sp", gcol, gcol_d, (), ("gcol",), "c3")
        ph.dma("sp", pidx, pidx_d, (), ("pidx",), "c4")
        ph.dma("sp", negm, negm_d, (), ("negm",), "c5")
        ph.dma("sp", dP, dP_d, (), ("dP",), "c6")
        ph.dma("sp", bglu, bglu_d, (), ("bglu",), "c7")
        ph.dma("sp", l4, lam4_d.broadcast_to([128, 256]), (), ("l4",), "c8")
        if NTP:
            ph.dma("sp", cosT[:, 0:NTP, :], ropeC_d[0:T, :].rearrange("(i p) f -> p i f", p=128), (), ("cosT",), "c9")
            ph.dma("sp", sinT[:, 0:NTP, :], ropeS_d[0:T, :].rearrange("(i p) f -> p i f", p=128), (), ("sinT",), "c10")
        ph.dma("sp", cosT[0:NS, NTP, :], ropeC_d[T:NT, :], (), ("cosTs",), "c11")
        ph.dma("sp", sinT[0:NS, NTP, :], ropeS_d[T:NT, :], (), ("sinTs",), "c12")
        ph.cp(identb, ident, ("ident",), ("identb",))
        ph.memset(onesf, 1.0, ("onesf",))
        ph.tt(pr.rearrange("p (a b) -> p a b", a=2), l4.rearrange("p (a two b) -> p a two b", a=2, two=2)[:, :, 0, :],
              l4.rearrange("p (a two b) -> p a two b", a=2, two=2)[:, :, 1, :], ALU.mult, ("l4",), ("pr",))
        ph.red(sm, pr.rearrange("p (a b) -> p a b", a=2), ALU.add, ("pr",), ("sm",))
        ph.act(ee, sm, AF.Exp, ("sm",), ("ee",))
        ph.tt(lam_t, ee[:, 0:1], ee[:, 1:2], ALU.subtract, ("ee",), ("lam0",))
        ph.ts(lam_t, lam_t, LAM_INIT, ALU.add, ("lam0",), ("lam",))
        ph.ts(nlam_t, lam_t, -1.0, ALU.mult, ("lam",), ("nlam",))
        ph.run()

        ph = Phase(ctx)
        xTb = ar.at(int(143.5 * KB), [128, 8, NT], BF16)
        sb = Bump(ar, 78, 94.5)
        xst = [sb([128, NT]), sb([128, NT])]
        wb_ = Bump(ar, 26.5, 43)
        wpc = [wb_([128, 8, 512], BF16), wb_([128, 8, 512], BF16)]
        tb_ = Bump(ar, 176.5, 207)
        wst = [tb_([128, 512]), tb_([128, 512])]
        ev = [tb_([128, 512]), tb_([128, 512])]
        ta = tb_([128, 256]); tbb = tb_([128, 256])
        for kc in range(8):
            s = kc % 2
            ph.dma("sp", xst[s], xT_d[kc * 128:(kc + 1) * 128, :], (), ("xst%d" % s,), "xst%d" % s)
            ph.cp(xTb[:, kc, :], xst[s], ("xst%d" % s,), ("xTb%d" % kc,), eng="act" if kc % 2 else "dve")
        xall = tuple("xTb%d" % k for k in range(8))
        nev = 0
        for grp in range(4):
            pw = wpc[grp % 2]
            pwn = "wpc%d" % (grp % 2)
            for kc in range(8):
                s = kc % 2
                ph.dma("act" if kc % 2 else "sp", wst[s], w_in_d[kc * 128:(kc + 1) * 128, grp * 512:(grp + 1) * 512],
                       (), ("wst%d" % s,), "wst%d" % s)
                ph.cp(pw[:, kc, :], wst[s], ("wst%d" % s,), (pwn,), eng="pool")
            if grp == 0:
                for c in range(4):
                    for bi, (t0, n) in enumerate(TB):
                        b = (c * len(TB) + bi) % 2
                        ph.mm([(bank(b, n), pw[:, kc, c * 128:(c + 1) * 128], xTb[:, kc, t0:t0 + n], kc == 0, kc == 7)
                               for kc in range(8)], xall + (pwn,), ("ps%d" % b,))
                        ph.cp(uT[:, c, t0:t0 + n], bank(b, n), ("ps%d" % b,), ("uT%d_%d" % (c, bi),), eng="act")
                continue
            def emit_mm(i, pw=pw, pwn=pwn):
                t0, n = TL[i]
                b = 2 + (i % 2)
                ph.mm([(ps_t[0:n, b * 512:(b + 1) * 512], xTb[:, kc, t0:t0 + n], pw[:, kc, :], kc == 0, kc == 7) for kc in range(8)],
                      xall + (pwn,), ("ps%d" % b,))
            emit_mm(0)
            for i, (t0, n) in enumerate(TL):
                b = 2 + (i % 2)
                pb = ps_t[0:n, b * 512:(b + 1) * 512]
                if i + 1 < len(TL):
                    emit_mm(i + 1)
                e_ = ev[nev % 2]; en = "ev%d" % (nev % 2); nev += 1
                eo = e_[0:n, :]
                if grp == 3:
                    ph.cp(eo, pb, ("ps%d" % b,), (en,), eng="act")
                    ph.dma("pool", vo_d[t0:t0 + n, :], eo, (en,), ("vo%d" % i,), "st_" + en)
                    if i < NTP:
                        ph.cp(vbf[:, i, :], pb, ("ps%d" % b,), ("vbf%d" % i,))
                    continue
                pv = pb.rearrange("p (g two f) -> p g two f", g=8, two=2)
                ov = eo.rearrange("p (g two f) -> p g two f", g=8, two=2)
                cs = cosT[0:n, i:i + 1, :].broadcast_to([n, 8, 32]); sn = sinT[0:n, i:i + 1, :].broadcast_to([n, 8, 32])
                t1 = ta[0:n, :].rearrange("p (g f) -> p g f", g=8); t2 = tbb[0:n, :].rearrange("p (g f) -> p g f", g=8)
                rp = ("ps%d" % b, "cosT", "sinT", "cosTs", "sinTs")
                ph.tt(t1, pv[:, :, 0, :], cs, ALU.mult, rp, ("ta",))
                ph.tt(t2, pv[:, :, 1, :], sn, ALU.mult, rp, ("tb",))
                ph.tt(ov[:, :, 0, :], t1, t2, ALU.subtract, ("ta", "tb"), (en,))
                ph.tt(t1, pv[:, :, 1, :], cs, ALU.mult, rp, ("ta",))
                ph.tt(t2, pv[:, :, 0, :], sn, ALU.mult, rp, ("tb",))
                ph.tt(ov[:, :, 1, :], t1, t2, ALU.add, ("ta", "tb"), (en + "b",))
                if grp == 2:
                    ph.dma("pool", ko_d[t0:t0 + n, :], eo, (en, en + "b"), ("ko%d" % i,), "st_" + en)
                if i >= NTP:
                    if grp == 1:
                        ph.cp(qs[0:n, :], eo, (en, en + "b"), ("qs",), eng="act")
                    continue
                tb2 = i % 2
                ph.tr([(bank(tb2)[:, h * 128:(h + 1) * 128], eo[:, h * 128:(h + 1) * 128], ident) for h in range(4)],
                      (en, en + "b", "ident"), ("ps%d" % tb2,))
                dst = qT if grp == 1 else kT
                ph.cp(dst[:, :, t0:t0 + 128], bank(tb2).rearrange("p (h t) -> p h t", h=4), ("ps%d" % tb2,),
                      ("%sT%d" % ("q" if grp == 1 else "k", i),), eng="act" if i % 2 else "dve")
        ph.run()

        if NTP:
            ph = Phase(ctx)
            tb_ = Bump(ar, 143.5, 207)
            Psb = [[tb_([128, T], BF16), tb_([128, T], BF16)], [tb_([128, T], BF16), tb_([128, T], BF16)]]
            PTs = [tb_([128, T], BF16), tb_([128, T], BF16)]
            mx = tb_([128, 2]); nb = tb_([128, 2]); lsum = tb_([128, 2]); rl = tb_([128, 2])
            tS = tb_([128, 128]); att = tb_([128, 128]); junk = tb_([128, 128]); ss = tb_([128, 1]); rs = tb_([128, 1])
            lsum2 = [lsum, tb_([128, 2])]

            def geom(qt, m):
                nk = qt + 1
                small = nk <= 8
                sb0 = 2 * m if small else 0
                SBK = tuple("ps%d" % (sb0 + k) for k in range(2 if small else 4))
                Sv = ps_t[:, sb0 * 512:sb0 * 512 + (1024 if small else 2048)]
                if small:
                    PTv = ps_t[:, (4 + m) * 512:(5 + m) * 512].bitcast(BF16); PTK = ("ps%d" % (4 + m),)
                else:
                    PTv = ps_t[:, 2048:3072].bitcast(BF16); PTK = ("ps4", "ps5")
                return nk, SBK, Sv, PTv, PTK

            def stageA(idx, h, qt, m):
                nk, SBK, Sv, PTv, PTK = geom(qt, m)
                ls = lsum2[idx % 2]; lname = "l%d_%d" % (idx % 2, m)
                items = []
                for j in range(0, nk, 4):
                    cols = min(4, nk - j) * 128
                    items.append((Sv[:, j * 128:j * 128 + cols], qT[m * 64:(m + 1) * 64, h, qt * 128:(qt + 1) * 128],
                                  kT[m * 64:(m + 1) * 64, h, j * 128:j * 128 + cols], True, True))
                ph.mm(items, (), SBK)
                ph.tt(Sv[:, qt * 128:(qt + 1) * 128], Sv[:, qt * 128:(qt + 1) * 128], tri, ALU.add, SBK + ("tri",), SBK)
                ph.red(mx[:, m:m + 1], Sv[:, 0:nk * 128], ALU.max, SBK, ("mx%d" % m,))
                ph.ts(nb[:, m:m + 1], mx[:, m:m + 1], -0.125, ALU.mult, ("mx%d" % m,), ("nb%d" % m,))
                ph.act(Psb[idx % 2][m][:, 0:nk * 128], Sv[:, 0:nk * 128], AF.Exp, SBK + ("nb%d" % m,), ("P%d_%d" % (idx % 2, m), lname),
                       bias=nb[:, m:m + 1], scale=0.125, accum=ls[:, m:m + 1])

            def stageB(idx, h, qt, m):
                nk, SBK, Sv, PTv, PTK = geom(qt, m)
                pn, tn = "P%d_%d" % (idx % 2, m), "PT%d" % m
                ph.tr([(PTv[:, k * 128:(k + 1) * 128], Psb[idx % 2][m][:, k * 128:(k + 1) * 128], identb) for k in range(nk)], (pn, "identb"), PTK)
                ph.cp(PTs[m][:, 0:nk * 128], PTv[:, 0:nk * 128], PTK, (tn,), eng="act" if m else "dve")
                ph.mm([(bank(6 + m)[:, 0:128], PTs[m][:, k * 128:(k + 1) * 128], vbf[:, k, h * 128:(h + 1) * 128],
                        k == 0, k == nk - 1) for k in range(nk)], (tn,), ("ps%d" % (6 + m),))

            def stageC(idx, h, qt):
                ls = lsum2[idx % 2]
                ph.recip(rl, ls, ("l%d_0" % (idx % 2), "l%d_1" % (idx % 2)), ("rl",))
                ph.tt(rl[:, 1:2], rl[:, 1:2], lam_t, ALU.mult, ("rl", "lam"), ("rl",))
                ph.ts(tS, bank(7)[:, 0:128], rl[:, 1:2], ALU.mult, ("ps7", "rl"), ("tS",))
                ph.stt(att, bank(6)[:, 0:128], rl[:, 0:1], tS, ALU.mult, ALU.subtract, ("ps6", "rl", "tS"), ("att",))
                ph.stt(junk, att, 1.0, att, ALU.mult, ALU.mult, ("att",), ("junk", "ss"), accum=ss)
                ph.ts(ss, ss, 1.0 / 128, ALU.mult, ("ss",), ("ss",), s2=RMS_EPS, op1=ALU.add)
                ph.act(ss, ss, AF.Ln, ("ss",), ("ss",))
                ph.act(rs, ss, AF.Exp, ("ss",), ("rs",), scale=-0.5)
                ph.ts(att, att, rs, ALU.mult, ("att", "rs"), ("att",), s2=1.0 - LAM_INIT, op1=ALU.mult)
                ph.tt(att, att, gsub, ALU.mult, ("att", "gsub"), ("att",))
                ph.tr([(bank(6)[:, 256:384], att, ident)], ("att", "ident"), ("ps6",))
                ph.cp(aoT[:, h, qt * 128:(qt + 1) * 128], bank(6)[:, 256:384], ("ps6",), ("aoT%d_%d" % (h, qt),), eng="act")

            iters = [(h, qt) for h in range(4) for qt in range(NTP)]
            stageA(0, iters[0][0], iters[0][1], 0); stageA(0, iters[0][0], iters[0][1], 1)
            for idx, (h, qt) in enumerate(iters):
                nxt = iters[idx + 1] if idx + 1 < len(iters) else None
                if nxt:
                    stageA(idx + 1, nxt[0], nxt[1], 0)
                stageB(idx, h, qt, 0); stageB(idx, h, qt, 1)
                if nxt:
                    stageA(idx + 1, nxt[0], nxt[1], 1)
                stageC(idx, h, qt)
            ph.run()

        ph = Phase(ctx)
        tb_ = Bump(ar, 94.5, 207)
        selB = tb_([128, NS * 128])
        pti = tb_([128, NS * NPG], I32); ptf = tb_([128, NS * NPG]); idx = tb_([128, NS * NPG], I32)
        NR = 3
        Kp = [tb_([128, 4, 512]) for _ in range(NR)]
        Vp = [tb_([128, 4, 512]) for _ in range(NR)]
        Ksf = [tb_([128, 512]), tb_([128, 512])]; Vsf = [tb_([128, 512]), tb_([128, 512])]
        prod = [tb_([128, 512]), tb_([128, 512])]
        Ss = tb_([128, NS, NSL, 8])
        mp = tb_([128, NS * 8]); gmx = tb_([128, 1]); dg = tb_([128, NS * 8]); rls = tb_([128, NS * 8])
        t1s = tb_([128, NS, 2]); atts = tb_([128, 4, NS]); sqs = tb_([128, 4 * NS]); rss = tb_([128, 4 * NS])
        H8 = NS * 8
        OTB = ("ps3", "ps4", "ps5", "ps6", "ps7")
        ph.dma("sp", selB[0:NS, :], selB_d, (), ("selB",), "d0")
        ph.dma("sp", pti, pt_d.broadcast_to([128, NS * NPG]), (), ("pti",), "d1")
        ph.cp(ptf, pti, ("pti",), ("ptf",))
        ph.ts(ptf, ptf, 128.0, ALU.mult, ("ptf", "pidx"), ("ptf2",), s2=pidx, op1=ALU.add)
        ph.cp(idx, ptf, ("ptf2",), ("idx",))
        for s in range(2):
            ph.memset(Ksf[s], 0.0, ("Ksf%d" % s,)); ph.memset(Vsf[s], 0.0, ("Vsf%d" % s,))
        ngr = (NPG + 3) // 4
        gi = 0
        for b in range(NS):
            qb = b % 2
            ph.mm([(bank(qb), selB[0:NS, b * 128:(b + 1) * 128], qs[0:NS, :], True, True)], ("selB", "qs"), ("ps%d" % qb,))
            for g in range(ngr):
                r = gi % NR; gi += 1
                for jj in range(min(4, NPG - g * 4)):
                    j = g * 4 + jj
                    col = b * NPG + j
                    bn = "Kp%d_%d" % (r, jj)
                    ph.add("pool", (lambda e, o=Kp[r][:, jj, :], ia=idx[:, col:col + 1]: e.indirect_dma_start(
                        out=o, out_offset=None, in_=ck_d, in_offset=bass.IndirectOffsetOnAxis(ap=ia, axis=0))),
                        ("idx",), (bn,), dma=bn)
                    p_ = prod[j % 2]; pn = "prod%d" % (j % 2)
                    ph.tt(p_, Kp[r][:, jj, :], bank(qb), ALU.mult, (bn, "ps%d" % qb), (pn,))
                    ph.red(Ss[:, b, j, :], p_.rearrange("p (g f) -> p g f", g=8), ALU.add, (pn,), ("Ss%d" % b,))
            s = b % 2
            ph.dma("sp", Ksf[s][0:1, :], ko_d[T + b:T + b + 1, :], ("ko%d" % NTP,), ("Ksf%d" % s,), "ksf%d" % s)
            ph.tt(prod[0], Ksf[s], bank(qb), ALU.mult, ("Ksf%d" % s, "ps%d" % qb), ("prod0",))
            ph.red(Ss[:, b, NPG, :], prod[0].rearrange("p (g f) -> p g f", g=8), ALU.add, ("prod0",), ("Ss%d" % b,))
        allS = tuple("Ss%d" % b for b in range(NS))
        ph.ts(Ss[:, :, NPG, :], Ss[:, :, NPG, :], negm, ALU.add, allS + ("negm",), ("SsA",))
        ph.red(mp.rearrange("p (b h) -> p b h", b=NS), Ss.rearrange("p b s h -> p b h s"), ALU.max, ("SsA",), ("mp",))
        ph.tr([(bank(2)[0:H8, 0:128], mp, ident)], ("mp", "ident"), ("ps2",))
        ph.red(gmx[0:H8, :], bank(2)[0:H8, 0:128], ALU.max, ("ps2",), ("gmx",))
        ph.ts(dg[0:H8, :], ident[0:H8, 0:H8], gmx[0:H8, :], ALU.mult, ("gmx", "ident"), ("dg",))
        ph.mm([(bank(2)[:, 0:H8], onesf[0:H8, :], dg[0:H8, :], True, True)], ("dg", "onesf"), ("ps2",))
        ph.tt(Ss, Ss, bank(2)[:, 0:H8].rearrange("p (b o h) -> p b o h", b=NS, o=1).broadcast_to([128, NS, NSL, 8]),
              ALU.subtract, ("SsA", "ps2"), ("SsB",))
        ph.act(Ss, Ss, AF.Exp, ("SsB",), ("P",), scale=0.125)
        gi = 0
        for b in range(NS):
            for g in range(ngr):
                r = gi % NR; gi += 1
                for jj in range(min(4, NPG - g * 4)):
                    j = g * 4 + jj
                    col = b * NPG + j
                    bn = "Vp%d_%d" % (r, jj)
                    ph.add("pool", (lambda e, o=Vp[r][:, jj, :], ia=idx[:, col:col + 1]: e.indirect_dma_start(
                        out=o, out_offset=None, in_=cv_d, in_offset=bass.IndirectOffsetOnAxis(ap=ia, axis=0))),
                        ("idx",), (bn,), dma=bn)
                    items = [(bank(3 + h)[:, b * 2:b * 2 + 2], Vp[r][:, jj, h * 128:(h + 1) * 128], Ss[:, b, j, 2 * h:2 * h + 2],
                              j == 0, False) for h in range(4)]
                    items.append((bank(7)[:, b * 8:b * 8 + 8], onesf, Ss[:, b, j, :], j == 0, False))
                    ph.mm(items, (bn, "P", "onesf"), OTB)
            s = b % 2
            ph.dma("sp", Vsf[s][0:1, :], vo_d[T + b:T + b + 1, :], ("vo%d" % NTP,), ("Vsf%d" % s,), "vsf%d" % s)
            items = [(bank(3 + h)[:, b * 2:b * 2 + 2], Vsf[s][:, h * 128:(h + 1) * 128], Ss[:, b, NPG, 2 * h:2 * h + 2],
                      False, True) for h in range(4)]
            items.append((bank(7)[:, b * 8:b * 8 + 8], onesf, Ss[:, b, NPG, :], False, True))
            ph.mm(items, ("Vsf%d" % s, "P", "onesf"), OTB)
        ph.recip(rls, bank(7)[:, 0:H8], ("ps7",), ("rls",))
        rl4 = rls.rearrange("p (b h m) -> p b h m", b=NS, h=4)
        for h in range(4):
            ph.tt(t1s, bank(3 + h)[:, 0:2 * NS].rearrange("p (b m) -> p b m", b=NS), rl4[:, :, h, :], ALU.mult,
                  ("ps%d" % (3 + h), "rls"), ("t1s",))
            ph.stt(atts[:, h, :], t1s[:, :, 1], nlam_t, t1s[:, :, 0], ALU.mult, ALU.add, ("t1s", "nlam"), ("atts%d" % h,))
        alla = tuple("atts%d" % h for h in range(4))
        af = atts.rearrange("p h b -> p (h b)")
        ph.tt(sqs, af, af, ALU.mult, alla, ("sqs",))
        ph.mm([(bank(2)[:, 0:4 * NS], onesf, sqs, True, True)], ("sqs", "onesf"), ("ps2",))
        ph.ts(rss, bank(2)[:, 0:4 * NS], 1.0 / 128, ALU.mult, ("ps2",), ("rss0",), s2=RMS_EPS, op1=ALU.add)
        ph.act(rss, rss, AF.Sqrt, ("rss0",), ("rss1",))
        ph.recip(rss, rss, ("rss1",), ("rss2",))
        ph.tt(af, af, rss, ALU.mult, alla + ("rss2",), ("attn",))
        ph.ts(aoT[:, :, T:NT], atts, gcol, ALU.mult, ("attn", "gcol"), ("aoTs",), s2=1.0 - LAM_INIT, op1=ALU.mult)
        ph.run()

        E_LO = 92.5
        pb_ = Bump(ar, E_LO, 207)
        WB = pb_([128, 2, 16, 128])
        CTr = pb_([128, 16, 128], BF16); CTi = pb_([128, 16, 128], BF16)
        magP = pb_([128, 16]); ec1 = pb_([128, 16]); es1 = pb_([128, 16]); lbrP = pb_([128, 16, 1]); lbiP = pb_([128, 16, 1])
        sout = [pb_([128, 16, 1 + NS]), pb_([128, 16, 1 + NS])]
        e_mark = pb_.p
        ph = Phase(ctx)
        tb_ = Bump(ar, e_mark / 256.0, 207)
        aPt = tb_([128, 48]); bPt = tb_([128, 2, 16, 128]); cst = tb_([128, 2048])
        BB = tb_([128, 2, 16, 128]); w2 = tb_([128, 16, 128]); w3 = tb_([128, 16, 128])
        ph.dma("sp", aPt, aP_d, (), ("Pin",), "e0")
        ph.dma("sp", bPt[:, 0], bP_d[:, 0:2048].rearrange("p (a b) -> p a b", a=16), (), ("bPt0",), "e1")
        ph.dma("act", bPt[:, 1], bP_d[:, 2048:4096].rearrange("p (a b) -> p a b", a=16), (), ("bPt1",), "e2")
        ph.dma("sp", cst, cT_d[:, 0:2048], (), ("cst",), "e3")
        ph.cp(CTr.rearrange("p a b -> p (a b)"), cst, ("cst",), ("CTr",))
        ph.dma("sp", cst, cT_d[:, 2048:4096], ("CTr",), ("cst2",), "e3")
        ph.act(CTi.rearrange("p a b -> p (a b)"), cst, AF.Copy, ("cst2",), ("CTi",), scale=-1.0)

        def disc(tag, a_re, a_im, ldt, W, want_f):
            keys = ("ar", "dt", "xr", "th", "acc", "tmp", "s", "a", "x2", "u", "s2", "a2")
            t = {k: tb_([128, W]) for k in keys}
            N = lambda k: tag + k
            IN = tag + "in"

            def TS(o, i, s1, op0, s2=None, op1=None, extra=()):
                ph.ts(t[o], t[i], s1, op0, (N(i),) + extra, (N(o),), s2=s2, op1=op1)

            def TT(o, i0, i1, op):
                ph.tt(t[o], t[i0], t[i1], op, (N(i0), N(i1)), (N(o),))

            ph.ts(t["ar"], a_re, -1e-4, ALU.min, (IN,), (N("ar"),))
            ph.act(t["dt"], ldt, AF.Exp, (IN,), (N("dt"),))
            TT("xr", "ar", "dt", ALU.mult)
            ph.tt(t["th"], a_im, t["dt"], ALU.mult, (IN, N("dt")), (N("th"),))
            TS("acc", "xr", 0.1, ALU.mult, s2=1.0, op1=ALU.add)
            for k in range(9, 0, -1):
                TT("tmp", "xr", "acc", ALU.mult)
                if k > 1:
                    TS("acc", "tmp", 1.0 / k, ALU.mult, s2=1.0, op1=ALU.add)
            TS("acc", "tmp", 1.0, ALU.add)
            TS("u", "th", 1.0 / 1024, ALU.mult)
            TT("x2", "u", "u", ALU.mult)
            TS("s", "x2", -1.0 / 20, ALU.mult, s2=1.0, op1=ALU.add)
            TT("s", "s", "x2", ALU.mult)
            TS("s", "s", -1.0 / 6, ALU.mult, s2=1.0, op1=ALU.add)
            TT("s", "s", "u", ALU.mult)
            TS("a", "x2", -1.0 / 30, ALU.mult, s2=1.0, op1=ALU.add)
            TT("a", "a", "x2", ALU.mult)
            TS("a", "a", -1.0 / 12, ALU.mult, s2=1.0, op1=ALU.add)
            TT("a", "a", "x2", ALU.mult)
            TS("a", "a", 0.5, ALU.mult)
            s_, a_, s2_, a2_ = "s", "a", "s2", "a2"
            for _ in range(10):
                TS("x2", a_, -1.0, ALU.mult, s2=1.0, op1=ALU.add)
                ph.stt(t[a2_], t[s_], 2.0, t[s_], ALU.mult, ALU.mult, (N(s_),), (N(a2_),))
                ph.stt(t[s2_], t[s_], 2.0, t["x2"], ALU.mult, ALU.mult, (N(s_), N("x2")), (N(s2_),))
                s_, s2_, a_, a2_ = s2_, s_, a2_, a_
            res = dict(mag=t["acc"], magn=N("acc"), s=t[s_], sn=N(s_), a=t[a_], an=N(a_))
            if want_f:
                TT("x2", "acc", a_, ALU.mult)
                TT("x2", "tmp", "x2", ALU.subtract)
                TT("th", "acc", s_, ALU.mult)
                TT("dt", "ar", "ar", ALU.mult)
                ph.tt(t["xr"], a_im, a_im, ALU.mult, (IN,), (N("xr"),))
                TT("dt", "dt", "xr", ALU.add)
                ph.recip(t["dt"], t["dt"], (N("dt"),), (N("dt"),))
                TT("xr", "x2", "ar", ALU.mult)
                ph.tt(t["u"], t["th"], a_im, ALU.mult, (N("th"), IN), (N("u"),))
                TT("xr", "xr", "u", ALU.add)
                TT("xr", "xr", "dt", ALU.mult)
                TT(s2_, "th", "ar", ALU.mult)
                ph.tt(t["u"], t["x2"], a_im, ALU.mult, (N("x2"), IN), (N("u"),))
                TT(s2_, s2_, "u", ALU.subtract)
                TT(s2_, s2_, "dt", ALU.mult)
                res.update(fre=t["xr"], fren=N("xr"), fim=t[s2_], fimn=N(s2_))
            return res

        rP = disc("P", aPt[:, 0:16], aPt[:, 16:32], aPt[:, 32:48], 16, True)
        fre = rP["fre"].unsqueeze(2).broadcast_to([128, 16, 128]); fim = rP["fim"].unsqueeze(2).broadcast_to([128, 16, 128])
        ph.tt(w2, bPt[:, 0], fre, ALU.mult, (rP["fren"], "bPt0"), ("w2",))
        ph.tt(w3, bPt[:, 1], fim, ALU.mult, (rP["fimn"], "bPt1"), ("w3",))
        ph.tt(BB[:, 0], w2, w3, ALU.subtract, ("w2", "w3"), ("BB0",))
        ph.tt(w2, bPt[:, 1], fre, ALU.mult, (rP["fren"], "bPt1"), ("w2",))
        ph.tt(w3, bPt[:, 0], fim, ALU.mult, (rP["fimn"], "bPt0"), ("w3",))
        ph.tt(BB[:, 1], w2, w3, ALU.add, ("w2", "w3"), ("BB1",))
        for ri_ in range(2):
            for g4 in range(4):
                bk = (ri_ * 4 + g4) % 4
                ph.tr([(bank(bk)[:, q * 128:(q + 1) * 128], BB[:, ri_, g4 * 4 + q, :], ident) for q in range(4)],
                      ("BB%d" % ri_, "ident"), ("ps%d" % bk,))
                ph.cp(WB[:, ri_, g4 * 4:g4 * 4 + 4, :], bank(bk).rearrange("p (q f) -> p q f", q=4), ("ps%d" % bk,), ("WB",),
                      eng="act" if g4 % 2 else "dve")
        cP = tb_([128, 16]); nP = tb_([128, 16]); n2 = tb_([128, 16])
        ph.ts(cP, rP["a"], -1.0, ALU.mult, (rP["an"],), ("cP",), s2=1.0, op1=ALU.add)
        ph.tt(nP, cP, cP, ALU.mult, ("cP",), ("nP",))
        ph.tt(n2, rP["s"], rP["s"], ALU.mult, (rP["sn"],), ("n2",))
        ph.tt(nP, nP, n2, ALU.add, ("nP", "n2"), ("nP",))
        ph.ts(nP, nP, -0.5, ALU.mult, ("nP",), ("nP",), s2=1.5, op1=ALU.add)
        ph.tt(ec1, cP, nP, ALU.mult, ("cP", "nP"), ("ec1",))
        ph.tt(es1, rP["s"], nP, ALU.mult, (rP["sn"], "nP"), ("es1",))
        ph.cp(magP, rP["mag"], (rP["magn"],), ("magP",))
        ph.tt(lbrP.rearrange("p a b -> p (a b)"), rP["mag"], ec1, ALU.mult, (rP["magn"], "ec1"), ("lbrP",))
        ph.tt(lbiP.rearrange("p a b -> p (a b)"), rP["mag"], es1, ALU.mult, (rP["magn"], "es1"), ("lbiP",))
        ph.run()

        if NTP:
            ph = Phase(ctx)
            tb_ = Bump(ar, e_mark / 256.0, 207)
            Ec = tb_([128, T]); Es = tb_([128, T]); zr = tb_([128, T]); zi = tb_([128, T])
            rr = tb_([128, T]); ri = tb_([128, T]); tA = tb_([128, T]); tBt = tb_([128, T])
            Sr = tb_([128, T], BF16); Si = tb_([128, T], BF16)
            en_ = [tb_([128, 2]), tb_([128, 2])]; e2_ = tb_([128, 2]); u1 = tb_([128, 2])
            yv = tb_([128, 512]); g1 = tb_([128, 512]); g2 = tb_([128, 512])
            PB = [(t0, n) for (t0, n) in TB if t0 < T]
            LOGT = int(math.log2(T))
            assert 1 << LOGT == T
            for gp in range(16):
                c, rows = gp // 4, (gp % 4) * 32
                ph.memset(Ec[:, 0:1], 1.0, ("Ec",)); ph.memset(Es[:, 0:1], 0.0, ("Es",))
                ph.cp(en_[0][:, 0:1], ec1[:, gp:gp + 1], (), ("en0",)); ph.cp(en_[0][:, 1:2], es1[:, gp:gp + 1], (), ("en0",))
                for k in range(LOGT):
                    n = 1 << k
                    e0, e1_ = en_[k % 2], en_[(k + 1) % 2]
                    n0, n1 = "en%d" % (k % 2), "en%d" % ((k + 1) % 2)
                    ph.ts(tA[:, 0:n], Es[:, 0:n], e0[:, 1:2], ALU.mult, ("Es", n0), ("tA",))
                    ph.stt(Ec[:, n:2 * n], Ec[:, 0:n], e0[:, 0:1], tA[:, 0:n], ALU.mult, ALU.subtract, ("Ec", n0, "tA"), ("Ec",))
                    ph.ts(tBt[:, 0:n], Es[:, 0:n], e0[:, 0:1], ALU.mult, ("Es", n0), ("tB",))
                    ph.stt(Es[:, n:2 * n], Ec[:, 0:n], e0[:, 1:2], tBt[:, 0:n], ALU.mult, ALU.add, ("Ec", n0, "tB"), ("Es",))
                    if k < LOGT - 1:
                        ph.tt(e2_, e0, e0, ALU.mult, (n0,), ("e2",))
                        ph.tt(e1_[:, 0:1], e2_[:, 0:1], e2_[:, 1:2], ALU.subtract, ("e2",), (n1,))
                        ph.stt(e1_[:, 1:2], e0[:, 0:1], 2.0, e0[:, 1:2], ALU.mult, ALU.mult, (n0,), (n1,))
                for bi, (t0, n) in enumerate(PB):
                    xb = 2 * (bi % 2)
                    un = "uT%d_%d" % (c, bi)
                    ph.mm([(bank(xb, n), WB[:, 0, gp, :], uT[:, c, t0:t0 + n], True, True),
                           (bank(xb + 1, n), WB[:, 1, gp, :], uT[:, c, t0:t0 + n], True, True)],
                          (un,), ("ps%d" % xb, "ps%d" % (xb + 1)))
                    xn0, xn1 = "ps%d" % xb, "ps%d" % (xb + 1)
                    sl = slice(t0, t0 + n)
                    ph.tt(tA[:, sl], bank(xb, n), Ec[:, sl], ALU.mult, (xn0, "Ec"), ("tA",))
                    ph.tt(tBt[:, sl], bank(xb + 1, n), Es[:, sl], ALU.mult, (xn1, "Es"), ("tB",))
                    ph.tt(zr[:, sl], tA[:, sl], tBt[:, sl], ALU.add, ("tA", "tB"), ("zr",))
                    ph.tt(tA[:, sl], bank(xb + 1, n), Ec[:, sl], ALU.mult, (xn1, "Ec"), ("tA",))
                    ph.tt(tBt[:, sl], bank(xb, n), Es[:, sl], ALU.mult, (xn0, "Es"), ("tB",))
                    ph.tt(zi[:, sl], tA[:, sl], tBt[:, sl], ALU.subtract, ("tA", "tB"), ("zi",))
                dec = magP[:, gp:gp + 1].broadcast_to([128, T])
                ph.add("dve", (lambda e, o=rr, d0=dec, d1=zr: e.tensor_tensor_scan(o, d0, d1, 0.0, ALU.mult, ALU.add)), ("zr",), ("rr",))
                ph.add("dve", (lambda e, o=ri, d0=dec, d1=zi: e.tensor_tensor_scan(o, d0, d1, 0.0, ALU.mult, ALU.add)), ("zi",), ("ri",))
                ph.tt(tA, rr, Ec, ALU.mult, ("rr", "Ec"), ("tA",))
                ph.tt(tBt, ri, Es, ALU.mult, ("ri", "Es"), ("tB",))
                ph.tt(Sr, tA, tBt, ALU.subtract, ("tA", "tB"), ("Sr",))
                ph.tt(sout[0][:, gp, 0:1], tA[:, T - 1:T], tBt[:, T - 1:T], ALU.subtract, ("tA", "tB"), ("so_re",))
                ph.tt(tA, ri, Ec, ALU.mult, ("ri", "Ec"), ("tA",))
                ph.tt(tBt, rr, Es, ALU.mult, ("rr", "Es"), ("tB",))
                ph.tt(Si, tA, tBt, ALU.add, ("tA", "tB"), ("Si",))
                ph.tt(sout[1][:, gp, 0:1], tA[:, T - 1:T], tBt[:, T - 1:T], ALU.add, ("tA", "tB"), ("so_im",))
                for bi, (t0, n) in enumerate(PB):
                    ph.mm([(bank(4 + bi, n), CTr[:, gp, :], Sr[:, t0:t0 + n], gp % 4 == 0, False),
                           (bank(4 + bi, n), CTi[:, gp, :], Si[:, t0:t0 + n], False, gp % 4 == 3)], ("Sr", "Si"), ("ps%d" % (4 + bi),))
                if gp % 4 == 3:
                    for bi, (t0, n) in enumerate(PB):
                        y_ = yv[:, 0:n]; a_ = g1[:, 0:n]; b_ = g2[:, 0:n]
                        ph.stt(y_, uT[:, c, t0:t0 + n], dP[:, c:c + 1], bank(4 + bi, n), ALU.mult, ALU.add, ("ps%d" % (4 + bi),), ("yv",))
                        ph.tt(a_, y_, y_, ALU.mult, ("yv",), ("g1",))
                        ph.ts(a_, a_, 0.044715, ALU.mult, ("g1",), ("g1",), s2=1.0, op1=ALU.add)
                        ph.tt(a_, a_, y_, ALU.mult, ("g1", "yv"), ("g1",))
                        ph.act(b_, a_, AF.Sigmoid, ("g1",), ("g2",), scale=1.5957691216057308)
                        ph.tt(gss[:, c, t0:t0 + n], y_, b_, ALU.mult, ("yv", "g2"), ("gss%d_%d" % (c, bi),))
            ph.run()

        ph = Phase(ctx)
        tb_ = Bump(ar, e_mark / 256.0, 207)
        s0r = tb_([128, 16, NS]); s0i = tb_([128, 16, NS]); q1 = tb_([128, 16, NS]); q2 = tb_([128, 16, NS])
        Ssr = tb_([128, 16, NS], BF16); Ssi = tb_([128, 16, NS], BF16)
        yv = tb_([128, 4, NS]); g1 = tb_([128, 4, NS]); g2 = tb_([128, 4, NS])
        ph.dma("sp", s0r, st_re_d.rearrange("p (a b) -> p a b", a=16), (), ("s0r",), "f0")
        ph.dma("sp", s0i, st_im_d.rearrange("p (a b) -> p a b", a=16), (), ("s0i",), "f1")
        items = []
        for gp in range(16):
            c, rows = gp // 4, (gp % 4) * 32
            for ri_ in range(2):
                items.append((bank(0)[:, (gp * 2 + ri_) * NS:(gp * 2 + ri_ + 1) * NS], WB[:, ri_, gp, :], uT[:, c, T:NT], True, True))
        ph.mm(items, (), ("ps0",))
        Xv = bank(0)[:, 0:32 * NS].rearrange("p (g r b) -> p g r b", g=16, r=2)
        lbr = lbrP.broadcast_to([128, 16, NS]); lbi = lbiP.broadcast_to([128, 16, NS])
        ph.tt(q1, s0i, lbi, ALU.mult, ("s0i",), ("q1",))
        ph.tt(q2, s0r, lbr, ALU.mult, ("s0r",), ("q2",))
        ph.tt(q2, q2, q1, ALU.subtract, ("q1", "q2"), ("q2",))
        ph.tt(sout[0][:, :, 1:1 + NS], q2, Xv[:, :, 0, :], ALU.add, ("q2", "ps0"), ("so_re",))
        ph.cp(Ssr, sout[0][:, :, 1:1 + NS], ("so_re",), ("Ssr",))
        ph.tt(q1, s0r, lbi, ALU.mult, ("s0r", "q2"), ("q1",))
        ph.tt(q2, s0i, lbr, ALU.mult, ("s0i", "so_re"), ("q2",))
        ph.tt(q2, q2, q1, ALU.add, ("q1", "q2"), ("q2",))
        ph.tt(sout[1][:, :, 1:1 + NS], q2, Xv[:, :, 1, :], ALU.add, ("q2", "ps0"), ("so_im",))
        ph.cp(Ssi, sout[1][:, :, 1:1 + NS], ("so_im",), ("Ssi",))
        ph.dma("sp", sre_d.rearrange("p (a b) -> p a b", a=16), sout[0], ("so_re",), ("sre_d",), "f2")
        ph.dma("sp", sim_d.rearrange("p (a b) -> p a b", a=16), sout[1], ("so_im",), ("sim_d",), "f3")
        for c in range(4):
            items = []
            for gl in range(4):
                gp = 4 * c + gl
                items.append((bank(1)[:, c * NS:(c + 1) * NS], CTr[:, gp, :], Ssr[:, gp, :], gl == 0, False))
                items.append((bank(1)[:, c * NS:(c + 1) * NS], CTi[:, gp, :], Ssi[:, gp, :], False, gl == 3))
            ph.mm(items, ("Ssr", "Ssi"), ("ps1",))
        for c in range(4):
            ph.stt(yv[:, c, :], uT[:, c, T:NT], dP[:, c:c + 1], bank(1)[:, c * NS:(c + 1) * NS], ALU.mult, ALU.add, ("ps1",), ("yv%d" % c,))
        ally = tuple("yv%d" % c for c in range(4))
        ph.tt(g1, yv, yv, ALU.mult, ally, ("g1",))
        ph.ts(g1, g1, 0.044715, ALU.mult, ("g1",), ("g1",), s2=1.0, op1=ALU.add)
        ph.tt(g1, g1, yv, ALU.mult, ("g1",) + ally, ("g1",))
        ph.act(g2, g1, AF.Sigmoid, ("g1",), ("g2",), scale=1.5957691216057308)
        ph.tt(gss[:, :, T:NT], yv, g2, ALU.mult, ally + ("g2",), ("gss_s",))
        ph.run()

        ph = Phase(ctx)
        tb_ = Bump(ar, 92.5, 207)
        wgl = tb_([128, 4, 1024], BF16)
        wst = [tb_([128, 1024]), tb_([128, 1024])]
        sg = [tb_([128, 512]), tb_([128, 512])]
        for kc in range(4):
            s = kc % 2
            ph.dma("sp", wst[s], w_glu_d[kc * 128:(kc + 1) * 128, :], (), ("wst%d" % s,), "g%d" % s)
            ph.cp(wgl[:, kc, :], wst[s], ("wst%d" % s,), ("wgl",), eng="pool")
        it = 0
        for oc in range(4):
            for bi, (t0, n) in enumerate(TB):
                b = 2 * (it % 2); it += 1
                ph.mm([(bank(b, n), wgl[:, kc, oc * 128:(oc + 1) * 128], gss[:, kc, t0:t0 + n], kc == 0, kc == 3) for kc in range(4)]
                      + [(bank(b + 1, n), wgl[:, kc, 512 + oc * 128:512 + (oc + 1) * 128], gss[:, kc, t0:t0 + n], kc == 0, kc == 3) for kc in range(4)],
                      ("wgl",), ("ps%d" % b, "ps%d" % (b + 1)))
                s_ = sg[it % 2][:, 0:n]
                ph.act(s_, bank(b + 1, n), AF.Sigmoid, ("ps%d" % (b + 1),), ("sg%d" % (it % 2),), bias=bglu[:, 4 + oc:5 + oc])
                ph.stt(soT[:, oc, t0:t0 + n], bank(b, n), bglu[:, oc:oc + 1], s_, ALU.add, ALU.mult, ("ps%d" % b, "sg%d" % (it % 2)), ("soT",))
        ph.run()

        def layer_norm(ph, src, srcn, n, gam, bet, out_tile, outn, tmp, tmpn, stat):
            st6, mv, rs_, nmr = stat
            for j in range(2):
                ph.add("dve", (lambda e, o=st6[0:n, j, :], i=src[j]: e.bn_stats(o, i)), (srcn[j],), ("st6",))
            ph.add("dve", (lambda e, o=mv[0:n, :], i=st6[0:n, :, :].rearrange("p a b -> p (a b)"): e.bn_aggr(o, i)), ("st6",), ("mv",))
            ph.ts(rs_[0:n, :], mv[0:n, 1:2], LN_EPS, ALU.add, ("mv",), ("rs",))
            ph.act(rs_[0:n, :], rs_[0:n, :], AF.Ln, ("rs",), ("rs",))
            ph.act(rs_[0:n, :], rs_[0:n, :], AF.Exp, ("rs",), ("rs",), scale=-0.5)
            ph.stt(nmr[0:n, :], mv[0:n, 0:1], -1.0, rs_[0:n, :], ALU.mult, ALU.mult, ("mv", "rs"), ("nmr",))
            for j in range(2):
                ph.act(tmp[0:n, j * 512:(j + 1) * 512], src[j], AF.Identity, (srcn[j], "rs", "nmr"), (tmpn[j],),
                       bias=nmr[0:n, :], scale=rs_[0:n, :])
            ph.tt(tmp[0:n, :], tmp[0:n, :], gam[0:n, :], ALU.mult, tuple(tmpn) + ("lng",), tuple(tmpn))
            ph.tt(out_tile[0:n, :], tmp[0:n, :], bet[0:n, :], ALU.add, tuple(tmpn) + ("lng",), (outn,))

        ph = Phase(ctx)
        tb_ = Bump(ar, 150.5, 207)
        wob = tb_([128, 8, 1024], BF16)
        wst = [tb_([128, 1024]), tb_([128, 1024])]
        lng = tb_([128, 1024]); lnb = tb_([128, 1024])
        xt = [tb_([128, 1024]), tb_([128, 1024])]
        tl = tb_([128, 1024]); ht = tb_([128, 1024])
        wrt = tb_([128, 8, 32]); brt = tb_([128, 32])
        st6 = tb_([128, 2, 6]); mv = tb_([128, 2]); rs_ = tb_([128, 1]); nmr = tb_([128, 1])
        lg = tb_([128, 32]); m8 = tb_([128, 8]); sel = tb_([128, 32]); nm_ = tb_([128, 1]); ex = tb_([128, 32])
        den = tb_([128, 1]); gt = tb_([128, 32])
        for kc in range(8):
            s = kc % 2
            ph.dma("sp", wst[s], w_out_d[kc * 128:(kc + 1) * 128, :], (), ("wst%d" % s,), "h%d" % s)
            ph.cp(wob[:, kc, :], wst[s], ("wst%d" % s,), ("wob",), eng="pool")
        ph.dma("act", lng, ln_d[:, 0:D].broadcast_to([128, D]), (), ("lng",), "h2")
        ph.dma("act", lnb, ln_d[:, D:2 * D].broadcast_to([128, D]), (), ("lng",), "h3")
        ph.dma("act", wrt[:, :, 0:NE], wr_d[:, 0:NE].rearrange("(c p) e -> p c e", p=128), (), ("wrt",), "h4")
        ph.dma("act", brt[:, 0:NE], br_d.broadcast_to([128, NE]), (), ("brt",), "h5")
        for i, (t0, n) in enumerate(TL):
            s = i % 2
            ph.dma("sp", xt[s][0:n, :], x_d[t0:t0 + n, :], (), ("xt%d" % s,), "x%d" % s)
            for j in range(2):
                ph.mm([(ps_t[0:n, j * 512:(j + 1) * 512], (soT[:, kc, t0:t0 + n] if kc < 4 else aoT[:, kc - 4, t0:t0 + n]),
                        wob[:, kc, j * 512:(j + 1) * 512], kc == 0, kc == 7) for kc in range(8)], ("wob",), ("ps%d" % j,))
                ph.stt(tl[0:n, j * 512:(j + 1) * 512], xt[s][0:n, j * 512:(j + 1) * 512], ALPHA, ps_t[0:n, j * 512:(j + 1) * 512],
                       ALU.mult, ALU.add, ("xt%d" % s, "ps%d" % j), ("tl%d" % j,))
            layer_norm(ph, [tl[0:n, 0:512], tl[0:n, 512:1024]], ("tl0", "tl1"), n, lng, lnb, ht, "ht", tl, ("tl0", "tl1"),
                       (st6, mv, rs_, nmr))
            for j in range(2):
                ph.tr([(ps_t[:, (2 + j) * 512 + cc * 128:(2 + j) * 512 + cc * 128 + n], ht[0:n, (4 * j + cc) * 128:(4 * j + cc + 1) * 128],
                        ident[0:n, 0:n]) for cc in range(4)], ("ht", "ident"), ("ps%d" % (2 + j),))
                src = bank(2 + j).rearrange("p (c t) -> p c t", c=4)[:, :, 0:n]
                ph.act(fT[:, 4 * j:4 * j + 4, t0:t0 + n], src, AF.Copy, ("ps%d" % (2 + j),), ("fT%d" % i,), scale=ALPHA)
                ph.cp(hTb[:, 4 * j:4 * j + 4, t0:t0 + n], src, ("ps%d" % (2 + j),), ("hTb%d" % i,))
            ph.mm([(ps_t[0:n, 4 * 512:4 * 512 + NE], fT[:, c, t0:t0 + n], wrt[:, c, 0:NE], c == 0, c == 7) for c in range(8)],
                  ("fT%d" % i, "wrt"), ("ps4",))
            ph.stt(lg[0:n, 0:NE], ps_t[0:n, 4 * 512:4 * 512 + NE], 1.0 / ALPHA, brt[0:n, 0:NE], ALU.mult, ALU.add, ("ps4", "brt"), ("lg",))
            ph.add("dve", (lambda e, o=m8[0:n, :], i_=lg[0:n, 0:NE]: e.max(o, i_)), ("lg",), ("m8",))
            ph.ts(sel[0:n, 0:NE], lg[0:n, 0:NE], m8[0:n, cfg.TOPK - 1:cfg.TOPK], ALU.is_ge, ("lg", "m8"), ("sel",))
            ph.ts(nm_[0:n, :], m8[0:n, 0:1], -1.0, ALU.mult, ("m8",), ("nm",))
            ph.act(ex[0:n, 0:NE], lg[0:n, 0:NE], AF.Exp, ("lg", "nm"), ("ex",), bias=nm_[0:n, :])
            ph.tt(ex[0:n, 0:NE], ex[0:n, 0:NE], sel[0:n, 0:NE], ALU.mult, ("ex", "sel"), ("ex2",))
            ph.red(den[0:n, :], ex[0:n, 0:NE], ALU.add, ("ex2",), ("den",))
            ph.recip(den[0:n, :], den[0:n, :], ("den",), ("den2",))
            ph.ts(gt[0:n, 0:NE], ex[0:n, 0:NE], den[0:n, :], ALU.mult, ("ex2", "den2"), ("gt",))
            ph.tr([(ps_t[0:NE, 5 * 512:5 * 512 + n], gt[0:n, 0:NE], ident[0:n, 0:n])], ("gt", "ident"), ("ps5",))
            ph.cp(gatesT[0:NE, t0:t0 + n], ps_t[0:NE, 5 * 512:5 * 512 + n], ("ps5",), ("gatesT",), eng="act")
        ph.run()

        ph = Phase(ctx)
        tb_ = Bump(ar, 159, 207)
        bdn = tb_([128, D])
        ph.dma("act", bdn[0:NE, :], bdn_d, (), ("bdn",), "m1")
        for dc in range(8):
            for bi, (t0, n) in enumerate(TB):
                b = (dc * len(TB) + bi) % 4
                ph.mm([(bank(b, n), bdn[0:NE, dc * 128:(dc + 1) * 128], gatesT[0:NE, t0:t0 + n], True, True)], ("bdn",), ("ps%d" % b,))
                ph.tt(fT[:, dc, t0:t0 + n], fT[:, dc, t0:t0 + n], bank(b, n), ALU.add, ("ps%d" % b,), ("fT%d_%d" % (dc, bi),))
        ph.run()

        ph = Phase(ctx)
        GeS = ar.at(int(150.5 * KB), [128, NT])
        tb_ = Bump(ar, 159, 207)
        stg = [tb_([128, 2048]), tb_([128, 2048])]
        wpb = [tb_([128, 8, 256], BF16) for _ in range(2)]
        Gt = [tb_([128, 512]) for _ in range(3)]; Sg = [tb_([128, 512]) for _ in range(3)]; Lt = [tb_([128, 512]) for _ in range(3)]
        bguT = tb_([128, NE, 16]); bl1 = tb_([128, NE, 8]); selt = [tb_([128, 128]), tb_([128, 128])]
        ph.dma("act", bguT, bgu_d.rearrange("p (e c) -> p e c", e=NE), (), ("bguT",), "m0")
        ph.ts(bl1, bguT[:, :, 8:16], 1.0, ALU.add, ("bguT",), ("bl1",))
        pieces = [(e_, kind, j) for e_ in range(NE) for kind in ("gu", "dn") for j in range(8)]
        cnt = dict(it=0, un=0)

        def emit_load(i):
            e_, kind, j = pieces[i]
            s2_ = i % 2
            row0 = (e_ * 8 + j) * 128
            if kind == "gu":
                ph.dma("sp", stg[s2_], wgu_d[row0:row0 + 128, :], (), ("stg%d" % s2_,), "stg%d" % s2_)
                ph.cp(wpb[s2_].rearrange("p a b -> p (a b)"), stg[s2_], ("stg%d" % s2_,), ("wpb%d" % s2_,), eng="act")
            else:
                ph.dma("sp", stg[s2_][:, 0:1024], wdn_d[row0:row0 + 128, :], (), ("stg%d" % s2_,), "stg%d" % s2_)
                ph.cp(wpb[s2_].rearrange("p a b -> p (a b)")[:, 0:1024], stg[s2_][:, 0:1024], ("stg%d" % s2_,), ("wpb%d" % s2_,), eng="act")

        def emit_compute(i):
            e_, kind, j = pieces[i]
            s2_ = i % 2
            if kind == "gu" and j == 0:
                st_ = selt[e_ % 2]; sn_ = "selt%d" % (e_ % 2)
                ph.cp(st_[0:NE, :], ident[0:NE, e_:e_ + 1].broadcast_to([NE, 128]), (), (sn_,), eng="pool")
                for bi, (t0, n) in enumerate(TB):
                    b = 6 + bi % 2
                    ph.mm([(bank(b, n), st_[0:NE, :], gatesT[0:NE, t0:t0 + n], True, True)], (sn_,), ("ps%d" % b,))
                    ph.cp(GeS[:, t0:t0 + n], bank(b, n), ("ps%d" % b,), ("GeS%d" % bi,), eng="act")
            if kind == "gu":
                fc = j
                for bi, (t0, n) in enumerate(TB):
                    b = 2 * (cnt["it"] % 3); cnt["it"] += 1
                    k3 = cnt["un"] % 3; cnt["un"] += 1
                    ph.mm([(bank(b, n), wpb[s2_][:, kc, 0:128], hTb[:, kc, t0:t0 + n], kc == 0, kc == 7) for kc in range(8)]
                          + [(bank(b + 1, n), wpb[s2_][:, kc, 128:256], hTb[:, kc, t0:t0 + n], kc == 0, kc == 7) for kc in range(8)],
                          ("wpb%d" % s2_,), ("ps%d" % b, "ps%d" % (b + 1)))
                    G_ = Gt[k3][:, 0:n]; S_ = Sg[k3][:, 0:n]; L_ = Lt[k3][:, 0:n]
                    gn, sn, ln_ = "G%d" % k3, "S%d" % k3, "L%d" % k3
                    ph.ts(G_, bank(b, n), bguT[:, e_, fc:fc + 1], ALU.add, ("ps%d" % b, "bguT"), (gn,), s2=SW_LIM, op1=ALU.min)
                    ph.act(L_, bank(b + 1, n), AF.Identity, ("ps%d" % (b + 1), "bl1"), (ln_,), bias=bl1[:, e_, fc:fc + 1])
                    ph.act(S_, G_, AF.Sigmoid, (gn,), (sn,), scale=SW_ALPHA)
                    ph.ts(L_, L_, 1.0 - SW_LIM, ALU.max, (ln_,), (ln_,), s2=SW_LIM + 1.0, op1=ALU.min)
                    ph.tt(L_, L_, G_, ALU.mult, (ln_, gn), (ln_,))
                    ph.tt(S_, S_, L_, ALU.mult, (sn, ln_), (sn,), eng="pool")
                    ph.tt(actT[:, fc, t0:t0 + n], S_, GeS[:, t0:t0 + n], ALU.mult, (sn, "GeS%d" % bi), ("actT%d_%d" % (fc, bi),), eng="pool")
            else:
                dc = j
                wd3 = wpb[s2_].rearrange("p a b -> p (a b)")[:, 0:1024].rearrange("p (a b) -> p a b", a=8)
                for bi, (t0, n) in enumerate(TB):
                    b = 6 + (cnt["it"] % 2); cnt["it"] += 1
                    ph.mm([(bank(b, n), wd3[:, f, :], actT[:, f, t0:t0 + n], f == 0, f == 7) for f in range(8)],
                          ("wpb%d" % s2_,) + tuple("actT%d_%d" % (f, bi) for f in range(8)), ("ps%d" % b,))
                    ph.tt(fT[:, dc, t0:t0 + n], fT[:, dc, t0:t0 + n], bank(b, n), ALU.add, ("ps%d" % b,), ("fT%d_%d" % (dc, bi),))

        emit_load(0)
        for i in range(len(pieces)):
            if i + 1 < len(pieces):
                emit_load(i + 1)
            emit_compute(i)
        ph.run()

        ph = Phase(ctx)
        tb_ = Bump(ar, 150.5, 207)
        lng = tb_([128, 1024]); lnb = tb_([128, 1024])
        yt = [tb_([128, 1024]), tb_([128, 1024])]; tmp = tb_([128, 1024])
        st6 = tb_([128, 2, 6]); mv = tb_([128, 2]); rs_ = tb_([128, 1]); nmr = tb_([128, 1])
        ph.dma("sp", lng, ln_d[:, 2 * D:3 * D].broadcast_to([128, D]), (), ("lng",), "n0")
        ph.dma("sp", lnb, ln_d[:, 3 * D:4 * D].broadcast_to([128, D]), (), ("lng",), "n1")
        for i, (t0, n) in enumerate(TL):
            s = i % 2
            bb = 2 * (i % 2)
            for j in range(2):
                ph.tr([(ps_t[0:n, (bb + j) * 512 + cc * 128:(bb + j) * 512 + (cc + 1) * 128], fT[:, 4 * j + cc, t0:t0 + n], ident)
                       for cc in range(4)], ("ident",), ("ps%d" % (bb + j),))
            layer_norm(ph, [ps_t[0:n, (bb + j) * 512:(bb + j + 1) * 512] for j in range(2)], ("ps%d" % bb, "ps%d" % (bb + 1)), n, lng, lnb,
                       yt[s], "yt%d" % s, tmp, ("tmp0", "tmp1"), (st6, mv, rs_, nmr))
            ph.add("pool", (lambda e, o=y_d[t0:t0 + n, :], i_=yt[s][0:n, :]: e.dma_start(out=o, in_=i_)), ("yt%d" % s,), ("yd%d" % i,), dma="y%d" % s)
        ph.run()
    return nc


def _consts(cfg, past_len):
    T, NS, NT = cfg.T, cfg.NS, cfg.NT
    half = 32
    inv = (np.float32(10000.0) ** (-np.arange(half, dtype=np.float32) / np.float32(half))).astype(np.float32)
    pos = np.concatenate([np.arange(T, dtype=np.float32), np.full((NS,), past_len, np.float32)])
    ang = (pos[:, None] * inv[None, :]).astype(np.float32)
    ropeC = np.cos(ang.astype(np.float64)).astype(np.float32)
    ropeS = np.sin(ang.astype(np.float64)).astype(np.float32)
    ident = np.eye(128, dtype=np.float32)
    tri = np.where(np.arange(128)[None, :] <= np.arange(128)[:, None], 0.0, -30000.0).astype(np.float32)
    selB = np.zeros((NS, NS, 128), np.float32)
    for b in range(NS):
        selB[b, b, :] = 1.0
    pidx = np.arange(128, dtype=np.float32)[:, None].copy()
    negm = np.full((128, 1), -30000.0, np.float32); negm[0, 0] = 0.0
    return dict(ropeC=ropeC, ropeS=ropeS, ident=ident, tri=tri, selB=selB.reshape(NS, NS * 128), pidx=pidx, negm=negm)


def _shared(cfg, I):
    NE = cfg.NE
    f = lambda a: np.ascontiguousarray(a, dtype=np.float32)
    a_re, a_im, ldt = I["ssm_a_re"][0], I["ssm_a_im"][0], I["ssm_log_dt"][0]
    toP = lambda a: a.reshape(16, 2, 64).transpose(1, 2, 0).reshape(128, 16)
    aP = np.concatenate([toP(a_re), toP(a_im), toP(np.repeat(ldt[:, None], 64, 1))], axis=1)

    def bP(b):
        out = np.zeros((2, 64, 16, 4, 2, 16), np.float32)
        v = b.reshape(16, 2, 64, 16)
        for gp in range(16):
            for g2 in range(2):
                out[g2, :, gp, gp % 4, g2, :] = v[gp, g2]
        return out.reshape(128, 2048)
    bPc = np.concatenate([bP(I["ssm_b_re"][0]), bP(I["ssm_b_im"][0])], axis=1)

    def cT(cm):
        out = np.zeros((2, 64, 16, 4, 2, 16), np.float32)
        v = cm.reshape(16, 2, 16, 64)
        for gp in range(16):
            for g2 in range(2):
                out[g2, :, gp, gp % 4, g2, :] = v[gp, g2].T
        return out.reshape(128, 2048)
    cTc = np.concatenate([cT(I["ssm_c_re"][0]), cT(I["ssm_c_im"][0])], axis=1)
    dP = I["ssm_d"][0].reshape(4, 128).T
    bglu = I["b_glu"][0].reshape(8, 128).T
    lam4 = np.concatenate([I["lambda_q1"][0], I["lambda_k1"][0], I["lambda_q2"][0], I["lambda_k2"][0]])[None, :]
    ln = np.concatenate([I["ln1_g"][0], I["ln1_b"][0], I["ln2_g"][0], I["ln2_b"][0]])[None, :]
    wgu = I["w_gate_up"][0]
    wg = wgu[:, :, :1024].reshape(NE, 8, 128, 8, 128)
    wl = wgu[:, :, 1024:].reshape(NE, 8, 128, 8, 128)
    wgu_t = np.empty((NE, 8, 128, 8, 256), np.float32)
    wgu_t[..., :128] = wg.transpose(0, 3, 2, 1, 4)
    wgu_t[..., 128:] = wl.transpose(0, 3, 2, 1, 4)
    wdn = I["w_down"][0].reshape(NE, 8, 128, 8, 128)
    wdn_t = np.ascontiguousarray(wdn.transpose(0, 3, 2, 1, 4))
    bgu = I["b_gate_up"][0].reshape(NE, 16, 128).transpose(2, 0, 1).reshape(128, NE * 16)
    wr = I["w_router"][0]
    if wr.shape[1] < 32:
        wr = np.concatenate([wr, np.zeros((D, 32 - wr.shape[1]), np.float32)], axis=1)
    return dict(
        cache_k=f(I["cache_k"][0].reshape(-1, 512)), cache_v=f(I["cache_v"][0].reshape(-1, 512)),
        w_in=f(I["w_in"][0]), w_out=f(I["w_out"][0]), aP=f(aP), bP=f(bPc), cT=f(cTc), dP=f(dP),
        w_glu=f(I["w_glu"][0]), bglu=f(bglu), lam4=f(lam4), gsub=f(I["subln_g"][0][None, :]), gcol=f(I["subln_g"][0][:, None]),
        ln=f(ln), wr=f(wr), br=f(I["b_router"][0][None, :]), wgu=f(wgu_t.reshape(NE * 8 * 128, 2048)), bgu=f(bgu),
        wdn=f(wdn_t.reshape(NE * 8 * 128, 1024)), bdn=f(I["b_down"][0]))


def run(cfg, I, trace=False, stop_after=None, max_ops=None):
    T, NS, NPG = cfg.T, cfg.NS, cfg.NPG
    nc = build(cfg, stop_after, max_ops)
    shared = _shared(cfg, I)
    shared.update(_consts(cfg, NPG * 128))
    in_maps = []
    for c in range(NCORES):
        x = np.concatenate([I["x_prompt"][c], I["x_sample"][c * NS:(c + 1) * NS, 0]], axis=0).astype(np.float32)
        m = dict(shared)
        m["x"] = np.ascontiguousarray(x)
        m["xT"] = np.ascontiguousarray(x.T)
        m["pt"] = np.ascontiguousarray(I["page_table"][c * NS:(c + 1) * NS].reshape(1, NS * NPG).astype(np.int32))
        for k_, nm in (("state_ssm_re", "st_re"), ("state_ssm_im", "st_im")):
            s = I[k_][0, c * NS:(c + 1) * NS].reshape(NS, 16, 2, 64)
            m[nm] = np.ascontiguousarray(s.transpose(2, 3, 1, 0).reshape(128, 16 * NS).astype(np.float32))
        in_maps.append(m)
    res = run_bass_kernel_spmd(nc, in_maps, core_ids=list(range(NCORES)), trace=trace) if trace else \
        run_bass_kernel_spmd(nc, in_maps, core_ids=list(range(NCORES)))
    R = res.results
    B = NCORES
    y = np.stack([r["y"] for r in R])
    ko = np.stack([r["ko"] for r in R]); vo = np.stack([r["vo"] for r in R])
    sre = np.stack([r["sre"].reshape(2, 64, 16, 1 + NS) for r in R])
    sim = np.stack([r["sim"].reshape(2, 64, 16, 1 + NS) for r in R])

    def st_p(s):
        return np.ascontiguousarray(s[..., 0].transpose(0, 3, 1, 2).reshape(B, 32, 64))[None]

    def st_s(s):
        v = s[..., 1:].transpose(0, 4, 3, 1, 2)
        return np.ascontiguousarray(v.reshape(B * NS, 32, 64))[None]
    outs = (
        np.ascontiguousarray(y[:, :T]), np.ascontiguousarray(y[:, T:].reshape(B * NS, 1, D)),
        np.ascontiguousarray(ko[:, :T].reshape(B, T, 4, 128))[None], np.ascontiguousarray(vo[:, :T].reshape(B, T, 4, 128))[None],
        st_p(sre), st_p(sim),
        np.ascontiguousarray(ko[:, T:].reshape(B * NS, 1, 4, 128))[None], np.ascontiguousarray(vo[:, T:].reshape(B * NS, 1, 4, 128))[None],
        st_s(sre), st_s(sim))
    return tuple(o.astype(np.float32) for o in outs), res


def kernel(**inputs):
    I = {k: np.asarray(v) for k, v in inputs.items()}
    outs, _ = run(FULL, I)
    return outs
```

```python
import math
from contextlib import ExitStack

import numpy as np
import concourse.bass as bass
import concourse.mybir as mybir
from concourse.bass_utils import run_bass_kernel_spmd

F32 = mybir.dt.float32
BF16 = mybir.dt.bfloat16
I32 = mybir.dt.int32
AF = mybir.ActivationFunctionType
ALU = mybir.AluOpType
AX = mybir.AxisListType

D = 1024
NCORES = 8
LN_EPS = 1e-5
RMS_EPS = 1e-5
LAM_INIT = 0.8 - 0.6 * math.exp(-0.3 * 0)
ALPHA = (2 * 1) ** 0.25
SW_ALPHA = 1.702
SW_LIM = 7.0
ARENA_W = 52992


class Cfg:
    def __init__(self, T=2048, NS=16, NPG=16, NPHYS=2560, NE=32, TOPK=4):
        self.T, self.NS, self.NPG, self.NPHYS, self.NE, self.TOPK = T, NS, NPG, NPHYS, NE, TOPK
        self.NT = T + NS
        self.NTP = T // 128
        self.TB = [(i * 512, min(512, T - i * 512)) for i in range((T + 511) // 512)] + [(T, NS)]
        self.TL = [(i * 128, 128) for i in range(self.NTP)] + [(T, NS)]


FULL = Cfg()

ENGS = ("pe", "act", "dve", "pool", "sp")


class Ctx:
    def __init__(self, nc, stack):
        self.nc, self.stack = nc, stack
        self.esem = {e: stack.enter_context(nc.semaphore("es_" + e)) for e in ("pe", "act", "dve", "pool")}
        self.ecnt = {e: 0 for e in self.esem}
        self.dsem, self.dcnt = {}, {}
        self.known = {e: {} for e in ENGS}
        self.phase_no = 0
        self.stop_after = None
        self.max_ops = None

    def dma_slot(self, slot):
        if slot not in self.dsem:
            self.dsem[slot] = self.stack.enter_context(self.nc.semaphore("ds%d" % len(self.dsem)))
            self.dcnt[slot] = 0
            assert len(self.dsem) < 150, "too many dma semaphores"
        return self.dsem[slot]


class Phase:
    def __init__(self, ctx):
        self.ctx, self.ops = ctx, []

    def add(self, eng, fn, r=(), w=(), dma=None):
        self.ops.append(dict(eng=eng, fn=fn, r=tuple(r), w=tuple(w), dma=dma, dep=False))

    def mm(self, items, r, w):
        def fn(e, items=items):
            return [e.matmul(o, l, rh, start=s, stop=t) for (o, l, rh, s, t) in items]
        self.add("pe", fn, r, w)

    def tr(self, items, r, w):
        def fn(e, items=items):
            return [e.transpose(o, i, idn) for (o, i, idn) in items]
        self.add("pe", fn, r, w)

    def act(self, out, in_, func, r, w, bias=None, scale=None, accum=None):
        def fn(e):
            kw = {}
            if bias is not None:
                kw["bias"] = bias
            if scale is not None:
                kw["scale"] = scale
            if accum is not None:
                kw["accum_out"] = accum
            return e.activation(out, in_, func, **kw)
        self.add("act", fn, r, w)

    def ts(self, out, in0, s1, op0, r, w, s2=None, op1=None, eng="dve"):
        def fn(e):
            if op1 is None:
                return e.tensor_scalar(out, in0, s1, None, op0)
            return e.tensor_scalar(out, in0, s1, s2, op0, op1)
        self.add(eng, fn, r, w)

    def stt(self, out, in0, scalar, in1, op0, op1, r, w, accum=None):
        if accum is None:
            self.add("dve", lambda e: e.scalar_tensor_tensor(out, in0, scalar, in1, op0, op1), r, w)
        else:
            self.add("dve", lambda e: e.scalar_tensor_tensor(out, in0, scalar, in1, op0, op1, accum_out=accum), r, w)

    def tt(self, out, in0, in1, op, r, w, eng="dve"):
        self.add(eng, lambda e: e.tensor_tensor(out, in0, in1, op), r, w)

    def cp(self, out, in_, r, w, eng="dve"):
        if eng == "act":
            self.add("act", lambda e: e.copy(out, in_), r, w)
        else:
            self.add(eng, lambda e: e.tensor_copy(out, in_), r, w)

    def red(self, out, in_, op, r, w, axis=None):
        ax = AX.X if axis is None else axis
        self.add("dve", lambda e: e.tensor_reduce(out, in_, ax, op), r, w)

    def memset(self, out, val, w, eng="dve"):
        self.add(eng, lambda e: e.memset(out, val), (), w)

    def recip(self, out, in_, r, w):
        self.add("dve", lambda e: e.reciprocal(out, in_), r, w)

    def dma(self, q, out, in_, r, w, slot):
        self.add(q, lambda e: e.dma_start(out=out, in_=in_), r, w, dma=slot)

    def run(self):
        ops, ctx, nc = self.ops, self.ctx, self.ctx.nc
        ctx.phase_no += 1
        if ctx.stop_after is not None and ctx.phase_no > ctx.stop_after:
            return
        if ctx.stop_after is not None and ctx.phase_no == ctx.stop_after and ctx.max_ops is not None:
            print("[bisect] phase %d has %d ops, keeping %d; last kept: %s" % (
                ctx.phase_no, len(ops), ctx.max_ops, [(o["eng"], o["r"], o["w"], o["dma"]) for o in ops[max(0, ctx.max_ops - 2):ctx.max_ops]]))
            del ops[ctx.max_ops:]
        lastw, readers = {}, {}
        def _excl(b):
            return len(b) == 3 and b[:2] == "ps" and b[2].isdigit()
        for o in ops:
            xr = tuple(b for b in o["r"] if _excl(b))
            if xr:
                o["w"] = tuple(o["w"]) + xr
                o["r"] = tuple(b for b in o["r"] if not _excl(b))
        for i, o in enumerate(ops):
            deps = set()
            for b in o["r"]:
                if b in lastw:
                    deps.add(lastw[b])
            for b in o["w"]:
                if b in lastw:
                    deps.add(lastw[b])
                deps |= readers.get(b, set())
            deps.discard(i)
            o["deps"] = deps
            for d in deps:
                ops[d]["dep"] = True
            for b in o["r"]:
                readers.setdefault(b, set()).add(i)
            for b in o["w"]:
                lastw[b] = i
                readers[b] = set()
        touched = []
        for o in ops:
            if o["dma"] is not None:
                sem = ctx.dma_slot(o["dma"])
                ctx.dcnt[o["dma"]] += 16
                o["tok"] = ("d:" + o["dma"], sem, ctx.dcnt[o["dma"]])
                if o["dma"] not in touched:
                    touched.append(o["dma"])
            elif o["dep"]:
                e = o["eng"]
                ctx.ecnt[e] += 1
                o["tok"] = ("e:" + e, ctx.esem[e], ctx.ecnt[e])
            else:
                o["tok"] = None
        per = {e: [o for o in ops if o["eng"] == e] for e in ENGS}

        def mk(ename):
            def body(eng):
                kn = ctx.known[ename]
                for o in per[ename]:
                    for d in sorted(o["deps"]):
                        key, sem, val = ops[d]["tok"]
                        if kn.get(key, 0) < val:
                            eng.wait_ge(sem, val)
                            kn[key] = val
                    res = o["fn"](eng)
                    last = res[-1] if isinstance(res, (list, tuple)) else res
                    if o["tok"] is not None:
                        last.then_inc(o["tok"][1], 16 if o["dma"] is not None else 1)
                if ename == "sp":
                    for slot in touched:
                        key, val = "d:" + slot, ctx.dcnt[slot]
                        if kn.get(key, 0) < val:
                            eng.wait_ge(ctx.dsem[slot], val)
                            kn[key] = val
            return body

        with nc.Block() as blk:
            blk.tensor(mk("pe"))
            blk.scalar(mk("act"))
            blk.vector(mk("dve"))
            blk.gpsimd(mk("pool"))
            blk.sync(mk("sp"))


class Arena:
    def __init__(self, ap_all):
        self.a = ap_all

    def at(self, off_w, shape, dt=F32, parts=128):
        n = int(np.prod(shape[1:]))
        words = n if dt in (F32, I32) else (n + 1) // 2
        assert off_w + words <= ARENA_W, ("arena overflow", off_w, words)
        v = self.a[0:parts, off_w:off_w + words]
        if dt not in (F32,):
            v = v.bitcast(dt)
            if dt == BF16 and n % 2:
                v = v[:, 0:n]
        if len(shape) == 3:
            v = v.rearrange("p (a b) -> p a b", a=shape[1])
        elif len(shape) == 4:
            v = v.rearrange("p (a b c) -> p a b c", a=shape[1], b=shape[2])
        return v


class Bump:
    def __init__(self, arena, lo_kb, hi_kb):
        self.ar, self.p, self.hi = arena, int(lo_kb * 256), int(hi_kb * 256)

    def __call__(self, shape, dt=F32, parts=128):
        n = int(np.prod(shape[1:]))
        words = n if dt in (F32, I32) else (n + 1) // 2
        words = (words + 7) // 8 * 8
        v = self.ar.at(self.p, shape, dt, parts)
        self.p += words
        assert self.p <= self.hi, ("bump overflow", self.p, self.hi)
        return v


def build(cfg, stop_after=None, max_ops=None):
    T, NS, NT, NTP, NPG, NE = cfg.T, cfg.NS, cfg.NT, cfg.NTP, cfg.NPG, cfg.NE
    TB, TL = cfg.TB, cfg.TL
    TT = len(TL)
    NSL = NPG + 1
    nc = bass.Bass("TRN2", target_bir_lowering=False)

    def din(name, shape, dt=F32):
        return nc.dram_tensor(name, list(shape), dt, kind="ExternalInput").ap()

    def dout(name, shape, dt=F32):
        return nc.dram_tensor(name, list(shape), dt, kind="ExternalOutput").ap()

    xT_d = din("xT", [D, NT]); x_d = din("x", [NT, D])
    ck_d = din("cache_k", [cfg.NPHYS * 128, 512]); cv_d = din("cache_v", [cfg.NPHYS * 128, 512])
    pt_d = din("pt", [1, NS * NPG], I32)
    st_re_d = din("st_re", [128, 16 * NS]); st_im_d = din("st_im", [128, 16 * NS])
    w_in_d = din("w_in", [D, 2048]); w_out_d = din("w_out", [D, D])
    aP_d = din("aP", [128, 48])
    bP_d = din("bP", [128, 2 * 2048])
    cT_d = din("cT", [128, 2 * 2048])
    dP_d = din("dP", [128, 4])
    w_glu_d = din("w_glu", [512, 1024]); bglu_d = din("bglu", [128, 8])
    lam4_d = din("lam4", [1, 256])
    gsub_d = din("gsub", [1, 128]); gcol_d = din("gcol", [128, 1])
    ln_d = din("ln", [1, 4 * D])
    wr_d = din("wr", [D, 32 if NE <= 32 else NE]); br_d = din("br", [1, NE])
    wgu_d = din("wgu", [NE * 8 * 128, 2048]); bgu_d = din("bgu", [128, NE * 16])
    wdn_d = din("wdn", [NE * 8 * 128, 1024]); bdn_d = din("bdn", [NE, D])
    ropeC_d = din("ropeC", [NT, 32]); ropeS_d = din("ropeS", [NT, 32])
    ident_d = din("ident", [128, 128]); tri_d = din("tri", [128, 128])
    selB_d = din("selB", [NS, NS * 128])
    pidx_d = din("pidx", [128, 1]); negm_d = din("negm", [128, 1])

    y_d = dout("y", [NT, D]); ko_d = dout("ko", [NT, 512]); vo_d = dout("vo", [NT, 512])
    sre_d = dout("sre", [128, 16 * (1 + NS)]); sim_d = dout("sim", [128, 16 * (1 + NS)])

    stack = ExitStack()
    with stack:
        arena_t = stack.enter_context(nc.sbuf_tensor("arena", [128, ARENA_W], F32))
        ps_t = stack.enter_context(nc.psum_tensor("ps", [128, 4096], F32))
        ar = Arena(arena_t)
        ctx = Ctx(nc, stack)
        ctx.stop_after = stop_after
        ctx.max_ops = max_ops

        def bank(b, n=512):
            return ps_t[:, b * 512:b * 512 + n]

        def bank_bf(b):
            return ps_t[:, b * 512:(b + 1) * 512].bitcast(BF16)

        KB = 256
        cb = Bump(ar, 0, 10)
        ident = cb([128, 128]); identb = cb([128, 128], BF16); tri = cb([128, 128]); onesf = cb([128, 128])
        cosT = cb([128, TT, 32]); sinT = cb([128, TT, 32])
        gsub = cb([128, 128]); gcol = cb([128, 1]); lam_t = cb([128, 1]); nlam_t = cb([128, 1])
        pidx = cb([128, 1]); negm = cb([128, 1]); dP = cb([128, 4]); bglu = cb([128, 8])
        aoT = ar.at(10 * KB, [128, 4, NT], BF16)
        soT = ar.at(int(26.5 * KB), [128, 4, NT], BF16)
        actT = ar.at(10 * KB, [128, 8, NT], BF16)
        uT = ar.at(43 * KB, [128, 4, NT])
        gss = ar.at(76 * KB, [128, 4, NT], BF16)
        qs = ar.at(76 * KB, [128, 512])
        qT = ar.at(int(94.5 * KB), [128, 4, NT], BF16)
        kT = ar.at(int(94.5 * KB) + 2 * NT, [128, 4, NT], BF16)
        vbf = ar.at(int(127.5 * KB), [128, max(NTP, 1), 512], BF16)
        fT = ar.at(43 * KB, [128, 8, NT])
        hTb = ar.at(109 * KB, [128, 8, NT], BF16)
        gatesT = ar.at(142 * KB, [128, NT])

        ph = Phase(ctx)
        tb_ = Bump(ar, 150, 207)
        l4 = tb_([128, 256]); pr = tb_([128, 128]); sm = tb_([128, 2]); ee = tb_([128, 2])
        ph.dma("sp", ident, ident_d, (), ("ident",), "c0")
        ph.dma("sp", tri, tri_d, (), ("tri",), "c1")
        ph.dma("sp", gsub, gsub_d.broadcast_to([128, 128]), (), ("gsub",), "c2")
        ph.dma("sp", gcol, gcol_d, (), ("gcol",), "c3")
        ph.dma("sp", pidx, pidx_d, (), ("pidx",), "c4")
        ph.dma("sp", negm, negm_d, (), ("negm",), "c5")
        ph.dma("sp", dP, dP_d, (), ("dP",), "c6")
        ph.dma("sp", bglu, bglu_d, (), ("bglu",), "c7")
        ph.dma("sp", l4, lam4_d.broadcast_to([128, 256]), (), ("l4",), "c8")
        if NTP:
            ph.dma("sp", cosT[:, 0:NTP, :], ropeC_d[0:T, :].rearrange("(i p) f -> p i f", p=128), (), ("cosT",), "c9")
            ph.dma("sp", sinT[:, 0:NTP, :], ropeS_d[0:T, :].rearrange("(i p) f -> p i f", p=128), (), ("sinT",), "c10")
        ph.dma("sp", cosT[0:NS, NTP, :], ropeC_d[T:NT, :], (), ("cosTs",), "c11")
        ph.dma("sp", sinT[0:NS, NTP, :], ropeS_d[T:NT, :], (), ("sinTs",), "c12")
        ph.cp(identb, ident, ("ident",), ("identb",))
        ph.memset(onesf, 1.0, ("onesf",))
        ph.tt(pr.rearrange("p (a b) -> p a b", a=2), l4.rearrange("p (a two b) -> p a two b", a=2, two=2)[:, :, 0, :],
              l4.rearrange("p (a two b) -> p a two b", a=2, two=2)[:, :, 1, :], ALU.mult, ("l4",), ("pr",))
        ph.red(sm, pr.rearrange("p (a b) -> p a b", a=2), ALU.add, ("pr",), ("sm",))
        ph.act(ee, sm, AF.Exp, ("sm",), ("ee",))
        ph.tt(lam_t, ee[:, 0:1], ee[:, 1:2], ALU.subtract, ("ee",), ("lam0",))
        ph.ts(lam_t, lam_t, LAM_INIT, ALU.add, ("lam0",), ("lam",))
        ph.ts(nlam_t, lam_t, -1.0, ALU.mult, ("lam",), ("nlam",))
        ph.run()

        ph = Phase(ctx)
        xTb = ar.at(int(143.5 * KB), [128, 8, NT], BF16)
        sb = Bump(ar, 78, 94.5)
        xst = [sb([128, NT]), sb([128, NT])]
        wb_ = Bump(ar, 26.5, 43)
        wpc = [wb_([128, 8, 512], BF16), wb_([128, 8, 512], BF16)]
        tb_ = Bump(ar, 176.5, 207)
        wst = [tb_([128, 512]), tb_([128, 512])]
        ev = [tb_([128, 512]), tb_([128, 512])]
        ta = tb_([128, 256]); tbb = tb_([128, 256])
        for kc in range(8):
            s = kc % 2
            ph.dma("sp", xst[s], xT_d[kc * 128:(kc + 1) * 128, :], (), ("xst%d" % s,), "xst%d" % s)
            ph.cp(xTb[:, kc, :], xst[s], ("xst%d" % s,), ("xTb%d" % kc,), eng="act" if kc % 2 else "dve")
        xall = tuple("xTb%d" % k for k in range(8))
        nev = 0
        for grp in range(4):
            pw = wpc[grp % 2]
            pwn = "wpc%d" % (grp % 2)
            for kc in range(8):
                s = kc % 2
                ph.dma("act" if kc % 2 else "sp", wst[s], w_in_d[kc * 128:(kc + 1) * 128, grp * 512:(grp + 1) * 512],
                       (), ("wst%d" % s,), "wst%d" % s)
                ph.cp(pw[:, kc, :], wst[s], ("wst%d" % s,), (pwn,), eng="pool")
            if grp == 0:
                for c in range(4):
                    for bi, (t0, n) in enumerate(TB):
                        b = (c * len(TB) + bi) % 2
                        ph.mm([(bank(b, n), pw[:, kc, c * 128:(c + 1) * 128], xTb[:, kc, t0:t0 + n], kc == 0, kc == 7)
                               for kc in range(8)], xall + (pwn,), ("ps%d" % b,))
                        ph.cp(uT[:, c, t0:t0 + n], bank(b, n), ("ps%d" % b,), ("uT%d_%d" % (c, bi),), eng="act")
                continue
            def emit_mm(i, pw=pw, pwn=pwn):
                t0, n = TL[i]
                b = 2 + (i % 2)
                ph.mm([(ps_t[0:n, b * 512:(b + 1) * 512], xTb[:, kc, t0:t0 + n], pw[:, kc, :], kc == 0, kc == 7) for kc in range(8)],
                      xall + (pwn,), ("ps%d" % b,))
            emit_mm(0)
            for i, (t0, n) in enumerate(TL):
                b = 2 + (i % 2)
                pb = ps_t[0:n, b * 512:(b + 1) * 512]
                if i + 1 < len(TL):
                    emit_mm(i + 1)
                e_ = ev[nev % 2]; en = "ev%d" % (nev % 2); nev += 1
                eo = e_[0:n, :]
                if grp == 3:
                    ph.cp(eo, pb, ("ps%d" % b,), (en,), eng="act")
                    ph.dma("pool", vo_d[t0:t0 + n, :], eo, (en,), ("vo%d" % i,), "st_" + en)
                    if i < NTP:
                        ph.cp(vbf[:, i, :], pb, ("ps%d" % b,), ("vbf%d" % i,))
                    continue
                pv = pb.rearrange("p (g two f) -> p g two f", g=8, two=2)
                ov = eo.rearrange("p (g two f) -> p g two f", g=8, two=2)
                cs = cosT[0:n, i:i + 1, :].broadcast_to([n, 8, 32]); sn = sinT[0:n, i:i + 1, :].broadcast_to([n, 8, 32])
                t1 = ta[0:n, :].rearrange("p (g f) -> p g f", g=8); t2 = tbb[0:n, :].rearrange("p (g f) -> p g f", g=8)
                rp = ("ps%d" % b, "cosT", "sinT", "cosTs", "sinTs")
                ph.tt(t1, pv[:, :, 0, :], cs, ALU.mult, rp, ("ta",))
                ph.tt(t2, pv[:, :, 1, :], sn, ALU.mult, rp, ("tb",))
                ph.tt(ov[:, :, 0, :], t1, t2, ALU.subtract, ("ta", "tb"), (en,))
                ph.tt(t1, pv[:, :, 1, :], cs, ALU.mult, rp, ("ta",))
                ph.tt(t2, pv[:, :, 0, :], sn, ALU.mult, rp, ("tb",))
                ph.tt(ov[:, :, 1, :], t1, t2, ALU.add, ("ta", "tb"), (en + "b",))
                if grp == 2:
                    ph.dma("pool", ko_d[t0:t0 + n, :], eo, (en, en + "b"), ("ko%d" % i,), "st_" + en)
                if i >= NTP:
                    if grp == 1:
                        ph.cp(qs[0:n, :], eo, (en, en + "b"), ("qs",), eng="act")
                    continue
                tb2 = i % 2
                ph.tr([(bank(tb2)[:, h * 128:(h + 1) * 128], eo[:, h * 128:(h + 1) * 128], ident) for h in range(4)],
                      (en, en + "b", "ident"), ("ps%d" % tb2,))
                dst = qT if grp == 1 else kT
                ph.cp(dst[:, :, t0:t0 + 128], bank(tb2).rearrange("p (h t) -> p h t", h=4), ("ps%d" % tb2,),
                      ("%sT%d" % ("q" if grp == 1 else "k", i),), eng="act" if i % 2 else "dve")
        ph.run()

        if NTP:
            ph = Phase(ctx)
            tb_ = Bump(ar, 143.5, 207)
            Psb = [[tb_([128, T], BF16), tb_([128, T], BF16)], [tb_([128, T], BF16), tb_([128, T], BF16)]]
            PTs = [tb_([128, T], BF16), tb_([128, T], BF16)]
            mx = tb_([128, 2]); nb = tb_([128, 2]); lsum = tb_([128, 2]); rl = tb_([128, 2])
            tS = tb_([128, 128]); att = tb_([128, 128]); junk = tb_([128, 128]); ss = tb_([128, 1]); rs = tb_([128, 1])
            lsum2 = [lsum, tb_([128, 2])]

            def geom(qt, m):
                nk = qt + 1
                small = nk <= 8
                sb0 = 2 * m if small else 0
                SBK = tuple("ps%d" % (sb0 + k) for k in range(2 if small else 4))
                Sv = ps_t[:, sb0 * 512:sb0 * 512 + (1024 if small else 2048)]
                if small:
                    PTv = ps_t[:, (4 + m) * 512:(5 + m) * 512].bitcast(BF16); PTK = ("ps%d" % (4 + m),)
                else:
                    PTv = ps_t[:, 2048:3072].bitcast(BF16); PTK = ("ps4", "ps5")
                return nk, SBK, Sv, PTv, PTK

            def stageA(idx, h, qt, m):
                nk, SBK, Sv, PTv, PTK = geom(qt, m)
                ls = lsum2[idx % 2]; lname = "l%d_%d" % (idx % 2, m)
                items = []
                for j in range(0, nk, 4):
                    cols = min(4, nk - j) * 128
                    items.append((Sv[:, j * 128:j * 128 + cols], qT[m * 64:(m + 1) * 64, h, qt * 128:(qt + 1) * 128],
                                  kT[m * 64:(m + 1) * 64, h, j * 128:j * 128 + cols], True, True))
                ph.mm(items, (), SBK)
                ph.tt(Sv[:, qt * 128:(qt + 1) * 128], Sv[:, qt * 128:(qt + 1) * 128], tri, ALU.add, SBK + ("tri",), SBK)
                ph.red(mx[:, m:m + 1], Sv[:, 0:nk * 128], ALU.max, SBK, ("mx%d" % m,))
                ph.ts(nb[:, m:m + 1], mx[:, m:m + 1], -0.125, ALU.mult, ("mx%d" % m,), ("nb%d" % m,))
                ph.act(Psb[idx % 2][m][:, 0:nk * 128], Sv[:, 0:nk * 128], AF.Exp, SBK + ("nb%d" % m,), ("P%d_%d" % (idx % 2, m), lname),
                       bias=nb[:, m:m + 1], scale=0.125, accum=ls[:, m:m + 1])

            def stageB(idx, h, qt, m):
                nk, SBK, Sv, PTv, PTK = geom(qt, m)
                pn, tn = "P%d_%d" % (idx % 2, m), "PT%d" % m
                ph.tr([(PTv[:, k * 128:(k + 1) * 128], Psb[idx % 2][m][:, k * 128:(k + 1) * 128], identb) for k in range(nk)], (pn, "identb"), PTK)
                ph.cp(PTs[m][:, 0:nk * 128], PTv[:, 0:nk * 128], PTK, (tn,), eng="act" if m else "dve")
                ph.mm([(bank(6 + m)[:, 0:128], PTs[m][:, k * 128:(k + 1) * 128], vbf[:, k, h * 128:(h + 1) * 128],
                        k == 0, k == nk - 1) for k in range(nk)], (tn,), ("ps%d" % (6 + m),))

            def stageC(idx, h, qt):
                ls = lsum2[idx % 2]
                ph.recip(rl, ls, ("l%d_0" % (idx % 2), "l%d_1" % (idx % 2)), ("rl",))
                ph.tt(rl[:, 1:2], rl[:, 1:2], lam_t, ALU.mult, ("rl", "lam"), ("rl",))
                ph.ts(tS, bank(7)[:, 0:128], rl[:, 1:2], ALU.mult, ("ps7", "rl"), ("tS",))
                ph.stt(att, bank(6)[:, 0:128], rl[:, 0:1], tS, ALU.mult, ALU.subtract, ("ps6", "rl", "tS"), ("att",))
                ph.stt(junk, att, 1.0, att, ALU.mult, ALU.mult, ("att",), ("junk", "ss"), accum=ss)
                ph.ts(ss, ss, 1.0 / 128, ALU.mult, ("ss",), ("ss",), s2=RMS_EPS, op1=ALU.add)
                ph.act(ss, ss, AF.Ln, ("ss",), ("ss",))
                ph.act(rs, ss, AF.Exp, ("ss",), ("rs",), scale=-0.5)
                ph.ts(att, att, rs, ALU.mult, ("att", "rs"), ("att",), s2=1.0 - LAM_INIT, op1=ALU.mult)
                ph.tt(att, att, gsub, ALU.mult, ("att", "gsub"), ("att",))
                ph.tr([(bank(6)[:, 256:384], att, ident)], ("att", "ident"), ("ps6",))
                ph.cp(aoT[:, h, qt * 128:(qt + 1) * 128], bank(6)[:, 256:384], ("ps6",), ("aoT%d_%d" % (h, qt),), eng="act")

            iters = [(h, qt) for h in range(4) for qt in range(NTP)]
            stageA(0, iters[0][0], iters[0][1], 0); stageA(0, iters[0][0], iters[0][1], 1)
            for idx, (h, qt) in enumerate(iters):
                nxt = iters[idx + 1] if idx + 1 < len(iters) else None
                if nxt:
                    stageA(idx + 1, nxt[0], nxt[1], 0)
                stageB(idx, h, qt, 0); stageB(idx, h, qt, 1)
                if nxt:
                    stageA(idx + 1, nxt[0], nxt[1], 1)
                stageC(idx, h, qt)
            ph.run()

        ph = Phase(ctx)
        tb_ = Bump(ar, 94.5, 207)
        selB = tb_([128, NS * 128])
        pti = tb_([128, NS * NPG], I32); ptf = tb_([128, NS * NPG]); idx = tb_([128, NS * NPG], I32)
        NR = 3
        Kp = [tb_([128, 4, 512]) for _ in range(NR)]
        Vp = [tb_([128, 4, 512]) for _ in range(NR)]
        Ksf = [tb_([128, 512]), tb_([128, 512])]; Vsf = [tb_([128, 512]), tb_([128, 512])]
        prod = [tb_([128, 512]), tb_([128, 512])]
        Ss = tb_([128, NS, NSL, 8])
        mp = tb_([128, NS * 8]); gmx = tb_([128, 1]); dg = tb_([128, NS * 8]); rls = tb_([128, NS * 8])
        t1s = tb_([128, NS, 2]); atts = tb_([128, 4, NS]); sqs = tb_([128, 4 * NS]); rss = tb_([128, 4 * NS])
        H8 = NS * 8
        OTB = ("ps3", "ps4", "ps5", "ps6", "ps7")
        NVB = 4
        Vb = [tb_([128, 512], BF16) for _ in range(NVB)]
        Pb = tb_([128, NS, NSL, 8], BF16); onesb = tb_([128, 128], BF16)
        ph.memset(onesb, 1.0, ("onesb",))
        ph.dma("sp", selB[0:NS, :], selB_d, (), ("selB",), "d0")
        ph.dma("sp", pti, pt_d.broadcast_to([128, NS * NPG]), (), ("pti",), "d1")
        ph.cp(ptf, pti, ("pti",), ("ptf",))
        ph.ts(ptf, ptf, 128.0, ALU.mult, ("ptf", "pidx"), ("ptf2",), s2=pidx, op1=ALU.add)
        ph.cp(idx, ptf, ("ptf2",), ("idx",))
        for s in range(2):
            ph.memset(Ksf[s], 0.0, ("Ksf%d" % s,)); ph.memset(Vsf[s], 0.0, ("Vsf%d" % s,))
        ngr = (NPG + 3) // 4
        gi = 0
        for b in range(NS):
            qb = b % 2
            ph.mm([(bank(qb), selB[0:NS, b * 128:(b + 1) * 128], qs[0:NS, :], True, True)], ("selB", "qs"), ("ps%d" % qb,))
            for g in range(ngr):
                r = gi % NR; gi += 1
                for jj in range(min(4, NPG - g * 4)):
                    j = g * 4 + jj
                    col = b * NPG + j
                    bn = "Kp%d_%d" % (r, jj)
                    ph.add("pool", (lambda e, o=Kp[r][:, jj, :], ia=idx[:, col:col + 1]: e.indirect_dma_start(
                        out=o, out_offset=None, in_=ck_d, in_offset=bass.IndirectOffsetOnAxis(ap=ia, axis=0))),
                        ("idx",), (bn,), dma=bn)
                    p_ = prod[j % 2]; pn = "prod%d" % (j % 2)
                    ph.tt(p_, Kp[r][:, jj, :], bank(qb), ALU.mult, (bn, "ps%d" % qb), (pn,))
                    ph.red(Ss[:, b, j, :], p_.rearrange("p (g f) -> p g f", g=8), ALU.add, (pn,), ("Ss%d" % b,))
            s = b % 2
            ph.dma("sp", Ksf[s][0:1, :], ko_d[T + b:T + b + 1, :], ("ko%d" % NTP,), ("Ksf%d" % s,), "ksf%d" % s)
            ph.tt(prod[0], Ksf[s], bank(qb), ALU.mult, ("Ksf%d" % s, "ps%d" % qb), ("prod0",))
            ph.red(Ss[:, b, NPG, :], prod[0].rearrange("p (g f) -> p g f", g=8), ALU.add, ("prod0",), ("Ss%d" % b,))
        allS = tuple("Ss%d" % b for b in range(NS))
        ph.ts(Ss[:, :, NPG, :], Ss[:, :, NPG, :], negm, ALU.add, allS + ("negm",), ("SsA",))
        ph.red(mp.rearrange("p (b h) -> p b h", b=NS), Ss.rearrange("p b s h -> p b h s"), ALU.max, ("SsA",), ("mp",))
        ph.tr([(bank(2)[0:H8, 0:128], mp, ident)], ("mp", "ident"), ("ps2",))
        ph.red(gmx[0:H8, :], bank(2)[0:H8, 0:128], ALU.max, ("ps2",), ("gmx",))
        ph.ts(dg[0:H8, :], ident[0:H8, 0:H8], gmx[0:H8, :], ALU.mult, ("gmx", "ident"), ("dg",))
        ph.mm([(bank(2)[:, 0:H8], onesf[0:H8, :], dg[0:H8, :], True, True)], ("dg", "onesf"), ("ps2",))
        ph.tt(Ss, Ss, bank(2)[:, 0:H8].rearrange("p (b o h) -> p b o h", b=NS, o=1).broadcast_to([128, NS, NSL, 8]),
              ALU.subtract, ("SsA", "ps2"), ("SsB",))
        ph.act(Ss, Ss, AF.Exp, ("SsB",), ("P",), scale=0.125)
        ph.cp(Pb.rearrange("p b s h -> p (b s h)"), Ss.rearrange("p b s h -> p (b s h)"), ("P",), ("Pb",))
        gi = 0
        vi = 0
        for b in range(NS):
            for g in range(ngr):
                r = gi % NR; gi += 1
                for jj in range(min(4, NPG - g * 4)):
                    j = g * 4 + jj
                    col = b * NPG + j
                    bn = "Vp%d_%d" % (r, jj)
                    ph.add("pool", (lambda e, o=Vp[r][:, jj, :], ia=idx[:, col:col + 1]: e.indirect_dma_start(
                        out=o, out_offset=None, in_=cv_d, in_offset=bass.IndirectOffsetOnAxis(ap=ia, axis=0))),
                        ("idx",), (bn,), dma=bn)
                    vb = Vb[vi % NVB]; vn = "Vb%d" % (vi % NVB); vi += 1
                    ph.cp(vb, Vp[r][:, jj, :], (bn,), (vn,), eng="act")
                    items = [(bank(3 + h)[:, b * 2:b * 2 + 2], vb[:, h * 128:(h + 1) * 128], Pb[:, b, j, 2 * h:2 * h + 2],
                              j == 0, False) for h in range(4)]
                    items.append((bank(7)[:, b * 8:b * 8 + 8], onesb, Pb[:, b, j, :], j == 0, False))
                    ph.mm(items, (vn, "Pb", "onesb"), OTB)
            s = b % 2
            ph.dma("sp", Vsf[s][0:1, :], vo_d[T + b:T + b + 1, :], ("vo%d" % NTP,), ("Vsf%d" % s,), "vsf%d" % s)
            items = [(bank(3 + h)[:, b * 2:b * 2 + 2], Vsf[s][:, h * 128:(h + 1) * 128], Ss[:, b, NPG, 2 * h:2 * h + 2],
                      False, True) for h in range(4)]
            items.append((bank(7)[:, b * 8:b * 8 + 8], onesf, Ss[:, b, NPG, :], False, True))
            ph.mm(items, ("Vsf%d" % s, "P", "onesf"), OTB)
        ph.recip(rls, bank(7)[:, 0:H8], ("ps7",), ("rls",))
        rl4 = rls.rearrange("p (b h m) -> p b h m", b=NS, h=4)
        for h in range(4):
            ph.tt(t1s, bank(3 + h)[:, 0:2 * NS].rearrange("p (b m) -> p b m", b=NS), rl4[:, :, h, :], ALU.mult,
                  ("ps%d" % (3 + h), "rls"), ("t1s",))
            ph.stt(atts[:, h, :], t1s[:, :, 1], nlam_t, t1s[:, :, 0], ALU.mult, ALU.add, ("t1s", "nlam"), ("atts%d" % h,))
        alla = tuple("atts%d" % h for h in range(4))
        af = atts.rearrange("p h b -> p (h b)")
        ph.tt(sqs, af, af, ALU.mult, alla, ("sqs",))
        ph.mm([(bank(2)[:, 0:4 * NS], onesf, sqs, True, True)], ("sqs", "onesf"), ("ps2",))
        ph.ts(rss, bank(2)[:, 0:4 * NS], 1.0 / 128, ALU.mult, ("ps2",), ("rss0",), s2=RMS_EPS, op1=ALU.add)
        ph.act(rss, rss, AF.Sqrt, ("rss0",), ("rss1",))
        ph.recip(rss, rss, ("rss1",), ("rss2",))
        ph.tt(af, af, rss, ALU.mult, alla + ("rss2",), ("attn",))
        ph.ts(aoT[:, :, T:NT], atts, gcol, ALU.mult, ("attn", "gcol"), ("aoTs",), s2=1.0 - LAM_INIT, op1=ALU.mult)
        ph.run()

        E_LO = 92.5
        pb_ = Bump(ar, E_LO, 207)
        WB = pb_([128, 2, 16, 128])
        CTr = pb_([128, 16, 128], BF16); CTi = pb_([128, 16, 128], BF16)
        magP = pb_([128, 16]); ec1 = pb_([128, 16]); es1 = pb_([128, 16]); lbrP = pb_([128, 16, 1]); lbiP = pb_([128, 16, 1])
        sout = [pb_([128, 16, 1 + NS]), pb_([128, 16, 1 + NS])]
        e_mark = pb_.p
        ph = Phase(ctx)
        tb_ = Bump(ar, e_mark / 256.0, 207)
        aPt = tb_([128, 48]); bPt = tb_([128, 2, 16, 128]); cst = tb_([128, 2048])
        BB = tb_([128, 2, 16, 128]); w2 = tb_([128, 16, 128]); w3 = tb_([128, 16, 128])
        ph.dma("sp", aPt, aP_d, (), ("Pin",), "e0")
        ph.dma("sp", bPt[:, 0], bP_d[:, 0:2048].rearrange("p (a b) -> p a b", a=16), (), ("bPt0",), "e1")
        ph.dma("act", bPt[:, 1], bP_d[:, 2048:4096].rearrange("p (a b) -> p a b", a=16), (), ("bPt1",), "e2")
        ph.dma("sp", cst, cT_d[:, 0:2048], (), ("cst",), "e3")
        ph.cp(CTr.rearrange("p a b -> p (a b)"), cst, ("cst",), ("CTr",))
        ph.dma("sp", cst, cT_d[:, 2048:4096], ("CTr",), ("cst2",), "e3")
        ph.act(CTi.rearrange("p a b -> p (a b)"), cst, AF.Copy, ("cst2",), ("CTi",), scale=-1.0)

        def disc(tag, a_re, a_im, ldt, W, want_f):
            keys = ("ar", "dt", "xr", "th", "acc", "tmp", "s", "a", "x2", "u", "s2", "a2")
            t = {k: tb_([128, W]) for k in keys}
            N = lambda k: tag + k
            IN = tag + "in"

            def TS(o, i, s1, op0, s2=None, op1=None, extra=()):
                ph.ts(t[o], t[i], s1, op0, (N(i),) + extra, (N(o),), s2=s2, op1=op1)

            def TT(o, i0, i1, op):
                ph.tt(t[o], t[i0], t[i1], op, (N(i0), N(i1)), (N(o),))

            ph.ts(t["ar"], a_re, -1e-4, ALU.min, (IN,), (N("ar"),))
            ph.act(t["dt"], ldt, AF.Exp, (IN,), (N("dt"),))
            TT("xr", "ar", "dt", ALU.mult)
            ph.tt(t["th"], a_im, t["dt"], ALU.mult, (IN, N("dt")), (N("th"),))
            TS("acc", "xr", 0.1, ALU.mult, s2=1.0, op1=ALU.add)
            for k in range(9, 0, -1):
                TT("tmp", "xr", "acc", ALU.mult)
                if k > 1:
                    TS("acc", "tmp", 1.0 / k, ALU.mult, s2=1.0, op1=ALU.add)
            TS("acc", "tmp", 1.0, ALU.add)
            TS("u", "th", 1.0 / 1024, ALU.mult)
            TT("x2", "u", "u", ALU.mult)
            TS("s", "x2", -1.0 / 20, ALU.mult, s2=1.0, op1=ALU.add)
            TT("s", "s", "x2", ALU.mult)
            TS("s", "s", -1.0 / 6, ALU.mult, s2=1.0, op1=ALU.add)
            TT("s", "s", "u", ALU.mult)
            TS("a", "x2", -1.0 / 30, ALU.mult, s2=1.0, op1=ALU.add)
            TT("a", "a", "x2", ALU.mult)
            TS("a", "a", -1.0 / 12, ALU.mult, s2=1.0, op1=ALU.add)
            TT("a", "a", "x2", ALU.mult)
            TS("a", "a", 0.5, ALU.mult)
            s_, a_, s2_, a2_ = "s", "a", "s2", "a2"
            for _ in range(10):
                TS("x2", a_, -1.0, ALU.mult, s2=1.0, op1=ALU.add)
                ph.stt(t[a2_], t[s_], 2.0, t[s_], ALU.mult, ALU.mult, (N(s_),), (N(a2_),))
                ph.stt(t[s2_], t[s_], 2.0, t["x2"], ALU.mult, ALU.mult, (N(s_), N("x2")), (N(s2_),))
                s_, s2_, a_, a2_ = s2_, s_, a2_, a_
            res = dict(mag=t["acc"], magn=N("acc"), s=t[s_], sn=N(s_), a=t[a_], an=N(a_))
            if want_f:
                TT("x2", "acc", a_, ALU.mult)
                TT("x2", "tmp", "x2", ALU.subtract)
                TT("th", "acc", s_, ALU.mult)
                TT("dt", "ar", "ar", ALU.mult)
                ph.tt(t["xr"], a_im, a_im, ALU.mult, (IN,), (N("xr"),))
                TT("dt", "dt", "xr", ALU.add)
                ph.recip(t["dt"], t["dt"], (N("dt"),), (N("dt"),))
                TT("xr", "x2", "ar", ALU.mult)
                ph.tt(t["u"], t["th"], a_im, ALU.mult, (N("th"), IN), (N("u"),))
                TT("xr", "xr", "u", ALU.add)
                TT("xr", "xr", "dt", ALU.mult)
                TT(s2_, "th", "ar", ALU.mult)
                ph.tt(t["u"], t["x2"], a_im, ALU.mult, (N("x2"), IN), (N("u"),))
                TT(s2_, s2_, "u", ALU.subtract)
                TT(s2_, s2_, "dt", ALU.mult)
                res.update(fre=t["xr"], fren=N("xr"), fim=t[s2_], fimn=N(s2_))
            return res

        rP = disc("P", aPt[:, 0:16], aPt[:, 16:32], aPt[:, 32:48], 16, True)
        fre = rP["fre"].unsqueeze(2).broadcast_to([128, 16, 128]); fim = rP["fim"].unsqueeze(2).broadcast_to([128, 16, 128])
        ph.tt(w2, bPt[:, 0], fre, ALU.mult, (rP["fren"], "bPt0"), ("w2",))
        ph.tt(w3, bPt[:, 1], fim, ALU.mult, (rP["fimn"], "bPt1"), ("w3",))
        ph.tt(BB[:, 0], w2, w3, ALU.subtract, ("w2", "w3"), ("BB0",))
        ph.tt(w2, bPt[:, 1], fre, ALU.mult, (rP["fren"], "bPt1"), ("w2",))
        ph.tt(w3, bPt[:, 0], fim, ALU.mult, (rP["fimn"], "bPt0"), ("w3",))
        ph.tt(BB[:, 1], w2, w3, ALU.add, ("w2", "w3"), ("BB1",))
        for ri_ in range(2):
            for g4 in range(4):
                bk = (ri_ * 4 + g4) % 4
                ph.tr([(bank(bk)[:, q * 128:(q + 1) * 128], BB[:, ri_, g4 * 4 + q, :], ident) for q in range(4)],
                      ("BB%d" % ri_, "ident"), ("ps%d" % bk,))
                ph.cp(WB[:, ri_, g4 * 4:g4 * 4 + 4, :], bank(bk).rearrange("p (q f) -> p q f", q=4), ("ps%d" % bk,), ("WB",),
                      eng="act" if g4 % 2 else "dve")
        cP = tb_([128, 16]); nP = tb_([128, 16]); n2 = tb_([128, 16])
        ph.ts(cP, rP["a"], -1.0, ALU.mult, (rP["an"],), ("cP",), s2=1.0, op1=ALU.add)
        ph.tt(nP, cP, cP, ALU.mult, ("cP",), ("nP",))
        ph.tt(n2, rP["s"], rP["s"], ALU.mult, (rP["sn"],), ("n2",))
        ph.tt(nP, nP, n2, ALU.add, ("nP", "n2"), ("nP",))
        ph.ts(nP, nP, -0.5, ALU.mult, ("nP",), ("nP",), s2=1.5, op1=ALU.add)
        ph.tt(ec1, cP, nP, ALU.mult, ("cP", "nP"), ("ec1",))
        ph.tt(es1, rP["s"], nP, ALU.mult, (rP["sn"], "nP"), ("es1",))
        ph.cp(magP, rP["mag"], (rP["magn"],), ("magP",))
        ph.tt(lbrP.rearrange("p a b -> p (a b)"), rP["mag"], ec1, ALU.mult, (rP["magn"], "ec1"), ("lbrP",))
        ph.tt(lbiP.rearrange("p a b -> p (a b)"), rP["mag"], es1, ALU.mult, (rP["magn"], "es1"), ("lbiP",))
        ph.run()

        if NTP:
            ph = Phase(ctx)
            tb_ = Bump(ar, e_mark / 256.0, 207)
            Ec = tb_([128, T]); Es = tb_([128, T]); zr = tb_([128, T]); zi = tb_([128, T])
            rr = tb_([128, T]); ri = tb_([128, T]); tA = tb_([128, T]); tBt = tb_([128, T])
            Sr = tb_([128, T], BF16); Si = tb_([128, T], BF16)
            en_ = [tb_([128, 2]), tb_([128, 2])]; e2_ = tb_([128, 2]); u1 = tb_([128, 2])
            yv = tb_([128, 512]); g1 = tb_([128, 512]); g2 = tb_([128, 512])
            PB = [(t0, n) for (t0, n) in TB if t0 < T]
            LOGT = int(math.log2(T))
            assert 1 << LOGT == T
            for gp in range(16):
                c, rows = gp // 4, (gp % 4) * 32
                ph.memset(Ec[:, 0:1], 1.0, ("Ec",)); ph.memset(Es[:, 0:1], 0.0, ("Es",))
                ph.cp(en_[0][:, 0:1], ec1[:, gp:gp + 1], (), ("en0",)); ph.cp(en_[0][:, 1:2], es1[:, gp:gp + 1], (), ("en0",))
                for k in range(LOGT):
                    n = 1 << k
                    e0, e1_ = en_[k % 2], en_[(k + 1) % 2]
                    n0, n1 = "en%d" % (k % 2), "en%d" % ((k + 1) % 2)
                    ph.ts(tA[:, 0:n], Es[:, 0:n], e0[:, 1:2], ALU.mult, ("Es", n0), ("tA",))
                    ph.stt(Ec[:, n:2 * n], Ec[:, 0:n], e0[:, 0:1], tA[:, 0:n], ALU.mult, ALU.subtract, ("Ec", n0, "tA"), ("Ec",))
                    ph.ts(tBt[:, 0:n], Es[:, 0:n], e0[:, 0:1], ALU.mult, ("Es", n0), ("tB",))
                    ph.stt(Es[:, n:2 * n], Ec[:, 0:n], e0[:, 1:2], tBt[:, 0:n], ALU.mult, ALU.add, ("Ec", n0, "tB"), ("Es",))
                    if k < LOGT - 1:
                        ph.tt(e2_, e0, e0, ALU.mult, (n0,), ("e2",))
                        ph.tt(e1_[:, 0:1], e2_[:, 0:1], e2_[:, 1:2], ALU.subtract, ("e2",), (n1,))
                        ph.stt(e1_[:, 1:2], e0[:, 0:1], 2.0, e0[:, 1:2], ALU.mult, ALU.mult, (n0,), (n1,))
                for bi, (t0, n) in enumerate(PB):
                    xb = 2 * (bi % 2)
                    un = "uT%d_%d" % (c, bi)
                    ph.mm([(bank(xb, n), WB[:, 0, gp, :], uT[:, c, t0:t0 + n], True, True),
                           (bank(xb + 1, n), WB[:, 1, gp, :], uT[:, c, t0:t0 + n], True, True)],
                          (un,), ("ps%d" % xb, "ps%d" % (xb + 1)))
                    xn0, xn1 = "ps%d" % xb, "ps%d" % (xb + 1)
                    sl = slice(t0, t0 + n)
                    ph.tt(tA[:, sl], bank(xb, n), Ec[:, sl], ALU.mult, (xn0, "Ec"), ("tA",))
                    ph.tt(tBt[:, sl], bank(xb + 1, n), Es[:, sl], ALU.mult, (xn1, "Es"), ("tB",))
                    ph.tt(zr[:, sl], tA[:, sl], tBt[:, sl], ALU.add, ("tA", "tB"), ("zr",))
                    ph.tt(tA[:, sl], bank(xb + 1, n), Ec[:, sl], ALU.mult, (xn1, "Ec"), ("tA",))
                    ph.tt(tBt[:, sl], bank(xb, n), Es[:, sl], ALU.mult, (xn0, "Es"), ("tB",))
                    ph.tt(zi[:, sl], tA[:, sl], tBt[:, sl], ALU.subtract, ("tA", "tB"), ("zi",))
                dec = magP[:, gp:gp + 1].broadcast_to([128, T])
                ph.add("dve", (lambda e, o=rr, d0=dec, d1=zr: e.tensor_tensor_scan(o, d0, d1, 0.0, ALU.mult, ALU.add)), ("zr",), ("rr",))
                ph.add("dve", (lambda e, o=ri, d0=dec, d1=zi: e.tensor_tensor_scan(o, d0, d1, 0.0, ALU.mult, ALU.add)), ("zi",), ("ri",))
                ph.tt(tA, rr, Ec, ALU.mult, ("rr", "Ec"), ("tA",))
                ph.tt(tBt, ri, Es, ALU.mult, ("ri", "Es"), ("tB",))
                ph.tt(Sr, tA, tBt, ALU.subtract, ("tA", "tB"), ("Sr",))
                ph.tt(sout[0][:, gp, 0:1], tA[:, T - 1:T], tBt[:, T - 1:T], ALU.subtract, ("tA", "tB"), ("so_re",))
                ph.tt(tA, ri, Ec, ALU.mult, ("ri", "Ec"), ("tA",))
                ph.tt(tBt, rr, Es, ALU.mult, ("rr", "Es"), ("tB",))
                ph.tt(Si, tA, tBt, ALU.add, ("tA", "tB"), ("Si",))
                ph.tt(sout[1][:, gp, 0:1], tA[:, T - 1:T], tBt[:, T - 1:T], ALU.add, ("tA", "tB"), ("so_im",))
                for bi, (t0, n) in enumerate(PB):
                    ph.mm([(bank(4 + bi, n), CTr[:, gp, :], Sr[:, t0:t0 + n], gp % 4 == 0, False),
                           (bank(4 + bi, n), CTi[:, gp, :], Si[:, t0:t0 + n], False, gp % 4 == 3)], ("Sr", "Si"), ("ps%d" % (4 + bi),))
                if gp % 4 == 3:
                    for bi, (t0, n) in enumerate(PB):
                        y_ = yv[:, 0:n]; a_ = g1[:, 0:n]; b_ = g2[:, 0:n]
                        ph.stt(y_, uT[:, c, t0:t0 + n], dP[:, c:c + 1], bank(4 + bi, n), ALU.mult, ALU.add, ("ps%d" % (4 + bi),), ("yv",))
                        ph.tt(a_, y_, y_, ALU.mult, ("yv",), ("g1",))
                        ph.ts(a_, a_, 0.044715, ALU.mult, ("g1",), ("g1",), s2=1.0, op1=ALU.add)
                        ph.tt(a_, a_, y_, ALU.mult, ("g1", "yv"), ("g1",))
                        ph.act(b_, a_, AF.Sigmoid, ("g1",), ("g2",), scale=1.5957691216057308)
                        ph.tt(gss[:, c, t0:t0 + n], y_, b_, ALU.mult, ("yv", "g2"), ("gss%d_%d" % (c, bi),))
            ph.run()

        ph = Phase(ctx)
        tb_ = Bump(ar, e_mark / 256.0, 207)
        s0r = tb_([128, 16, NS]); s0i = tb_([128, 16, NS]); q1 = tb_([128, 16, NS]); q2 = tb_([128, 16, NS])
        Ssr = tb_([128, 16, NS], BF16); Ssi = tb_([128, 16, NS], BF16)
        yv = tb_([128, 4, NS]); g1 = tb_([128, 4, NS]); g2 = tb_([128, 4, NS])
        ph.dma("sp", s0r, st_re_d.rearrange("p (a b) -> p a b", a=16), (), ("s0r",), "f0")
        ph.dma("sp", s0i, st_im_d.rearrange("p (a b) -> p a b", a=16), (), ("s0i",), "f1")
        items = []
        for gp in range(16):
            c, rows = gp // 4, (gp % 4) * 32
            for ri_ in range(2):
                items.append((bank(0)[:, (gp * 2 + ri_) * NS:(gp * 2 + ri_ + 1) * NS], WB[:, ri_, gp, :], uT[:, c, T:NT], True, True))
        ph.mm(items, (), ("ps0",))
        Xv = bank(0)[:, 0:32 * NS].rearrange("p (g r b) -> p g r b", g=16, r=2)
        lbr = lbrP.broadcast_to([128, 16, NS]); lbi = lbiP.broadcast_to([128, 16, NS])
        ph.tt(q1, s0i, lbi, ALU.mult, ("s0i",), ("q1",))
        ph.tt(q2, s0r, lbr, ALU.mult, ("s0r",), ("q2",))
        ph.tt(q2, q2, q1, ALU.subtract, ("q1", "q2"), ("q2",))
        ph.tt(sout[0][:, :, 1:1 + NS], q2, Xv[:, :, 0, :], ALU.add, ("q2", "ps0"), ("so_re",))
        ph.cp(Ssr, sout[0][:, :, 1:1 + NS], ("so_re",), ("Ssr",))
        ph.tt(q1, s0r, lbi, ALU.mult, ("s0r", "q2"), ("q1",))
        ph.tt(q2, s0i, lbr, ALU.mult, ("s0i", "so_re"), ("q2",))
        ph.tt(q2, q2, q1, ALU.add, ("q1", "q2"), ("q2",))
        ph.tt(sout[1][:, :, 1:1 + NS], q2, Xv[:, :, 1, :], ALU.add, ("q2", "ps0"), ("so_im",))
        ph.cp(Ssi, sout[1][:, :, 1:1 + NS], ("so_im",), ("Ssi",))
        ph.dma("sp", sre_d.rearrange("p (a b) -> p a b", a=16), sout[0], ("so_re",), ("sre_d",), "f2")
        ph.dma("sp", sim_d.rearrange("p (a b) -> p a b", a=16), sout[1], ("so_im",), ("sim_d",), "f3")
        for c in range(4):
            items = []
            for gl in range(4):
                gp = 4 * c + gl
                items.append((bank(1)[:, c * NS:(c + 1) * NS], CTr[:, gp, :], Ssr[:, gp, :], gl == 0, False))
                items.append((bank(1)[:, c * NS:(c + 1) * NS], CTi[:, gp, :], Ssi[:, gp, :], False, gl == 3))
            ph.mm(items, ("Ssr", "Ssi"), ("ps1",))
        for c in range(4):
            ph.stt(yv[:, c, :], uT[:, c, T:NT], dP[:, c:c + 1], bank(1)[:, c * NS:(c + 1) * NS], ALU.mult, ALU.add, ("ps1",), ("yv%d" % c,))
        ally = tuple("yv%d" % c for c in range(4))
        ph.tt(g1, yv, yv, ALU.mult, ally, ("g1",))
        ph.ts(g1, g1, 0.044715, ALU.mult, ("g1",), ("g1",), s2=1.0, op1=ALU.add)
        ph.tt(g1, g1, yv, ALU.mult, ("g1",) + ally, ("g1",))
        ph.act(g2, g1, AF.Sigmoid, ("g1",), ("g2",), scale=1.5957691216057308)
        ph.tt(gss[:, :, T:NT], yv, g2, ALU.mult, ally + ("g2",), ("gss_s",))
        ph.run()

        ph = Phase(ctx)
        tb_ = Bump(ar, 92.5, 207)
        wgl = tb_([128, 4, 1024], BF16)
        wst = [tb_([128, 1024]), tb_([128, 1024])]
        sg = [tb_([128, 512]), tb_([128, 512])]
        for kc in range(4):
            s = kc % 2
            ph.dma("sp", wst[s], w_glu_d[kc * 128:(kc + 1) * 128, :], (), ("wst%d" % s,), "g%d" % s)
            ph.cp(wgl[:, kc, :], wst[s], ("wst%d" % s,), ("wgl",), eng="pool")
        it = 0
        for oc in range(4):
            for bi, (t0, n) in enumerate(TB):
                b = 2 * (it % 2); it += 1
                ph.mm([(bank(b, n), wgl[:, kc, oc * 128:(oc + 1) * 128], gss[:, kc, t0:t0 + n], kc == 0, kc == 3) for kc in range(4)]
                      + [(bank(b + 1, n), wgl[:, kc, 512 + oc * 128:512 + (oc + 1) * 128], gss[:, kc, t0:t0 + n], kc == 0, kc == 3) for kc in range(4)],
                      ("wgl",), ("ps%d" % b, "ps%d" % (b + 1)))
                s_ = sg[it % 2][:, 0:n]
                ph.act(s_, bank(b + 1, n), AF.Sigmoid, ("ps%d" % (b + 1),), ("sg%d" % (it % 2),), bias=bglu[:, 4 + oc:5 + oc])
                ph.stt(soT[:, oc, t0:t0 + n], bank(b, n), bglu[:, oc:oc + 1], s_, ALU.add, ALU.mult, ("ps%d" % b, "sg%d" % (it % 2)), ("soT",))
        ph.run()

        def layer_norm(ph, src, srcn, n, gam, bet, out_tile, outn, tmp, tmpn, stat):
            st6, mv, rs_, nmr = stat
            for j in range(2):
                ph.add("dve", (lambda e, o=st6[0:n, j, :], i=src[j]: e.bn_stats(o, i)), (srcn[j],), ("st6",))
            ph.add("dve", (lambda e, o=mv[0:n, :], i=st6[0:n, :, :].rearrange("p a b -> p (a b)"): e.bn_aggr(o, i)), ("st6",), ("mv",))
            ph.ts(rs_[0:n, :], mv[0:n, 1:2], LN_EPS, ALU.add, ("mv",), ("rs",))
            ph.act(rs_[0:n, :], rs_[0:n, :], AF.Ln, ("rs",), ("rs",))
            ph.act(rs_[0:n, :], rs_[0:n, :], AF.Exp, ("rs",), ("rs",), scale=-0.5)
            ph.stt(nmr[0:n, :], mv[0:n, 0:1], -1.0, rs_[0:n, :], ALU.mult, ALU.mult, ("mv", "rs"), ("nmr",))
            for j in range(2):
                ph.act(tmp[0:n, j * 512:(j + 1) * 512], src[j], AF.Identity, (srcn[j], "rs", "nmr"), (tmpn[j],),
                       bias=nmr[0:n, :], scale=rs_[0:n, :])
            ph.tt(tmp[0:n, :], tmp[0:n, :], gam[0:n, :], ALU.mult, tuple(tmpn) + ("lng",), tuple(tmpn))
            ph.tt(out_tile[0:n, :], tmp[0:n, :], bet[0:n, :], ALU.add, tuple(tmpn) + ("lng",), (outn,))

        ph = Phase(ctx)
        tb_ = Bump(ar, 150.5, 207)
        wob = tb_([128, 8, 1024], BF16)
        wst = [tb_([128, 1024]), tb_([128, 1024])]
        lng = tb_([128, 1024]); lnb = tb_([128, 1024])
        xt = [tb_([128, 1024]), tb_([128, 1024])]
        tl = tb_([128, 1024]); ht = tb_([128, 1024])
        wrt = tb_([128, 8, 32]); brt = tb_([128, 32])
        st6 = tb_([128, 2, 6]); mv = tb_([128, 2]); rs_ = tb_([128, 1]); nmr = tb_([128, 1])
        lg = tb_([128, 32]); m8 = tb_([128, 8]); sel = tb_([128, 32]); nm_ = tb_([128, 1]); ex = tb_([128, 32])
        den = tb_([128, 1]); gt = tb_([128, 32])
        for kc in range(8):
            s = kc % 2
            ph.dma("sp", wst[s], w_out_d[kc * 128:(kc + 1) * 128, :], (), ("wst%d" % s,), "h%d" % s)
            ph.cp(wob[:, kc, :], wst[s], ("wst%d" % s,), ("wob",), eng="pool")
        ph.dma("act", lng, ln_d[:, 0:D].broadcast_to([128, D]), (), ("lng",), "h2")
        ph.dma("act", lnb, ln_d[:, D:2 * D].broadcast_to([128, D]), (), ("lng",), "h3")
        ph.dma("act", wrt[:, :, 0:NE], wr_d[:, 0:NE].rearrange("(c p) e -> p c e", p=128), (), ("wrt",), "h4")
        ph.dma("act", brt[:, 0:NE], br_d.broadcast_to([128, NE]), (), ("brt",), "h5")
        for i, (t0, n) in enumerate(TL):
            s = i % 2
            ph.dma("sp", xt[s][0:n, :], x_d[t0:t0 + n, :], (), ("xt%d" % s,), "x%d" % s)
            for j in range(2):
                ph.mm([(ps_t[0:n, j * 512:(j + 1) * 512], (soT[:, kc, t0:t0 + n] if kc < 4 else aoT[:, kc - 4, t0:t0 + n]),
                        wob[:, kc, j * 512:(j + 1) * 512], kc == 0, kc == 7) for kc in range(8)], ("wob",), ("ps%d" % j,))
                ph.stt(tl[0:n, j * 512:(j + 1) * 512], xt[s][0:n, j * 512:(j + 1) * 512], ALPHA, ps_t[0:n, j * 512:(j + 1) * 512],
                       ALU.mult, ALU.add, ("xt%d" % s, "ps%d" % j), ("tl%d" % j,))
            layer_norm(ph, [tl[0:n, 0:512], tl[0:n, 512:1024]], ("tl0", "tl1"), n, lng, lnb, ht, "ht", tl, ("tl0", "tl1"),
                       (st6, mv, rs_, nmr))
            for j in range(2):
                ph.tr([(ps_t[:, (2 + j) * 512 + cc * 128:(2 + j) * 512 + cc * 128 + n], ht[0:n, (4 * j + cc) * 128:(4 * j + cc + 1) * 128],
                        ident[0:n, 0:n]) for cc in range(4)], ("ht", "ident"), ("ps%d" % (2 + j),))
                src = bank(2 + j).rearrange("p (c t) -> p c t", c=4)[:, :, 0:n]
                ph.act(fT[:, 4 * j:4 * j + 4, t0:t0 + n], src, AF.Copy, ("ps%d" % (2 + j),), ("fT%d" % i,), scale=ALPHA)
                ph.cp(hTb[:, 4 * j:4 * j + 4, t0:t0 + n], src, ("ps%d" % (2 + j),), ("hTb%d" % i,))
            ph.mm([(ps_t[0:n, 4 * 512:4 * 512 + NE], fT[:, c, t0:t0 + n], wrt[:, c, 0:NE], c == 0, c == 7) for c in range(8)],
                  ("fT%d" % i, "wrt"), ("ps4",))
            ph.stt(lg[0:n, 0:NE], ps_t[0:n, 4 * 512:4 * 512 + NE], 1.0 / ALPHA, brt[0:n, 0:NE], ALU.mult, ALU.add, ("ps4", "brt"), ("lg",))
            ph.add("dve", (lambda e, o=m8[0:n, :], i_=lg[0:n, 0:NE]: e.max(o, i_)), ("lg",), ("m8",))
            ph.ts(sel[0:n, 0:NE], lg[0:n, 0:NE], m8[0:n, cfg.TOPK - 1:cfg.TOPK], ALU.is_ge, ("lg", "m8"), ("sel",))
            ph.ts(nm_[0:n, :], m8[0:n, 0:1], -1.0, ALU.mult, ("m8",), ("nm",))
            ph.act(ex[0:n, 0:NE], lg[0:n, 0:NE], AF.Exp, ("lg", "nm"), ("ex",), bias=nm_[0:n, :])
            ph.tt(ex[0:n, 0:NE], ex[0:n, 0:NE], sel[0:n, 0:NE], ALU.mult, ("ex", "sel"), ("ex2",))
            ph.red(den[0:n, :], ex[0:n, 0:NE], ALU.add, ("ex2",), ("den",))
            ph.recip(den[0:n, :], den[0:n, :], ("den",), ("den2",))
            ph.ts(gt[0:n, 0:NE], ex[0:n, 0:NE], den[0:n, :], ALU.mult, ("ex2", "den2"), ("gt",))
            ph.tr([(ps_t[0:NE, 5 * 512:5 * 512 + n], gt[0:n, 0:NE], ident[0:n, 0:n])], ("gt", "ident"), ("ps5",))
            ph.cp(gatesT[0:NE, t0:t0 + n], ps_t[0:NE, 5 * 512:5 * 512 + n], ("ps5",), ("gatesT",), eng="act")
        ph.run()

        ph = Phase(ctx)
        tb_ = Bump(ar, 159, 207)
        bdn = tb_([128, D])
        ph.dma("act", bdn[0:NE, :], bdn_d, (), ("bdn",), "m1")
        for dc in range(8):
            for bi, (t0, n) in enumerate(TB):
                b = (dc * len(TB) + bi) % 4
                ph.mm([(bank(b, n), bdn[0:NE, dc * 128:(dc + 1) * 128], gatesT[0:NE, t0:t0 + n], True, True)], ("bdn",), ("ps%d" % b,))
                ph.tt(fT[:, dc, t0:t0 + n], fT[:, dc, t0:t0 + n], bank(b, n), ALU.add, ("ps%d" % b,), ("fT%d_%d" % (dc, bi),))
        ph.run()

        ph = Phase(ctx)
        GeS = ar.at(int(150.5 * KB), [128, NT])
        tb_ = Bump(ar, 159, 207)
        stg = [tb_([128, 2048]), tb_([128, 2048])]
        wpb = [tb_([128, 8, 256], BF16) for _ in range(2)]
        Gt = [tb_([128, 512]) for _ in range(3)]; Sg = [tb_([128, 512]) for _ in range(3)]; Lt = [tb_([128, 512]) for _ in range(3)]
        bguT = tb_([128, NE, 16]); bl1 = tb_([128, NE, 8]); selt = [tb_([128, 128]), tb_([128, 128])]
        ph.dma("act", bguT, bgu_d.rearrange("p (e c) -> p e c", e=NE), (), ("bguT",), "m0")
        ph.ts(bl1, bguT[:, :, 8:16], 1.0, ALU.add, ("bguT",), ("bl1",))
        pieces = [(e_, kind, j) for e_ in range(NE) for kind in ("gu", "dn") for j in range(8)]
        cnt = dict(it=0, un=0)

        def emit_load(i):
            e_, kind, j = pieces[i]
            s2_ = i % 2
            row0 = (e_ * 8 + j) * 128
            if kind == "gu":
                ph.dma("sp", stg[s2_], wgu_d[row0:row0 + 128, :], (), ("stg%d" % s2_,), "stg%d" % s2_)
                ph.cp(wpb[s2_].rearrange("p a b -> p (a b)"), stg[s2_], ("stg%d" % s2_,), ("wpb%d" % s2_,), eng="act")
            else:
                ph.dma("sp", stg[s2_][:, 0:1024], wdn_d[row0:row0 + 128, :], (), ("stg%d" % s2_,), "stg%d" % s2_)
                ph.cp(wpb[s2_].rearrange("p a b -> p (a b)")[:, 0:1024], stg[s2_][:, 0:1024], ("stg%d" % s2_,), ("wpb%d" % s2_,), eng="act")

        def emit_compute(i):
            e_, kind, j = pieces[i]
            s2_ = i % 2
            if kind == "gu" and j == 0:
                st_ = selt[e_ % 2]; sn_ = "selt%d" % (e_ % 2)
                ph.cp(st_[0:NE, :], ident[0:NE, e_:e_ + 1].broadcast_to([NE, 128]), (), (sn_,), eng="pool")
                for bi, (t0, n) in enumerate(TB):
                    b = 6 + bi % 2
                    ph.mm([(bank(b, n), st_[0:NE, :], gatesT[0:NE, t0:t0 + n], True, True)], (sn_,), ("ps%d" % b,))
                    ph.cp(GeS[:, t0:t0 + n], bank(b, n), ("ps%d" % b,), ("GeS%d" % bi,), eng="act")
            if kind == "gu":
                fc = j
                for bi, (t0, n) in enumerate(TB):
                    b = 2 * (cnt["it"] % 3); cnt["it"] += 1
                    k3 = cnt["un"] % 3; cnt["un"] += 1
                    ph.mm([(bank(b, n), wpb[s2_][:, kc, 0:128], hTb[:, kc, t0:t0 + n], kc == 0, kc == 7) for kc in range(8)]
                          + [(bank(b + 1, n), wpb[s2_][:, kc, 128:256], hTb[:, kc, t0:t0 + n], kc == 0, kc == 7) for kc in range(8)],
                          ("wpb%d" % s2_,), ("ps%d" % b, "ps%d" % (b + 1)))
                    G_ = Gt[k3][:, 0:n]; S_ = Sg[k3][:, 0:n]; L_ = Lt[k3][:, 0:n]
                    gn, sn, ln_ = "G%d" % k3, "S%d" % k3, "L%d" % k3
                    ph.ts(G_, bank(b, n), bguT[:, e_, fc:fc + 1], ALU.add, ("ps%d" % b, "bguT"), (gn,), s2=SW_LIM, op1=ALU.min)
                    ph.act(L_, bank(b + 1, n), AF.Identity, ("ps%d" % (b + 1), "bl1"), (ln_,), bias=bl1[:, e_, fc:fc + 1])
                    ph.act(S_, G_, AF.Sigmoid, (gn,), (sn,), scale=SW_ALPHA)
                    ph.ts(L_, L_, 1.0 - SW_LIM, ALU.max, (ln_,), (ln_,), s2=SW_LIM + 1.0, op1=ALU.min)
                    ph.tt(L_, L_, G_, ALU.mult, (ln_, gn), (ln_,))
                    ph.tt(S_, S_, L_, ALU.mult, (sn, ln_), (sn,), eng="pool")
                    ph.tt(actT[:, fc, t0:t0 + n], S_, GeS[:, t0:t0 + n], ALU.mult, (sn, "GeS%d" % bi), ("actT%d_%d" % (fc, bi),), eng="pool")
            else:
                dc = j
                wd3 = wpb[s2_].rearrange("p a b -> p (a b)")[:, 0:1024].rearrange("p (a b) -> p a b", a=8)
                for bi, (t0, n) in enumerate(TB):
                    b = 6 + (cnt["it"] % 2); cnt["it"] += 1
                    ph.mm([(bank(b, n), wd3[:, f, :], actT[:, f, t0:t0 + n], f == 0, f == 7) for f in range(8)],
                          ("wpb%d" % s2_,) + tuple("actT%d_%d" % (f, bi) for f in range(8)), ("ps%d" % b,))
                    ph.tt(fT[:, dc, t0:t0 + n], fT[:, dc, t0:t0 + n], bank(b, n), ALU.add, ("ps%d" % b,), ("fT%d_%d" % (dc, bi),))

        emit_load(0)
        for i in range(len(pieces)):
            if i + 1 < len(pieces):
                emit_load(i + 1)
            emit_compute(i)
        ph.run()

        ph = Phase(ctx)
        tb_ = Bump(ar, 150.5, 207)
        lng = tb_([128, 1024]); lnb = tb_([128, 1024])
        yt = [tb_([128, 1024]), tb_([128, 1024])]; tmp = tb_([128, 1024])
        st6 = tb_([128, 2, 6]); mv = tb_([128, 2]); rs_ = tb_([128, 1]); nmr = tb_([128, 1])
        ph.dma("sp", lng, ln_d[:, 2 * D:3 * D].broadcast_to([128, D]), (), ("lng",), "n0")
        ph.dma("sp", lnb, ln_d[:, 3 * D:4 * D].broadcast_to([128, D]), (), ("lng",), "n1")
        for i, (t0, n) in enumerate(TL):
            s = i % 2
            bb = 2 * (i % 2)
            for j in range(2):
                ph.tr([(ps_t[0:n, (bb + j) * 512 + cc * 128:(bb + j) * 512 + (cc + 1) * 128], fT[:, 4 * j + cc, t0:t0 + n], ident)
                       for cc in range(4)], ("ident",), ("ps%d" % (bb + j),))
            layer_norm(ph, [ps_t[0:n, (bb + j) * 512:(bb + j + 1) * 512] for j in range(2)], ("ps%d" % bb, "ps%d" % (bb + 1)), n, lng, lnb,
                       yt[s], "yt%d" % s, tmp, ("tmp0", "tmp1"), (st6, mv, rs_, nmr))
            ph.add("pool", (lambda e, o=y_d[t0:t0 + n, :], i_=yt[s][0:n, :]: e.dma_start(out=o, in_=i_)), ("yt%d" % s,), ("yd%d" % i,), dma="y%d" % s)
        ph.run()
    return nc


def _consts(cfg, past_len):
    T, NS, NT = cfg.T, cfg.NS, cfg.NT
    half = 32
    inv = (np.float32(10000.0) ** (-np.arange(half, dtype=np.float32) / np.float32(half))).astype(np.float32)
    pos = np.concatenate([np.arange(T, dtype=np.float32), np.full((NS,), past_len, np.float32)])
    ang = (pos[:, None] * inv[None, :]).astype(np.float32)
    ropeC = np.cos(ang.astype(np.float64)).astype(np.float32)
    ropeS = np.sin(ang.astype(np.float64)).astype(np.float32)
    ident = np.eye(128, dtype=np.float32)
    tri = np.where(np.arange(128)[None, :] <= np.arange(128)[:, None], 0.0, -30000.0).astype(np.float32)
    selB = np.zeros((NS, NS, 128), np.float32)
    for b in range(NS):
        selB[b, b, :] = 1.0
    pidx = np.arange(128, dtype=np.float32)[:, None].copy()
    negm = np.full((128, 1), -30000.0, np.float32); negm[0, 0] = 0.0
    return dict(ropeC=ropeC, ropeS=ropeS, ident=ident, tri=tri, selB=selB.reshape(NS, NS * 128), pidx=pidx, negm=negm)


def _shared(cfg, I):
    NE = cfg.NE
    f = lambda a: np.ascontiguousarray(a, dtype=np.float32)
    a_re, a_im, ldt = I["ssm_a_re"][0], I["ssm_a_im"][0], I["ssm_log_dt"][0]
    toP = lambda a: a.reshape(16, 2, 64).transpose(1, 2, 0).reshape(128, 16)
    aP = np.concatenate([toP(a_re), toP(a_im), toP(np.repeat(ldt[:, None], 64, 1))], axis=1)

    def bP(b):
        out = np.zeros((2, 64, 16, 4, 2, 16), np.float32)
        v = b.reshape(16, 2, 64, 16)
        for gp in range(16):
            for g2 in range(2):
                out[g2, :, gp, gp % 4, g2, :] = v[gp, g2]
        return out.reshape(128, 2048)
    bPc = np.concatenate([bP(I["ssm_b_re"][0]), bP(I["ssm_b_im"][0])], axis=1)

    def cT(cm):
        out = np.zeros((2, 64, 16, 4, 2, 16), np.float32)
        v = cm.reshape(16, 2, 16, 64)
        for gp in range(16):
            for g2 in range(2):
                out[g2, :, gp, gp % 4, g2, :] = v[gp, g2].T
        return out.reshape(128, 2048)
    cTc = np.concatenate([cT(I["ssm_c_re"][0]), cT(I["ssm_c_im"][0])], axis=1)
    dP = I["ssm_d"][0].reshape(4, 128).T
    bglu = I["b_glu"][0].reshape(8, 128).T
    lam4 = np.concatenate([I["lambda_q1"][0], I["lambda_k1"][0], I["lambda_q2"][0], I["lambda_k2"][0]])[None, :]
    ln = np.concatenate([I["ln1_g"][0], I["ln1_b"][0], I["ln2_g"][0], I["ln2_b"][0]])[None, :]
    wgu = I["w_gate_up"][0]
    wg = wgu[:, :, :1024].reshape(NE, 8, 128, 8, 128)
    wl = wgu[:, :, 1024:].reshape(NE, 8, 128, 8, 128)
    wgu_t = np.empty((NE, 8, 128, 8, 256), np.float32)
    wgu_t[..., :128] = wg.transpose(0, 3, 2, 1, 4)
    wgu_t[..., 128:] = wl.transpose(0, 3, 2, 1, 4)
    wdn = I["w_down"][0].reshape(NE, 8, 128, 8, 128)
    wdn_t = np.ascontiguousarray(wdn.transpose(0, 3, 2, 1, 4))
    bgu = I["b_gate_up"][0].reshape(NE, 16, 128).transpose(2, 0, 1).reshape(128, NE * 16)
    wr = I["w_router"][0]
    if wr.shape[1] < 32:
        wr = np.concatenate([wr, np.zeros((D, 32 - wr.shape[1]), np.float32)], axis=1)
    return dict(
        cache_k=f(I["cache_k"][0].reshape(-1, 512)), cache_v=f(I["cache_v"][0].reshape(-1, 512)),
        w_in=f(I["w_in"][0]), w_out=f(I["w_out"][0]), aP=f(aP), bP=f(bPc), cT=f(cTc), dP=f(dP),
        w_glu=f(I["w_glu"][0]), bglu=f(bglu), lam4=f(lam4), gsub=f(I["subln_g"][0][None, :]), gcol=f(I["subln_g"][0][:, None]),
        ln=f(ln), wr=f(wr), br=f(I["b_router"][0][None, :]), wgu=f(wgu_t.reshape(NE * 8 * 128, 2048)), bgu=f(bgu),
        wdn=f(wdn_t.reshape(NE * 8 * 128, 1024)), bdn=f(I["b_down"][0]))


def run(cfg, I, trace=False, stop_after=None, max_ops=None):
    T, NS, NPG = cfg.T, cfg.NS, cfg.NPG
    nc = build(cfg, stop_after, max_ops)
    shared = _shared(cfg, I)
    shared.update(_consts(cfg, NPG * 128))
    in_maps = []
    for c in range(NCORES):
        x = np.concatenate([I["x_prompt"][c], I["x_sample"][c * NS:(c + 1) * NS, 0]], axis=0).astype(np.float32)
        m = dict(shared)
        m["x"] = np.ascontiguousarray(x)
        m["xT"] = np.ascontiguousarray(x.T)
        m["pt"] = np.ascontiguousarray(I["page_table"][c * NS:(c + 1) * NS].reshape(1, NS * NPG).astype(np.int32))
        for k_, nm in (("state_ssm_re", "st_re"), ("state_ssm_im", "st_im")):
            s = I[k_][0, c * NS:(c + 1) * NS].reshape(NS, 16, 2, 64)
            m[nm] = np.ascontiguousarray(s.transpose(2, 3, 1, 0).reshape(128, 16 * NS).astype(np.float32))
        in_maps.append(m)
    res = run_bass_kernel_spmd(nc, in_maps, core_ids=list(range(NCORES)), trace=trace) if trace else \
        run_bass_kernel_spmd(nc, in_maps, core_ids=list(range(NCORES)))
    R = res.results
    B = NCORES
    y = np.stack([r["y"] for r in R])
    ko = np.stack([r["ko"] for r in R]); vo = np.stack([r["vo"] for r in R])
    sre = np.stack([r["sre"].reshape(2, 64, 16, 1 + NS) for r in R])
    sim = np.stack([r["sim"].reshape(2, 64, 16, 1 + NS) for r in R])

    def st_p(s):
        return np.ascontiguousarray(s[..., 0].transpose(0, 3, 1, 2).reshape(B, 32, 64))[None]

    def st_s(s):
        v = s[..., 1:].transpose(0, 4, 3, 1, 2)
        return np.ascontiguousarray(v.reshape(B * NS, 32, 64))[None]
    outs = (
        np.ascontiguousarray(y[:, :T]), np.ascontiguousarray(y[:, T:].reshape(B * NS, 1, D)),
        np.ascontiguousarray(ko[:, :T].reshape(B, T, 4, 128))[None], np.ascontiguousarray(vo[:, :T].reshape(B, T, 4, 128))[None],
        st_p(sre), st_p(sim),
        np.ascontiguousarray(ko[:, T:].reshape(B * NS, 1, 4, 128))[None], np.ascontiguousarray(vo[:, T:].reshape(B * NS, 1, 4, 128))[None],
        st_s(sre), st_s(sim))
    return tuple(o.astype(np.float32) for o in outs), res


def kernel(**inputs):
    I = {k: np.asarray(v) for k, v in inputs.items()}
    outs, _ = run(FULL, I)
    return outs
```

```python
import math
from contextlib import ExitStack

import numpy as np
import concourse.bass as bass
import concourse.mybir as mybir
from concourse.bass_utils import run_bass_kernel_spmd

F32 = mybir.dt.float32
BF16 = mybir.dt.bfloat16
I32 = mybir.dt.int32
AF = mybir.ActivationFunctionType
ALU = mybir.AluOpType
AX = mybir.AxisListType

D = 1024
NCORES = 8
LN_EPS = 1e-5
RMS_EPS = 1e-5
LAM_INIT = 0.8 - 0.6 * math.exp(-0.3 * 0)
ALPHA = (2 * 1) ** 0.25
SW_ALPHA = 1.702
SW_LIM = 7.0
ARENA_W = 52992


class Cfg:
    def __init__(self, T=2048, NS=16, NPG=16, NPHYS=2560, NE=32, TOPK=4):
        self.T, self.NS, self.NPG, self.NPHYS, self.NE, self.TOPK = T, NS, NPG, NPHYS, NE, TOPK
        self.NT = T + NS
        self.NTP = T // 128
        self.TB = [(i * 512, min(512, T - i * 512)) for i in range((T + 511) // 512)] + [(T, NS)]
        self.TL = [(i * 128, 128) for i in range(self.NTP)] + [(T, NS)]


FULL = Cfg()

ENGS = ("pe", "act", "dve", "pool", "sp")


class Ctx:
    def __init__(self, nc, stack):
        self.nc, self.stack = nc, stack
        self.esem = {e: stack.enter_context(nc.semaphore("es_" + e)) for e in ("pe", "act", "dve", "pool")}
        self.ecnt = {e: 0 for e in self.esem}
        self.dsem, self.dcnt = {}, {}
        self.known = {e: {} for e in ENGS}
        self.phase_no = 0
        self.stop_after = None
        self.max_ops = None

    def dma_slot(self, slot):
        if slot not in self.dsem:
            self.dsem[slot] = self.stack.enter_context(self.nc.semaphore("ds%d" % len(self.dsem)))
            self.dcnt[slot] = 0
            assert len(self.dsem) < 150, "too many dma semaphores"
        return self.dsem[slot]


class Phase:
    def __init__(self, ctx):
        self.ctx, self.ops = ctx, []

    def add(self, eng, fn, r=(), w=(), dma=None):
        self.ops.append(dict(eng=eng, fn=fn, r=tuple(r), w=tuple(w), dma=dma, dep=False))

    def mm(self, items, r, w):
        def fn(e, items=items):
            return [e.matmul(o, l, rh, start=s, stop=t) for (o, l, rh, s, t) in items]
        self.add("pe", fn, r, w)

    def tr(self, items, r, w):
        def fn(e, items=items):
            return [e.transpose(o, i, idn) for (o, i, idn) in items]
        self.add("pe", fn, r, w)

    def act(self, out, in_, func, r, w, bias=None, scale=None, accum=None):
        def fn(e):
            kw = {}
            if bias is not None:
                kw["bias"] = bias
            if scale is not None:
                kw["scale"] = scale
            if accum is not None:
                kw["accum_out"] = accum
            return e.activation(out, in_, func, **kw)
        self.add("act", fn, r, w)

    def ts(self, out, in0, s1, op0, r, w, s2=None, op1=None, eng="dve"):
        def fn(e):
            if op1 is None:
                return e.tensor_scalar(out, in0, s1, None, op0)
            return e.tensor_scalar(out, in0, s1, s2, op0, op1)
        self.add(eng, fn, r, w)

    def stt(self, out, in0, scalar, in1, op0, op1, r, w, accum=None):
        if accum is None:
            self.add("dve", lambda e: e.scalar_tensor_tensor(out, in0, scalar, in1, op0, op1), r, w)
        else:
            self.add("dve", lambda e: e.scalar_tensor_tensor(out, in0, scalar, in1, op0, op1, accum_out=accum), r, w)

    def tt(self, out, in0, in1, op, r, w, eng="dve"):
        self.add(eng, lambda e: e.tensor_tensor(out, in0, in1, op), r, w)

    def cp(self, out, in_, r, w, eng="dve"):
        if eng == "act":
            self.add("act", lambda e: e.copy(out, in_), r, w)
        else:
            self.add(eng, lambda e: e.tensor_copy(out, in_), r, w)

    def red(self, out, in_, op, r, w, axis=None):
        ax = AX.X if axis is None else axis
        self.add("dve", lambda e: e.tensor_reduce(out, in_, ax, op), r, w)

    def memset(self, out, val, w, eng="dve"):
        self.add(eng, lambda e: e.memset(out, val), (), w)

    def recip(self, out, in_, r, w):
        self.add("dve", lambda e: e.reciprocal(out, in_), r, w)

    def dma(self, q, out, in_, r, w, slot):
        self.add(q, lambda e: e.dma_start(out=out, in_=in_), r, w, dma=slot)

    def run(self):
        ops, ctx, nc = self.ops, self.ctx, self.ctx.nc
        ctx.phase_no += 1
        if ctx.stop_after is not None and ctx.phase_no > ctx.stop_after:
            return
        if ctx.stop_after is not None and ctx.phase_no == ctx.stop_after and ctx.max_ops is not None:
            print("[bisect] phase %d has %d ops, keeping %d; last kept: %s" % (
                ctx.phase_no, len(ops), ctx.max_ops, [(o["eng"], o["r"], o["w"], o["dma"]) for o in ops[max(0, ctx.max_ops - 2):ctx.max_ops]]))
            del ops[ctx.max_ops:]
        lastw, readers = {}, {}
        def _excl(b):
            return len(b) == 3 and b[:2] == "ps" and b[2].isdigit()
        for o in ops:
            xr = tuple(b for b in o["r"] if _excl(b))
            if xr:
                o["w"] = tuple(o["w"]) + xr
                o["r"] = tuple(b for b in o["r"] if not _excl(b))
        for i, o in enumerate(ops):
            deps = set()
            for b in o["r"]:
                if b in lastw:
                    deps.add(lastw[b])
            for b in o["w"]:
                if b in lastw:
                    deps.add(lastw[b])
                deps |= readers.get(b, set())
            deps.discard(i)
            o["deps"] = deps
            for d in deps:
                ops[d]["dep"] = True
            for b in o["r"]:
                readers.setdefault(b, set()).add(i)
            for b in o["w"]:
                lastw[b] = i
                readers[b] = set()
        touched = []
        for o in ops:
            if o["dma"] is not None:
                sem = ctx.dma_slot(o["dma"])
                ctx.dcnt[o["dma"]] += 16
                o["tok"] = ("d:" + o["dma"], sem, ctx.dcnt[o["dma"]])
                if o["dma"] not in touched:
                    touched.append(o["dma"])
            elif o["dep"]:
                e = o["eng"]
                ctx.ecnt[e] += 1
                o["tok"] = ("e:" + e, ctx.esem[e], ctx.ecnt[e])
            else:
                o["tok"] = None
        per = {e: [o for o in ops if o["eng"] == e] for e in ENGS}

        def mk(ename):
            def body(eng):
                kn = ctx.known[ename]
                for o in per[ename]:
                    for d in sorted(o["deps"]):
                        key, sem, val = ops[d]["tok"]
                        if kn.get(key, 0) < val:
                            eng.wait_ge(sem, val)
                            kn[key] = val
                    res = o["fn"](eng)
                    last = res[-1] if isinstance(res, (list, tuple)) else res
                    if o["tok"] is not None:
                        last.then_inc(o["tok"][1], 16 if o["dma"] is not None else 1)
                if ename == "sp":
                    for slot in touched:
                        key, val = "d:" + slot, ctx.dcnt[slot]
                        if kn.get(key, 0) < val:
                            eng.wait_ge(ctx.dsem[slot], val)
                            kn[key] = val
            return body

        with nc.Block() as blk:
            blk.tensor(mk("pe"))
            blk.scalar(mk("act"))
            blk.vector(mk("dve"))
            blk.gpsimd(mk("pool"))
            blk.sync(mk("sp"))


class Arena:
    def __init__(self, ap_all):
        self.a = ap_all

    def at(self, off_w, shape, dt=F32, parts=128):
        n = int(np.prod(shape[1:]))
        words = n if dt in (F32, I32) else (n + 1) // 2
        assert off_w + words <= ARENA_W, ("arena overflow", off_w, words)
        v = self.a[0:parts, off_w:off_w + words]
        if dt not in (F32,):
            v = v.bitcast(dt)
            if dt == BF16 and n % 2:
                v = v[:, 0:n]
        if len(shape) == 3:
            v = v.rearrange("p (a b) -> p a b", a=shape[1])
        elif len(shape) == 4:
            v = v.rearrange("p (a b c) -> p a b c", a=shape[1], b=shape[2])
        return v


class Bump:
    def __init__(self, arena, lo_kb, hi_kb):
        self.ar, self.p, self.hi = arena, int(lo_kb * 256), int(hi_kb * 256)

    def __call__(self, shape, dt=F32, parts=128):
        n = int(np.prod(shape[1:]))
        words = n if dt in (F32, I32) else (n + 1) // 2
        words = (words + 7) // 8 * 8
        v = self.ar.at(self.p, shape, dt, parts)
        self.p += words
        assert self.p <= self.hi, ("bump overflow", self.p, self.hi)
        return v


def build(cfg, stop_after=None, max_ops=None):
    T, NS, NT, NTP, NPG, NE = cfg.T, cfg.NS, cfg.NT, cfg.NTP, cfg.NPG, cfg.NE
    TB, TL = cfg.TB, cfg.TL
    TT = len(TL)
    NSL = NPG + 1
    nc = bass.Bass("TRN2", target_bir_lowering=False)

    def din(name, shape, dt=F32):
        return nc.dram_tensor(name, list(shape), dt, kind="ExternalInput").ap()

    def dout(name, shape, dt=F32):
        return nc.dram_tensor(name, list(shape), dt, kind="ExternalOutput").ap()

    xT_d = din("xT", [D, NT]); x_d = din("x", [NT, D])
    ck_d = din("cache_k", [cfg.NPHYS * 128, 512]); cv_d = din("cache_v", [cfg.NPHYS * 128, 512])
    pt_d = din("pt", [1, NS * NPG], I32)
    st_re_d = din("st_re", [128, 16 * NS]); st_im_d = din("st_im", [128, 16 * NS])
    w_in_d = din("w_in", [D, 2048]); w_out_d = din("w_out", [D, D])
    aP_d = din("aP", [128, 48])
    bP_d = din("bP", [128, 2 * 2048])
    cT_d = din("cT", [128, 2 * 2048])
    dP_d = din("dP", [128, 4])
    w_glu_d = din("w_glu", [512, 1024]); bglu_d = din("bglu", [128, 8])
    lam4_d = din("lam4", [1, 256])
    gsub_d = din("gsub", [1, 128]); gcol_d = din("gcol", [128, 1])
    ln_d = din("ln", [1, 4 * D])
    wr_d = din("wr", [D, 32 if NE <= 32 else NE]); br_d = din("br", [1, NE])
    wgu_d = din("wgu", [NE * 8 * 128, 2048]); bgu_d = din("bgu", [128, NE * 16])
    wdn_d = din("wdn", [NE * 8 * 128, 1024]); bdn_d = din("bdn", [NE, D])
    ropeC_d = din("ropeC", [NT, 32]); ropeS_d = din("ropeS", [NT, 32])
    ident_d = din("ident", [128, 128]); tri_d = din("tri", [128, 128])
    selB_d = din("selB", [NS, NS * 128])
    pidx_d = din("pidx", [128, 1]); negm_d = din("negm", [128, 1])

    y_d = dout("y", [NT, D]); ko_d = dout("ko", [NT, 512]); vo_d = dout("vo", [NT, 512])
    sre_d = dout("sre", [128, 16 * (1 + NS)]); sim_d = dout("sim", [128, 16 * (1 + NS)])

    stack = ExitStack()
    with stack:
        arena_t = stack.enter_context(nc.sbuf_tensor("arena", [128, ARENA_W], F32))
        ps_t = stack.enter_context(nc.psum_tensor("ps", [128, 4096], F32))
        ar = Arena(arena_t)
        ctx = Ctx(nc, stack)
        ctx.stop_after = stop_after
        ctx.max_ops = max_ops

        def bank(b, n=512):
            return ps_t[:, b * 512:b * 512 + n]

        def bank_bf(b):
            return ps_t[:, b * 512:(b + 1) * 512].bitcast(BF16)

        KB = 256
        cb = Bump(ar, 0, 10)
        ident = cb([128, 128]); identb = cb([128, 128], BF16); tri = cb([128, 128]); onesf = cb([128, 128])
        cosT = cb([128, TT, 32]); sinT = cb([128, TT, 32])
        gsub = cb([128, 128]); gcol = cb([128, 1]); lam_t = cb([128, 1]); nlam_t = cb([128, 1])
        pidx = cb([128, 1]); negm = cb([128, 1]); dP = cb([128, 4]); bglu = cb([128, 8])
        aoT = ar.at(10 * KB, [128, 4, NT], BF16)
        soT = ar.at(int(26.5 * KB), [128, 4, NT], BF16)
        actT = ar.at(10 * KB, [128, 8, NT], BF16)
        uT = ar.at(43 * KB, [128, 4, NT])
        gss = ar.at(76 * KB, [128, 4, NT], BF16)
        qs = ar.at(76 * KB, [128, 512])
        qT = ar.at(int(94.5 * KB), [128, 4, NT], BF16)
        kT = ar.at(int(94.5 * KB) + 2 * NT, [128, 4, NT], BF16)
        vbf = ar.at(int(127.5 * KB), [128, max(NTP, 1), 512], BF16)
        fT = ar.at(43 * KB, [128, 8, NT])
        hTb = ar.at(109 * KB, [128, 8, NT], BF16)
        gatesT = ar.at(142 * KB, [128, NT])

        ph = Phase(ctx)
        tb_ = Bump(ar, 150, 207)
        l4 = tb_([128, 256]); pr = tb_([128, 128]); sm = tb_([128, 2]); ee = tb_([128, 2])
        ph.dma("sp", ident, ident_d, (), ("ident",), "c0")
        ph.dma("sp", tri, tri_d, (), ("tri",), "c1")
        ph.dma("sp", gsub, gsub_d.broadcast_to([128, 128]), (), ("gsub",), "c2")
        ph.dma("sp", gcol, gcol_d, (), ("gcol",), "c3")
        ph.dma("sp", pidx, pidx_d, (), ("pidx",), "c4")
        ph.dma("sp", negm, negm_d, (), ("negm",), "c5")
        ph.dma("sp", dP, dP_d, (), ("dP",), "c6")
        ph.dma("sp", bglu, bglu_d, (), ("bglu",), "c7")
        ph.dma("sp", l4, lam4_d.broadcast_to([128, 256]), (), ("l4",), "c8")
        if NTP:
            ph.dma("sp", cosT[:, 0:NTP, :], ropeC_d[0:T, :].rearrange("(i p) f -> p i f", p=128), (), ("cosT",), "c9")
            ph.dma("sp", sinT[:, 0:NTP, :], ropeS_d[0:T, :].rearrange("(i p) f -> p i f", p=128), (), ("sinT",), "c10")
        ph.dma("sp", cosT[0:NS, NTP, :], ropeC_d[T:NT, :], (), ("cosTs",), "c11")
        ph.dma("sp", sinT[0:NS, NTP, :], ropeS_d[T:NT, :], (), ("sinTs",), "c12")
        ph.cp(identb, ident, ("ident",), ("identb",))
        ph.memset(onesf, 1.0, ("onesf",))
        ph.tt(pr.rearrange("p (a b) -> p a b", a=2), l4.rearrange("p (a two b) -> p a two b", a=2, two=2)[:, :, 0, :],
              l4.rearrange("p (a two b) -> p a two b", a=2, two=2)[:, :, 1, :], ALU.mult, ("l4",), ("pr",))
        ph.red(sm, pr.rearrange("p (a b) -> p a b", a=2), ALU.add, ("pr",), ("sm",))
        ph.act(ee, sm, AF.Exp, ("sm",), ("ee",))
        ph.tt(lam_t, ee[:, 0:1], ee[:, 1:2], ALU.subtract, ("ee",), ("lam0",))
        ph.ts(lam_t, lam_t, LAM_INIT, ALU.add, ("lam0",), ("lam",))
        ph.ts(nlam_t, lam_t, -1.0, ALU.mult, ("lam",), ("nlam",))
        ph.run()

        ph = Phase(ctx)
        xTb = ar.at(int(143.5 * KB), [128, 8, NT], BF16)
        sb = Bump(ar, 78, 94.5)
        xst = [sb([128, NT]), sb([128, NT])]
        wb_ = Bump(ar, 26.5, 43)
        wpc = [wb_([128, 8, 512], BF16), wb_([128, 8, 512], BF16)]
        tb_ = Bump(ar, 176.5, 207)
        wst = [tb_([128, 512]), tb_([128, 512])]
        ev = [tb_([128, 512]), tb_([128, 512])]
        ta = tb_([128, 256]); tbb = tb_([128, 256])
        for kc in range(8):
            s = kc % 2
            ph.dma("sp", xst[s], xT_d[kc * 128:(kc + 1) * 128, :], (), ("xst%d" % s,), "xst%d" % s)
            ph.cp(xTb[:, kc, :], xst[s], ("xst%d" % s,), ("xTb%d" % kc,), eng="act" if kc % 2 else "dve")
        xall = tuple("xTb%d" % k for k in range(8))
        nev = 0
        for grp in range(4):
            pw = wpc[grp % 2]
            pwn = "wpc%d" % (grp % 2)
            for kc in range(8):
                s = kc % 2
                ph.dma("act" if kc % 2 else "sp", wst[s], w_in_d[kc * 128:(kc + 1) * 128, grp * 512:(grp + 1) * 512],
                       (), ("wst%d" % s,), "wst%d" % s)
                ph.cp(pw[:, kc, :], wst[s], ("wst%d" % s,), (pwn,), eng="pool")
            if grp == 0:
                for c in range(4):
                    for bi, (t0, n) in enumerate(TB):
                        b = (c * len(TB) + bi) % 2
                        ph.mm([(bank(b, n), pw[:, kc, c * 128:(c + 1) * 128], xTb[:, kc, t0:t0 + n], kc == 0, kc == 7)
                               for kc in range(8)], xall + (pwn,), ("ps%d" % b,))
                        ph.cp(uT[:, c, t0:t0 + n], bank(b, n), ("ps%d" % b,), ("uT%d_%d" % (c, bi),), eng="act")
                continue
            def emit_mm(i, pw=pw, pwn=pwn):
                t0, n = TL[i]
                b = 2 + (i % 2)
                ph.mm([(ps_t[0:n, b * 512:(b + 1) * 512], xTb[:, kc, t0:t0 + n], pw[:, kc, :], kc == 0, kc == 7) for kc in range(8)],
                      xall + (pwn,), ("ps%d" % b,))
            emit_mm(0)
            for i, (t0, n) in enumerate(TL):
                b = 2 + (i % 2)
                pb = ps_t[0:n, b * 512:(b + 1) * 512]
                if i + 1 < len(TL):
                    emit_mm(i + 1)
                e_ = ev[nev % 2]; en = "ev%d" % (nev % 2); nev += 1
                eo = e_[0:n, :]
                if grp == 3:
                    ph.cp(eo, pb, ("ps%d" % b,), (en,), eng="act")
                    ph.dma("pool", vo_d[t0:t0 + n, :], eo, (en,), ("vo%d" % i,), "st_" + en)
                    if i < NTP:
                        ph.cp(vbf[:, i, :], pb, ("ps%d" % b,), ("vbf%d" % i,))
                    continue
                pv = pb.rearrange("p (g two f) -> p g two f", g=8, two=2)
                ov = eo.rearrange("p (g two f) -> p g two f", g=8, two=2)
                cs = cosT[0:n, i:i + 1, :].broadcast_to([n, 8, 32]); sn = sinT[0:n, i:i + 1, :].broadcast_to([n, 8, 32])
                t1 = ta[0:n, :].rearrange("p (g f) -> p g f", g=8); t2 = tbb[0:n, :].rearrange("p (g f) -> p g f", g=8)
                rp = ("ps%d" % b, "cosT", "sinT", "cosTs", "sinTs")
                ph.tt(t1, pv[:, :, 0, :], cs, ALU.mult, rp, ("ta",))
                ph.tt(t2, pv[:, :, 1, :], sn, ALU.mult, rp, ("tb",))
                ph.tt(ov[:, :, 0, :], t1, t2, ALU.subtract, ("ta", "tb"), (en,))
                ph.tt(t1, pv[:, :, 1, :], cs, ALU.mult, rp, ("ta",))
                ph.tt(t2, pv[:, :, 0, :], sn, ALU.mult, rp, ("tb",))
                ph.tt(ov[:, :, 1, :], t1, t2, ALU.add, ("ta", "tb"), (en + "b",))
                if grp == 2:
                    ph.dma("pool", ko_d[t0:t0 + n, :], eo, (en, en + "b"), ("ko%d" % i,), "st_" + en)
                if i >= NTP:
                    if grp == 1:
                        ph.cp(qs[0:n, :], eo, (en, en + "b"), ("qs",), eng="act")
                    continue
                tb2 = i % 2
                ph.tr([(bank(tb2)[:, h * 128:(h + 1) * 128], eo[:, h * 128:(h + 1) * 128], ident) for h in range(4)],
                      (en, en + "b", "ident"), ("ps%d" % tb2,))
                dst = qT if grp == 1 else kT
                ph.cp(dst[:, :, t0:t0 + 128], bank(tb2).rearrange("p (h t) -> p h t", h=4), ("ps%d" % tb2,),
                      ("%sT%d" % ("q" if grp == 1 else "k", i),), eng="act" if i % 2 else "dve")
        ph.run()

        if NTP:
            ph = Phase(ctx)
            tb_ = Bump(ar, 143.5, 207)
            Psb = [[tb_([128, T], BF16), tb_([128, T], BF16)], [tb_([128, T], BF16), tb_([128, T], BF16)]]
            PTs = [tb_([128, T], BF16), tb_([128, T], BF16)]
            mx = tb_([128, 2]); nb = tb_([128, 2]); lsum = tb_([128, 2]); rl = tb_([128, 2])
            tS = tb_([128, 128]); att = tb_([128, 128]); junk = tb_([128, 128]); ss = tb_([128, 1]); rs = tb_([128, 1])
            lsum2 = [lsum, tb_([128, 2])]

            def geom(qt, m):
                nk = qt + 1
                small = nk <= 8
                sb0 = 2 * m if small else 0
                SBK = tuple("ps%d" % (sb0 + k) for k in range(2 if small else 4))
                Sv = ps_t[:, sb0 * 512:sb0 * 512 + (1024 if small else 2048)]
                if small:
                    PTv = ps_t[:, (4 + m) * 512:(5 + m) * 512].bitcast(BF16); PTK = ("ps%d" % (4 + m),)
                else:
                    PTv = ps_t[:, 2048:3072].bitcast(BF16); PTK = ("ps4", "ps5")
                return nk, SBK, Sv, PTv, PTK

            def stageA(idx, h, qt, m):
                nk, SBK, Sv, PTv, PTK = geom(qt, m)
                ls = lsum2[idx % 2]; lname = "l%d_%d" % (idx % 2, m)
                items = []
                for j in range(0, nk, 4):
                    cols = min(4, nk - j) * 128
                    items.append((Sv[:, j * 128:j * 128 + cols], qT[m * 64:(m + 1) * 64, h, qt * 128:(qt + 1) * 128],
                                  kT[m * 64:(m + 1) * 64, h, j * 128:j * 128 + cols], True, True))
                ph.mm(items, (), SBK)
                ph.tt(Sv[:, qt * 128:(qt + 1) * 128], Sv[:, qt * 128:(qt + 1) * 128], tri, ALU.add, SBK + ("tri",), SBK)
                ph.red(mx[:, m:m + 1], Sv[:, 0:nk * 128], ALU.max, SBK, ("mx%d" % m,))
                ph.ts(nb[:, m:m + 1], mx[:, m:m + 1], -0.125, ALU.mult, ("mx%d" % m,), ("nb%d" % m,))
                ph.act(Psb[idx % 2][m][:, 0:nk * 128], Sv[:, 0:nk * 128], AF.Exp, SBK + ("nb%d" % m,), ("P%d_%d" % (idx % 2, m), lname),
                       bias=nb[:, m:m + 1], scale=0.125, accum=ls[:, m:m + 1])

            def stageB(idx, h, qt, m):
                nk, SBK, Sv, PTv, PTK = geom(qt, m)
                pn, tn = "P%d_%d" % (idx % 2, m), "PT%d" % m
                ph.tr([(PTv[:, k * 128:(k + 1) * 128], Psb[idx % 2][m][:, k * 128:(k + 1) * 128], identb) for k in range(nk)], (pn, "identb"), PTK)
                ph.cp(PTs[m][:, 0:nk * 128], PTv[:, 0:nk * 128], PTK, (tn,), eng="act" if m else "dve")
                ph.mm([(bank(6 + m)[:, 0:128], PTs[m][:, k * 128:(k + 1) * 128], vbf[:, k, h * 128:(h + 1) * 128],
                        k == 0, k == nk - 1) for k in range(nk)], (tn,), ("ps%d" % (6 + m),))

            def stageC(idx, h, qt):
                ls = lsum2[idx % 2]
                ph.recip(rl, ls, ("l%d_0" % (idx % 2), "l%d_1" % (idx % 2)), ("rl",))
                ph.tt(rl[:, 1:2], rl[:, 1:2], lam_t, ALU.mult, ("rl", "lam"), ("rl",))
                ph.ts(tS, bank(7)[:, 0:128], rl[:, 1:2], ALU.mult, ("ps7", "rl"), ("tS",))
                ph.stt(att, bank(6)[:, 0:128], rl[:, 0:1], tS, ALU.mult, ALU.subtract, ("ps6", "rl", "tS"), ("att",))
                ph.stt(junk, att, 1.0, att, ALU.mult, ALU.mult, ("att",), ("junk", "ss"), accum=ss)
                ph.ts(ss, ss, 1.0 / 128, ALU.mult, ("ss",), ("ss",), s2=RMS_EPS, op1=ALU.add)
                ph.act(ss, ss, AF.Ln, ("ss",), ("ss",))
                ph.act(rs, ss, AF.Exp, ("ss",), ("rs",), scale=-0.5)
                ph.ts(att, att, rs, ALU.mult, ("att", "rs"), ("att",), s2=1.0 - LAM_INIT, op1=ALU.mult)
                ph.tt(att, att, gsub, ALU.mult, ("att", "gsub"), ("att",))
                ph.tr([(bank(6)[:, 256:384], att, ident)], ("att", "ident"), ("ps6",))
                ph.cp(aoT[:, h, qt * 128:(qt + 1) * 128], bank(6)[:, 256:384], ("ps6",), ("aoT%d_%d" % (h, qt),), eng="act")

            iters = [(h, qt) for h in range(4) for qt in range(NTP)]
            stageA(0, iters[0][0], iters[0][1], 0); stageA(0, iters[0][0], iters[0][1], 1)
            for idx, (h, qt) in enumerate(iters):
                nxt = iters[idx + 1] if idx + 1 < len(iters) else None
                if nxt:
                    stageA(idx + 1, nxt[0], nxt[1], 0)
                stageB(idx, h, qt, 0); stageB(idx, h, qt, 1)
                if nxt:
                    stageA(idx + 1, nxt[0], nxt[1], 1)
                stageC(idx, h, qt)
            ph.run()

        ph = Phase(ctx)
        tb_ = Bump(ar, 94.5, 207)
        selB = tb_([128, NS * 128])
        pti = tb_([128, NS * NPG], I32); ptf = tb_([128, NS * NPG]); idx = tb_([128, NS * NPG], I32)
        NR = 3
        Kp = [tb_([128, 4, 512]) for _ in range(NR)]
        Vp = [tb_([128, 4, 512]) for _ in range(NR)]
        Ksf = [tb_([128, 512]), tb_([128, 512])]; Vsf = [tb_([128, 512]), tb_([128, 512])]
        prod = [tb_([128, 512]), tb_([128, 512])]
        Ss = tb_([128, NS, NSL, 8])
        mp = tb_([128, NS * 8]); gmx = tb_([128, 1]); dg = tb_([128, NS * 8]); rls = tb_([128, NS * 8])
        t1s = tb_([128, NS, 2]); atts = tb_([128, 4, NS]); sqs = tb_([128, 4 * NS]); rss = tb_([128, 4 * NS])
        H8 = NS * 8
        OTB = ("ps3", "ps4", "ps5", "ps6", "ps7")
        NVB = 4
        Vb = [tb_([128, 512], BF16) for _ in range(NVB)]
        Pb = tb_([128, NS, NSL, 8], BF16); onesb = tb_([128, 128], BF16)
        ph.memset(onesb, 1.0, ("onesb",))
        ph.dma("sp", selB[0:NS, :], selB_d, (), ("selB",), "d0")
        ph.dma("sp", pti, pt_d.broadcast_to([128, NS * NPG]), (), ("pti",), "d1")
        ph.cp(ptf, pti, ("pti",), ("ptf",))
        ph.ts(ptf, ptf, 128.0, ALU.mult, ("ptf", "pidx"), ("ptf2",), s2=pidx, op1=ALU.add)
        ph.cp(idx, ptf, ("ptf2",), ("idx",))
        for s in range(2):
            ph.memset(Ksf[s], 0.0, ("Ksf%d" % s,)); ph.memset(Vsf[s], 0.0, ("Vsf%d" % s,))
        ngr = (NPG + 3) // 4
        gi = 0
        for b in range(NS):
            qb = b % 2
            ph.mm([(bank(qb), selB[0:NS, b * 128:(b + 1) * 128], qs[0:NS, :], True, True)], ("selB", "qs"), ("ps%d" % qb,))
            for g in range(ngr):
                r = gi % NR; gi += 1
                for jj in range(min(4, NPG - g * 4)):
                    j = g * 4 + jj
                    col = b * NPG + j
                    bn = "Kp%d_%d" % (r, jj)
                    ph.add("pool", (lambda e, o=Kp[r][:, jj, :], ia=idx[:, col:col + 1]: e.indirect_dma_start(
                        out=o, out_offset=None, in_=ck_d, in_offset=bass.IndirectOffsetOnAxis(ap=ia, axis=0))),
                        ("idx",), (bn,), dma=bn)
                    p_ = prod[j % 2]; pn = "prod%d" % (j % 2)
                    ph.tt(p_, Kp[r][:, jj, :], bank(qb), ALU.mult, (bn, "ps%d" % qb), (pn,))
                    ph.red(Ss[:, b, j, :], p_.rearrange("p (g f) -> p g f", g=8), ALU.add, (pn,), ("Ss%d" % b,))
            s = b % 2
            ph.dma("sp", Ksf[s][0:1, :], ko_d[T + b:T + b + 1, :], ("ko%d" % NTP,), ("Ksf%d" % s,), "ksf%d" % s)
            ph.tt(prod[0], Ksf[s], bank(qb), ALU.mult, ("Ksf%d" % s, "ps%d" % qb), ("prod0",))
            ph.red(Ss[:, b, NPG, :], prod[0].rearrange("p (g f) -> p g f", g=8), ALU.add, ("prod0",), ("Ss%d" % b,))
        allS = tuple("Ss%d" % b for b in range(NS))
        ph.ts(Ss[:, :, NPG, :], Ss[:, :, NPG, :], negm, ALU.add, allS + ("negm",), ("SsA",))
        ph.red(mp.rearrange("p (b h) -> p b h", b=NS), Ss.rearrange("p b s h -> p b h s"), ALU.max, ("SsA",), ("mp",))
        ph.tr([(bank(2)[0:H8, 0:128], mp, ident)], ("mp", "ident"), ("ps2",))
        ph.red(gmx[0:H8, :], bank(2)[0:H8, 0:128], ALU.max, ("ps2",), ("gmx",))
        ph.ts(dg[0:H8, :], ident[0:H8, 0:H8], gmx[0:H8, :], ALU.mult, ("gmx", "ident"), ("dg",))
        ph.mm([(bank(2)[:, 0:H8], onesf[0:H8, :], dg[0:H8, :], True, True)], ("dg", "onesf"), ("ps2",))
        ph.tt(Ss, Ss, bank(2)[:, 0:H8].rearrange("p (b o h) -> p b o h", b=NS, o=1).broadcast_to([128, NS, NSL, 8]),
              ALU.subtract, ("SsA", "ps2"), ("SsB",))
        ph.act(Ss, Ss, AF.Exp, ("SsB",), ("P",), scale=0.125)
        ph.cp(Pb.rearrange("p b s h -> p (b s h)"), Ss.rearrange("p b s h -> p (b s h)"), ("P",), ("Pb",))
        gi = 0
        vi = 0
        for b in range(NS):
            for g in range(ngr):
                r = gi % NR; gi += 1
                for jj in range(min(4, NPG - g * 4)):
                    j = g * 4 + jj
                    col = b * NPG + j
                    bn = "Vp%d_%d" % (r, jj)
                    ph.add("pool", (lambda e, o=Vp[r][:, jj, :], ia=idx[:, col:col + 1]: e.indirect_dma_start(
                        out=o, out_offset=None, in_=cv_d, in_offset=bass.IndirectOffsetOnAxis(ap=ia, axis=0))),
                        ("idx",), (bn,), dma=bn)
                    vb = Vb[vi % NVB]; vn = "Vb%d" % (vi % NVB); vi += 1
                    ph.cp(vb, Vp[r][:, jj, :], (bn,), (vn,), eng="act")
                    items = [(bank(3 + h)[:, b * 2:b * 2 + 2], vb[:, h * 128:(h + 1) * 128], Pb[:, b, j, 2 * h:2 * h + 2],
                              j == 0, False) for h in range(4)]
                    items.append((bank(7)[:, b * 8:b * 8 + 8], onesb, Pb[:, b, j, :], j == 0, False))
                    ph.mm(items, (vn, "Pb", "onesb"), OTB)
            s = b % 2
            ph.dma("sp", Vsf[s][0:1, :], vo_d[T + b:T + b + 1, :], ("vo%d" % NTP,), ("Vsf%d" % s,), "vsf%d" % s)
            items = [(bank(3 + h)[:, b * 2:b * 2 + 2], Vsf[s][:, h * 128:(h + 1) * 128], Ss[:, b, NPG, 2 * h:2 * h + 2],
                      False, True) for h in range(4)]
            items.append((bank(7)[:, b * 8:b * 8 + 8], onesf, Ss[:, b, NPG, :], False, True))
            ph.mm(items, ("Vsf%d" % s, "P", "onesf"), OTB)
        ph.recip(rls, bank(7)[:, 0:H8], ("ps7",), ("rls",))
        rl4 = rls.rearrange("p (b h m) -> p b h m", b=NS, h=4)
        for h in range(4):
            ph.tt(t1s, bank(3 + h)[:, 0:2 * NS].rearrange("p (b m) -> p b m", b=NS), rl4[:, :, h, :], ALU.mult,
                  ("ps%d" % (3 + h), "rls"), ("t1s",))
            ph.stt(atts[:, h, :], t1s[:, :, 1], nlam_t, t1s[:, :, 0], ALU.mult, ALU.add, ("t1s", "nlam"), ("atts%d" % h,))
        alla = tuple("atts%d" % h for h in range(4))
        af = atts.rearrange("p h b -> p (h b)")
        ph.tt(sqs, af, af, ALU.mult, alla, ("sqs",))
        ph.mm([(bank(2)[:, 0:4 * NS], onesf, sqs, True, True)], ("sqs", "onesf"), ("ps2",))
        ph.ts(rss, bank(2)[:, 0:4 * NS], 1.0 / 128, ALU.mult, ("ps2",), ("rss0",), s2=RMS_EPS, op1=ALU.add)
        ph.act(rss, rss, AF.Sqrt, ("rss0",), ("rss1",))
        ph.recip(rss, rss, ("rss1",), ("rss2",))
        ph.tt(af, af, rss, ALU.mult, alla + ("rss2",), ("attn",))
        ph.ts(aoT[:, :, T:NT], atts, gcol, ALU.mult, ("attn", "gcol"), ("aoTs",), s2=1.0 - LAM_INIT, op1=ALU.mult)
        ph.run()

        E_LO = 92.5
        pb_ = Bump(ar, E_LO, 207)
        WB = pb_([128, 2, 16, 128])
        CTr = pb_([128, 16, 128], BF16); CTi = pb_([128, 16, 128], BF16)
        magP = pb_([128, 16]); ec1 = pb_([128, 16]); es1 = pb_([128, 16]); lbrP = pb_([128, 16, 1]); lbiP = pb_([128, 16, 1])
        sout = [pb_([128, 16, 1 + NS]), pb_([128, 16, 1 + NS])]
        e_mark = pb_.p
        ph = Phase(ctx)
        tb_ = Bump(ar, e_mark / 256.0, 207)
        aPt = tb_([128, 48]); bPt = tb_([128, 2, 16, 128]); cst = tb_([128, 2048])
        BB = tb_([128, 2, 16, 128]); w2 = tb_([128, 16, 128]); w3 = tb_([128, 16, 128])
        ph.dma("sp", aPt, aP_d, (), ("Pin",), "e0")
        ph.dma("sp", bPt[:, 0], bP_d[:, 0:2048].rearrange("p (a b) -> p a b", a=16), (), ("bPt0",), "e1")
        ph.dma("act", bPt[:, 1], bP_d[:, 2048:4096].rearrange("p (a b) -> p a b", a=16), (), ("bPt1",), "e2")
        ph.dma("sp", cst, cT_d[:, 0:2048], (), ("cst",), "e3")
        ph.cp(CTr.rearrange("p a b -> p (a b)"), cst, ("cst",), ("CTr",))
        ph.dma("sp", cst, cT_d[:, 2048:4096], ("CTr",), ("cst2",), "e3")
        ph.act(CTi.rearrange("p a b -> p (a b)"), cst, AF.Copy, ("cst2",), ("CTi",), scale=-1.0)

        def disc(tag, a_re, a_im, ldt, W, want_f):
            keys = ("ar", "dt", "xr", "th", "acc", "tmp", "s", "a", "x2", "u", "s2", "a2")
            t = {k: tb_([128, W]) for k in keys}
            N = lambda k: tag + k
            IN = tag + "in"

            def TS(o, i, s1, op0, s2=None, op1=None, extra=()):
                ph.ts(t[o], t[i], s1, op0, (N(i),) + extra, (N(o),), s2=s2, op1=op1)

            def TT(o, i0, i1, op):
                ph.tt(t[o], t[i0], t[i1], op, (N(i0), N(i1)), (N(o),))

            ph.ts(t["ar"], a_re, -1e-4, ALU.min, (IN,), (N("ar"),))
            ph.act(t["dt"], ldt, AF.Exp, (IN,), (N("dt"),))
            TT("xr", "ar", "dt", ALU.mult)
            ph.tt(t["th"], a_im, t["dt"], ALU.mult, (IN, N("dt")), (N("th"),))
            TS("acc", "xr", 0.1, ALU.mult, s2=1.0, op1=ALU.add)
            for k in range(9, 0, -1):
                TT("tmp", "xr", "acc", ALU.mult)
                if k > 1:
                    TS("acc", "tmp", 1.0 / k, ALU.mult, s2=1.0, op1=ALU.add)
            TS("acc", "tmp", 1.0, ALU.add)
            TS("u", "th", 1.0 / 1024, ALU.mult)
            TT("x2", "u", "u", ALU.mult)
            TS("s", "x2", -1.0 / 20, ALU.mult, s2=1.0, op1=ALU.add)
            TT("s", "s", "x2", ALU.mult)
            TS("s", "s", -1.0 / 6, ALU.mult, s2=1.0, op1=ALU.add)
            TT("s", "s", "u", ALU.mult)
            TS("a", "x2", -1.0 / 30, ALU.mult, s2=1.0, op1=ALU.add)
            TT("a", "a", "x2", ALU.mult)
            TS("a", "a", -1.0 / 12, ALU.mult, s2=1.0, op1=ALU.add)
            TT("a", "a", "x2", ALU.mult)
            TS("a", "a", 0.5, ALU.mult)
            s_, a_, s2_, a2_ = "s", "a", "s2", "a2"
            for _ in range(10):
                TS("x2", a_, -1.0, ALU.mult, s2=1.0, op1=ALU.add)
                ph.stt(t[a2_], t[s_], 2.0, t[s_], ALU.mult, ALU.mult, (N(s_),), (N(a2_),))
                ph.stt(t[s2_], t[s_], 2.0, t["x2"], ALU.mult, ALU.mult, (N(s_), N("x2")), (N(s2_),))
                s_, s2_, a_, a2_ = s2_, s_, a2_, a_
            res = dict(mag=t["acc"], magn=N("acc"), s=t[s_], sn=N(s_), a=t[a_], an=N(a_))
            if want_f:
                TT("x2", "acc", a_, ALU.mult)
                TT("x2", "tmp", "x2", ALU.subtract)
                TT("th", "acc", s_, ALU.mult)
                TT("dt", "ar", "ar", ALU.mult)
                ph.tt(t["xr"], a_im, a_im, ALU.mult, (IN,), (N("xr"),))
                TT("dt", "dt", "xr", ALU.add)
                ph.recip(t["dt"], t["dt"], (N("dt"),), (N("dt"),))
                TT("xr", "x2", "ar", ALU.mult)
                ph.tt(t["u"], t["th"], a_im, ALU.mult, (N("th"), IN), (N("u"),))
                TT("xr", "xr", "u", ALU.add)
                TT("xr", "xr", "dt", ALU.mult)
                TT(s2_, "th", "ar", ALU.mult)
                ph.tt(t["u"], t["x2"], a_im, ALU.mult, (N("x2"), IN), (N("u"),))
                TT(s2_, s2_, "u", ALU.subtract)
                TT(s2_, s2_, "dt", ALU.mult)
                res.update(fre=t["xr"], fren=N("xr"), fim=t[s2_], fimn=N(s2_))
            return res

        rP = disc("P", aPt[:, 0:16], aPt[:, 16:32], aPt[:, 32:48], 16, True)
        fre = rP["fre"].unsqueeze(2).broadcast_to([128, 16, 128]); fim = rP["fim"].unsqueeze(2).broadcast_to([128, 16, 128])
        ph.tt(w2, bPt[:, 0], fre, ALU.mult, (rP["fren"], "bPt0"), ("w2",))
        ph.tt(w3, bPt[:, 1], fim, ALU.mult, (rP["fimn"], "bPt1"), ("w3",))
        ph.tt(BB[:, 0], w2, w3, ALU.subtract, ("w2", "w3"), ("BB0",))
        ph.tt(w2, bPt[:, 1], fre, ALU.mult, (rP["fren"], "bPt1"), ("w2",))
        ph.tt(w3, bPt[:, 0], fim, ALU.mult, (rP["fimn"], "bPt0"), ("w3",))
        ph.tt(BB[:, 1], w2, w3, ALU.add, ("w2", "w3"), ("BB1",))
        for ri_ in range(2):
            for g4 in range(4):
                bk = (ri_ * 4 + g4) % 4
                ph.tr([(bank(bk)[:, q * 128:(q + 1) * 128], BB[:, ri_, g4 * 4 + q, :], ident) for q in range(4)],
                      ("BB%d" % ri_, "ident"), ("ps%d" % bk,))
                ph.cp(WB[:, ri_, g4 * 4:g4 * 4 + 4, :], bank(bk).rearrange("p (q f) -> p q f", q=4), ("ps%d" % bk,), ("WB",),
                      eng="act" if g4 % 2 else "dve")
        cP = tb_([128, 16]); nP = tb_([128, 16]); n2 = tb_([128, 16])
        ph.ts(cP, rP["a"], -1.0, ALU.mult, (rP["an"],), ("cP",), s2=1.0, op1=ALU.add)
        ph.tt(nP, cP, cP, ALU.mult, ("cP",), ("nP",))
        ph.tt(n2, rP["s"], rP["s"], ALU.mult, (rP["sn"],), ("n2",))
        ph.tt(nP, nP, n2, ALU.add, ("nP", "n2"), ("nP",))
        ph.ts(nP, nP, -0.5, ALU.mult, ("nP",), ("nP",), s2=1.5, op1=ALU.add)
        ph.tt(ec1, cP, nP, ALU.mult, ("cP", "nP"), ("ec1",))
        ph.tt(es1, rP["s"], nP, ALU.mult, (rP["sn"], "nP"), ("es1",))
        ph.cp(magP, rP["mag"], (rP["magn"],), ("magP",))
        ph.tt(lbrP.rearrange("p a b -> p (a b)"), rP["mag"], ec1, ALU.mult, (rP["magn"], "ec1"), ("lbrP",))
        ph.tt(lbiP.rearrange("p a b -> p (a b)"), rP["mag"], es1, ALU.mult, (rP["magn"], "es1"), ("lbiP",))
        ph.run()

        if NTP:
            ph = Phase(ctx)
            tb_ = Bump(ar, e_mark / 256.0, 207)
            Ec = tb_([128, T]); Es = tb_([128, T]); zr = tb_([128, T]); zi = tb_([128, T])
            rr = tb_([128, T]); ri = tb_([128, T]); tA = tb_([128, T]); tBt = tb_([128, T])
            Sr = tb_([128, T], BF16); Si = tb_([128, T], BF16)
            en_ = [tb_([128, 2]), tb_([128, 2])]; e2_ = tb_([128, 2]); u1 = tb_([128, 2])
            yv = tb_([128, 512]); g1 = tb_([128, 512]); g2 = tb_([128, 512])
            PB = [(t0, n) for (t0, n) in TB if t0 < T]
            LOGT = int(math.log2(T))
            assert 1 << LOGT == T
            for gp in range(16):
                c, rows = gp // 4, (gp % 4) * 32
                ph.memset(Ec[:, 0:1], 1.0, ("Ec",)); ph.memset(Es[:, 0:1], 0.0, ("Es",))
                ph.cp(en_[0][:, 0:1], ec1[:, gp:gp + 1], (), ("en0",)); ph.cp(en_[0][:, 1:2], es1[:, gp:gp + 1], (), ("en0",))
                for k in range(LOGT):
                    n = 1 << k
                    e0, e1_ = en_[k % 2], en_[(k + 1) % 2]
                    n0, n1 = "en%d" % (k % 2), "en%d" % ((k + 1) % 2)
                    ph.ts(tA[:, 0:n], Es[:, 0:n], e0[:, 1:2], ALU.mult, ("Es", n0), ("tA",))
                    ph.stt(Ec[:, n:2 * n], Ec[:, 0:n], e0[:, 0:1], tA[:, 0:n], ALU.mult, ALU.subtract, ("Ec", n0, "tA"), ("Ec",))
                    ph.ts(tBt[:, 0:n], Es[:, 0:n], e0[:, 0:1], ALU.mult, ("Es", n0), ("tB",))
                    ph.stt(Es[:, n:2 * n], Ec[:, 0:n], e0[:, 1:2], tBt[:, 0:n], ALU.mult, ALU.add, ("Ec", n0, "tB"), ("Es",))
                    if k < LOGT - 1:
                        ph.tt(e2_, e0, e0, ALU.mult, (n0,), ("e2",))
                        ph.tt(e1_[:, 0:1], e2_[:, 0:1], e2_[:, 1:2], ALU.subtract, ("e2",), (n1,))
                        ph.stt(e1_[:, 1:2], e0[:, 0:1], 2.0, e0[:, 1:2], ALU.mult, ALU.mult, (n0,), (n1,))
                for bi, (t0, n) in enumerate(PB):
                    xb = 2 * (bi % 2)
                    un = "uT%d_%d" % (c, bi)
                    ph.mm([(bank(xb, n), WB[:, 0, gp, :], uT[:, c, t0:t0 + n], True, True),
                           (bank(xb + 1, n), WB[:, 1, gp, :], uT[:, c, t0:t0 + n], True, True)],
                          (un,), ("ps%d" % xb, "ps%d" % (xb + 1)))
                    xn0, xn1 = "ps%d" % xb, "ps%d" % (xb + 1)
                    sl = slice(t0, t0 + n)
                    ph.tt(tA[:, sl], bank(xb, n), Ec[:, sl], ALU.mult, (xn0, "Ec"), ("tA",))
                    ph.tt(tBt[:, sl], bank(xb + 1, n), Es[:, sl], ALU.mult, (xn1, "Es"), ("tB",))
                    ph.tt(zr[:, sl], tA[:, sl], tBt[:, sl], ALU.add, ("tA", "tB"), ("zr",))
                    ph.tt(tA[:, sl], bank(xb + 1, n), Ec[:, sl], ALU.mult, (xn1, "Ec"), ("tA",))
                    ph.tt(tBt[:, sl], bank(xb, n), Es[:, sl], ALU.mult, (xn0, "Es"), ("tB",))
                    ph.tt(zi[:, sl], tA[:, sl], tBt[:, sl], ALU.subtract, ("tA", "tB"), ("zi",))
                dec = magP[:, gp:gp + 1].broadcast_to([128, T])
                ph.add("dve", (lambda e, o=rr, d0=dec, d1=zr: e.tensor_tensor_scan(o, d0, d1, 0.0, ALU.mult, ALU.add)), ("zr",), ("rr",))
                ph.add("dve", (lambda e, o=ri, d0=dec, d1=zi: e.tensor_tensor_scan(o, d0, d1, 0.0, ALU.mult, ALU.add)), ("zi",), ("ri",))
                ph.tt(tA, rr, Ec, ALU.mult, ("rr", "Ec"), ("tA",))
                ph.tt(tBt, ri, Es, ALU.mult, ("ri", "Es"), ("tB",))
                ph.tt(Sr, tA, tBt, ALU.subtract, ("tA", "tB"), ("Sr",))
                ph.tt(sout[0][:, gp, 0:1], tA[:, T - 1:T], tBt[:, T - 1:T], ALU.subtract, ("tA", "tB"), ("so_re",))
                ph.tt(tA, ri, Ec, ALU.mult, ("ri", "Ec"), ("tA",))
                ph.tt(tBt, rr, Es, ALU.mult, ("rr", "Es"), ("tB",))
                ph.tt(Si, tA, tBt, ALU.add, ("tA", "tB"), ("Si",))
                ph.tt(sout[1][:, gp, 0:1], tA[:, T - 1:T], tBt[:, T - 1:T], ALU.add, ("tA", "tB"), ("so_im",))
                for bi, (t0, n) in enumerate(PB):
                    ph.mm([(bank(4 + bi, n), CTr[:, gp, :], Sr[:, t0:t0 + n], gp % 4 == 0, False),
                           (bank(4 + bi, n), CTi[:, gp, :], Si[:, t0:t0 + n], False, gp % 4 == 3)], ("Sr", "Si"), ("ps%d" % (4 + bi),))
                if gp % 4 == 3:
                    for bi, (t0, n) in enumerate(PB):
                        y_ = yv[:, 0:n]; a_ = g1[:, 0:n]; b_ = g2[:, 0:n]
                        ph.stt(y_, uT[:, c, t0:t0 + n], dP[:, c:c + 1], bank(4 + bi, n), ALU.mult, ALU.add, ("ps%d" % (4 + bi),), ("yv",))
                        ph.tt(a_, y_, y_, ALU.mult, ("yv",), ("g1",))
                        ph.ts(a_, a_, 0.044715, ALU.mult, ("g1",), ("g1",), s2=1.0, op1=ALU.add)
                        ph.tt(a_, a_, y_, ALU.mult, ("g1", "yv"), ("g1",))
                        ph.act(b_, a_, AF.Sigmoid, ("g1",), ("g2",), scale=1.5957691216057308)
                        ph.tt(gss[:, c, t0:t0 + n], y_, b_, ALU.mult, ("yv", "g2"), ("gss%d_%d" % (c, bi),))
            ph.run()

        ph = Phase(ctx)
        tb_ = Bump(ar, e_mark / 256.0, 207)
        s0r = tb_([128, 16, NS]); s0i = tb_([128, 16, NS]); q1 = tb_([128, 16, NS]); q2 = tb_([128, 16, NS])
        Ssr = tb_([128, 16, NS], BF16); Ssi = tb_([128, 16, NS], BF16)
        yv = tb_([128, 4, NS]); g1 = tb_([128, 4, NS]); g2 = tb_([128, 4, NS])
        ph.dma("sp", s0r, st_re_d.rearrange("p (a b) -> p a b", a=16), (), ("s0r",), "f0")
        ph.dma("sp", s0i, st_im_d.rearrange("p (a b) -> p a b", a=16), (), ("s0i",), "f1")
        items = []
        for gp in range(16):
            c, rows = gp // 4, (gp % 4) * 32
            for ri_ in range(2):
                items.append((bank(0)[:, (gp * 2 + ri_) * NS:(gp * 2 + ri_ + 1) * NS], WB[:, ri_, gp, :], uT[:, c, T:NT], True, True))
        ph.mm(items, (), ("ps0",))
        Xv = bank(0)[:, 0:32 * NS].rearrange("p (g r b) -> p g r b", g=16, r=2)
        lbr = lbrP.broadcast_to([128, 16, NS]); lbi = lbiP.broadcast_to([128, 16, NS])
        ph.tt(q1, s0i, lbi, ALU.mult, ("s0i",), ("q1",))
        ph.tt(q2, s0r, lbr, ALU.mult, ("s0r",), ("q2",))
        ph.tt(q2, q2, q1, ALU.subtract, ("q1", "q2"), ("q2",))
        ph.tt(sout[0][:, :, 1:1 + NS], q2, Xv[:, :, 0, :], ALU.add, ("q2", "ps0"), ("so_re",))
        ph.cp(Ssr, sout[0][:, :, 1:1 + NS], ("so_re",), ("Ssr",))
        ph.tt(q1, s0r, lbi, ALU.mult, ("s0r", "q2"), ("q1",))
        ph.tt(q2, s0i, lbr, ALU.mult, ("s0i", "so_re"), ("q2",))
        ph.tt(q2, q2, q1, ALU.add, ("q1", "q2"), ("q2",))
        ph.tt(sout[1][:, :, 1:1 + NS], q2, Xv[:, :, 1, :], ALU.add, ("q2", "ps0"), ("so_im",))
        ph.cp(Ssi, sout[1][:, :, 1:1 + NS], ("so_im",), ("Ssi",))
        ph.dma("sp", sre_d.rearrange("p (a b) -> p a b", a=16), sout[0], ("so_re",), ("sre_d",), "f2")
        ph.dma("sp", sim_d.rearrange("p (a b) -> p a b", a=16), sout[1], ("so_im",), ("sim_d",), "f3")
        for c in range(4):
            items = []
            for gl in range(4):
                gp = 4 * c + gl
                items.append((bank(1)[:, c * NS:(c + 1) * NS], CTr[:, gp, :], Ssr[:, gp, :], gl == 0, False))
                items.append((bank(1)[:, c * NS:(c + 1) * NS], CTi[:, gp, :], Ssi[:, gp, :], False, gl == 3))
            ph.mm(items, ("Ssr", "Ssi"), ("ps1",))
        for c in range(4):
            ph.stt(yv[:, c, :], uT[:, c, T:NT], dP[:, c:c + 1], bank(1)[:, c * NS:(c + 1) * NS], ALU.mult, ALU.add, ("ps1",), ("yv%d" % c,))
        ally = tuple("yv%d" % c for c in range(4))
        ph.tt(g1, yv, yv, ALU.mult, ally, ("g1",))
        ph.ts(g1, g1, 0.044715, ALU.mult, ("g1",), ("g1",), s2=1.0, op1=ALU.add)
        ph.tt(g1, g1, yv, ALU.mult, ("g1",) + ally, ("g1",))
        ph.act(g2, g1, AF.Sigmoid, ("g1",), ("g2",), scale=1.5957691216057308)
        ph.tt(gss[:, :, T:NT], yv, g2, ALU.mult, ally + ("g2",), ("gss_s",))
        ph.run()

        ph = Phase(ctx)
        tb_ = Bump(ar, 92.5, 207)
        wgl = tb_([128, 4, 1024], BF16)
        wst = [tb_([128, 1024]), tb_([128, 1024])]
        sg = [tb_([128, 512]), tb_([128, 512])]
        for kc in range(4):
            s = kc % 2
            ph.dma("sp", wst[s], w_glu_d[kc * 128:(kc + 1) * 128, :], (), ("wst%d" % s,), "g%d" % s)
            ph.cp(wgl[:, kc, :], wst[s], ("wst%d" % s,), ("wgl",), eng="pool")
        it = 0
        for oc in range(4):
            for bi, (t0, n) in enumerate(TB):
                b = 2 * (it % 2); it += 1
                ph.mm([(bank(b, n), wgl[:, kc, oc * 128:(oc + 1) * 128], gss[:, kc, t0:t0 + n], kc == 0, kc == 3) for kc in range(4)]
                      + [(bank(b + 1, n), wgl[:, kc, 512 + oc * 128:512 + (oc + 1) * 128], gss[:, kc, t0:t0 + n], kc == 0, kc == 3) for kc in range(4)],
                      ("wgl",), ("ps%d" % b, "ps%d" % (b + 1)))
                s_ = sg[it % 2][:, 0:n]
                ph.act(s_, bank(b + 1, n), AF.Sigmoid, ("ps%d" % (b + 1),), ("sg%d" % (it % 2),), bias=bglu[:, 4 + oc:5 + oc])
                ph.stt(soT[:, oc, t0:t0 + n], bank(b, n), bglu[:, oc:oc + 1], s_, ALU.add, ALU.mult, ("ps%d" % b, "sg%d" % (it % 2)), ("soT",))
        ph.run()

        def layer_norm(ph, src, srcn, n, gam, bet, out_tile, outn, tmp, tmpn, stat):
            st6, mv, rs_, nmr = stat
            for j in range(2):
                ph.add("dve", (lambda e, o=st6[0:n, j, :], i=src[j]: e.bn_stats(o, i)), (srcn[j],), ("st6",))
            ph.add("dve", (lambda e, o=mv[0:n, :], i=st6[0:n, :, :].rearrange("p a b -> p (a b)"): e.bn_aggr(o, i)), ("st6",), ("mv",))
            ph.ts(rs_[0:n, :], mv[0:n, 1:2], LN_EPS, ALU.add, ("mv",), ("rs",))
            ph.act(rs_[0:n, :], rs_[0:n, :], AF.Ln, ("rs",), ("rs",))
            ph.act(rs_[0:n, :], rs_[0:n, :], AF.Exp, ("rs",), ("rs",), scale=-0.5)
            ph.stt(nmr[0:n, :], mv[0:n, 0:1], -1.0, rs_[0:n, :], ALU.mult, ALU.mult, ("mv", "rs"), ("nmr",))
            for j in range(2):
                ph.act(tmp[0:n, j * 512:(j + 1) * 512], src[j], AF.Identity, (srcn[j], "rs", "nmr"), (tmpn[j],),
                       bias=nmr[0:n, :], scale=rs_[0:n, :])
            ph.tt(tmp[0:n, :], tmp[0:n, :], gam[0:n, :], ALU.mult, tuple(tmpn) + ("lng",), tuple(tmpn))
            ph.tt(out_tile[0:n, :], tmp[0:n, :], bet[0:n, :], ALU.add, tuple(tmpn) + ("lng",), (outn,))

        ph = Phase(ctx)
        tb_ = Bump(ar, 150.5, 207)
        wob = tb_([128, 8, 1024], BF16)
        wst = [tb_([128, 1024]), tb_([128, 1024])]
        lng = tb_([128, 1024]); lnb = tb_([128, 1024])
        xt = [tb_([128, 1024]), tb_([128, 1024])]
        tl = tb_([128, 1024]); ht = tb_([128, 1024])
        wrt = tb_([128, 8, 32]); brt = tb_([128, 32])
        st6 = tb_([128, 2, 6]); mv = tb_([128, 2]); rs_ = tb_([128, 1]); nmr = tb_([128, 1])
        lg = tb_([128, 32]); m8 = tb_([128, 8]); sel = tb_([128, 32]); nm_ = tb_([128, 1]); ex = tb_([128, 32])
        den = tb_([128, 1]); gt = tb_([128, 32])
        for kc in range(8):
            s = kc % 2
            ph.dma("sp", wst[s], w_out_d[kc * 128:(kc + 1) * 128, :], (), ("wst%d" % s,), "h%d" % s)
            ph.cp(wob[:, kc, :], wst[s], ("wst%d" % s,), ("wob",), eng="pool")
        ph.dma("act", lng, ln_d[:, 0:D].broadcast_to([128, D]), (), ("lng",), "h2")
        ph.dma("act", lnb, ln_d[:, D:2 * D].broadcast_to([128, D]), (), ("lng",), "h3")
        ph.dma("act", wrt[:, :, 0:NE], wr_d[:, 0:NE].rearrange("(c p) e -> p c e", p=128), (), ("wrt",), "h4")
        ph.dma("act", brt[:, 0:NE], br_d.broadcast_to([128, NE]), (), ("brt",), "h5")
        for i, (t0, n) in enumerate(TL):
            s = i % 2
            ph.dma("sp", xt[s][0:n, :], x_d[t0:t0 + n, :], (), ("xt%d" % s,), "x%d" % s)
            for j in range(2):
                ph.mm([(ps_t[0:n, j * 512:(j + 1) * 512], (soT[:, kc, t0:t0 + n] if kc < 4 else aoT[:, kc - 4, t0:t0 + n]),
                        wob[:, kc, j * 512:(j + 1) * 512], kc == 0, kc == 7) for kc in range(8)], ("wob",), ("ps%d" % j,))
                ph.stt(tl[0:n, j * 512:(j + 1) * 512], xt[s][0:n, j * 512:(j + 1) * 512], ALPHA, ps_t[0:n, j * 512:(j + 1) * 512],
                       ALU.mult, ALU.add, ("xt%d" % s, "ps%d" % j), ("tl%d" % j,))
            layer_norm(ph, [tl[0:n, 0:512], tl[0:n, 512:1024]], ("tl0", "tl1"), n, lng, lnb, ht, "ht", tl, ("tl0", "tl1"),
                       (st6, mv, rs_, nmr))
            for j in range(2):
                ph.tr([(ps_t[:, (2 + j) * 512 + cc * 128:(2 + j) * 512 + cc * 128 + n], ht[0:n, (4 * j + cc) * 128:(4 * j + cc + 1) * 128],
                        ident[0:n, 0:n]) for cc in range(4)], ("ht", "ident"), ("ps%d" % (2 + j),))
                src = bank(2 + j).rearrange("p (c t) -> p c t", c=4)[:, :, 0:n]
                ph.act(fT[:, 4 * j:4 * j + 4, t0:t0 + n], src, AF.Copy, ("ps%d" % (2 + j),), ("fT%d" % i,), scale=ALPHA)
                ph.cp(hTb[:, 4 * j:4 * j + 4, t0:t0 + n], src, ("ps%d" % (2 + j),), ("hTb%d" % i,))
            ph.mm([(ps_t[0:n, 4 * 512:4 * 512 + NE], fT[:, c, t0:t0 + n], wrt[:, c, 0:NE], c == 0, c == 7) for c in range(8)],
                  ("fT%d" % i, "wrt"), ("ps4",))
            ph.stt(lg[0:n, 0:NE], ps_t[0:n, 4 * 512:4 * 512 + NE], 1.0 / ALPHA, brt[0:n, 0:NE], ALU.mult, ALU.add, ("ps4", "brt"), ("lg",))
            ph.add("dve", (lambda e, o=m8[0:n, :], i_=lg[0:n, 0:NE]: e.max(o, i_)), ("lg",), ("m8",))
            ph.ts(sel[0:n, 0:NE], lg[0:n, 0:NE], m8[0:n, cfg.TOPK - 1:cfg.TOPK], ALU.is_ge, ("lg", "m8"), ("sel",))
            ph.ts(nm_[0:n, :], m8[0:n, 0:1], -1.0, ALU.mult, ("m8",), ("nm",))
            ph.act(ex[0:n, 0:NE], lg[0:n, 0:NE], AF.Exp, ("lg", "nm"), ("ex",), bias=nm_[0:n, :])
            ph.tt(ex[0:n, 0:NE], ex[0:n, 0:NE], sel[0:n, 0:NE], ALU.mult, ("ex", "sel"), ("ex2",))
            ph.red(den[0:n, :], ex[0:n, 0:NE], ALU.add, ("ex2",), ("den",))
            ph.recip(den[0:n, :], den[0:n, :], ("den",), ("den2",))
            ph.ts(gt[0:n, 0:NE], ex[0:n, 0:NE], den[0:n, :], ALU.mult, ("ex2", "den2"), ("gt",))
            ph.tr([(ps_t[0:NE, 5 * 512:5 * 512 + n], gt[0:n, 0:NE], ident[0:n, 0:n])], ("gt", "ident"), ("ps5",))
            ph.cp(gatesT[0:NE, t0:t0 + n], ps_t[0:NE, 5 * 512:5 * 512 + n], ("ps5",), ("gatesT",), eng="act")
        ph.run()

        ph = Phase(ctx)
        tb_ = Bump(ar, 159, 207)
        bdn = tb_([128, D])
        ph.dma("act", bdn[0:NE, :], bdn_d, (), ("bdn",), "m1")
        for dc in range(8):
            for bi, (t0, n) in enumerate(TB):
                b = (dc * len(TB) + bi) % 4
                ph.mm([(bank(b, n), bdn[0:NE, dc * 128:(dc + 1) * 128], gatesT[0:NE, t0:t0 + n], True, True)], ("bdn",), ("ps%d" % b,))
                ph.tt(fT[:, dc, t0:t0 + n], fT[:, dc, t0:t0 + n], bank(b, n), ALU.add, ("ps%d" % b,), ("fT%d_%d" % (dc, bi),))
        ph.run()

        ph = Phase(ctx)
        GeS = ar.at(int(150.5 * KB), [128, NT])
        tb_ = Bump(ar, 159, 207)
        stg = [tb_([128, 2048]), tb_([128, 2048])]
        wpb = [tb_([128, 8, 256], BF16) for _ in range(2)]
        Gt = [tb_([128, 512]) for _ in range(3)]; Sg = [tb_([128, 512]) for _ in range(3)]; Lt = [tb_([128, 512]) for _ in range(3)]
        bguT = tb_([128, NE, 16]); bl1 = tb_([128, NE, 8]); selt = [tb_([128, 128]), tb_([128, 128])]
        ph.dma("act", bguT, bgu_d.rearrange("p (e c) -> p e c", e=NE), (), ("bguT",), "m0")
        ph.ts(bl1, bguT[:, :, 8:16], 1.0, ALU.add, ("bguT",), ("bl1",))
        pieces = [(e_, kind, j) for e_ in range(NE) for kind in ("gu", "dn") for j in range(8)]
        cnt = dict(it=0, un=0)

        def emit_load(i):
            e_, kind, j = pieces[i]
            s2_ = i % 2
            row0 = (e_ * 8 + j) * 128
            if kind == "gu":
                ph.dma("sp", stg[s2_], wgu_d[row0:row0 + 128, :], (), ("stg%d" % s2_,), "stg%d" % s2_)
                ph.cp(wpb[s2_].rearrange("p a b -> p (a b)"), stg[s2_], ("stg%d" % s2_,), ("wpb%d" % s2_,), eng="act")
            else:
                ph.dma("sp", stg[s2_][:, 0:1024], wdn_d[row0:row0 + 128, :], (), ("stg%d" % s2_,), "stg%d" % s2_)
                ph.cp(wpb[s2_].rearrange("p a b -> p (a b)")[:, 0:1024], stg[s2_][:, 0:1024], ("stg%d" % s2_,), ("wpb%d" % s2_,), eng="act")

        def emit_compute(i):
            e_, kind, j = pieces[i]
            s2_ = i % 2
            if kind == "gu" and j == 0:
                st_ = selt[e_ % 2]; sn_ = "selt%d" % (e_ % 2)
                ph.cp(st_[0:NE, :], ident[0:NE, e_:e_ + 1].broadcast_to([NE, 128]), (), (sn_,), eng="pool")
                for bi, (t0, n) in enumerate(TB):
                    b = 6 + bi % 2
                    ph.mm([(bank(b, n), st_[0:NE, :], gatesT[0:NE, t0:t0 + n], True, True)], (sn_,), ("ps%d" % b,))
                    ph.cp(GeS[:, t0:t0 + n], bank(b, n), ("ps%d" % b,), ("GeS%d" % bi,), eng="act" if bi % 2 == 0 else "dve")
            if kind == "gu":
                fc = j
                for bi, (t0, n) in enumerate(TB):
                    b = 2 * (cnt["it"] % 3); cnt["it"] += 1
                    k3 = cnt["un"] % 3; cnt["un"] += 1
                    ph.mm([(bank(b, n), wpb[s2_][:, kc, 0:128], hTb[:, kc, t0:t0 + n], kc == 0, kc == 7) for kc in range(8)]
                          + [(bank(b + 1, n), wpb[s2_][:, kc, 128:256], hTb[:, kc, t0:t0 + n], kc == 0, kc == 7) for kc in range(8)],
                          ("wpb%d" % s2_,), ("ps%d" % b, "ps%d" % (b + 1)))
                    G_ = Gt[k3][:, 0:n]; S_ = Sg[k3][:, 0:n]; L_ = Lt[k3][:, 0:n]
                    gn, sn, ln_ = "G%d" % k3, "S%d" % k3, "L%d" % k3
                    ph.ts(G_, bank(b, n), bguT[:, e_, fc:fc + 1], ALU.add, ("ps%d" % b, "bguT"), (gn,), s2=SW_LIM, op1=ALU.min)
                    ph.act(L_, bank(b + 1, n), AF.Identity, ("ps%d" % (b + 1), "bl1"), (ln_,), bias=bl1[:, e_, fc:fc + 1])
                    ph.act(S_, G_, AF.Sigmoid, (gn,), (sn,), scale=SW_ALPHA)
                    ph.ts(L_, L_, 1.0 - SW_LIM, ALU.max, (ln_,), (ln_,), s2=SW_LIM + 1.0, op1=ALU.min)
                    ph.tt(L_, L_, G_, ALU.mult, (ln_, gn), (ln_,))
                    ph.tt(S_, S_, L_, ALU.mult, (sn, ln_), (sn,), eng="pool")
                    ph.tt(actT[:, fc, t0:t0 + n], S_, GeS[:, t0:t0 + n], ALU.mult, (sn, "GeS%d" % bi), ("actT%d_%d" % (fc, bi),), eng="pool")
            else:
                dc = j
                wd3 = wpb[s2_].rearrange("p a b -> p (a b)")[:, 0:1024].rearrange("p (a b) -> p a b", a=8)
                for bi, (t0, n) in enumerate(TB):
                    b = 6 + (cnt["it"] % 2); cnt["it"] += 1
                    ph.mm([(bank(b, n), wd3[:, f, :], actT[:, f, t0:t0 + n], f == 0, f == 7) for f in range(8)],
                          ("wpb%d" % s2_,) + tuple("actT%d_%d" % (f, bi) for f in range(8)), ("ps%d" % b,))
                    ph.tt(fT[:, dc, t0:t0 + n], fT[:, dc, t0:t0 + n], bank(b, n), ALU.add, ("ps%d" % b,), ("fT%d_%d" % (dc, bi),))

        emit_load(0)
        for i in range(len(pieces)):
            if i + 1 < len(pieces):
                emit_load(i + 1)
            emit_compute(i)
        ph.run()

        ph = Phase(ctx)
        tb_ = Bump(ar, 150.5, 207)
        lng = tb_([128, 1024]); lnb = tb_([128, 1024])
        yt = [tb_([128, 1024]), tb_([128, 1024])]; tmp = tb_([128, 1024])
        st6 = tb_([128, 2, 6]); mv = tb_([128, 2]); rs_ = tb_([128, 1]); nmr = tb_([128, 1])
        ph.dma("sp", lng, ln_d[:, 2 * D:3 * D].broadcast_to([128, D]), (), ("lng",), "n0")
        ph.dma("sp", lnb, ln_d[:, 3 * D:4 * D].broadcast_to([128, D]), (), ("lng",), "n1")
        for i, (t0, n) in enumerate(TL):
            s = i % 2
            bb = 2 * (i % 2)
            for j in range(2):
                ph.tr([(ps_t[0:n, (bb + j) * 512 + cc * 128:(bb + j) * 512 + (cc + 1) * 128], fT[:, 4 * j + cc, t0:t0 + n], ident)
                       for cc in range(4)], ("ident",), ("ps%d" % (bb + j),))
            layer_norm(ph, [ps_t[0:n, (bb + j) * 512:(bb + j + 1) * 512] for j in range(2)], ("ps%d" % bb, "ps%d" % (bb + 1)), n, lng, lnb,
                       yt[s], "yt%d" % s, tmp, ("tmp0", "tmp1"), (st6, mv, rs_, nmr))
            ph.add("pool", (lambda e, o=y_d[t0:t0 + n, :], i_=yt[s][0:n, :]: e.dma_start(out=o, in_=i_)), ("yt%d" % s,), ("yd%d" % i,), dma="y%d" % s)
        ph.run()
    return nc


def _consts(cfg, past_len):
    T, NS, NT = cfg.T, cfg.NS, cfg.NT
    half = 32
    inv = (np.float32(10000.0) ** (-np.arange(half, dtype=np.float32) / np.float32(half))).astype(np.float32)
    pos = np.concatenate([np.arange(T, dtype=np.float32), np.full((NS,), past_len, np.float32)])
    ang = (pos[:, None] * inv[None, :]).astype(np.float32)
    ropeC = np.cos(ang.astype(np.float64)).astype(np.float32)
    ropeS = np.sin(ang.astype(np.float64)).astype(np.float32)
    ident = np.eye(128, dtype=np.float32)
    tri = np.where(np.arange(128)[None, :] <= np.arange(128)[:, None], 0.0, -30000.0).astype(np.float32)
    selB = np.zeros((NS, NS, 128), np.float32)
    for b in range(NS):
        selB[b, b, :] = 1.0
    pidx = np.arange(128, dtype=np.float32)[:, None].copy()
    negm = np.full((128, 1), -30000.0, np.float32); negm[0, 0] = 0.0
    return dict(ropeC=ropeC, ropeS=ropeS, ident=ident, tri=tri, selB=selB.reshape(NS, NS * 128), pidx=pidx, negm=negm)


def _shared(cfg, I):
    NE = cfg.NE
    f = lambda a: np.ascontiguousarray(a, dtype=np.float32)
    a_re, a_im, ldt = I["ssm_a_re"][0], I["ssm_a_im"][0], I["ssm_log_dt"][0]
    toP = lambda a: a.reshape(16, 2, 64).transpose(1, 2, 0).reshape(128, 16)
    aP = np.concatenate([toP(a_re), toP(a_im), toP(np.repeat(ldt[:, None], 64, 1))], axis=1)

    def bP(b):
        out = np.zeros((2, 64, 16, 4, 2, 16), np.float32)
        v = b.reshape(16, 2, 64, 16)
        for gp in range(16):
            for g2 in range(2):
                out[g2, :, gp, gp % 4, g2, :] = v[gp, g2]
        return out.reshape(128, 2048)
    bPc = np.concatenate([bP(I["ssm_b_re"][0]), bP(I["ssm_b_im"][0])], axis=1)

    def cT(cm):
        out = np.zeros((2, 64, 16, 4, 2, 16), np.float32)
        v = cm.reshape(16, 2, 16, 64)
        for gp in range(16):
            for g2 in range(2):
                out[g2, :, gp, gp % 4, g2, :] = v[gp, g2].T
        return out.reshape(128, 2048)
    cTc = np.concatenate([cT(I["ssm_c_re"][0]), cT(I["ssm_c_im"][0])], axis=1)
    dP = I["ssm_d"][0].reshape(4, 128).T
    bglu = I["b_glu"][0].reshape(8, 128).T
    lam4 = np.concatenate([I["lambda_q1"][0], I["lambda_k1"][0], I["lambda_q2"][0], I["lambda_k2"][0]])[None, :]
    ln = np.concatenate([I["ln1_g"][0], I["ln1_b"][0], I["ln2_g"][0], I["ln2_b"][0]])[None, :]
    wgu = I["w_gate_up"][0]
    wg = wgu[:, :, :1024].reshape(NE, 8, 128, 8, 128)
    wl = wgu[:, :, 1024:].reshape(NE, 8, 128, 8, 128)
    wgu_t = np.empty((NE, 8, 128, 8, 256), np.float32)
    wgu_t[..., :128] = wg.transpose(0, 3, 2, 1, 4)
    wgu_t[..., 128:] = wl.transpose(0, 3, 2, 1, 4)
    wdn = I["w_down"][0].reshape(NE, 8, 128, 8, 128)
    wdn_t = np.ascontiguousarray(wdn.transpose(0, 3, 2, 1, 4))
    bgu = I["b_gate_up"][0].reshape(NE, 16, 128).transpose(2, 0, 1).reshape(128, NE * 16)
    wr = I["w_router"][0]
    if wr.shape[1] < 32:
        wr = np.concatenate([wr, np.zeros((D, 32 - wr.shape[1]), np.float32)], axis=1)
    return dict(
        cache_k=f(I["cache_k"][0].reshape(-1, 512)), cache_v=f(I["cache_v"][0].reshape(-1, 512)),
        w_in=f(I["w_in"][0]), w_out=f(I["w_out"][0]), aP=f(aP), bP=f(bPc), cT=f(cTc), dP=f(dP),
        w_glu=f(I["w_glu"][0]), bglu=f(bglu), lam4=f(lam4), gsub=f(I["subln_g"][0][None, :]), gcol=f(I["subln_g"][0][:, None]),
        ln=f(ln), wr=f(wr), br=f(I["b_router"][0][None, :]), wgu=f(wgu_t.reshape(NE * 8 * 128, 2048)), bgu=f(bgu),
        wdn=f(wdn_t.reshape(NE * 8 * 128, 1024)), bdn=f(I["b_down"][0]))


def run(cfg, I, trace=False, stop_after=None, max_ops=None):
    T, NS, NPG = cfg.T, cfg.NS, cfg.NPG
    nc = build(cfg, stop_after, max_ops)
    shared = _shared(cfg, I)
    shared.update(_consts(cfg, NPG * 128))
    in_maps = []
    for c in range(NCORES):
        x = np.concatenate([I["x_prompt"][c], I["x_sample"][c * NS:(c + 1) * NS, 0]], axis=0).astype(np.float32)
        m = dict(shared)
        m["x"] = np.ascontiguousarray(x)
        m["xT"] = np.ascontiguousarray(x.T)
        m["pt"] = np.ascontiguousarray(I["page_table"][c * NS:(c + 1) * NS].reshape(1, NS * NPG).astype(np.int32))
        for k_, nm in (("state_ssm_re", "st_re"), ("state_ssm_im", "st_im")):
            s = I[k_][0, c * NS:(c + 1) * NS].reshape(NS, 16, 2, 64)
            m[nm] = np.ascontiguousarray(s.transpose(2, 3, 1, 0).reshape(128, 16 * NS).astype(np.float32))
        in_maps.append(m)
    res = run_bass_kernel_spmd(nc, in_maps, core_ids=list(range(NCORES)), trace=trace) if trace else \
        run_bass_kernel_spmd(nc, in_maps, core_ids=list(range(NCORES)))
    R = res.results
    B = NCORES
    y = np.stack([r["y"] for r in R])
    ko = np.stack([r["ko"] for r in R]); vo = np.stack([r["vo"] for r in R])
    sre = np.stack([r["sre"].reshape(2, 64, 16, 1 + NS) for r in R])
    sim = np.stack([r["sim"].reshape(2, 64, 16, 1 + NS) for r in R])

    def st_p(s):
        return np.ascontiguousarray(s[..., 0].transpose(0, 3, 1, 2).reshape(B, 32, 64))[None]

    def st_s(s):
        v = s[..., 1:].transpose(0, 4, 3, 1, 2)
        return np.ascontiguousarray(v.reshape(B * NS, 32, 64))[None]
    outs = (
        np.ascontiguousarray(y[:, :T]), np.ascontiguousarray(y[:, T:].reshape(B * NS, 1, D)),
        np.ascontiguousarray(ko[:, :T].reshape(B, T, 4, 128))[None], np.ascontiguousarray(vo[:, :T].reshape(B, T, 4, 128))[None],
        st_p(sre), st_p(sim),
        np.ascontiguousarray(ko[:, T:].reshape(B * NS, 1, 4, 128))[None], np.ascontiguousarray(vo[:, T:].reshape(B * NS, 1, 4, 128))[None],
        st_s(sre), st_s(sim))
    return tuple(o.astype(np.float32) for o in outs), res


def kernel(**inputs):
    I = {k: np.asarray(v) for k, v in inputs.items()}
    outs, _ = run(FULL, I)
    return outs
```
